# Optimizing a Trainium2 kernel written in Bass

```python
import jax, jax.numpy as jnp
from jax import lax
import numpy as np

D_MODEL = 2048
BATCH = 2
SEQ = 8192
DEPTH = 2

EPS = 1e-6
Q_BLOCK = 128
MLA_HEADS = 8
Q_LORA = 512
KV_LORA = 512
NOPE_DIM = 128
ROPE_DIM = 64
MLA_V_DIM = 128
ROPE_THETA = 10000.0
MLA_OUT = MLA_HEADS * MLA_V_DIM
DIL_WINDOWS = (128, 512, 2048)
DIL_RATES = (1, 4, 16)
DIL_GROUPS = 3
DIL_HEADS = 4
DIL_HEAD_DIM = 128
DIL_KEYS = DIL_WINDOWS[0] // DIL_RATES[0] + 1
DIL_WIDTH = DIL_GROUPS * DIL_HEADS * DIL_HEAD_DIM
DIL_OUT = DIL_HEADS * DIL_HEAD_DIM
ALIBI_MAX_BIAS = 8.0
SB_HEADS = 8
SB_HEAD_DIM = 128
SB_WIDTH = SB_HEADS * SB_HEAD_DIM
N_BRANCHES = 3
IN_SIZES = (Q_LORA, KV_LORA, ROPE_DIM, 3 * DIL_WIDTH, 3 * SB_WIDTH, N_BRANCHES * D_MODEL)
IN_COLS = Q_LORA + KV_LORA + ROPE_DIM + 3 * DIL_WIDTH + 3 * SB_WIDTH + N_BRANCHES * D_MODEL
N_EXPERTS = 16
N_GROUPS = 4
EXPERTS_PER_GROUP = N_EXPERTS // N_GROUPS
TOP_K = 2
D_EXPERT = 512

kernel_name = 'hybrid_gated_mla_dilated_stickbreaking_grouped_moe'


def rmsnorm(x, g):
    xf = x.astype(jnp.float32)
    y = xf * lax.rsqrt(jnp.mean(xf * xf, axis=-1, keepdims=True) + EPS)
    return (y * g.astype(jnp.float32)).astype(x.dtype)


def rope(x, positions):
    half = ROPE_DIM // 2
    inv_freq = ROPE_THETA ** (-jnp.arange(half, dtype=jnp.float32) / half)
    ang = positions.astype(jnp.float32)[..., None] * inv_freq
    ang = ang.reshape(ang.shape[:2] + (1,) * (x.ndim - 3) + (half,))
    cos, sin = jnp.cos(ang), jnp.sin(ang)
    x1 = x[..., :half].astype(jnp.float32)
    x2 = x[..., half:].astype(jnp.float32)
    return jnp.concatenate([x1 * cos - x2 * sin, x2 * cos + x1 * sin], axis=-1).astype(x.dtype)


def to_blocks(t):
    b, s = t.shape[:2]
    return jnp.moveaxis(t.reshape((b, s // Q_BLOCK, Q_BLOCK) + t.shape[2:]), 1, 0)


def from_blocks(t):
    nb, b = t.shape[:2]
    return jnp.moveaxis(t, 0, 1).reshape((b, nb * Q_BLOCK) + t.shape[3:])


def mla_attention(c_q, c_kv, k_rope, positions, g_q, w_uq, g_kv, w_ukv):
    b, s, _ = c_q.shape
    q = (rmsnorm(c_q, g_q) @ w_uq).reshape(b, s, MLA_HEADS, NOPE_DIM + ROPE_DIM)
    q_nope, q_rope = q[..., :NOPE_DIM], rope(q[..., NOPE_DIM:], positions)
    kv = (rmsnorm(c_kv, g_kv) @ w_ukv).reshape(b, s, MLA_HEADS, NOPE_DIM + MLA_V_DIM)
    k_nope, v = kv[..., :NOPE_DIM], kv[..., NOPE_DIM:]
    k_rope = rope(k_rope, positions)
    scale = (NOPE_DIM + ROPE_DIM) ** -0.5
    key_idx = jnp.arange(s)

    def block(args):
        qn, qr, i = args
        t = i * Q_BLOCK + jnp.arange(Q_BLOCK)
        z = (jnp.einsum('bqhd,bkhd->bhqk', qn, k_nope)
             + jnp.einsum('bqhr,bkr->bhqk', qr, k_rope)).astype(jnp.float32) * scale
        z = jnp.where(key_idx[None, :] <= t[:, None], z, -jnp.inf)
        p = jax.nn.softmax(z, axis=-1).astype(v.dtype)
        return jnp.einsum('bhqk,bkhd->bqhd', p, v)

    o = lax.map(block, (to_blocks(q_nope), to_blocks(q_rope), jnp.arange(s // Q_BLOCK)))
    return from_blocks(o).reshape(b, s, MLA_OUT)


def dilated_attention(q, k, v, positions):
    b, s = q.shape[:2]
    rates = jnp.array(DIL_RATES, dtype=jnp.int32)
    n_h = DIL_GROUPS * DIL_HEADS
    slopes = (2.0 ** (-ALIBI_MAX_BIAS * jnp.arange(1, n_h + 1, dtype=jnp.float32) / n_h)).reshape(DIL_GROUPS, DIL_HEADS)
    scale = DIL_HEAD_DIM ** -0.5
    offsets = jnp.arange(DIL_KEYS)
    pos_f = positions.astype(jnp.float32)

    def group(qg, kg, vg, rate, slope, t):
        idx = t[:, None] - offsets[None, :] * rate
        valid = idx >= 0
        idx = jnp.maximum(idx, 0)
        kk = kg[:, idx]
        vv = vg[:, idx]
        z = jnp.einsum('bqhd,bqjhd->bhqj', qg, kk).astype(jnp.float32) * scale
        dist = jnp.abs(pos_f[:, t][:, :, None] - pos_f[:, idx])
        z = z - slope[None, :, None, None] * dist[:, None]
        z = jnp.where(valid[None, None], z, -jnp.inf)
        m = jnp.max(z, axis=-1, keepdims=True)
        e = jnp.exp(z - m)
        den = jnp.sum(e, axis=-1, keepdims=True)
        o = jnp.einsum('bhqj,bqjhd->bqhd', (e / den).astype(vg.dtype), vv)
        lse = (m + jnp.log(den))[..., 0]
        return o, jnp.moveaxis(lse, 1, 2)

    def block(args):
        qb, i = args
        t = i * Q_BLOCK + jnp.arange(Q_BLOCK)
        o, lse = jax.vmap(group, in_axes=(2, 2, 2, 0, 0, None), out_axes=2)(qb, k, v, rates, slopes, t)
        w = jax.nn.softmax(lse, axis=2)
        return jnp.einsum('bqgh,bqghd->bqhd', w.astype(o.dtype), o)

    o = lax.map(block, (to_blocks(q), jnp.arange(s // Q_BLOCK)))
    return from_blocks(o).reshape(b, s, DIL_OUT)


def stick_breaking_attention(q, k, v):
    b, s = q.shape[:2]
    scale = SB_HEAD_DIM ** -0.5
    key_idx = jnp.arange(s)

    def block(args):
        qb, i = args
        t = i * Q_BLOCK + jnp.arange(Q_BLOCK)
        z = jnp.einsum('bqhd,bkhd->bhqk', qb, k).astype(jnp.float32) * scale
        before = key_idx[None, :] < t[:, None]
        log_keep = jnp.where(before, jax.nn.log_sigmoid(-z), 0.0)
        later = lax.cumsum(log_keep, axis=3, reverse=True) - log_keep
        log_a = jnp.where(before, jax.nn.log_sigmoid(z) + later, -jnp.inf)
        a = jnp.exp(log_a).astype(v.dtype)
        return jnp.einsum('bhqk,bkhd->bqhd', a, v)

    o = lax.map(block, (to_blocks(q), jnp.arange(s // Q_BLOCK)))
    return from_blocks(o).reshape(b, s, SB_WIDTH)


def grouped_moe(a, w_router, b_router, w_gate, w_up, w_down):
    b, s, d = a.shape
    xt = a.reshape(b * s, d)
    scores = jax.nn.sigmoid((xt @ w_router).astype(jnp.float32))
    grouped = (scores + b_router.astype(jnp.float32)).reshape(-1, N_GROUPS, EXPERTS_PER_GROUP)
    group_score = jnp.sum(lax.top_k(grouped, TOP_K)[0], axis=-1)
    g_sel = jnp.argmax(group_score, axis=-1)
    in_group = jnp.take_along_axis(grouped, g_sel[:, None, None], axis=1)[:, 0]
    _, local = lax.top_k(in_group, TOP_K)
    expert_idx = g_sel[:, None] * EXPERTS_PER_GROUP + local
    sel = jnp.take_along_axis(scores, expert_idx, axis=1)
    weights = sel / jnp.sum(sel, axis=-1, keepdims=True)
    gates = jnp.sum(jax.nn.one_hot(expert_idx, N_EXPERTS, dtype=jnp.float32) * weights[..., None], axis=1)
    y = jnp.zeros((b * s, d), jnp.float32)
    for e in range(N_EXPERTS):
        h = jax.nn.silu(xt @ w_gate[e]) * (xt @ w_up[e])
        y = y + gates[:, e:e + 1] * (h @ w_down[e]).astype(jnp.float32)
    return y.astype(a.dtype).reshape(b, s, d)


def setup_inputs(seed: int = 0) -> dict:
    key = jax.random.key(seed)
    ks = jax.random.split(key, 24)
    f32 = jnp.float32

    def nrm(k, shape, fan_in, mult=1.0):
        return jax.random.normal(k, shape, f32) * (mult * fan_in ** -0.5)

    def gain(k, shape):
        return 1.0 + 0.01 * jax.random.normal(k, shape, f32)

    positions = (jnp.arange(SEQ, dtype=jnp.int32)[None, :]
                 + jax.random.randint(ks[2], (BATCH, 1), 0, 1024, dtype=jnp.int32))
    return {
        'x': jax.random.normal(ks[0], (BATCH, SEQ, D_MODEL), f32),
        'c': jax.random.normal(ks[1], (BATCH, D_MODEL), f32),
        'positions': positions,
        'w_ada': nrm(ks[3], (DEPTH, D_MODEL, 6 * D_MODEL), D_MODEL, 0.5),
        'b_ada': 0.01 * jax.random.normal(ks[4], (DEPTH, 6 * D_MODEL), f32),
        'g_mix': gain(ks[5], (DEPTH, D_MODEL)),
        'g_moe': gain(ks[6], (DEPTH, D_MODEL)),
        'w_in': nrm(ks[7], (DEPTH, D_MODEL, IN_COLS), D_MODEL),
        'g_q': gain(ks[8], (DEPTH, Q_LORA)),
        'w_uq': nrm(ks[9], (DEPTH, Q_LORA, MLA_HEADS * (NOPE_DIM + ROPE_DIM)), Q_LORA),
        'g_kv': gain(ks[10], (DEPTH, KV_LORA)),
        'w_ukv': nrm(ks[11], (DEPTH, KV_LORA, MLA_HEADS * (NOPE_DIM + MLA_V_DIM)), KV_LORA),
        'w_o_mla': nrm(ks[12], (DEPTH, MLA_OUT, D_MODEL), MLA_OUT),
        'w_o_dil': nrm(ks[13], (DEPTH, DIL_OUT, D_MODEL), DIL_OUT),
        'w_o_sb': nrm(ks[14], (DEPTH, SB_WIDTH, D_MODEL), SB_WIDTH),
        'w_out': nrm(ks[15], (DEPTH, D_MODEL, D_MODEL), D_MODEL),
        'w_router': nrm(ks[16], (D_MODEL, N_EXPERTS), D_MODEL),
        'b_router': 0.01 * jax.random.normal(ks[17], (N_EXPERTS,), f32),
        'w_gate': nrm(ks[18], (DEPTH, N_EXPERTS, D_MODEL, D_EXPERT), D_MODEL),
        'w_up': nrm(ks[19], (DEPTH, N_EXPERTS, D_MODEL, D_EXPERT), D_MODEL),
        'w_down': nrm(ks[20], (DEPTH, N_EXPERTS, D_EXPERT, D_MODEL), D_EXPERT),
        'g_final': gain(ks[21], (D_MODEL,)),
    }


def reference(x, c, positions, w_ada, b_ada, g_mix, g_moe, w_in, g_q, w_uq, g_kv, w_ukv,
              w_o_mla, w_o_dil, w_o_sb, w_out, w_router, b_router, w_gate, w_up, w_down, g_final):
    b, s, d = x.shape
    split_at = [int(v) for v in np.cumsum(IN_SIZES)[:-1]]
    c_act = jax.nn.silu(c)
    for l in range(DEPTH):
        mod = (c_act @ w_ada[l] + b_ada[l])[:, None, :]
        shift_a, scale_a, gate_a, shift_m, scale_m, gate_m = jnp.split(mod, 6, axis=-1)

        a = rmsnorm(x, g_mix[l]) * (1 + scale_a) + shift_a
        proj = a @ w_in[l]
        c_q, c_kv, k_rope, qkv_dil, qkv_sb, gate_logits = jnp.split(proj, split_at, axis=-1)

        y_mla = mla_attention(c_q, c_kv, k_rope, positions, g_q[l], w_uq[l], g_kv[l], w_ukv[l])
        qkv_dil = qkv_dil.reshape(b, s, 3, DIL_GROUPS, DIL_HEADS, DIL_HEAD_DIM)
        y_dil = dilated_attention(qkv_dil[:, :, 0], qkv_dil[:, :, 1], qkv_dil[:, :, 2], positions)
        qkv_sb = qkv_sb.reshape(b, s, 3, SB_HEADS, SB_HEAD_DIM)
        y_sb = stick_breaking_attention(qkv_sb[:, :, 0], qkv_sb[:, :, 1], qkv_sb[:, :, 2])

        g = jax.nn.sigmoid(gate_logits.reshape(b, s, N_BRANCHES, d))
        merged = (g[:, :, 0] * (y_mla @ w_o_mla[l])
                  + g[:, :, 1] * (y_dil @ w_o_dil[l])
                  + g[:, :, 2] * (y_sb @ w_o_sb[l]))
        x = x + gate_a * (merged @ w_out[l])

        a2 = rmsnorm(x, g_moe[l]) * (1 + scale_m) + shift_m
        x = x + gate_m * grouped_moe(a2, w_router, b_router, w_gate[l], w_up[l], w_down[l])
    return rmsnorm(x, g_final)
```

```python
import math
import numpy as np
import concourse.bass as bass
import concourse.mybir as mybir
from concourse.bass_utils import run_bass_kernel_spmd

F32 = mybir.dt.float32
BF16 = mybir.dt.bfloat16
I32 = mybir.dt.int32
AF = mybir.ActivationFunctionType
ALU = mybir.AluOpType
AX = mybir.AxisListType


class Buf:
    __slots__ = ("name", "w", "r")

    def __init__(self, name):
        self.name = name
        self.w = None
        self.r = {}


class _Rec:
    def __init__(self):
        self.call = None

    def __getattr__(self, name):
        def f(*a, **kw):
            self.call = (name, a, kw)
            return self
        return f


def _record(fn):
    r = _Rec()
    fn(r)
    assert r.call is not None
    return r.call


class Sched:
    ENGS = ("pe", "act", "dve", "pool", "sp")
    NDMA = 12

    def __init__(self, nc):
        self.nc = nc
        self.streams = {e: [] for e in self.ENGS}
        self.cnt = {e: 0 for e in self.ENGS}
        self.seen = {e: {} for e in self.ENGS}
        self.dma_i = {"sp": 0, "pool": 0, "act": 0}
        self.dma_val = {}
        self.sems = {}
        self._ctx = []
        self._perm = []
        self.cc_val = {}
        self._phase_mark = 0
        self.uses_dyn = False
        self._phase_no = 0
        self.jt_ap = None
        self.jsb = None
        self.jt_cnt = 0
        for e in ("pe", "act", "dve", "pool"):
            self._mk_sem("E_" + e)
        for q in ("sp", "pool", "act"):
            for k in range(self.NDMA):
                self._mk_sem("D_%s%d" % (q, k))
                self.dma_val["D_%s%d" % (q, k)] = 0

    def _mk_sem(self, key):
        cm = self.nc.semaphore(key)
        self.sems[key] = cm.__enter__()
        self._perm.append(cm)

    _mk_sem_perm = _mk_sem

    def enable_dyn(self, jt_ap):
        self.jt_ap = jt_ap
        self._mk_sem("JT")
        cm = self.nc.sbuf_tensor("jsb", [1, 2], I32)
        self.jsb = cm.__enter__()
        self._perm.append(cm)

    def sbuf(self, name, shape, dtype):
        cm = self.nc.sbuf_tensor("%s_p%d" % (name, self._phase_no), shape, dtype)
        t = cm.__enter__()
        self._ctx.append(cm)
        return t

    def psum(self, name, shape, dtype):
        cm = self.nc.psum_tensor("%s_p%d" % (name, self._phase_no), shape, dtype)
        t = cm.__enter__()
        self._ctx.append(cm)
        return t

    def _deps(self, eng, reads, writes):
        deps = {}

        def add(tok):
            if tok is None:
                return
            k, v = tok
            if deps.get(k, -1) < v:
                deps[k] = v
        for b in reads:
            add(b.w)
        for b in writes:
            add(b.w)
            for k, v in b.r.items():
                add((k, v))
        out = []
        seen = self.seen[eng]
        for k, v in deps.items():
            if eng == "pe" and k == "E_pe":
                continue
            if seen.get(k, -1) >= v:
                continue
            seen[k] = v
            out.append((k, v))
        return out

    def _mark(self, tok, reads, writes):
        k, v = tok
        for b in reads:
            if b.r.get(k, -1) < v:
                b.r[k] = v
        for b in writes:
            b.w = tok
            b.r = {}

    def op(self, eng, fn, reads=(), writes=()):
        waits = self._deps(eng, reads, writes)
        self.cnt[eng] += 1
        tok = ("E_" + eng, self.cnt[eng])
        self.streams[eng].append((waits, _record(fn), tok[0], 1))
        self._mark(tok, reads, writes)

    def dma(self, q, fn, reads=(), writes=()):
        i = self.dma_i[q]
        self.dma_i[q] += 1
        key = "D_%s%d" % (q, i % self.NDMA)
        waits = self._deps(q, reads, writes)
        prev = self.dma_val[key]
        if prev > 0 and self.seen[q].get(key, -1) < prev:
            self.seen[q][key] = prev
            waits.append((key, prev))
        self.dma_val[key] = prev + 16
        tok = (key, prev + 16)
        self.streams[q].append((waits, _record(fn), key, 16))
        self._mark(tok, reads, writes)

    def begin_phase(self):
        self._phase_mark = len(self._ctx)
        self._phase_no += 1

    def barrier(self):
        allv = {}
        for e in ("pe", "act", "dve", "pool"):
            if self.cnt[e] > 0:
                allv["E_" + e] = self.cnt[e]
        for k, v in self.dma_val.items():
            if v > 0:
                allv[k] = v
        for k, v in self.cc_val.items():
            if v > 0:
                allv[k] = v
        for eng in self.ENGS:
            waits = []
            for k, v in allv.items():
                if self.seen[eng].get(k, -1) < v:
                    self.seen[eng][k] = v
                    waits.append((k, v))
            self.streams[eng].append((waits, None, None, 0))

    def end_phase(self):
        self.barrier()
        self.emit()
        self.streams = {e: [] for e in self.ENGS}
        while len(self._ctx) > self._phase_mark:
            self._ctx.pop().__exit__(None, None, None)

    def cc(self, fn, reads=(), writes=()):
        key = "CC"
        if key not in self.sems:
            self._mk_sem_perm(key)
            self.cc_val[key] = 0
        n = self.cc_val[key] + 1
        self.cc_val[key] = n
        waits = self._deps("pool", reads, writes)
        self.streams["pool"].append((waits, _record(fn), key, None))
        self._mark((key, n), reads, writes)

    def dma_dyn(self, out_ap, tensor, jmul, const, ap_list, reads=(), writes=(), which=0):
        q = "sp"
        i = self.dma_i[q]
        self.dma_i[q] += 1
        key = "D_%s%d" % (q, i % self.NDMA)
        waits = self._deps(q, reads, writes)
        prev = self.dma_val[key]
        if prev > 0 and self.seen[q].get(key, -1) < prev:
            self.seen[q][key] = prev
            waits.append((key, prev))
        self.dma_val[key] = prev + 16
        tok = (key, prev + 16)
        self.streams[q].append((waits, ("__dyn__", (out_ap, tensor, int(jmul), int(const), [list(x) for x in ap_list], which), {}), key, 16))
        self._mark(tok, reads, writes)
        self.uses_dyn = True

    def final_wait(self, eng, bufs):
        waits = self._deps(eng, bufs, ())
        self.streams[eng].append((waits, None, None, 0))

    def emit(self):
        nc = self.nc
        sems = self.sems
        streams = self.streams

        def run(engine, lst, regs=None):
            for waits, fn, key, inc in lst:
                for k, v in waits:
                    engine.wait_ge(sems[k], v)
                if fn is not None:
                    name, a, kw = fn
                    if name == "__dyn__":
                        out_ap, tensor, jmul, const, ap_list, which = a
                        rj, ro = regs[which], regs[2]
                        engine.reg_mul(ro, rj, jmul)
                        engine.reg_add(ro, ro, const)
                        ins = engine.dma_start(out=out_ap, in_=bass.AP(tensor, ro, ap_list))
                    else:
                        ins = getattr(engine, name)(*a, **kw)
                    if inc is None:
                        ins.then_inc(sems[key])
                    else:
                        ins.then_inc(sems[key], inc)

        with nc.Block() as block:
            @block.tensor
            def _(e):
                run(e, streams["pe"])

            @block.scalar
            def _(e):
                run(e, streams["act"])

            @block.vector
            def _(e):
                run(e, streams["dve"])

            @block.gpsimd
            def _(e):
                run(e, streams["pool"])

            @block.sync
            def _(e):
                if any(fn is not None and fn[0] == "__dyn__" for _, fn, _, _ in streams["sp"]):
                    self.jt_cnt += 1
                    with e.register("rj%d" % self.jt_cnt) as rj, e.register("ry%d" % self.jt_cnt) as ry, e.register("ro%d" % self.jt_cnt) as ro:
                        e.dma_start(out=self.jsb[:, :], in_=self.jt_ap).then_inc(sems["JT"], 16)
                        e.wait_ge(sems["JT"], 16 * self.jt_cnt)
                        e.reg_load(rj, self.jsb[0:1, 0:1])
                        e.reg_load(ry, self.jsb[0:1, 1:2])
                        run(e, streams["sp"], (rj, ry, ro))
                else:
                    run(e, streams["sp"])

    def close(self):
        for cm in reversed(self._ctx):
            cm.__exit__(None, None, None)
        self._ctx = []
        for cm in reversed(self._perm):
            cm.__exit__(None, None, None)
        self._perm = []


D = 2048
NK = 16
TS = 1024
TP = 128
EPS = 1e-6
TWO_PI = 2.0 * math.pi
C1 = 6.28125
C2 = TWO_PI - C1


def emit_A(S, nc, d, l, xT, QKs, Vs, gT, b_QKs_l, b_Vs_l, b_gT, ntok=2048, after_sup=None, tick=None):
    cT = d['cT']; wada = d['wada'][l]; bada = d['bada_a'][l]; gmix = d['gmix'][l]; win = d['win'][l]; wsw = d['wsw'][l]
    gq = d['gq'][l]; gkv = d['gkv'][l]; wuq = d['wuq'][l]; wuqs = d['wuqs'][l]; wukv = d['wukv'][l]
    pos = d['pos']; invf = d['invf']; sgn = d['sgn']
    b_QKs = b_QKs_l[0]; b_Vs = b_Vs_l[0]
    b_projT = b_QKs; b_mlaq = b_QKs; b_mlakv = b_QKs; b_mlakr = b_QKs
    aT = S.sbuf("aT", [128, NK, TS], BF16); b_aT = [Buf("aT%d" % i) for i in range(TS // TP)]
    wb = [S.sbuf("wb%d" % i, [128, NK, 512], BF16) for i in range(2)]; b_wb = [Buf("wb0"), Buf("wb1")]
    xt = S.sbuf("xt", [128, NK, TP], F32); b_xt = Buf("xt")
    sq = S.sbuf("sq", [128, NK, TP], BF16); b_sq = Buf("sq")
    rstd = S.sbuf("rstd", [128, 512], F32); b_rstd = Buf("rstd")
    lnt = S.sbuf("lnt", [128, 512], F32); b_lnt = Buf("lnt")
    tmp = [S.sbuf("tmp%d" % i, [128, 512], F32) for i in range(2)]; b_tmp = [Buf("tmp0"), Buf("tmp1")]
    ones = S.sbuf("ones", [128, 128], BF16); b_ones = Buf("ones")
    cin = S.sbuf("cin", [128, NK], F32); b_cin = Buf("cin")
    cact = S.sbuf("cact", [128, NK], BF16); b_cact = Buf("cact")
    badat = S.sbuf("badat", [128, 32], F32); b_badat = Buf("badat")
    gmixt = S.sbuf("gmixt", [128, NK], F32); b_gmixt = Buf("gmixt")
    modt = S.sbuf("modt", [128, 32], F32); b_modt = Buf("modt")
    s1 = S.sbuf("s1", [128, NK], F32); b_s1 = Buf("s1")
    cq = S.sbuf("cq", [128, 4, TS], F32); b_cq = Buf("cq")
    ckv = S.sbuf("ckv", [128, 4, TS], F32); b_ckv = Buf("ckv")
    kr = S.sbuf("kr", [64, TS], F32); b_kr = Buf("kr")
    krs = S.sbuf("krs", [64, TS], F32); b_krs = Buf("krs")
    wswb = S.sbuf("wswb", [128, NK, 64], BF16); b_wswb = Buf("wswb")
    wuqb = S.sbuf("wuqb", [128, 4, 1536], BF16); b_wuqb = Buf("wuqb")
    wuqsb = S.sbuf("wuqsb", [128, 4, 512], BF16); b_wuqsb = Buf("wuqsb")
    wukvb = S.sbuf("wukvb", [128, 4, 2048], BF16); b_wukvb = Buf("wukvb")
    gqt = S.sbuf("gqt", [128, 4], F32); b_gqt = Buf("gqt")
    gkvt = S.sbuf("gkvt", [128, 4], F32); b_gkvt = Buf("gkvt")
    ob = [S.sbuf("ob%d" % i, [128, TS], BF16) for i in range(2)]; b_ob = [Buf("ob0"), Buf("ob1")]
    of = [S.sbuf("of%d" % i, [128, TS], F32) for i in range(2)]; b_of = [Buf("of0"), Buf("of1")]
    lat = S.sbuf("lat", [128, 4, 512], BF16); b_lat = Buf("lat")
    posi = S.sbuf("posi", [64, 512], I32); b_posi = Buf("posi")
    ang = S.sbuf("ang", [64, 512], F32); b_ang = Buf("ang")
    kf = S.sbuf("kf", [64, 512], F32); b_kf = Buf("kf")
    ki = posi; b_ki = b_posi
    rr = S.sbuf("rr", [64, 512], F32); b_rr = Buf("rr")
    rc = S.sbuf("rc", [64, 512], F32); b_rc = Buf("rc")
    mm = S.sbuf("mm", [64, 512], F32); b_mm = Buf("mm")
    CS = S.sbuf("CS", [64, 512], F32); b_CS = Buf("CS")
    SN = S.sbuf("SN", [64, 512], F32); b_SN = Buf("SN")
    invft = S.sbuf("invft", [64, 1], F32); b_invft = Buf("invft")
    sgnt = S.sbuf("sgnt", [64, 1], F32); b_sgnt = Buf("sgnt")
    t1 = ang; b_t1 = b_ang
    t2 = kf; b_t2 = b_kf
    ps = S.psum("ps", [128, 8, 512], F32); b_ps = [Buf("ps%d" % i) for i in range(8)]
    st = {"bank": 0, "w": 0, "ob": 0, "of": 0, "tmp": 0, "ev": 0}

    def nbank():
        i = st["bank"]; st["bank"] = (i + 1) % 8
        return i

    def load_w(dst, bdst, src, nk, c0, ncols):
        srcv = src.rearrange("(k p) c -> p k c", p=128)
        h = max(1, nk // 2)
        for k0 in range(0, nk, h):
            S.dma("pool", lambda e, k0=k0: e.dma_start(out=dst[:, k0:k0 + h, 0:ncols],
                                                      in_=srcv[:, k0:k0 + h, c0:c0 + ncols]),
                  writes=[bdst])

    S.op("dve", lambda e: e.memset(ones[:], 1.0), writes=[b_ones])
    S.dma("sp", lambda e: e.dma_start(out=cin[:], in_=cT[:, :]), writes=[b_cin])
    S.dma("sp", lambda e: e.dma_start(out=badat[:], in_=bada[:, :]), writes=[b_badat])
    S.dma("sp", lambda e: e.dma_start(out=gmixt[:], in_=gmix[:, :]), writes=[b_gmixt])
    S.dma("sp", lambda e: e.dma_start(out=gqt[:], in_=gq[:, :]), writes=[b_gqt])
    S.dma("sp", lambda e: e.dma_start(out=gkvt[:], in_=gkv[:, :]), writes=[b_gkvt])
    S.dma("sp", lambda e: e.dma_start(out=invft[:], in_=invf[:, :]), writes=[b_invft])
    S.dma("sp", lambda e: e.dma_start(out=sgnt[:], in_=sgn[:, :]), writes=[b_sgnt])
    S.op("act", lambda e: e.activation(out=cact[:], in_=cin[:], func=AF.Silu), reads=[b_cin], writes=[b_cact])

    mb = nbank()
    for g in range(8):
        wi = st["w"]; st["w"] ^= 1
        load_w(wb[wi], b_wb[wi], wada, NK, g * 512, 512)
        for m in range(4):
            n = g * 4 + m
            for k in range(NK):
                S.op("pe", lambda e, wi=wi, m=m, k=k, n=n: e.matmul(
                    ps[:, mb, n:n + 1], lhsT=wb[wi][:, k, m * 128:(m + 1) * 128], rhs=cact[:, k:k + 1],
                    start=(k == 0), stop=(k == NK - 1)), reads=[b_wb[wi], b_cact], writes=[b_ps[mb]])
    S.op("dve", lambda e: e.tensor_tensor(out=modt[:], in0=ps[:, mb, 0:32], in1=badat[:], op=ALU.add),
         reads=[b_ps[mb], b_badat], writes=[b_modt])
    S.op("dve", lambda e: e.scalar_tensor_tensor(out=s1[:], in0=modt[:, 16:32], scalar=1.0, in1=gmixt[:],
                                                 op0=ALU.add, op1=ALU.mult),
         reads=[b_modt, b_gmixt], writes=[b_s1])

    load_w(wswb, b_wswb, wsw, NK, 0, 64)
    load_w(wuqb, b_wuqb, wuq, 4, 0, 1536)
    load_w(wuqsb, b_wuqsb, wuqs, 4, 0, 512)
    load_w(wukvb, b_wukvb, wukv, 4, 0, 2048)

    def rms_rstd(src_sq, bsrc, nk, width, dim):
        bk = nbank()
        for k in range(nk):
            S.op("pe", lambda e, k=k: e.matmul(ps[:, bk, 0:width], lhsT=ones[:], rhs=src_sq[:, k, 0:width],
                                               start=(k == 0), stop=(k == nk - 1)),
                 reads=[bsrc, b_ones], writes=[b_ps[bk]])
        S.op("act", lambda e: e.activation(out=lnt[:, 0:width], in_=ps[:, bk, 0:width], func=AF.Ln,
                                           scale=1.0 / dim, bias=EPS), reads=[b_ps[bk]], writes=[b_lnt])
        S.op("act", lambda e: e.activation(out=rstd[:, 0:width], in_=lnt[:, 0:width], func=AF.Exp, scale=-0.5),
             reads=[b_lnt], writes=[b_rstd])

    xTv = xT.rearrange("(k p) t -> p k t", p=128)
    for sup in range(ntok // TS):
        t0s = sup * TS
        b_QKs = b_QKs_l[sup]; b_Vs = b_Vs_l[sup]
        for pt in range(TS // TP):
            tok0 = t0s + pt * TP
            for k0 in (0, 8):
                S.dma("sp", lambda e, k0=k0, tok0=tok0: e.dma_start(out=xt[:, k0:k0 + 8, :],
                                                                    in_=xTv[:, k0:k0 + 8, tok0:tok0 + TP]),
                      writes=[b_xt])
            S.op("act", lambda e: e.activation(out=sq[:], in_=xt[:], func=AF.Square), reads=[b_xt], writes=[b_sq])
            rms_rstd(sq, b_sq, NK, TP, float(D))
            for k in range(NK):
                ti = st["tmp"]; st["tmp"] ^= 1
                S.op("dve", lambda e, k=k, ti=ti: e.tensor_tensor(out=tmp[ti][:, 0:TP], in0=xt[:, k, :],
                                                                  in1=rstd[:, 0:TP], op=ALU.mult),
                     reads=[b_xt, b_rstd], writes=[b_tmp[ti]])
                S.op("act", lambda e, k=k, ti=ti, pt=pt: e.activation(
                    out=aT[:, k, pt * TP:(pt + 1) * TP], in_=tmp[ti][:, 0:TP], func=AF.Identity,
                    scale=s1[:, k:k + 1], bias=modt[:, k:k + 1]),
                    reads=[b_tmp[ti], b_s1, b_modt], writes=[b_aT[pt]])

        def gemm_group(src, c0, ncols, epilogue):
            wi = st["w"]; st["w"] ^= 1
            load_w(wb[wi], b_wb[wi], src, NK, c0, ncols)
            if tick is not None:
                tick()
            for m in range((ncols + 127) // 128):
                mc = min(128, ncols - m * 128)
                for t in range(TS // 512):
                    bk = nbank()
                    for k in range(NK):
                        S.op("pe", lambda e, wi=wi, m=m, mc=mc, t=t, k=k, bk=bk: e.matmul(
                            ps[0:mc, bk, :], lhsT=wb[wi][:, k, m * 128:m * 128 + mc],
                            rhs=aT[:, k, t * 512:(t + 1) * 512], start=(k == 0), stop=(k == NK - 1)),
                            reads=[b_wb[wi]] + b_aT[4 * t:4 * t + 4], writes=[b_ps[bk]])
                    epilogue(m, mc, t, bk)

        def evac(out_ap, bout, bk, mc, scale=1.0, func=None):
            st["ev"] ^= 1
            if func is not None or st["ev"]:
                f = func if func is not None else AF.Copy
                S.op("act", lambda e: e.activation(out=out_ap, in_=ps[0:mc, bk, :], func=f, scale=scale),
                     reads=[b_ps[bk]], writes=[bout])
            else:
                S.op("dve", lambda e: e.tensor_scalar(out=out_ap, in0=ps[0:mc, bk, :], scalar1=scale, scalar2=None,
                                                      op0=ALU.mult), reads=[b_ps[bk]], writes=[bout])

        gemm_group(win, 0, 512, lambda m, mc, t, bk: evac(cq[:, m, t * 512:(t + 1) * 512], b_cq, bk, mc))
        gemm_group(win, 512, 512, lambda m, mc, t, bk: evac(ckv[:, m, t * 512:(t + 1) * 512], b_ckv, bk, mc))
        gemm_group(win, 1024, 64, lambda m, mc, t, bk: evac(kr[:, t * 512:(t + 1) * 512], b_kr, bk, mc))
        gemm_group(wsw, 0, 64, lambda m, mc, t, bk: evac(krs[:, t * 512:(t + 1) * 512], b_krs, bk, mc))

        def out_bf(dst, bdst, row0, scale):
            cur = {}

            def ep(m, mc, t, bk):
                if t == 0:
                    cur["i"] = st["ob"]; st["ob"] ^= 1
                i = cur["i"]
                evac(ob[i][:, t * 512:(t + 1) * 512], b_ob[i], bk, mc, scale=scale)
                if t == TS // 512 - 1:
                    r = row0 + m * 128
                    S.dma("sp", lambda e, i=i, r=r: e.dma_start(out=dst[r:r + 128, t0s:t0s + TS], in_=ob[i][:, :]),
                          reads=[b_ob[i]], writes=[bdst])
            return ep

        def out_f32(dst, bdst, row0, func):
            cur = {}

            def ep(m, mc, t, bk):
                if t == 0:
                    cur["i"] = st["of"]; st["of"] ^= 1
                i = cur["i"]
                evac(of[i][:, t * 512:(t + 1) * 512], b_of[i], bk, mc, func=func)
                if t == TS // 512 - 1:
                    r = row0 + m * 128
                    S.dma("sp", lambda e, i=i, r=r: e.dma_start(out=dst[r:r + 128, t0s:t0s + TS], in_=of[i][:, :]),
                          reads=[b_of[i]], writes=[bdst])
            return ep

        sc = 128.0 ** -0.5
        def out_qk(rowfn, scale):
            cur = {}

            def ep(m, mc, t, bk):
                if t == 0:
                    cur["i"] = st["ob"]; st["ob"] ^= 1
                i = cur["i"]
                evac(ob[i][:, t * 512:(t + 1) * 512], b_ob[i], bk, mc, scale=scale)
                if t == TS // 512 - 1:
                    sh, r0 = rowfn(m)
                    S.dma("sp", lambda e: e.dma_start(out=QKs[sh, sup, r0:r0 + 128, :], in_=ob[i][:, :]),
                          reads=[b_ob[i]], writes=[b_QKs])
            return ep

        def gemm_group_tm(c0, store):
            wi = st["w"]; st["w"] ^= 1
            load_w(wb[wi], b_wb[wi], win, NK, c0, 512)
            if tick is not None:
                tick()
            for s_ in range(TS // 128):
                bk = nbank()
                for k in range(NK):
                    S.op("pe", lambda e: e.matmul(ps[:, bk, :], lhsT=aT[:, k, s_ * 128:(s_ + 1) * 128], rhs=wb[wi][:, k, 0:512],
                                                  start=(k == 0), stop=(k == NK - 1)), reads=[b_wb[wi], b_aT[s_]], writes=[b_ps[bk]])
                oi = st["ob"]; st["ob"] ^= 1
                evac(ob[oi][:, 0:512], b_ob[oi], bk, 128)
                store(s_, oi)

        for g in range(3):
            gemm_group(win, 1088 + g * 512, 512, out_qk(lambda m, g=g: (m, g * 128), sc))
        for g in range(3):
            gemm_group(win, 1088 + 1536 + g * 512, 512, out_qk(lambda m, g=g: (m, 384 + g * 128), 1.0))
        for g in range(3):
            def st_dv(s_, oi, g=g):
                tk = t0s + s_ * 128
                S.dma("sp", lambda e: e.dma_start(out=Vs[:, tk:tk + 128, g * 128:(g + 1) * 128].rearrange("h p c -> p h c"),
                                                  in_=ob[oi][:, 0:512].rearrange("p (h c) -> p h c", c=128)),
                      reads=[b_ob[oi]], writes=[b_Vs])
            gemm_group_tm(1088 + 3072 + g * 512, st_dv)
        for gi in range(2):
            gemm_group(win, 5696 + gi * 512, 512, out_qk(lambda m, gi=gi: ((4 * gi + m) // 2, 768 + (m % 2) * 128), sc))
        for gi in range(2):
            gemm_group(win, 5696 + 1024 + gi * 512, 512, out_qk(lambda m, gi=gi: ((4 * gi + m) // 2, 1024 + (m % 2) * 128), 1.0))
        for gi in range(2):
            def st_sv(s_, oi, gi=gi):
                tk = t0s + s_ * 128
                S.dma("sp", lambda e: e.dma_start(out=Vs[2 * gi:2 * gi + 2, tk:tk + 128, 384:640].rearrange("j p c -> p j c"),
                                                  in_=ob[oi][:, 0:512].rearrange("p (j c) -> p j c", c=256)),
                      reads=[b_ob[oi]], writes=[b_Vs])
            gemm_group_tm(5696 + 2048 + gi * 512, st_sv)
        for gi in range(12):
            gemm_group(win, 8768 + gi * 512, 512, out_f32(gT, b_gT, gi * 512, AF.Sigmoid))

        scm = 192.0 ** -0.5
        for tt in range(TS // 512):
            tok0 = t0s + tt * 512
            tsl = slice(tt * 512, (tt + 1) * 512)
            S.dma("sp", lambda e, tok0=tok0: e.dma_start(out=posi[:], in_=pos[0:1, tok0:tok0 + 512].partition_broadcast(64)),
                  writes=[b_posi])
            S.op("dve", lambda e: e.tensor_copy(out=ang[:], in_=posi[:]), reads=[b_posi], writes=[b_ang])
            S.op("dve", lambda e: e.tensor_scalar(out=ang[:], in0=ang[:], scalar1=invft[:, 0:1], scalar2=None, op0=ALU.mult),
                 reads=[b_ang, b_invft], writes=[b_ang])
            S.op("dve", lambda e: e.tensor_scalar(out=kf[:], in0=ang[:], scalar1=1.0 / TWO_PI, scalar2=None, op0=ALU.mult),
                 reads=[b_ang], writes=[b_kf])
            S.op("dve", lambda e: e.tensor_copy(out=ki[:], in_=kf[:]), reads=[b_kf], writes=[b_ki])
            S.op("dve", lambda e: e.tensor_copy(out=kf[:], in_=ki[:]), reads=[b_ki], writes=[b_kf])
            S.op("dve", lambda e: e.scalar_tensor_tensor(out=rr[:], in0=kf[:], scalar=-C1, in1=ang[:], op0=ALU.mult, op1=ALU.add),
                 reads=[b_kf, b_ang], writes=[b_rr])
            S.op("dve", lambda e: e.scalar_tensor_tensor(out=rr[:], in0=kf[:], scalar=-C2, in1=rr[:], op0=ALU.mult, op1=ALU.add),
                 reads=[b_kf, b_rr], writes=[b_rr])

            def wrap(r, br):
                S.op("dve", lambda e: e.tensor_scalar(out=mm[:], in0=r[:], scalar1=math.pi, scalar2=-TWO_PI, op0=ALU.is_gt, op1=ALU.mult),
                     reads=[br], writes=[b_mm])
                S.op("dve", lambda e: e.tensor_tensor(out=r[:], in0=r[:], in1=mm[:], op=ALU.add), reads=[br, b_mm], writes=[br])
                S.op("dve", lambda e: e.tensor_scalar(out=mm[:], in0=r[:], scalar1=-math.pi, scalar2=TWO_PI, op0=ALU.is_lt, op1=ALU.mult),
                     reads=[br], writes=[b_mm])
                S.op("dve", lambda e: e.tensor_tensor(out=r[:], in0=r[:], in1=mm[:], op=ALU.add), reads=[br, b_mm], writes=[br])
                S.op("dve", lambda e: e.tensor_scalar(out=r[:], in0=r[:], scalar1=3.1415925, scalar2=-3.1415925, op0=ALU.min, op1=ALU.max),
                     reads=[br], writes=[br])
            wrap(rr, b_rr)
            S.op("dve", lambda e: e.tensor_scalar(out=rc[:], in0=rr[:], scalar1=math.pi / 2, scalar2=None, op0=ALU.add),
                 reads=[b_rr], writes=[b_rc])
            wrap(rc, b_rc)
            S.op("act", lambda e: e.activation(out=CS[:], in_=rc[:], func=AF.Sin), reads=[b_rc], writes=[b_CS])
            S.op("act", lambda e: e.activation(out=SN[:], in_=rr[:], func=AF.Sin, scale=sgnt[:, 0:1]), reads=[b_rr, b_sgnt], writes=[b_SN])

            def rope_out(src_r, bsr, src_s, bss, scale, dst_ap, bdst):
                S.op("dve", lambda e: e.scalar_tensor_tensor(out=t1[:], in0=src_r, scalar=scale, in1=CS[:], op0=ALU.mult, op1=ALU.mult),
                     reads=[bsr, b_CS], writes=[b_t1])
                S.op("dve", lambda e: e.scalar_tensor_tensor(out=t2[:], in0=src_s, scalar=scale, in1=SN[:], op0=ALU.mult, op1=ALU.mult),
                     reads=[bss, b_SN], writes=[b_t2])
                S.op("dve", lambda e: e.tensor_tensor(out=dst_ap, in0=t1[:], in1=t2[:], op=ALU.add),
                     reads=[b_t1, b_t2], writes=[bdst])

            oi = st["ob"]; st["ob"] ^= 1
            rope_out(kr[:, tsl], b_kr, krs[:, tsl], b_krs, 1.0, ob[oi][0:64, 0:512], b_ob[oi])
            for sh in range(4):
                S.dma("sp", lambda e: e.dma_start(out=QKs[sh, sup, 1920:1984, tok0 - t0s:tok0 - t0s + 512], in_=ob[oi][0:64, 0:512]),
                      reads=[b_ob[oi]], writes=[b_QKs])

            def latent_norm(src, bsrc, gt, bgt):
                sqv = sq[:, :, :].rearrange("p (k a) t -> p k (a t)", a=4)
                S.op("act", lambda e: e.activation(out=sqv, in_=src[:, :, tsl], func=AF.Square), reads=[bsrc], writes=[b_sq])
                rms_rstd(sqv, b_sq, 4, 512, 512.0)
                for k in range(4):
                    S.op("dve", lambda e, k=k: e.scalar_tensor_tensor(out=lat[:, k, :], in0=src[:, k, tsl], scalar=gt[:, k:k + 1],
                                                                      in1=rstd[:, :], op0=ALU.mult, op1=ALU.mult),
                         reads=[bsrc, bgt, b_rstd], writes=[b_lat])

            latent_norm(cq, b_cq, gqt, b_gqt)
            for h in range(8):
                bk = nbank()
                for k in range(4):
                    S.op("pe", lambda e, h=h, k=k, bk=bk: e.matmul(ps[:, bk, :], lhsT=wuqb[:, k, h * 192:h * 192 + 128], rhs=lat[:, k, :],
                                                                   start=(k == 0), stop=(k == 3)), reads=[b_wuqb, b_lat], writes=[b_ps[bk]])
                oi = st["ob"]; st["ob"] ^= 1
                evac(ob[oi][:, 0:512], b_ob[oi], bk, 128, scale=scm)
                S.dma("sp", lambda e, oi=oi, h=h, tok0=tok0: e.dma_start(out=QKs[h // 2, sup, 1280 + (h % 2) * 128:1280 + (h % 2) * 128 + 128, tok0 - t0s:tok0 - t0s + 512], in_=ob[oi][:, 0:512]),
                      reads=[b_ob[oi]], writes=[b_mlaq])
                bk1 = nbank(); bk2 = nbank()
                for k in range(4):
                    S.op("pe", lambda e, h=h, k=k, bk1=bk1: e.matmul(ps[0:64, bk1, :], lhsT=wuqb[:, k, h * 192 + 128:h * 192 + 192], rhs=lat[:, k, :],
                                                                     start=(k == 0), stop=(k == 3)), reads=[b_wuqb, b_lat], writes=[b_ps[bk1]])
                for k in range(4):
                    S.op("pe", lambda e, h=h, k=k, bk2=bk2: e.matmul(ps[0:64, bk2, :], lhsT=wuqsb[:, k, h * 64:(h + 1) * 64], rhs=lat[:, k, :],
                                                                     start=(k == 0), stop=(k == 3)), reads=[b_wuqsb, b_lat], writes=[b_ps[bk2]])
                oi = st["ob"]; st["ob"] ^= 1
                rope_out(ps[0:64, bk1, :], b_ps[bk1], ps[0:64, bk2, :], b_ps[bk2], scm, ob[oi][0:64, 0:512], b_ob[oi])
                S.dma("sp", lambda e, oi=oi, h=h, tok0=tok0: e.dma_start(out=QKs[h // 2, sup, 1792 + (h % 2) * 64:1792 + (h % 2) * 64 + 64, tok0 - t0s:tok0 - t0s + 512], in_=ob[oi][0:64, 0:512]),
                      reads=[b_ob[oi]], writes=[b_mlaq])
            latent_norm(ckv, b_ckv, gkvt, b_gkvt)
            for h in range(8):
                bk = nbank()
                for k in range(4):
                    S.op("pe", lambda e: e.matmul(ps[:, bk, :], lhsT=wukvb[:, k, h * 256:h * 256 + 128], rhs=lat[:, k, :],
                                                  start=(k == 0), stop=(k == 3)), reads=[b_wukvb, b_lat], writes=[b_ps[bk]])
                oi = st["ob"]; st["ob"] ^= 1
                evac(ob[oi][:, 0:512], b_ob[oi], bk, 128)
                S.dma("sp", lambda e: e.dma_start(out=QKs[h // 2, sup, 1536 + (h % 2) * 128:1536 + (h % 2) * 128 + 128, tok0 - t0s:tok0 - t0s + 512], in_=ob[oi][:, 0:512]),
                      reads=[b_ob[oi]], writes=[b_QKs])
            wv = wukvb[:, :, :].rearrange("p k (h c) -> p k h c", c=256)
            for s4 in range(4):
                for hg in range(2):
                    bk = nbank()
                    for k in range(4):
                        S.op("pe", lambda e: e.matmul(ps[:, bk, :].rearrange("p (h c) -> p h c", c=128), lhsT=lat[:, k, s4 * 128:(s4 + 1) * 128],
                                                      rhs=wv[:, k, hg * 4:(hg + 1) * 4, 128:256], start=(k == 0), stop=(k == 3)),
                             reads=[b_wukvb, b_lat], writes=[b_ps[bk]])
                    oi = st["ob"]; st["ob"] ^= 1
                    evac(ob[oi][:, 0:512], b_ob[oi], bk, 128)
                    tk = tok0 + s4 * 128
                    S.dma("sp", lambda e: e.dma_start(out=Vs[2 * hg:2 * hg + 2, tk:tk + 128, 640:896].rearrange("j p c -> p j c"),
                                                      in_=ob[oi][:, 0:512].rearrange("p (j c) -> p j c", c=256)),
                          reads=[b_ob[oi]], writes=[b_Vs])
        if after_sup is not None:
            after_sup(sup)


SEQ = 8192
NB = SEQ // 128
RATES = (1, 4, 16)
BIG = 1.0e6


def emit_B(S, nc, d, QKl, Vl, Ysrc, b_QKg, b_Vg, b_Ys, seq=SEQ, do_mla=True, do_sb=True, do_dil=True, mix_head=None):
    NBk = seq // 128
    NG4 = seq // 512
    dposk = d['dposk']; dposq = d['dposq']; nslope = d['nslope']
    maskc_d = d['maskc']; masks_d = d['masks']; maskd_d = d['maskd']; m1_d = d['m1']; m2_d = d['m2']
    b_ymla = b_Ys; b_ysb = b_Ys; b_ydil = b_Ys
    SHR = 1984; SHV = 896
    Q1 = S.sbuf("Q1", [128, seq], BF16); bQ1 = Buf("Q1")
    K1 = S.sbuf("K1", [128, seq], BF16); bK1 = Buf("K1")
    mix = mix_head is not None
    if do_mla or mix:
        Q2 = S.sbuf("Q2", [64, seq], BF16); bQ2 = Buf("Q2")
        K2 = S.sbuf("K2", [64, seq], BF16); bK2 = Buf("K2")
    V1 = S.sbuf("V1", [128, NBk, 128], BF16); bV1 = Buf("V1")
    two = do_mla or do_sb or mix
    if two:
        Q1b = S.sbuf("Q1b", [128, seq], BF16); bQ1b = Buf("Q1b")
        K1b = S.sbuf("K1b", [128, seq], BF16); bK1b = Buf("K1b")
        V1b = S.sbuf("V1b", [128, NBk, 128], BF16); bV1b = Buf("V1b")
        if do_mla:
            Q2b = S.sbuf("Q2b", [64, seq], BF16); bQ2b = Buf("Q2b")
    NP = 8
    PTm = [S.sbuf("PTm%d" % i, [128, 512], BF16) for i in range(4)]; bPTm = [Buf("PTm%d" % i) for i in range(4)]
    cntm = {"pt": 0}
    PT = [S.sbuf("PT%d" % i, [128, 512], BF16) for i in range(NP)]; bPT = [Buf("PT%d" % i) for i in range(NP)]
    SP = [S.sbuf("SP%d" % i, [128, 512], BF16) for i in range(NP)]; bSP = [Buf("SP%d" % i) for i in range(NP)]
    EN = [S.sbuf("EN%d" % i, [128, 512], F32) for i in range(4)]; bEN = [Buf("EN%d" % i) for i in range(4)]
    SNt = [S.sbuf("SN%d" % i, [128, 512], F32) for i in range(NP)]; bSN = [Buf("SN%d" % i) for i in range(NP)]
    UU = [S.sbuf("UU%d" % i, [128, 512], F32) for i in range(4)]; bUU = [Buf("UU%d" % i) for i in range(4)]
    YS = [S.sbuf("YS%d" % i, [128, 512], BF16) for i in range(2)]; bYS = [Buf("YS%d" % i) for i in range(2)]
    RD = S.sbuf("RD", [128, 512], F32); bRD = Buf("RD")
    maskc = S.sbuf("maskc_t", [128, 4, 512], BF16); bmaskc = Buf("maskc")
    masks = S.sbuf("masks_t", [128, 4, 512], BF16); bmasks = Buf("masks")
    maskd = S.sbuf("maskd_t", [128, 256], F32); bmaskd = Buf("maskd")
    M1 = S.sbuf("M1", [128, 128], BF16); bM1 = Buf("M1")
    M2 = S.sbuf("M2", [128, 128], BF16); bM2 = Buf("M2")
    ones = S.sbuf("ones", [128, 128], BF16); bones = Buf("ones")
    nsl = S.sbuf("nsl", [128, 3], F32); bnsl = Buf("nsl")
    ps = S.psum("ps", [128, 8, 512], F32); bps = [Buf("ps%d" % i) for i in range(8)]

    S.op("dve", lambda e: e.memset(ones[:], 1.0), writes=[bones])
    S.dma("sp", lambda e: e.dma_start(out=maskc[:], in_=maskc_d[:, :, :]), writes=[bmaskc])
    S.dma("sp", lambda e: e.dma_start(out=masks[:], in_=masks_d[:, :, :]), writes=[bmasks])
    S.dma("sp", lambda e: e.dma_start(out=maskd[:], in_=maskd_d[:, :]), writes=[bmaskd])
    S.dma("sp", lambda e: e.dma_start(out=M1[:], in_=m1_d[:, :]), writes=[bM1])
    S.dma("sp", lambda e: e.dma_start(out=M2[:], in_=m2_d[:, :]), writes=[bM2])
    S.dma("sp", lambda e: e.dma_start(out=nsl[:], in_=nslope[:, :]), writes=[bnsl])

    QK5 = QKl.rearrange("c r (p a) t -> c r p (a t)", a=1) if False else QKl
    Vl4 = Vl
    Vl6 = Vl.rearrange("c r (b p) d -> c r b p d", p=128)

    def load_fm(dst, bdst, R0, rows=128):
        dv4 = dst[0:rows, :].rearrange("p (r u t) -> p r u t", r=4, u=2)
        for u in range(2):
            S.dma("sp", lambda e: e.dma_start(out=dv4[:, :, u, :],
                                              in_=QK5[u * 4 + R0 // 512, :, R0 % 512:R0 % 512 + rows, :].rearrange("r p t -> p r t")),
                  reads=[b_QKg], writes=[bdst])

    def load_v(c0, Vt=None, bVt=None):
        Vt = V1 if Vt is None else Vt
        bVt = bV1 if bVt is None else bVt
        for r in range(4):
            for cp in range(4):
                S.dma("sp", lambda e: e.dma_start(out=Vt[:, r * 16 + cp * 4:r * 16 + cp * 4 + 4, :],
                                                  in_=Vl6[cp, r, :, :, c0:c0 + 128].rearrange("b p d -> p b d")),
                      reads=[b_Vg], writes=[bVt])

    def load_v_perm(c0, rt):
        if rt == 1:
            load_v(c0)
        elif rt == 4:
            for s_ in range(4):
                for r in range(4):
                    S.dma("sp", lambda e: e.dma_start(out=V1[:, s_ * 16 + 4 * r:s_ * 16 + 4 * r + 4, :],
                                                      in_=Vl4[:, r, s_:s_ + 4 * 127 + 1:4, c0:c0 + 128].rearrange("c p d -> p c d")),
                          reads=[b_Vg], writes=[bV1])
        else:
            for s_ in range(16):
                for cp in range(4):
                    S.dma("sp", lambda e: e.dma_start(out=V1[32 * cp:32 * cp + 32, s_ * 4:s_ * 4 + 4, :],
                                                      in_=Vl4[cp, :, s_:s_ + 16 * 31 + 1:16, c0:c0 + 128].rearrange("m p d -> p m d")),
                          reads=[b_Vg], writes=[bV1])

    Ys5 = Ysrc

    def yout(blk, g4):
        return Ys5[g4 // 8, blk, :, (g4 % 8) * 512:(g4 % 8) * 512 + 512]

    def pipeline(steps):
        prev = None
        for s1, s2 in steps:
            s1()
            if prev is not None:
                prev()
            prev = s2
        if prev is not None:
            prev()

    cnt = {"z": 0, "o": 0, "pt": 0, "ys": 0, "en": 0, "uu": 0}

    def interleave(a, b):
        out = []
        for x, y in zip(a, b):
            out.append(x); out.append(y)
        return out

    if do_mla:
        load_fm(K2, bK2, 1920, 64)
        hs = [dict(Q=Q1, bQ=bQ1, Qr=Q2, bQr=bQ2, K=K1, bK=bK1, V=V1, bV=bV1, zb=(0, 1), ob=2, db=3),
              dict(Q=Q1b, bQ=bQ1b, Qr=Q2b, bQr=bQ2b, K=K1b, bK=bK1b, V=V1b, bV=bV1b, zb=(6, 7), ob=4, db=5)]
        allsteps = []
        for h in range(2):
            allsteps.append(mla_steps(h, hs[h]))
        pipeline(interleave(allsteps[0], allsteps[1]))

    def mla_steps(h, H):
        if True:
            load_fm(H["Q"], H["bQ"], 1280 + h * 128)
            load_fm(H["Qr"], H["bQr"], 1792 + h * 64, 64)
            load_fm(H["K"], H["bK"], 1536 + h * 128)
            load_v(640 + h * 128, H["V"], H["bV"])
            steps = []
            zc = 0
            for g4 in range(NG4):
                qs = slice(g4 * 512, (g4 + 1) * 512)
                ob = H["ob"]; db = H["db"]
                nst = 4 * g4 + 4
                for j in range(nst):
                    zb = H["zb"][zc % 2]; zc += 1
                    ks = slice(j * 128, (j + 1) * 128)
                    box = {}

                    def s1(qs=qs, j=j, zb=zb, ks=ks, g4=g4, H=H, box=box):
                        pi = cntm["pt"] % 4; cntm["pt"] += 1
                        box["pi"] = pi
                        S.op("pe", lambda e: e.matmul(ps[:, zb, :], lhsT=H["K"][:, ks], rhs=H["Q"][:, qs], start=True, stop=False),
                             reads=[H["bK"], H["bQ"]], writes=[bps[zb]])
                        S.op("pe", lambda e: e.matmul(ps[:, zb, :], lhsT=K2[0:64, ks], rhs=H["Qr"][0:64, qs], start=False, stop=True),
                             reads=[bK2, H["bQr"]], writes=[bps[zb]])
                        S.op("act", lambda e: e.activation(out=PTm[pi][:], in_=ps[:, zb, :], func=AF.Exp),
                             reads=[bps[zb]], writes=[bPTm[pi]])
                        if j >= 4 * g4:
                            S.op("dve", lambda e: e.tensor_tensor(out=PTm[pi][:], in0=PTm[pi][:], in1=maskc[:, j - 4 * g4, :], op=ALU.mult),
                                 reads=[bPTm[pi], bmaskc], writes=[bPTm[pi]])

                    def s2(j=j, ob=ob, db=db, nst=nst, h=h, g4=g4, H=H, box=box):
                        pi = box["pi"]
                        S.op("pe", lambda e: e.matmul(ps[:, ob, :], lhsT=H["V"][:, j, :], rhs=PTm[pi][:], start=(j == 0), stop=(j == nst - 1)),
                             reads=[H["bV"], bPTm[pi]], writes=[bps[ob]])
                        S.op("pe", lambda e: e.matmul(ps[:, db, :], lhsT=ones[:], rhs=PTm[pi][:], start=(j == 0), stop=(j == nst - 1)),
                             reads=[bones, bPTm[pi]], writes=[bps[db]])
                        if j == nst - 1:
                            yi = cnt["ys"] % 2; cnt["ys"] += 1
                            ri = cnt["uu"] % 4; cnt["uu"] += 1
                            S.op("dve", lambda e: e.reciprocal(out=UU[ri][:], in_=ps[:, db, :]), reads=[bps[db]], writes=[bUU[ri]])
                            S.op("dve", lambda e: e.tensor_tensor(out=YS[yi][:], in0=ps[:, ob, :], in1=UU[ri][:], op=ALU.mult),
                                 reads=[bps[ob], bUU[ri]], writes=[bYS[yi]])
                            S.dma("sp", lambda e: e.dma_start(out=yout(h, g4), in_=YS[yi][:]), reads=[bYS[yi]], writes=[b_ymla])
                    steps.append((s1, s2))
            return steps

    if do_sb:
        hs = [dict(Q=Q1, bQ=bQ1, K=K1, bK=bK1, V=V1, bV=bV1, zb=(0, 1), ob=2, rb=3),
              dict(Q=Q1b, bQ=bQ1b, K=K1b, bK=bK1b, V=V1b, bV=bV1b, zb=(6, 7), ob=4, rb=5)]
        allsteps = [sb_steps(0, hs[0]), sb_steps(1, hs[1])]
        run_sb(allsteps)

    def sb_steps(h, H):
        if True:
            load_fm(H["Q"], H["bQ"], 768 + h * 128)
            load_fm(H["K"], H["bK"], 1024 + h * 128)
            load_v(384 + h * 128, H["V"], H["bV"])
            steps = []
            zc = 0
            for g4 in range(NG4):
                qs = slice(g4 * 512, (g4 + 1) * 512)
                ob = H["ob"]; rb = H["rb"]
                nst = 4 * g4 + 4
                for idx, j in enumerate(reversed(range(nst))):
                    first = idx == 0; last = idx == nst - 1
                    zb = H["zb"][zc % 2]; zc += 1
                    ks = slice(j * 128, (j + 1) * 128)
                    box = {}

                    def t1(qs=qs, j=j, zb=zb, ks=ks, g4=g4, H=H, box=box):
                        pi = cnt["pt"] % NP; cnt["pt"] += 1
                        ei = cnt["en"] % 4; cnt["en"] += 1
                        box["pi"] = pi
                        S.op("pe", lambda e: e.matmul(ps[:, zb, :], lhsT=H["K"][:, ks], rhs=H["Q"][:, qs], start=True, stop=True),
                             reads=[H["bK"], H["bQ"]], writes=[bps[zb]])
                        S.op("act", lambda e: e.activation(out=EN[ei][:], in_=ps[:, zb, :], func=AF.Exp, scale=-1.0),
                             reads=[bps[zb]], writes=[bEN[ei]])
                        S.op("act", lambda e: e.activation(out=SNt[pi][:], in_=EN[ei][:], func=AF.Ln, bias=1.0),
                             reads=[bEN[ei]], writes=[bSN[pi]])
                        S.op("dve", lambda e: e.tensor_tensor(out=SP[pi][:], in0=ps[:, zb, :], in1=SNt[pi][:], op=ALU.add),
                             reads=[bps[zb], bSN[pi]], writes=[bSP[pi]])
                        if j >= 4 * g4:
                            S.op("dve", lambda e: e.tensor_tensor(out=SP[pi][:], in0=SP[pi][:], in1=masks[:, j - 4 * g4, :], op=ALU.mult),
                                 reads=[bSP[pi], bmasks], writes=[bSP[pi]])

                    def t2(j=j, rb=rb, first=first, g4=g4, box=box):
                        pi = box["pi"]
                        ui = cnt["uu"] % 4; cnt["uu"] += 1
                        S.op("pe", lambda e: e.matmul(ps[:, rb, :], lhsT=M1[:], rhs=SP[pi][:], start=first, stop=False, skip_group_check=True),
                             reads=[bM1, bSP[pi]], writes=[bps[rb]])
                        S.op("dve", lambda e: e.tensor_tensor(out=UU[ui][:], in0=SNt[pi][:], in1=ps[:, rb, :], op=ALU.add),
                             reads=[bSN[pi], bps[rb]], writes=[bUU[ui]])
                        S.op("act", lambda e: e.activation(out=PT[pi][:], in_=UU[ui][:], func=AF.Exp, scale=-1.0),
                             reads=[bUU[ui]], writes=[bPT[pi]])
                        if j >= 4 * g4:
                            S.op("dve", lambda e: e.tensor_tensor(out=PT[pi][:], in0=PT[pi][:], in1=masks[:, j - 4 * g4, :], op=ALU.mult),
                                 reads=[bPT[pi], bmasks], writes=[bPT[pi]])

                    def t3(rb=rb, last=last, box=box):
                        pi = box["pi"]
                        S.op("pe", lambda e: e.matmul(ps[:, rb, :], lhsT=M2[:], rhs=SP[pi][:], start=False, stop=last, skip_group_check=True),
                             reads=[bM2, bSP[pi]], writes=[bps[rb]])

                    def t4(j=j, ob=ob, first=first, last=last, h=h, g4=g4, H=H, box=box):
                        pi = box["pi"]
                        S.op("pe", lambda e: e.matmul(ps[:, ob, :], lhsT=H["V"][:, j, :], rhs=PT[pi][:], start=first, stop=last),
                             reads=[H["bV"], bPT[pi]], writes=[bps[ob]])
                        if last:
                            yi = cnt["ys"] % 2; cnt["ys"] += 1
                            S.op("act", lambda e: e.activation(out=YS[yi][:], in_=ps[:, ob, :], func=AF.Copy),
                                 reads=[bps[ob]], writes=[bYS[yi]])
                            S.dma("sp", lambda e: e.dma_start(out=yout(3 + h, g4), in_=YS[yi][:]), reads=[bYS[yi]], writes=[b_ysb])
                    steps.append((t1, t2, t3, t4))
            return steps

    def run_sb(allsteps, extra=None):
        N_ = len(allsteps[0])
        if extra is not None:
            extra[0][0]()
        for X in allsteps:
            X[0][0]()
        for k in range(N_):
            for X in allsteps:
                X[k][1]()
            if k > 0:
                for X in allsteps:
                    X[k - 1][3]()
            if extra is not None and k + 1 < N_:
                extra[k + 1][0]()
            if k + 1 < N_:
                for X in allsteps:
                    X[k + 1][0]()
            if extra is not None:
                extra[k][1]()
            for X in allsteps:
                X[k][2]()
        for X in allsteps:
            X[N_ - 1][3]()

    if mix:
        HA = dict(Q=Q1, bQ=bQ1, Qr=Q2, bQr=bQ2, K=K1, bK=bK1, V=V1, bV=bV1, zb=(0, 1), ob=2, db=3)
        HB = dict(Q=Q1b, bQ=bQ1b, K=K1b, bK=bK1b, V=V1b, bV=bV1b, zb=(6, 7), ob=4, rb=5)
        load_fm(K2, bK2, 1920, 64)
        ms = mla_steps(mix_head, HA)
        ss = sb_steps(mix_head, HB)
        run_sb([ss], extra=ms)

    if do_dil:
        accn = S.sbuf("accn", [128, seq], F32); baccn = Buf("accn")
        accd = S.sbuf("accd", [128, seq], F32); baccd = Buf("accd")
        posk_i = S.sbuf("posk_i", [128, NBk], I32); bposk_i = Buf("posk_i")
        posk = S.sbuf("posk", [128, NBk], F32); bposk = Buf("posk")
        pq_i = [S.sbuf("pq_i%d" % i, [128, 128], I32) for i in range(2)]; bpq_i = [Buf("pq_i0"), Buf("pq_i1")]
        pq = [S.sbuf("pq%d" % i, [128, 128], F32) for i in range(2)]; bpq = [Buf("pq0"), Buf("pq1")]
        dd = [S.sbuf("dd%d" % i, [128, 256], F32) for i in range(2)]; bdd = [Buf("dd0"), Buf("dd1")]
        zt = [S.sbuf("zt%d" % i, [128, 256], F32) for i in range(2)]; bzt = [Buf("zt0"), Buf("zt1")]
        for g in range(3):
            r = RATES[g]
            L = seq // r
            nb = L // 128
            load_fm(Q1, bQ1, g * 128)
            load_fm(K1, bK1, 384 + g * 128)
            load_v_perm(g * 128, r)
            S.dma("sp", lambda e: e.dma_start(out=posk_i[:], in_=dposk[g, :, :]), writes=[bposk_i])
            S.op("dve", lambda e: e.tensor_copy(out=posk[:], in_=posk_i[:]), reads=[bposk_i], writes=[bposk])
            S.op("dve", lambda e: e.tensor_scalar(out=posk[:], in0=posk[:], scalar1=-1.0, scalar2=None, op0=ALU.mult), reads=[bposk], writes=[bposk])
            dsteps = []
            for i in range(NBk):
                s, m = divmod(i, nb)
                hp = m > 0
                c0 = 0 if hp else 128
                zb = cnt["z"] % 2; cnt["z"] += 1
                ob = 2 + (cnt["o"] % 2); db = 4 + (cnt["o"] % 2); cnt["o"] += 1
                pi = cnt["pt"] % NP; cnt["pt"] += 1
                bi = i % 2
                st0 = s + 128 * r * m
                qsl = slice(st0, st0 + 127 * r + 1, r) if r > 1 else slice(st0, st0 + 128)
                psl = (slice(st0 - 128 * r, st0 - 128 * r + 127 * r + 1, r) if r > 1 else slice(st0 - 128, st0)) if hp else None
                def s1(i=i, hp=hp, c0=c0, zb=zb, pi=pi, bi=bi, qsl=qsl, psl=psl, g=g):
                    if hp:
                        S.op("pe", lambda e: e.matmul(ps[:, zb, 0:128], lhsT=K1[:, psl], rhs=Q1[:, qsl], start=True, stop=True),
                             reads=[bK1, bQ1], writes=[bps[zb]])
                    S.op("pe", lambda e: e.matmul(ps[:, zb, 128:256], lhsT=K1[:, qsl], rhs=Q1[:, qsl], start=True, stop=True),
                         reads=[bK1, bQ1], writes=[bps[zb]])
                    S.dma("sp", lambda e: e.dma_start(out=pq_i[bi][:], in_=dposq[g:g + 1, i * 128:(i + 1) * 128].partition_broadcast(128)),
                          writes=[bpq_i[bi]])
                    S.op("dve", lambda e: e.tensor_copy(out=pq[bi][:], in_=pq_i[bi][:]), reads=[bpq_i[bi]], writes=[bpq[bi]])
                    if hp:
                        S.op("act", lambda e: e.activation(out=dd[bi][:, 0:128], in_=pq[bi][:], func=AF.Abs, bias=posk[:, i - 1:i], scale=1.0),
                             reads=[bpq[bi], bposk], writes=[bdd[bi]])
                    S.op("act", lambda e: e.activation(out=dd[bi][:, 128:256], in_=pq[bi][:], func=AF.Abs, bias=posk[:, i:i + 1], scale=1.0),
                         reads=[bpq[bi], bposk], writes=[bdd[bi]])
                    S.op("dve", lambda e: e.tensor_tensor(out=dd[bi][:, c0:256], in0=dd[bi][:, c0:256], in1=maskd[:, c0:256], op=ALU.add),
                         reads=[bdd[bi], bmaskd], writes=[bdd[bi]])
                    S.op("dve", lambda e: e.scalar_tensor_tensor(out=zt[bi][:, c0:256], in0=dd[bi][:, c0:256], scalar=nsl[:, g:g + 1],
                                                                 in1=ps[:, zb, c0:256], op0=ALU.mult, op1=ALU.add),
                         reads=[bdd[bi], bnsl, bps[zb]], writes=[bzt[bi]])
                    S.op("act", lambda e: e.activation(out=PT[pi][:, c0:256], in_=zt[bi][:, c0:256], func=AF.Exp),
                         reads=[bzt[bi]], writes=[bPT[pi]])

                def s2(i=i, hp=hp, ob=ob, db=db, pi=pi, s=s, m=m, r=r, g=g, st0=st0):
                    if hp:
                        S.op("pe", lambda e: e.matmul(ps[:, ob, 0:128], lhsT=V1[:, i - 1, :], rhs=PT[pi][:, 0:128], start=True, stop=False),
                             reads=[bV1, bPT[pi]], writes=[bps[ob]])
                    S.op("pe", lambda e: e.matmul(ps[:, ob, 0:128], lhsT=V1[:, i, :], rhs=PT[pi][:, 128:256], start=(not hp), stop=True),
                         reads=[bV1, bPT[pi]], writes=[bps[ob]])
                    if hp:
                        S.op("pe", lambda e: e.matmul(ps[:, db, 0:128], lhsT=ones[:], rhs=PT[pi][:, 0:128], start=True, stop=False),
                             reads=[bones, bPT[pi]], writes=[bps[db]])
                    S.op("pe", lambda e: e.matmul(ps[:, db, 0:128], lhsT=ones[:], rhs=PT[pi][:, 128:256], start=(not hp), stop=True),
                         reads=[bones, bPT[pi]], writes=[bps[db]])
                    an = accn[:, st0:st0 + 127 * r + 1:r] if r > 1 else accn[:, st0:st0 + 128]
                    ad = accd[:, st0:st0 + 127 * r + 1:r] if r > 1 else accd[:, st0:st0 + 128]
                    if g == 0:
                        S.op("act", lambda e: e.activation(out=an, in_=ps[:, ob, 0:128], func=AF.Copy), reads=[bps[ob]], writes=[baccn])
                        S.op("dve", lambda e: e.tensor_copy(out=ad, in_=ps[:, db, 0:128]), reads=[bps[db]], writes=[baccd])
                    else:
                        S.op("dve", lambda e: e.tensor_tensor(out=an, in0=an, in1=ps[:, ob, 0:128], op=ALU.add), reads=[baccn, bps[ob]], writes=[baccn])
                        S.op("dve", lambda e: e.tensor_tensor(out=ad, in0=ad, in1=ps[:, db, 0:128], op=ALU.add), reads=[baccd, bps[db]], writes=[baccd])
                dsteps.append((s1, s2))
            pipeline(dsteps)
        for c in range(seq // 512):
            cs = slice(c * 512, (c + 1) * 512)
            yi = cnt["ys"] % 2; cnt["ys"] += 1
            S.op("dve", lambda e: e.reciprocal(out=RD[:], in_=accd[:, cs]), reads=[baccd], writes=[bRD])
            S.op("dve", lambda e: e.tensor_tensor(out=YS[yi][:], in0=accn[:, cs], in1=RD[:], op=ALU.mult), reads=[baccn, bRD], writes=[bYS[yi]])
            S.dma("sp", lambda e: e.dma_start(out=yout(2, c), in_=YS[yi][:]), reads=[bYS[yi]], writes=[b_ydil])


def consts_B():
    import ml_dtypes
    k = np.arange(128)[:, None]
    q = np.arange(512)[None, :]
    maskc = np.stack([(128 * i0 + k <= q) for i0 in range(4)], axis=1).astype(np.float32)
    masks = np.stack([(128 * i0 + k < q) for i0 in range(4)], axis=1).astype(np.float32)
    kl = np.arange(128)[:, None]; ql = np.arange(128)[None, :]
    maskd = np.concatenate([np.where(kl >= ql, 0.0, BIG), np.where(kl <= ql, 0.0, BIG)], axis=1).astype(np.float32)
    p = np.arange(128)[:, None]; m = np.arange(128)[None, :]
    m1 = (p > m).astype(np.float32); m2 = (p <= m).astype(np.float32)
    bf = ml_dtypes.bfloat16
    return dict(maskc=maskc.astype(bf), masks=masks.astype(bf), maskd=maskd, m1=m1.astype(bf), m2=m2.astype(bf))


D = 2048
NK = 16
EPS = 1e-6
NE = 16
BIGR = 1.0e4


def emit_C(S, nc, d, l, Yl, gT, xT, x2T, xfT, b_Yg, b_gT, b_xsrc, b_x2T, b_xfT, ntok=2048):
    NT = ntok // 512
    cT = d['cT']; wada = d['wada'][l]; bada = d['bada_c'][l]; gmoe = d['gmoe'][l]; gfin = d['gfin']
    womla = d['womla'][l]; wodil = d['wodil'][l]; wosb = d['wosb'][l]; wout = d['wout'][l]
    wr = d['wr']; br = d['br']; wg = d['wg'][l]; wu = d['wu'][l]; wd = d['wd'][l]; ident = d['ident']
    DBG = False
    xt = S.sbuf("xt", [128, NK, 512], F32); b_xt = [Buf("xt%d" % k) for k in range(NK)]
    scr = S.sbuf("scr", [128, 20, 512], BF16); b_scr = Buf("scr")
    mg = S.sbuf("mg", [128, NK, 512], BF16); b_mg = Buf("mg")
    a2b = S.sbuf("a2b", [128, NK, 512], BF16); b_a2b = Buf("a2b")
    NW = 5
    W = [S.sbuf("W%d" % i, [128, NK, 512], BF16) for i in range(NW)]; b_W = [Buf("W%d" % i) for i in range(NW)]
    gt = [S.sbuf("gt%d" % i, [128, 3, 512], F32) for i in range(2)]; b_gt = [Buf("gt0"), Buf("gt1")]
    tmp = [S.sbuf("tmp%d" % i, [128, 512], F32) for i in range(4)]; b_tmp = [Buf("tmp%d" % i) for i in range(4)]
    hp = [S.sbuf("hp%d" % i, [128, 4, 512], BF16) for i in range(2)]; b_hp = [Buf("hp0"), Buf("hp1")]
    rstd = S.sbuf("rstd", [128, 512], F32); b_rstd = Buf("rstd")
    lnt = S.sbuf("lnt", [128, 512], F32); b_lnt = Buf("lnt")
    ones = S.sbuf("ones", [128, 128], BF16); b_ones = Buf("ones")
    onesf = S.sbuf("onesf", [128, 128], F32); b_onesf = Buf("onesf")
    idt = S.sbuf("idt", [128, 128], F32); b_idt = Buf("idt")
    Gm = [S.sbuf("Gm%d" % i, [128, 128], F32) for i in range(2)]; b_Gm = [Buf("Gm0"), Buf("Gm1")]
    cin = S.sbuf("cin", [128, NK], F32); b_cin = Buf("cin")
    cact = S.sbuf("cact", [128, NK], BF16); b_cact = Buf("cact")
    badat = S.sbuf("badat", [128, 64], F32); b_badat = Buf("badat")
    gmoet = S.sbuf("gmoet", [128, NK], F32); b_gmoet = Buf("gmoet")
    gfint = S.sbuf("gfint", [128, NK], F32); b_gfint = Buf("gfint")
    modt = S.sbuf("modt", [128, 64], F32); b_modt = Buf("modt")
    s1m = S.sbuf("s1m", [128, NK], F32); b_s1m = Buf("s1m")
    wrt = S.sbuf("wrt", [128, NK, NE], F32); b_wrt = Buf("wrt")
    brt = S.sbuf("brt", [128, NE], F32); b_brt = Buf("brt")
    lt = S.sbuf("lt", [16, 512], F32); b_lt = Buf("lt")
    sc = S.sbuf("sc", [128, 64], F32); b_sc = Buf("sc")
    bi = S.sbuf("bi", [128, 64], F32); b_bi = Buf("bi")
    bi2 = S.sbuf("bi2", [128, 64], F32); b_bi2 = Buf("bi2")
    eq = S.sbuf("eq", [128, 64], F32); b_eq = Buf("eq")
    m1 = S.sbuf("m1", [128, 16], F32); b_m1 = Buf("m1")
    m2 = S.sbuf("m2", [128, 16], F32); b_m2 = Buf("m2")
    gs = S.sbuf("gs", [128, 16], F32); b_gs = Buf("gs")
    gmax = S.sbuf("gmax", [128, 4], F32); b_gmax = Buf("gmax")
    gsel = S.sbuf("gsel", [128, 16], F32); b_gsel = Buf("gsel")
    wsel = S.sbuf("wsel", [128, 64], F32); b_wsel = Buf("wsel")
    den = S.sbuf("den", [128, 4], F32); b_den = Buf("den")
    gates = S.sbuf("gates", [128, 64], F32); b_gates = Buf("gates")
    ps = S.psum("ps", [128, 8, 512], F32); b_ps = [Buf("ps%d" % i) for i in range(8)]
    st = {"bank": 0, "w": 0, "gt": 0, "tmp": 0, "hp": 0, "gm": 0}

    def nbank():
        i = st["bank"]; st["bank"] = (i + 1) % 6
        return i

    def nw():
        i = st["w"]; st["w"] = (i + 1) % NW
        return i

    def ntmp():
        i = st["tmp"]; st["tmp"] = (i + 1) % 4
        return i

    def load_w(wi, src2d, nk, c0, ncols, k_off=0):
        srcv = src2d.rearrange("(k p) c -> p k c", p=128)
        h = max(1, nk // 2)
        for k0 in range(0, nk, h):
            S.dma("pool", lambda e: e.dma_start(out=W[wi][:, k_off + k0:k_off + k0 + h, 0:ncols],
                                                in_=srcv[:, k0:k0 + h, c0:c0 + ncols]), writes=[b_W[wi]])

    S.op("dve", lambda e: e.memset(ones[:], 1.0), writes=[b_ones])
    S.op("dve", lambda e: e.memset(onesf[:], 1.0), writes=[b_onesf])
    for dst, bd, src in ((cin, b_cin, cT), (badat, b_badat, bada), (gmoet, b_gmoet, gmoe), (gfint, b_gfint, gfin),
                         (brt, b_brt, br), (idt, b_idt, ident)):
        S.dma("sp", lambda e: e.dma_start(out=dst[:], in_=src[:, :]), writes=[bd])
    S.dma("sp", lambda e: e.dma_start(out=wrt[:], in_=wr.rearrange("(k p) e -> p k e", p=128)), writes=[b_wrt])
    S.op("act", lambda e: e.activation(out=cact[:], in_=cin[:], func=AF.Silu), reads=[b_cin], writes=[b_cact])

    mb = nbank()
    for g in range(16):
        wi = nw()
        load_w(wi, wada, NK, 4096 + g * 512, 512)
        for m in range(4):
            n = g * 4 + m
            for k in range(NK):
                S.op("pe", lambda e: e.matmul(ps[:, mb, n:n + 1], lhsT=W[wi][:, k, m * 128:(m + 1) * 128], rhs=cact[:, k:k + 1],
                                              start=(k == 0), stop=(k == NK - 1)), reads=[b_W[wi], b_cact], writes=[b_ps[mb]])
    S.op("dve", lambda e: e.tensor_tensor(out=modt[:], in0=ps[:, mb, 0:64], in1=badat[:], op=ALU.add),
         reads=[b_ps[mb], b_badat], writes=[b_modt])
    S.op("dve", lambda e: e.scalar_tensor_tensor(out=s1m[:], in0=modt[:, 32:48], scalar=1.0, in1=gmoet[:], op0=ALU.add, op1=ALU.mult),
         reads=[b_modt, b_gmoet], writes=[b_s1m])

    def rms_rstd(src3, bsrcs):
        S.op("act", lambda e: e.activation(out=scr[:, 0:NK, :], in_=src3, func=AF.Square), reads=bsrcs, writes=[b_scr])
        bk = nbank()
        for k in range(NK):
            S.op("pe", lambda e: e.matmul(ps[:, bk, :], lhsT=ones[:], rhs=scr[:, k, :], start=(k == 0), stop=(k == NK - 1)),
                 reads=[b_scr, b_ones], writes=[b_ps[bk]])
        S.op("act", lambda e: e.activation(out=lnt[:], in_=ps[:, bk, :], func=AF.Ln, scale=1.0 / D, bias=EPS),
             reads=[b_ps[bk]], writes=[b_lnt])
        S.op("act", lambda e: e.activation(out=rstd[:], in_=lnt[:], func=AF.Exp, scale=-0.5), reads=[b_lnt], writes=[b_rstd])

    xTv = xT.rearrange("(k p) t -> p k t", p=128)
    gTv = gT.rearrange("(b n p) t -> p b n t", b=3, p=128)
    x2v = x2T.rearrange("(k p) t -> p k t", p=128) if x2T is not None else None
    xfv = xfT.rearrange("(k p) t -> p k t", p=128) if xfT is not None else None

    for t in range(NT):
        ts = slice(t * 512, (t + 1) * 512)
        for k0 in range(0, NK, 4):
            S.dma("sp", lambda e: e.dma_start(out=xt[:, k0:k0 + 4, :], in_=xTv[:, k0:k0 + 4, ts]), reads=[b_xsrc], writes=b_xt[k0:k0 + 4])
        for i_ in range(2):
            S.dma("sp", lambda e: e.dma_start(out=scr[:, i_:8:2, :], in_=Yl[i_, :, :, ts].rearrange("j p t -> p j t")), reads=[b_Yg], writes=[b_scr])
            S.dma("sp", lambda e: e.dma_start(out=scr[:, 12 + i_:20:2, :], in_=Yl[3 + i_, :, :, ts].rearrange("j p t -> p j t")), reads=[b_Yg], writes=[b_scr])
        S.dma("sp", lambda e: e.dma_start(out=scr[:, 8:12, :], in_=Yl[2, :, :, ts].rearrange("j p t -> p j t")), reads=[b_Yg], writes=[b_scr])
        for ng in range(4):
            wa = nw(); load_w(wa, womla, 8, ng * 512, 512); load_w(wa, wodil, 4, ng * 512, 512, k_off=8)
            wb_ = nw(); load_w(wb_, wosb, 8, ng * 512, 512)
            for m in range(4):
                n = ng * 4 + m
                gi = st["gt"]; st["gt"] ^= 1
                S.dma("sp", lambda e: e.dma_start(out=gt[gi][:], in_=gTv[:, :, n, ts]), reads=[b_gT], writes=[b_gt[gi]])
                banks = []
                for brn, (wi, koff, nk, yoff) in enumerate(((wa, 0, 8, 0), (wa, 8, 4, 8), (wb_, 0, 8, 12))):
                    bk = nbank(); banks.append(bk)
                    for k in range(nk):
                        S.op("pe", lambda e: e.matmul(ps[:, bk, :], lhsT=W[wi][:, koff + k, m * 128:(m + 1) * 128], rhs=scr[:, yoff + k, :],
                                                      start=(k == 0), stop=(k == nk - 1)), reads=[b_W[wi], b_scr], writes=[b_ps[bk]])
                ta = ntmp(); tb = ntmp()
                S.op("dve", lambda e: e.tensor_tensor(out=tmp[ta][:], in0=ps[:, banks[0], :], in1=gt[gi][:, 0, :], op=ALU.mult),
                     reads=[b_ps[banks[0]], b_gt[gi]], writes=[b_tmp[ta]])
                S.op("dve", lambda e: e.tensor_tensor(out=tmp[tb][:], in0=ps[:, banks[1], :], in1=gt[gi][:, 1, :], op=ALU.mult),
                     reads=[b_ps[banks[1]], b_gt[gi]], writes=[b_tmp[tb]])
                S.op("pool", lambda e: e.tensor_tensor(out=tmp[ta][:], in0=tmp[ta][:], in1=tmp[tb][:], op=ALU.add),
                     reads=[b_tmp[ta], b_tmp[tb]], writes=[b_tmp[ta]])
                S.op("dve", lambda e: e.tensor_tensor(out=tmp[tb][:], in0=ps[:, banks[2], :], in1=gt[gi][:, 2, :], op=ALU.mult),
                     reads=[b_ps[banks[2]], b_gt[gi]], writes=[b_tmp[tb]])
                S.op("pool", lambda e: e.tensor_tensor(out=mg[:, n, :], in0=tmp[ta][:], in1=tmp[tb][:], op=ALU.add),
                     reads=[b_tmp[ta], b_tmp[tb]], writes=[b_mg])
        for ng in range(4):
            wi = nw(); load_w(wi, wout, NK, ng * 512, 512)
            for m in range(4):
                n = ng * 4 + m
                bk = nbank()
                for k in range(NK):
                    S.op("pe", lambda e: e.matmul(ps[:, bk, :], lhsT=W[wi][:, k, m * 128:(m + 1) * 128], rhs=mg[:, k, :],
                                                  start=(k == 0), stop=(k == NK - 1)), reads=[b_W[wi], b_mg], writes=[b_ps[bk]])
                S.op("dve", lambda e: e.scalar_tensor_tensor(out=xt[:, n, :], in0=ps[:, bk, :], scalar=modt[:, n:n + 1], in1=xt[:, n, :],
                                                             op0=ALU.mult, op1=ALU.add), reads=[b_ps[bk], b_modt, b_xt[n]], writes=[b_xt[n]])
        if DBG:
            x1v = x1T.rearrange("(k p) t -> p k t", p=128)
            for k0 in range(0, NK, 4):
                S.dma("sp", lambda e: e.dma_start(out=x1v[:, k0:k0 + 4, ts], in_=xt[:, k0:k0 + 4, :]), reads=b_xt[k0:k0 + 4], writes=[b_x1T])
        rms_rstd(xt[:, :, :], b_xt)
        lb = nbank()
        for k in range(NK):
            ta = ntmp(); tb = ntmp()
            S.op("dve", lambda e: e.tensor_tensor(out=tmp[ta][:], in0=xt[:, k, :], in1=rstd[:], op=ALU.mult),
                 reads=[b_xt[k], b_rstd], writes=[b_tmp[ta]])
            S.op("act", lambda e: e.activation(out=tmp[tb][:], in_=tmp[ta][:], func=AF.Identity, scale=s1m[:, k:k + 1], bias=modt[:, 16 + k:17 + k]),
                 reads=[b_tmp[ta], b_s1m, b_modt], writes=[b_tmp[tb]])
            S.op("act", lambda e: e.activation(out=a2b[:, k, :], in_=tmp[ta][:], func=AF.Identity, scale=s1m[:, k:k + 1], bias=modt[:, 16 + k:17 + k]),
                 reads=[b_tmp[ta], b_s1m, b_modt], writes=[b_a2b])
            S.op("pe", lambda e: e.matmul(ps[0:16, lb, :], lhsT=wrt[:, k, :], rhs=tmp[tb][:], start=(k == 0), stop=(k == NK - 1)),
                 reads=[b_wrt, b_tmp[tb]], writes=[b_ps[lb]])
        S.op("act", lambda e: e.activation(out=lt[:], in_=ps[0:16, lb, :], func=AF.Copy), reads=[b_ps[lb]], writes=[b_lt])
        tb_ = nbank()
        for sub in range(4):
            S.op("pe", lambda e: e.matmul(ps[:, tb_, sub * 16:(sub + 1) * 16], lhsT=lt[0:16, sub * 128:(sub + 1) * 128], rhs=idt[0:16, 0:16],
                                          start=True, stop=True), reads=[b_lt, b_idt], writes=[b_ps[tb_]])
        S.op("act", lambda e: e.activation(out=sc[:], in_=ps[:, tb_, 0:64], func=AF.Sigmoid), reads=[b_ps[tb_]], writes=[b_sc])
        for sub in range(4):
            S.op("dve", lambda e: e.tensor_tensor(out=bi[:, sub * 16:(sub + 1) * 16], in0=sc[:, sub * 16:(sub + 1) * 16], in1=brt[:], op=ALU.add),
                 reads=[b_sc, b_brt], writes=[b_bi])
        v3 = lambda tl: tl[:, :].rearrange("p (g e) -> p g e", e=4)
        S.op("dve", lambda e: e.tensor_reduce(out=m1[:], in_=v3(bi), axis=AX.X, op=ALU.max), reads=[b_bi], writes=[b_m1])
        for ee in range(4):
            S.op("dve", lambda e: e.tensor_tensor(out=v3(eq)[:, :, ee], in0=v3(bi)[:, :, ee], in1=m1[:], op=ALU.is_equal),
                 reads=[b_bi, b_m1], writes=[b_eq])
        S.op("dve", lambda e: e.scalar_tensor_tensor(out=bi2[:], in0=eq[:], scalar=-BIGR, in1=bi[:], op0=ALU.mult, op1=ALU.add),
             reads=[b_eq, b_bi], writes=[b_bi2])
        S.op("dve", lambda e: e.tensor_reduce(out=m2[:], in_=v3(bi2), axis=AX.X, op=ALU.max), reads=[b_bi2], writes=[b_m2])
        S.op("dve", lambda e: e.tensor_tensor(out=gs[:], in0=m1[:], in1=m2[:], op=ALU.add), reads=[b_m1, b_m2], writes=[b_gs])
        S.op("dve", lambda e: e.tensor_reduce(out=gmax[:], in_=gs[:, :].rearrange("p (s g) -> p s g", g=4), axis=AX.X, op=ALU.max),
             reads=[b_gs], writes=[b_gmax])
        for g in range(4):
            S.op("dve", lambda e: e.tensor_tensor(out=gsel[:, :].rearrange("p (s g) -> p s g", g=4)[:, :, g],
                                                  in0=gs[:, :].rearrange("p (s g) -> p s g", g=4)[:, :, g], in1=gmax[:], op=ALU.is_equal),
                 reads=[b_gs, b_gmax], writes=[b_gsel])
        for ee in range(4):
            S.op("dve", lambda e: e.tensor_tensor(out=v3(eq)[:, :, ee], in0=v3(bi)[:, :, ee], in1=m2[:], op=ALU.is_ge),
                 reads=[b_bi, b_m2], writes=[b_eq])
            S.op("dve", lambda e: e.tensor_tensor(out=v3(eq)[:, :, ee], in0=v3(eq)[:, :, ee], in1=gsel[:], op=ALU.mult),
                 reads=[b_eq, b_gsel], writes=[b_eq])
        S.op("dve", lambda e: e.tensor_tensor(out=wsel[:], in0=sc[:], in1=eq[:], op=ALU.mult), reads=[b_sc, b_eq], writes=[b_wsel])
        S.op("dve", lambda e: e.tensor_reduce(out=den[:], in_=wsel[:, :].rearrange("p (s e) -> p s e", e=16), axis=AX.X, op=ALU.add),
             reads=[b_wsel], writes=[b_den])
        S.op("dve", lambda e: e.reciprocal(out=den[:], in_=den[:]), reads=[b_den], writes=[b_den])
        for sub in range(4):
            S.op("dve", lambda e: e.tensor_scalar(out=gates[:, sub * 16:(sub + 1) * 16], in0=wsel[:, sub * 16:(sub + 1) * 16],
                                                  scalar1=den[:, sub:sub + 1], scalar2=None, op0=ALU.mult), reads=[b_wsel, b_den], writes=[b_gates])
        if DBG and t == 0:
            for i_, (tl, bb, w_) in enumerate(((sc, b_sc, 64), (bi, b_bi, 64), (m1, b_m1, 16), (m2, b_m2, 16), (gsel, b_gsel, 16), (eq, b_eq, 64), (gates, b_gates, 64), (den, b_den, 4))):
                S.dma("sp", lambda e: e.dma_start(out=dbg[:, i_, 0:w_], in_=tl[:, 0:w_]), reads=[bb], writes=[b_dbg])
            a2v = a2T.rearrange("(k p) t -> p k t", p=128)
            S.dma("sp", lambda e: e.dma_start(out=a2v[:, :, ts], in_=a2b[:, :, :]), reads=[b_a2b], writes=[b_a2T])
        for ex in range(NE):
            w1 = nw(); load_w(w1, wg[ex], NK, 0, 512)
            w2 = nw(); load_w(w2, wu[ex], NK, 0, 512)
            w3 = nw()
            wdv = wd[ex].rearrange("(k p) c -> p k c", p=128)
            W3v = W[w3][:, :, :].rearrange("p (k a) c -> p k (a c)", a=4)
            for k0 in range(0, 4, 2):
                S.dma("pool", lambda e: e.dma_start(out=W3v[:, k0:k0 + 2, :], in_=wdv[:, k0:k0 + 2, :]), writes=[b_W[w3]])
            gb = 6 + (ex % 2)
            for sub in range(4):
                gi = st["gm"]; st["gm"] ^= 1
                S.op("dve", lambda e: e.tensor_scalar(out=Gm[gi][:], in0=onesf[:], scalar1=gates[:, sub * 16 + ex:sub * 16 + ex + 1], scalar2=None,
                                                      op0=ALU.mult), reads=[b_onesf, b_gates], writes=[b_Gm[gi]])
                S.op("pe", lambda e: e.matmul(ps[:, gb, sub * 128:(sub + 1) * 128], lhsT=Gm[gi][:], rhs=idt[:], start=True, stop=True),
                     reads=[b_Gm[gi], b_idt], writes=[b_ps[gb]])
            hi = st["hp"]; st["hp"] ^= 1
            for dc in range(4):
                b1 = nbank()
                for k in range(NK):
                    S.op("pe", lambda e: e.matmul(ps[:, b1, :], lhsT=W[w1][:, k, dc * 128:(dc + 1) * 128], rhs=a2b[:, k, :],
                                                  start=(k == 0), stop=(k == NK - 1)), reads=[b_W[w1], b_a2b], writes=[b_ps[b1]])
                b2 = nbank()
                for k in range(NK):
                    S.op("pe", lambda e: e.matmul(ps[:, b2, :], lhsT=W[w2][:, k, dc * 128:(dc + 1) * 128], rhs=a2b[:, k, :],
                                                  start=(k == 0), stop=(k == NK - 1)), reads=[b_W[w2], b_a2b], writes=[b_ps[b2]])
                ta = ntmp(); tb = ntmp()
                S.op("act", lambda e: e.activation(out=tmp[ta][:], in_=ps[:, b1, :], func=AF.Silu), reads=[b_ps[b1]], writes=[b_tmp[ta]])
                S.op("dve", lambda e: e.tensor_tensor(out=tmp[tb][:], in0=tmp[ta][:], in1=ps[:, b2, :], op=ALU.mult),
                     reads=[b_tmp[ta], b_ps[b2]], writes=[b_tmp[tb]])
                S.op("dve", lambda e: e.tensor_tensor(out=hp[hi][:, dc, :], in0=tmp[tb][:], in1=ps[:, gb, :], op=ALU.mult),
                     reads=[b_tmp[tb], b_ps[gb]], writes=[b_hp[hi]])
            for n in range(NK):
                bk = nbank()
                for dc in range(4):
                    S.op("pe", lambda e: e.matmul(ps[:, bk, :], lhsT=W3v[:, dc, n * 128:(n + 1) * 128], rhs=hp[hi][:, dc, :],
                                                  start=(dc == 0), stop=(dc == 3)), reads=[b_W[w3], b_hp[hi]], writes=[b_ps[bk]])
                S.op("dve", lambda e: e.scalar_tensor_tensor(out=xt[:, n, :], in0=ps[:, bk, :], scalar=modt[:, 48 + n:49 + n], in1=xt[:, n, :],
                                                             op0=ALU.mult, op1=ALU.add), reads=[b_ps[bk], b_modt, b_xt[n]], writes=[b_xt[n]])
        if x2v is not None:
            for k0 in range(0, NK, 4):
                S.dma("sp", lambda e: e.dma_start(out=x2v[:, k0:k0 + 4, ts], in_=xt[:, k0:k0 + 4, :]), reads=b_xt[k0:k0 + 4], writes=[b_x2T])
        if xfv is not None:
            rms_rstd(xt[:, :, :], b_xt)
            for k in range(NK):
                ta = ntmp()
                S.op("dve", lambda e: e.scalar_tensor_tensor(out=tmp[ta][:], in0=xt[:, k, :], scalar=gfint[:, k:k + 1], in1=rstd[:],
                                                             op0=ALU.mult, op1=ALU.mult), reads=[b_xt[k], b_gfint, b_rstd], writes=[b_tmp[ta]])
                S.dma("sp", lambda e: e.dma_start(out=xfv[:, k, ts], in_=tmp[ta][:]), reads=[b_tmp[ta]], writes=[b_xfT])


def build_fused():
    nc = bass.Bass("TRN2", target_bir_lowering=False)
    ext = lambda n, s, dt: nc.dram_tensor(n, s, dt, kind="ExternalInput").ap()
    d = {}
    d['xT'] = ext("xT", [2048, 2048], F32)
    jt = ext("jt", [1, 2], I32)
    d['cT'] = ext("cT", [128, 16], F32)
    d['pos'] = ext("pos", [1, 2048], I32)
    d['invf'] = ext("invf", [64, 1], F32)
    d['sgn'] = ext("sgn", [64, 1], F32)
    d['wada'] = ext("wada", [2, 2048, 12288], F32)
    d['bada_a'] = ext("bada_a", [2, 128, 32], F32)
    d['bada_c'] = ext("bada_c", [2, 128, 64], F32)
    d['gmix'] = ext("gmix", [2, 128, 16], F32)
    d['gq'] = ext("gq", [2, 128, 4], F32)
    d['gkv'] = ext("gkv", [2, 128, 4], F32)
    d['gmoe'] = ext("gmoe", [2, 128, 16], F32)
    d['gfin'] = ext("gfin", [128, 16], F32)
    d['win'] = ext("win", [2, 2048, 14912], F32)
    d['wsw'] = ext("wsw", [2, 2048, 64], F32)
    d['wuq'] = ext("wuq", [2, 512, 1536], F32)
    d['wuqs'] = ext("wuqs", [2, 512, 512], F32)
    d['wukv'] = ext("wukv", [2, 512, 2048], F32)
    d['womla'] = ext("womla", [2, 1024, 2048], F32)
    d['wodil'] = ext("wodil", [2, 512, 2048], F32)
    d['wosb'] = ext("wosb", [2, 1024, 2048], F32)
    d['wout'] = ext("wout", [2, 2048, 2048], F32)
    d['wr'] = ext("wr", [2048, 16], F32)
    d['br'] = ext("br", [128, 16], F32)
    d['wg'] = ext("wg", [2, 16, 2048, 512], F32)
    d['wu'] = ext("wu", [2, 16, 2048, 512], F32)
    d['wd'] = ext("wd", [2, 16, 512, 2048], F32)
    d['ident'] = ext("ident", [128, 128], F32)
    d['maskc'] = ext("maskc", [128, 4, 512], BF16)
    d['masks'] = ext("masks", [128, 4, 512], BF16)
    d['maskd'] = ext("maskd", [128, 256], F32)
    d['m1'] = ext("m1", [128, 128], BF16)
    d['m2'] = ext("m2", [128, 128], BF16)
    d['nslope'] = ext("nslope", [128, 3], F32)
    d['dposk'] = ext("dposk", [3, 128, 64], I32)
    d['dposq'] = ext("dposq", [3, 8192], I32)
    xfT = nc.dram_tensor("xfT", [2048, 2048], F32, kind="ExternalOutput").ap()
    b_xfT = Buf("xfT")
    x2s = nc.dram_tensor("x2scr", [2048, 2048], F32).ap()
    b_x2s = Buf("x2s")
    b_x0 = Buf("x0")
    GROUPS = [[0, 1, 2, 3], [4, 5, 6, 7]]

    S = Sched(nc)
    S.enable_dyn(jt[:, :])
    CH = 128 * 4096
    QKs_h = nc.dram_tensor("QKs", [32, 128, 4096], BF16)
    Vs_h = nc.dram_tensor("Vs", [16, 128, 4096], BF16)
    QKg_h = nc.dram_tensor("QKg", [32, 512, 4096], BF16)
    Vg_h = nc.dram_tensor("Vg", [16, 512, 4096], BF16)
    Ys_h = nc.dram_tensor("Ys", [10, 128, 4096], BF16)
    Yg_h = nc.dram_tensor("Yg", [10, 512, 4096], BF16)
    gsc = nc.dram_tensor("gscr", [6144, 2048], F32).ap()
    QKl_h = nc.dram_tensor("QKl", [4096, 4096], BF16)
    Vl_h = nc.dram_tensor("Vl", [2048, 4096], BF16)
    Yl_h = nc.dram_tensor("Yl", [2560, 2048], BF16)
    for l in range(2):
        b_QKs, b_Vs, b_QKg, b_Vg, b_Ys, b_Yg, b_g = (Buf(n) for n in ("QKs", "Vs", "QKg", "Vg", "Ys", "Yg", "g"))
        b_QKl, b_Vl, b_Yl = Buf("QKl"), Buf("Vl"), Buf("Yl")
        xsrc = d['xT'] if l == 0 else x2s
        b_xsrc = b_x0 if l == 0 else b_x2s
        QKs_v = QKs_h.ap().rearrange("c p (a t) -> (c p a) t", a=4).rearrange("(s u r) t -> s u r t", s=4, u=2)
        Vs_v = Vs_h.ap().rearrange("c p (a d) -> (c p a) d", a=4).rearrange("(s t) d -> s t d", s=4)
        S.begin_phase()
        b_QKs2 = [Buf("QKs0"), Buf("QKs1")]
        b_Vs2 = [Buf("Vs0"), Buf("Vs1")]

        pending = []

        def after_sup(sup):
            for sh in range(4):
                for q in range(4):
                    pending.append((QKs_h, QKg_h, sh * 8 + sup * 4 + q, b_QKs2[sup], b_QKg))
            for sh in range(4):
                for q in range(2):
                    pending.append((Vs_h, Vg_h, sh * 4 + sup * 2 + q, b_Vs2[sup], b_Vg))
            if sup == 1:
                while pending:
                    tick()

        def tick():
            if pending:
                sh_, gh_, c, br_, bw_ = pending.pop(0)
                S.cc(lambda e: e.collective_compute("AllGather", ALU.bypass, replica_groups=GROUPS, ins=[sh_.ap()[c].opt()],
                                                    outs=[gh_.ap()[c].opt()]), reads=[br_], writes=[bw_])
        emit_A(S, nc, d, l, xsrc, QKs_v, Vs_v, gsc, b_QKs2, b_Vs2, b_g, after_sup=after_sup, tick=tick)
        S.end_phase()
        S.begin_phase()
        for hh in range(2):
            S.dma_dyn(QKl_h.ap()[hh * 2048:(hh + 1) * 2048, :], QKg_h, 8 * 4 * CH, hh * 2048 * 4096, [[4096, 2048], [1, 4096]],
                      reads=[b_QKg], writes=[b_QKl])
        S.dma_dyn(Vl_h.ap()[:, :], Vg_h, 4 * 4 * CH, 0, [[4096, 2048], [1, 4096]], reads=[b_Vg], writes=[b_Vl])
        QKl = QKl_h.ap().rearrange("(c r p) (a t) -> c r (p a) t", c=8, r=4, a=4)
        Vl = Vl_h.ap().rearrange("(c r p) (a d) -> c r (p a) d", c=4, r=4, a=4)
        Ys_v = Ys_h.ap().rearrange("(th b) p t -> th b p t", th=2)
        S.barrier()
        emit_B(S, nc, d, QKl, Vl, Ys_v, b_QKl, b_Vl, b_Ys, do_mla=False, do_sb=False, do_dil=False, mix_head=0)
        S.end_phase()
        S.begin_phase()
        emit_B(S, nc, d, QKl, Vl, Ys_v, b_QKl, b_Vl, b_Ys, do_mla=False, do_sb=False, do_dil=False, mix_head=1)
        S.end_phase()
        S.begin_phase()
        emit_B(S, nc, d, QKl, Vl, Ys_v, b_QKl, b_Vl, b_Ys, do_mla=False, do_sb=False, do_dil=True)
        for c in range(10):
            S.cc(lambda e: e.collective_compute("AllGather", ALU.bypass, replica_groups=GROUPS, ins=[Ys_h.ap()[c].opt()], outs=[Yg_h.ap()[c].opt()]),
                 reads=[b_Ys], writes=[b_Yg])
        S.end_phase()
        S.begin_phase()
        for b0, nb_ in ((0, 3), (3, 2)):
            S.dma_dyn(Yl_h.ap()[b0 * 512:(b0 + nb_) * 512, :], Yg_h, 1, b0 * 4 * CH, [[4 * CH, nb_], [4096, 512], [1, 2048]],
                      reads=[b_Yg], writes=[b_Yl], which=1)
        Yl = Yl_h.ap().rearrange("(b j p) t -> b j p t", b=5, j=4)
        emit_C(S, nc, d, l, Yl, gsc, xsrc, x2s if l == 0 else None, xfT if l == 1 else None,
               b_Yl, b_g, b_xsrc, b_x2s, b_xfT)
        S.end_phase()
    S.close()
    return nc


_PROG = {}


def _f(a):
    return np.ascontiguousarray(a)


def kernel(**inputs):
    inp = {k: np.asarray(v) for k, v in inputs.items()}
    B_, S_ = inp['x'].shape[:2]
    cores = list(range(8))
    pos = inp['positions'].astype(np.int32)
    perm = [np.concatenate([np.arange(s, S_, r) for s in range(r)]) for r in RATES]
    slopes = (np.float32(2.0) ** (np.float32(-8.0) * np.arange(1, 13, dtype=np.float32) / np.float32(12))).reshape(3, 4)
    half = 32
    invf = (np.float32(10000.0) ** (-(np.arange(half, dtype=np.float32)) / np.float32(half))).astype(np.float32)
    wu_ = inp['w_uq']
    wuqs = np.stack([np.concatenate([np.concatenate([wu_[l][:, h * 192 + 160:h * 192 + 192], wu_[l][:, h * 192 + 128:h * 192 + 160]], axis=1)
                                     for h in range(8)], axis=1) for l in range(2)])
    wi_ = inp['w_in']
    lay = lambda a, n: _f(a.reshape(a.shape[0], n, 128).transpose(0, 2, 1))
    shared = {
        'invf': _f(np.concatenate([invf, invf])[:, None]),
        'sgn': _f(np.concatenate([-np.ones(32, np.float32), np.ones(32, np.float32)])[:, None]),
        'wada': _f(inp['w_ada']),
        'bada_a': lay(inp['b_ada'][:, :4096], 32),
        'bada_c': lay(inp['b_ada'][:, 4096:], 64),
        'gmix': lay(inp['g_mix'], 16), 'gq': lay(inp['g_q'], 4), 'gkv': lay(inp['g_kv'], 4), 'gmoe': lay(inp['g_moe'], 16),
        'gfin': _f(inp['g_final'].reshape(16, 128).T),
        'win': _f(wi_), 'wsw': _f(np.concatenate([wi_[:, :, 1056:1088], wi_[:, :, 1024:1056]], axis=2)),
        'wuq': _f(wu_), 'wuqs': _f(wuqs), 'wukv': _f(inp['w_ukv']),
        'womla': _f(inp['w_o_mla']), 'wodil': _f(inp['w_o_dil']), 'wosb': _f(inp['w_o_sb']), 'wout': _f(inp['w_out']),
        'wr': _f(inp['w_router']), 'br': _f(np.broadcast_to(inp['b_router'][None, :], (128, 16))),
        'wg': _f(inp['w_gate']), 'wu': _f(inp['w_up']), 'wd': _f(inp['w_down']), 'ident': np.eye(128, dtype=np.float32),
    }
    shared.update(consts_B())
    maps = []
    for c in cores:
        b, j = c // 4, c % 4
        m = dict(shared)
        m['xT'] = _f(inp['x'][b, j * 2048:(j + 1) * 2048, :].T)
        m['jt'] = np.array([[j, (j // 2) * 5 * 4 * 128 * 4096 + (j % 2) * 2048]], np.int32)
        m['cT'] = _f(inp['c'][b].reshape(16, 128).T)
        m['pos'] = _f(pos[b, j * 2048:(j + 1) * 2048][None, :])
        pp = np.stack([pos[b][perm[g]] for g in range(3)]).astype(np.int32)
        m['dposq'] = _f(pp)
        m['dposk'] = _f(pp.reshape(3, S_ // 128, 128).transpose(0, 2, 1))
        m['nslope'] = _f(np.broadcast_to(-slopes[:, j][None, :], (128, 3)).astype(np.float32))
        maps.append(m)
    if "f" not in _PROG:
        _PROG["f"] = build_fused()
    res = run_bass_kernel_spmd(_PROG["f"], maps, core_ids=cores).results
    out = np.empty(inp['x'].shape, dtype=np.float32)
    for c in cores:
        b, j = c // 4, c % 4
        out[b, j * 2048:(j + 1) * 2048, :] = np.asarray(res[c]['xfT']).T
    return out
```

```python
import math
import numpy as np
import concourse.bass as bass
import concourse.mybir as mybir
from concourse.bass_utils import run_bass_kernel_spmd

F32 = mybir.dt.float32
BF16 = mybir.dt.bfloat16
I32 = mybir.dt.int32
AF = mybir.ActivationFunctionType
ALU = mybir.AluOpType
AX = mybir.AxisListType


class Buf:
    __slots__ = ("name", "w", "r")

    def __init__(self, name):
        self.name = name
        self.w = None
        self.r = {}


class _Rec:
    def __init__(self):
        self.call = None

    def __getattr__(self, name):
        def f(*a, **kw):
            self.call = (name, a, kw)
            return self
        return f


def _record(fn):
    r = _Rec()
    fn(r)
    assert r.call is not None
    return r.call


class Sched:
    ENGS = ("pe", "act", "dve", "pool", "sp")
    NDMA = 12

    def __init__(self, nc):
        self.nc = nc
        self.streams = {e: [] for e in self.ENGS}
        self.cnt = {e: 0 for e in self.ENGS}
        self.seen = {e: {} for e in self.ENGS}
        self.dma_i = {"sp": 0, "pool": 0, "act": 0}
        self.dma_val = {}
        self.sems = {}
        self._ctx = []
        self._perm = []
        self.cc_val = {}
        self._phase_mark = 0
        self.uses_dyn = False
        self._phase_no = 0
        self.jt_ap = None
        self.jsb = None
        self.jt_cnt = 0
        for e in ("pe", "act", "dve", "pool"):
            self._mk_sem("E_" + e)
        for q in ("sp", "pool", "act"):
            for k in range(self.NDMA):
                self._mk_sem("D_%s%d" % (q, k))
                self.dma_val["D_%s%d" % (q, k)] = 0

    def _mk_sem(self, key):
        cm = self.nc.semaphore(key)
        self.sems[key] = cm.__enter__()
        self._perm.append(cm)

    _mk_sem_perm = _mk_sem

    def enable_dyn(self, jt_ap):
        self.jt_ap = jt_ap
        self._mk_sem("JT")
        cm = self.nc.sbuf_tensor("jsb", [1, 2], I32)
        self.jsb = cm.__enter__()
        self._perm.append(cm)

    def sbuf(self, name, shape, dtype):
        cm = self.nc.sbuf_tensor("%s_p%d" % (name, self._phase_no), shape, dtype)
        t = cm.__enter__()
        self._ctx.append(cm)
        return t

    def psum(self, name, shape, dtype):
        cm = self.nc.psum_tensor("%s_p%d" % (name, self._phase_no), shape, dtype)
        t = cm.__enter__()
        self._ctx.append(cm)
        return t

    def _deps(self, eng, reads, writes):
        deps = {}

        def add(tok):
            if tok is None:
                return
            k, v = tok
            if deps.get(k, -1) < v:
                deps[k] = v
        for b in reads:
            add(b.w)
        for b in writes:
            add(b.w)
            for k, v in b.r.items():
                add((k, v))
        out = []
        seen = self.seen[eng]
        for k, v in deps.items():
            if eng == "pe" and k == "E_pe":
                continue
            if seen.get(k, -1) >= v:
                continue
            seen[k] = v
            out.append((k, v))
        return out

    def _mark(self, tok, reads, writes):
        k, v = tok
        for b in reads:
            if b.r.get(k, -1) < v:
                b.r[k] = v
        for b in writes:
            b.w = tok
            b.r = {}

    def op(self, eng, fn, reads=(), writes=()):
        waits = self._deps(eng, reads, writes)
        self.cnt[eng] += 1
        tok = ("E_" + eng, self.cnt[eng])
        self.streams[eng].append((waits, _record(fn), tok[0], 1))
        self._mark(tok, reads, writes)

    def dma(self, q, fn, reads=(), writes=()):
        i = self.dma_i[q]
        self.dma_i[q] += 1
        key = "D_%s%d" % (q, i % self.NDMA)
        waits = self._deps(q, reads, writes)
        prev = self.dma_val[key]
        if prev > 0 and self.seen[q].get(key, -1) < prev:
            self.seen[q][key] = prev
            waits.append((key, prev))
        self.dma_val[key] = prev + 16
        tok = (key, prev + 16)
        self.streams[q].append((waits, _record(fn), key, 16))
        self._mark(tok, reads, writes)

    def begin_phase(self):
        self._phase_mark = len(self._ctx)
        self._phase_no += 1

    def barrier(self, wait_cc=True):
        allv = {}
        for e in ("pe", "act", "dve", "pool"):
            if self.cnt[e] > 0:
                allv["E_" + e] = self.cnt[e]
        for k, v in self.dma_val.items():
            if v > 0:
                allv[k] = v
        for k, v in self.cc_val.items():
            if v > 0 and wait_cc:
                allv[k] = v
        for eng in self.ENGS:
            waits = []
            for k, v in allv.items():
                if self.seen[eng].get(k, -1) < v:
                    self.seen[eng][k] = v
                    waits.append((k, v))
            self.streams[eng].append((waits, None, None, 0))

    def end_phase(self, wait_cc=True):
        self.barrier(wait_cc)
        self.emit()
        self.streams = {e: [] for e in self.ENGS}
        while len(self._ctx) > self._phase_mark:
            self._ctx.pop().__exit__(None, None, None)

    def cc(self, fn, reads=(), writes=()):
        key = "CC"
        if key not in self.sems:
            self._mk_sem_perm(key)
            self.cc_val[key] = 0
        n = self.cc_val[key] + 1
        self.cc_val[key] = n
        waits = self._deps("pool", reads, writes)
        self.streams["pool"].append((waits, _record(fn), key, None))
        self._mark((key, n), reads, writes)

    def dma_dyn(self, out_ap, tensor, jmul, const, ap_list, reads=(), writes=(), which=0):
        q = "sp"
        i = self.dma_i[q]
        self.dma_i[q] += 1
        key = "D_%s%d" % (q, i % self.NDMA)
        waits = self._deps(q, reads, writes)
        prev = self.dma_val[key]
        if prev > 0 and self.seen[q].get(key, -1) < prev:
            self.seen[q][key] = prev
            waits.append((key, prev))
        self.dma_val[key] = prev + 16
        tok = (key, prev + 16)
        self.streams[q].append((waits, ("__dyn__", (out_ap, tensor, int(jmul), int(const), [list(x) for x in ap_list], which), {}), key, 16))
        self._mark(tok, reads, writes)
        self.uses_dyn = True

    def final_wait(self, eng, bufs):
        waits = self._deps(eng, bufs, ())
        self.streams[eng].append((waits, None, None, 0))

    def emit(self):
        nc = self.nc
        sems = self.sems
        streams = self.streams

        def run(engine, lst, regs=None):
            for waits, fn, key, inc in lst:
                for k, v in waits:
                    engine.wait_ge(sems[k], v)
                if fn is not None:
                    name, a, kw = fn
                    if name == "__dyn__":
                        out_ap, tensor, jmul, const, ap_list, which = a
                        rj, ro = regs[which], regs[2]
                        engine.reg_mul(ro, rj, jmul)
                        engine.reg_add(ro, ro, const)
                        ins = engine.dma_start(out=out_ap, in_=bass.AP(tensor, ro, ap_list))
                    else:
                        ins = getattr(engine, name)(*a, **kw)
                    if inc is None:
                        ins.then_inc(sems[key])
                    else:
                        ins.then_inc(sems[key], inc)

        with nc.Block() as block:
            @block.tensor
            def _(e):
                run(e, streams["pe"])

            @block.scalar
            def _(e):
                run(e, streams["act"])

            @block.vector
            def _(e):
                run(e, streams["dve"])

            @block.gpsimd
            def _(e):
                run(e, streams["pool"])

            @block.sync
            def _(e):
                if any(fn is not None and fn[0] == "__dyn__" for _, fn, _, _ in streams["sp"]):
                    self.jt_cnt += 1
                    with e.register("rj%d" % self.jt_cnt) as rj, e.register("ry%d" % self.jt_cnt) as ry, e.register("ro%d" % self.jt_cnt) as ro:
                        e.dma_start(out=self.jsb[:, :], in_=self.jt_ap).then_inc(sems["JT"], 16)
                        e.wait_ge(sems["JT"], 16 * self.jt_cnt)
                        e.reg_load(rj, self.jsb[0:1, 0:1])
                        e.reg_load(ry, self.jsb[0:1, 1:2])
                        run(e, streams["sp"], (rj, ry, ro))
                else:
                    run(e, streams["sp"])

    def close(self):
        for cm in reversed(self._ctx):
            cm.__exit__(None, None, None)
        self._ctx = []
        for cm in reversed(self._perm):
            cm.__exit__(None, None, None)
        self._perm = []


D = 2048
NK = 16
TS = 1024
TP = 128
EPS = 1e-6
TWO_PI = 2.0 * math.pi
C1 = 6.28125
C2 = TWO_PI - C1


def emit_A(S, nc, d, l, xT, QKs, Vs, gT, b_QKs_l, b_Vs_l, b_gT, ntok=2048, after_sup=None, tick=None):
    cT = d['cT']; wada = d['wada'][l]; bada = d['bada_a'][l]; gmix = d['gmix'][l]; win = d['win'][l]; wsw = d['wsw'][l]
    gq = d['gq'][l]; gkv = d['gkv'][l]; wuq = d['wuq'][l]; wuqs = d['wuqs'][l]; wukv = d['wukv'][l]
    pos = d['pos']; invf = d['invf']; sgn = d['sgn']
    b_QKs = b_QKs_l[0]; b_Vs = b_Vs_l[0]
    b_projT = b_QKs; b_mlaq = b_QKs; b_mlakv = b_QKs; b_mlakr = b_QKs
    aT = S.sbuf("aT", [128, NK, TS], BF16); b_aT = [Buf("aT%d" % i) for i in range(TS // TP)]
    wb = [S.sbuf("wb%d" % i, [128, NK, 512], BF16) for i in range(2)]; b_wb = [Buf("wb0"), Buf("wb1")]
    xt = S.sbuf("xt", [128, NK, TP], F32); b_xt = Buf("xt")
    sq = S.sbuf("sq", [128, NK, TP], BF16); b_sq = Buf("sq")
    rstd = S.sbuf("rstd", [128, 512], F32); b_rstd = Buf("rstd")
    lnt = S.sbuf("lnt", [128, 512], F32); b_lnt = Buf("lnt")
    tmp = [S.sbuf("tmp%d" % i, [128, 512], F32) for i in range(2)]; b_tmp = [Buf("tmp0"), Buf("tmp1")]
    ones = S.sbuf("ones", [128, 128], BF16); b_ones = Buf("ones")
    cin = S.sbuf("cin", [128, NK], F32); b_cin = Buf("cin")
    cact = S.sbuf("cact", [128, NK], BF16); b_cact = Buf("cact")
    badat = S.sbuf("badat", [128, 32], F32); b_badat = Buf("badat")
    gmixt = S.sbuf("gmixt", [128, NK], F32); b_gmixt = Buf("gmixt")
    modt = S.sbuf("modt", [128, 32], F32); b_modt = Buf("modt")
    s1 = S.sbuf("s1", [128, NK], F32); b_s1 = Buf("s1")
    cq = S.sbuf("cq", [128, 4, TS], F32); b_cq = Buf("cq")
    ckv = S.sbuf("ckv", [128, 4, TS], F32); b_ckv = Buf("ckv")
    kr = S.sbuf("kr", [64, TS], F32); b_kr = Buf("kr")
    krs = S.sbuf("krs", [64, TS], F32); b_krs = Buf("krs")
    wswb = S.sbuf("wswb", [128, NK, 64], BF16); b_wswb = Buf("wswb")
    wuqb = S.sbuf("wuqb", [128, 4, 1536], BF16); b_wuqb = Buf("wuqb")
    wuqsb = S.sbuf("wuqsb", [128, 4, 512], BF16); b_wuqsb = Buf("wuqsb")
    wukvb = S.sbuf("wukvb", [128, 4, 2048], BF16); b_wukvb = Buf("wukvb")
    gqt = S.sbuf("gqt", [128, 4], F32); b_gqt = Buf("gqt")
    gkvt = S.sbuf("gkvt", [128, 4], F32); b_gkvt = Buf("gkvt")
    ob = [S.sbuf("ob%d" % i, [128, TS], BF16) for i in range(2)]; b_ob = [Buf("ob0"), Buf("ob1")]
    of = [S.sbuf("of%d" % i, [128, TS], F32) for i in range(2)]; b_of = [Buf("of0"), Buf("of1")]
    lat = S.sbuf("lat", [128, 4, 512], BF16); b_lat = Buf("lat")
    posi = S.sbuf("posi", [64, 512], I32); b_posi = Buf("posi")
    ang = S.sbuf("ang", [64, 512], F32); b_ang = Buf("ang")
    kf = S.sbuf("kf", [64, 512], F32); b_kf = Buf("kf")
    ki = posi; b_ki = b_posi
    rr = S.sbuf("rr", [64, 512], F32); b_rr = Buf("rr")
    rc = S.sbuf("rc", [64, 512], F32); b_rc = Buf("rc")
    mm = S.sbuf("mm", [64, 512], F32); b_mm = Buf("mm")
    CS = S.sbuf("CS", [64, 512], F32); b_CS = Buf("CS")
    SN = S.sbuf("SN", [64, 512], F32); b_SN = Buf("SN")
    invft = S.sbuf("invft", [64, 1], F32); b_invft = Buf("invft")
    sgnt = S.sbuf("sgnt", [64, 1], F32); b_sgnt = Buf("sgnt")
    t1 = ang; b_t1 = b_ang
    t2 = kf; b_t2 = b_kf
    ps = S.psum("ps", [128, 8, 512], F32); b_ps = [Buf("ps%d" % i) for i in range(8)]
    st = {"bank": 0, "w": 0, "ob": 0, "of": 0, "tmp": 0, "ev": 0}

    def nbank():
        i = st["bank"]; st["bank"] = (i + 1) % 8
        return i

    def load_w(dst, bdst, src, nk, c0, ncols):
        srcv = src.rearrange("(k p) c -> p k c", p=128)
        h = max(1, nk // 2)
        for k0 in range(0, nk, h):
            S.dma("pool", lambda e, k0=k0: e.dma_start(out=dst[:, k0:k0 + h, 0:ncols],
                                                      in_=srcv[:, k0:k0 + h, c0:c0 + ncols]),
                  writes=[bdst])

    S.op("dve", lambda e: e.memset(ones[:], 1.0), writes=[b_ones])
    S.dma("sp", lambda e: e.dma_start(out=cin[:], in_=cT[:, :]), writes=[b_cin])
    S.dma("sp", lambda e: e.dma_start(out=badat[:], in_=bada[:, :]), writes=[b_badat])
    S.dma("sp", lambda e: e.dma_start(out=gmixt[:], in_=gmix[:, :]), writes=[b_gmixt])
    S.dma("sp", lambda e: e.dma_start(out=gqt[:], in_=gq[:, :]), writes=[b_gqt])
    S.dma("sp", lambda e: e.dma_start(out=gkvt[:], in_=gkv[:, :]), writes=[b_gkvt])
    S.dma("sp", lambda e: e.dma_start(out=invft[:], in_=invf[:, :]), writes=[b_invft])
    S.dma("sp", lambda e: e.dma_start(out=sgnt[:], in_=sgn[:, :]), writes=[b_sgnt])
    S.op("act", lambda e: e.activation(out=cact[:], in_=cin[:], func=AF.Silu), reads=[b_cin], writes=[b_cact])

    mb = nbank()
    for g in range(8):
        wi = st["w"]; st["w"] ^= 1
        load_w(wb[wi], b_wb[wi], wada, NK, g * 512, 512)
        for m in range(4):
            n = g * 4 + m
            for k in range(NK):
                S.op("pe", lambda e, wi=wi, m=m, k=k, n=n: e.matmul(
                    ps[:, mb, n:n + 1], lhsT=wb[wi][:, k, m * 128:(m + 1) * 128], rhs=cact[:, k:k + 1],
                    start=(k == 0), stop=(k == NK - 1)), reads=[b_wb[wi], b_cact], writes=[b_ps[mb]])
    S.op("dve", lambda e: e.tensor_tensor(out=modt[:], in0=ps[:, mb, 0:32], in1=badat[:], op=ALU.add),
         reads=[b_ps[mb], b_badat], writes=[b_modt])
    S.op("dve", lambda e: e.scalar_tensor_tensor(out=s1[:], in0=modt[:, 16:32], scalar=1.0, in1=gmixt[:],
                                                 op0=ALU.add, op1=ALU.mult),
         reads=[b_modt, b_gmixt], writes=[b_s1])

    load_w(wswb, b_wswb, wsw, NK, 0, 64)
    load_w(wuqb, b_wuqb, wuq, 4, 0, 1536)
    load_w(wuqsb, b_wuqsb, wuqs, 4, 0, 512)
    load_w(wukvb, b_wukvb, wukv, 4, 0, 2048)

    def rms_rstd(src_sq, bsrc, nk, width, dim):
        bk = nbank()
        for k in range(nk):
            S.op("pe", lambda e, k=k: e.matmul(ps[:, bk, 0:width], lhsT=ones[:], rhs=src_sq[:, k, 0:width],
                                               start=(k == 0), stop=(k == nk - 1)),
                 reads=[bsrc, b_ones], writes=[b_ps[bk]])
        S.op("act", lambda e: e.activation(out=lnt[:, 0:width], in_=ps[:, bk, 0:width], func=AF.Ln,
                                           scale=1.0 / dim, bias=EPS), reads=[b_ps[bk]], writes=[b_lnt])
        S.op("act", lambda e: e.activation(out=rstd[:, 0:width], in_=lnt[:, 0:width], func=AF.Exp, scale=-0.5),
             reads=[b_lnt], writes=[b_rstd])

    xTv = xT.rearrange("(k p) t -> p k t", p=128)
    for sup in range(ntok // TS):
        t0s = sup * TS
        b_QKs = b_QKs_l[sup]; b_Vs = b_Vs_l[sup]
        for pt in range(TS // TP):
            tok0 = t0s + pt * TP
            for k0 in (0, 8):
                S.dma("sp", lambda e, k0=k0, tok0=tok0: e.dma_start(out=xt[:, k0:k0 + 8, :],
                                                                    in_=xTv[:, k0:k0 + 8, tok0:tok0 + TP]),
                      writes=[b_xt])
            S.op("act", lambda e: e.activation(out=sq[:], in_=xt[:], func=AF.Square), reads=[b_xt], writes=[b_sq])
            rms_rstd(sq, b_sq, NK, TP, float(D))
            for k in range(NK):
                ti = st["tmp"]; st["tmp"] ^= 1
                S.op("dve", lambda e, k=k, ti=ti: e.tensor_tensor(out=tmp[ti][:, 0:TP], in0=xt[:, k, :],
                                                                  in1=rstd[:, 0:TP], op=ALU.mult),
                     reads=[b_xt, b_rstd], writes=[b_tmp[ti]])
                S.op("act", lambda e, k=k, ti=ti, pt=pt: e.activation(
                    out=aT[:, k, pt * TP:(pt + 1) * TP], in_=tmp[ti][:, 0:TP], func=AF.Identity,
                    scale=s1[:, k:k + 1], bias=modt[:, k:k + 1]),
                    reads=[b_tmp[ti], b_s1, b_modt], writes=[b_aT[pt]])

        def gemm_group(src, c0, ncols, epilogue):
            wi = st["w"]; st["w"] ^= 1
            load_w(wb[wi], b_wb[wi], src, NK, c0, ncols)
            if tick is not None:
                tick()
            for m in range((ncols + 127) // 128):
                mc = min(128, ncols - m * 128)
                for t in range(TS // 512):
                    bk = nbank()
                    for k in range(NK):
                        S.op("pe", lambda e, wi=wi, m=m, mc=mc, t=t, k=k, bk=bk: e.matmul(
                            ps[0:mc, bk, :], lhsT=wb[wi][:, k, m * 128:m * 128 + mc],
                            rhs=aT[:, k, t * 512:(t + 1) * 512], start=(k == 0), stop=(k == NK - 1)),
                            reads=[b_wb[wi]] + b_aT[4 * t:4 * t + 4], writes=[b_ps[bk]])
                    epilogue(m, mc, t, bk)

        def evac(out_ap, bout, bk, mc, scale=1.0, func=None):
            st["ev"] ^= 1
            if func is not None or st["ev"]:
                f = func if func is not None else AF.Copy
                S.op("act", lambda e: e.activation(out=out_ap, in_=ps[0:mc, bk, :], func=f, scale=scale),
                     reads=[b_ps[bk]], writes=[bout])
            else:
                S.op("dve", lambda e: e.tensor_scalar(out=out_ap, in0=ps[0:mc, bk, :], scalar1=scale, scalar2=None,
                                                      op0=ALU.mult), reads=[b_ps[bk]], writes=[bout])

        gemm_group(win, 0, 512, lambda m, mc, t, bk: evac(cq[:, m, t * 512:(t + 1) * 512], b_cq, bk, mc))
        gemm_group(win, 512, 512, lambda m, mc, t, bk: evac(ckv[:, m, t * 512:(t + 1) * 512], b_ckv, bk, mc))
        gemm_group(win, 1024, 64, lambda m, mc, t, bk: evac(kr[:, t * 512:(t + 1) * 512], b_kr, bk, mc))
        gemm_group(wsw, 0, 64, lambda m, mc, t, bk: evac(krs[:, t * 512:(t + 1) * 512], b_krs, bk, mc))

        def out_bf(dst, bdst, row0, scale):
            cur = {}

            def ep(m, mc, t, bk):
                if t == 0:
                    cur["i"] = st["ob"]; st["ob"] ^= 1
                i = cur["i"]
                evac(ob[i][:, t * 512:(t + 1) * 512], b_ob[i], bk, mc, scale=scale)
                if t == TS // 512 - 1:
                    r = row0 + m * 128
                    S.dma("sp", lambda e, i=i, r=r: e.dma_start(out=dst[r:r + 128, t0s:t0s + TS], in_=ob[i][:, :]),
                          reads=[b_ob[i]], writes=[bdst])
            return ep

        def out_f32(dst, bdst, row0, func):
            cur = {}

            def ep(m, mc, t, bk):
                if t == 0:
                    cur["i"] = st["of"]; st["of"] ^= 1
                i = cur["i"]
                evac(of[i][:, t * 512:(t + 1) * 512], b_of[i], bk, mc, func=func)
                if t == TS // 512 - 1:
                    r = row0 + m * 128
                    S.dma("sp", lambda e, i=i, r=r: e.dma_start(out=dst[r:r + 128, t0s:t0s + TS], in_=of[i][:, :]),
                          reads=[b_of[i]], writes=[bdst])
            return ep

        sc = 128.0 ** -0.5
        def out_qk(rowfn, scale):
            cur = {}

            def ep(m, mc, t, bk):
                if t == 0:
                    cur["i"] = st["ob"]; st["ob"] ^= 1
                i = cur["i"]
                evac(ob[i][:, t * 512:(t + 1) * 512], b_ob[i], bk, mc, scale=scale)
                if t == TS // 512 - 1:
                    sh, r0 = rowfn(m)
                    S.dma("sp", lambda e: e.dma_start(out=QKs[sh, sup, r0:r0 + 128, :], in_=ob[i][:, :]),
                          reads=[b_ob[i]], writes=[b_QKs])
            return ep

        def gemm_group_tm(c0, store):
            wi = st["w"]; st["w"] ^= 1
            load_w(wb[wi], b_wb[wi], win, NK, c0, 512)
            if tick is not None:
                tick()
            for s_ in range(TS // 128):
                bk = nbank()
                for k in range(NK):
                    S.op("pe", lambda e: e.matmul(ps[:, bk, :], lhsT=aT[:, k, s_ * 128:(s_ + 1) * 128], rhs=wb[wi][:, k, 0:512],
                                                  start=(k == 0), stop=(k == NK - 1)), reads=[b_wb[wi], b_aT[s_]], writes=[b_ps[bk]])
                oi = st["ob"]; st["ob"] ^= 1
                evac(ob[oi][:, 0:512], b_ob[oi], bk, 128)
                store(s_, oi)

        for g in range(3):
            gemm_group(win, 1088 + g * 512, 512, out_qk(lambda m, g=g: (m, g * 128), sc))
        for g in range(3):
            gemm_group(win, 1088 + 1536 + g * 512, 512, out_qk(lambda m, g=g: (m, 384 + g * 128), 1.0))
        for g in range(3):
            def st_dv(s_, oi, g=g):
                tk = t0s + s_ * 128
                S.dma("sp", lambda e: e.dma_start(out=Vs[:, tk:tk + 128, g * 128:(g + 1) * 128].rearrange("h p c -> p h c"),
                                                  in_=ob[oi][:, 0:512].rearrange("p (h c) -> p h c", c=128)),
                      reads=[b_ob[oi]], writes=[b_Vs])
            gemm_group_tm(1088 + 3072 + g * 512, st_dv)
        for gi in range(2):
            gemm_group(win, 5696 + gi * 512, 512, out_qk(lambda m, gi=gi: ((4 * gi + m) // 2, 768 + (m % 2) * 128), sc))
        for gi in range(2):
            gemm_group(win, 5696 + 1024 + gi * 512, 512, out_qk(lambda m, gi=gi: ((4 * gi + m) // 2, 1024 + (m % 2) * 128), 1.0))
        for gi in range(2):
            def st_sv(s_, oi, gi=gi):
                tk = t0s + s_ * 128
                S.dma("sp", lambda e: e.dma_start(out=Vs[2 * gi:2 * gi + 2, tk:tk + 128, 384:640].rearrange("j p c -> p j c"),
                                                  in_=ob[oi][:, 0:512].rearrange("p (j c) -> p j c", c=256)),
                      reads=[b_ob[oi]], writes=[b_Vs])
            gemm_group_tm(5696 + 2048 + gi * 512, st_sv)
        for gi in range(12):
            gemm_group(win, 8768 + gi * 512, 512, out_f32(gT, b_gT, gi * 512, AF.Sigmoid))

        scm = 192.0 ** -0.5
        for tt in range(TS // 512):
            tok0 = t0s + tt * 512
            tsl = slice(tt * 512, (tt + 1) * 512)
            S.dma("sp", lambda e, tok0=tok0: e.dma_start(out=posi[:], in_=pos[0:1, tok0:tok0 + 512].partition_broadcast(64)),
                  writes=[b_posi])
            S.op("dve", lambda e: e.tensor_copy(out=ang[:], in_=posi[:]), reads=[b_posi], writes=[b_ang])
            S.op("dve", lambda e: e.tensor_scalar(out=ang[:], in0=ang[:], scalar1=invft[:, 0:1], scalar2=None, op0=ALU.mult),
                 reads=[b_ang, b_invft], writes=[b_ang])
            S.op("dve", lambda e: e.tensor_scalar(out=kf[:], in0=ang[:], scalar1=1.0 / TWO_PI, scalar2=None, op0=ALU.mult),
                 reads=[b_ang], writes=[b_kf])
            S.op("dve", lambda e: e.tensor_copy(out=ki[:], in_=kf[:]), reads=[b_kf], writes=[b_ki])
            S.op("dve", lambda e: e.tensor_copy(out=kf[:], in_=ki[:]), reads=[b_ki], writes=[b_kf])
            S.op("dve", lambda e: e.scalar_tensor_tensor(out=rr[:], in0=kf[:], scalar=-C1, in1=ang[:], op0=ALU.mult, op1=ALU.add),
                 reads=[b_kf, b_ang], writes=[b_rr])
            S.op("dve", lambda e: e.scalar_tensor_tensor(out=rr[:], in0=kf[:], scalar=-C2, in1=rr[:], op0=ALU.mult, op1=ALU.add),
                 reads=[b_kf, b_rr], writes=[b_rr])

            def wrap(r, br):
                S.op("dve", lambda e: e.tensor_scalar(out=mm[:], in0=r[:], scalar1=math.pi, scalar2=-TWO_PI, op0=ALU.is_gt, op1=ALU.mult),
                     reads=[br], writes=[b_mm])
                S.op("dve", lambda e: e.tensor_tensor(out=r[:], in0=r[:], in1=mm[:], op=ALU.add), reads=[br, b_mm], writes=[br])
                S.op("dve", lambda e: e.tensor_scalar(out=mm[:], in0=r[:], scalar1=-math.pi, scalar2=TWO_PI, op0=ALU.is_lt, op1=ALU.mult),
                     reads=[br], writes=[b_mm])
                S.op("dve", lambda e: e.tensor_tensor(out=r[:], in0=r[:], in1=mm[:], op=ALU.add), reads=[br, b_mm], writes=[br])
                S.op("dve", lambda e: e.tensor_scalar(out=r[:], in0=r[:], scalar1=3.1415925, scalar2=-3.1415925, op0=ALU.min, op1=ALU.max),
                     reads=[br], writes=[br])
            wrap(rr, b_rr)
            S.op("dve", lambda e: e.tensor_scalar(out=rc[:], in0=rr[:], scalar1=math.pi / 2, scalar2=None, op0=ALU.add),
                 reads=[b_rr], writes=[b_rc])
            wrap(rc, b_rc)
            S.op("act", lambda e: e.activation(out=CS[:], in_=rc[:], func=AF.Sin), reads=[b_rc], writes=[b_CS])
            S.op("act", lambda e: e.activation(out=SN[:], in_=rr[:], func=AF.Sin, scale=sgnt[:, 0:1]), reads=[b_rr, b_sgnt], writes=[b_SN])

            def rope_out(src_r, bsr, src_s, bss, scale, dst_ap, bdst):
                S.op("dve", lambda e: e.scalar_tensor_tensor(out=t1[:], in0=src_r, scalar=scale, in1=CS[:], op0=ALU.mult, op1=ALU.mult),
                     reads=[bsr, b_CS], writes=[b_t1])
                S.op("dve", lambda e: e.scalar_tensor_tensor(out=t2[:], in0=src_s, scalar=scale, in1=SN[:], op0=ALU.mult, op1=ALU.mult),
                     reads=[bss, b_SN], writes=[b_t2])
                S.op("dve", lambda e: e.tensor_tensor(out=dst_ap, in0=t1[:], in1=t2[:], op=ALU.add),
                     reads=[b_t1, b_t2], writes=[bdst])

            oi = st["ob"]; st["ob"] ^= 1
            rope_out(kr[:, tsl], b_kr, krs[:, tsl], b_krs, 1.0, ob[oi][0:64, 0:512], b_ob[oi])
            for sh in range(4):
                S.dma("sp", lambda e: e.dma_start(out=QKs[sh, sup, 1920:1984, tok0 - t0s:tok0 - t0s + 512], in_=ob[oi][0:64, 0:512]),
                      reads=[b_ob[oi]], writes=[b_QKs])

            def latent_norm(src, bsrc, gt, bgt):
                sqv = sq[:, :, :].rearrange("p (k a) t -> p k (a t)", a=4)
                S.op("act", lambda e: e.activation(out=sqv, in_=src[:, :, tsl], func=AF.Square), reads=[bsrc], writes=[b_sq])
                rms_rstd(sqv, b_sq, 4, 512, 512.0)
                for k in range(4):
                    S.op("dve", lambda e, k=k: e.scalar_tensor_tensor(out=lat[:, k, :], in0=src[:, k, tsl], scalar=gt[:, k:k + 1],
                                                                      in1=rstd[:, :], op0=ALU.mult, op1=ALU.mult),
                         reads=[bsrc, bgt, b_rstd], writes=[b_lat])

            latent_norm(cq, b_cq, gqt, b_gqt)
            for h in range(8):
                bk = nbank()
                for k in range(4):
                    S.op("pe", lambda e, h=h, k=k, bk=bk: e.matmul(ps[:, bk, :], lhsT=wuqb[:, k, h * 192:h * 192 + 128], rhs=lat[:, k, :],
                                                                   start=(k == 0), stop=(k == 3)), reads=[b_wuqb, b_lat], writes=[b_ps[bk]])
                oi = st["ob"]; st["ob"] ^= 1
                evac(ob[oi][:, 0:512], b_ob[oi], bk, 128, scale=scm)
                S.dma("sp", lambda e, oi=oi, h=h, tok0=tok0: e.dma_start(out=QKs[h // 2, sup, 1280 + (h % 2) * 128:1280 + (h % 2) * 128 + 128, tok0 - t0s:tok0 - t0s + 512], in_=ob[oi][:, 0:512]),
                      reads=[b_ob[oi]], writes=[b_mlaq])
                bk1 = nbank(); bk2 = nbank()
                for k in range(4):
                    S.op("pe", lambda e, h=h, k=k, bk1=bk1: e.matmul(ps[0:64, bk1, :], lhsT=wuqb[:, k, h * 192 + 128:h * 192 + 192], rhs=lat[:, k, :],
                                                                     start=(k == 0), stop=(k == 3)), reads=[b_wuqb, b_lat], writes=[b_ps[bk1]])
                for k in range(4):
                    S.op("pe", lambda e, h=h, k=k, bk2=bk2: e.matmul(ps[0:64, bk2, :], lhsT=wuqsb[:, k, h * 64:(h + 1) * 64], rhs=lat[:, k, :],
                                                                     start=(k == 0), stop=(k == 3)), reads=[b_wuqsb, b_lat], writes=[b_ps[bk2]])
                oi = st["ob"]; st["ob"] ^= 1
                rope_out(ps[0:64, bk1, :], b_ps[bk1], ps[0:64, bk2, :], b_ps[bk2], scm, ob[oi][0:64, 0:512], b_ob[oi])
                S.dma("sp", lambda e, oi=oi, h=h, tok0=tok0: e.dma_start(out=QKs[h // 2, sup, 1792 + (h % 2) * 64:1792 + (h % 2) * 64 + 64, tok0 - t0s:tok0 - t0s + 512], in_=ob[oi][0:64, 0:512]),
                      reads=[b_ob[oi]], writes=[b_mlaq])
            latent_norm(ckv, b_ckv, gkvt, b_gkvt)
            for h in range(8):
                bk = nbank()
                for k in range(4):
                    S.op("pe", lambda e: e.matmul(ps[:, bk, :], lhsT=wukvb[:, k, h * 256:h * 256 + 128], rhs=lat[:, k, :],
                                                  start=(k == 0), stop=(k == 3)), reads=[b_wukvb, b_lat], writes=[b_ps[bk]])
                oi = st["ob"]; st["ob"] ^= 1
                evac(ob[oi][:, 0:512], b_ob[oi], bk, 128)
                S.dma("sp", lambda e: e.dma_start(out=QKs[h // 2, sup, 1536 + (h % 2) * 128:1536 + (h % 2) * 128 + 128, tok0 - t0s:tok0 - t0s + 512], in_=ob[oi][:, 0:512]),
                      reads=[b_ob[oi]], writes=[b_QKs])
            wv = wukvb[:, :, :].rearrange("p k (h c) -> p k h c", c=256)
            for s4 in range(4):
                for hg in range(2):
                    bk = nbank()
                    for k in range(4):
                        S.op("pe", lambda e: e.matmul(ps[:, bk, :].rearrange("p (h c) -> p h c", c=128), lhsT=lat[:, k, s4 * 128:(s4 + 1) * 128],
                                                      rhs=wv[:, k, hg * 4:(hg + 1) * 4, 128:256], start=(k == 0), stop=(k == 3)),
                             reads=[b_wukvb, b_lat], writes=[b_ps[bk]])
                    oi = st["ob"]; st["ob"] ^= 1
                    evac(ob[oi][:, 0:512], b_ob[oi], bk, 128)
                    tk = tok0 + s4 * 128
                    S.dma("sp", lambda e: e.dma_start(out=Vs[2 * hg:2 * hg + 2, tk:tk + 128, 640:896].rearrange("j p c -> p j c"),
                                                      in_=ob[oi][:, 0:512].rearrange("p (j c) -> p j c", c=256)),
                          reads=[b_ob[oi]], writes=[b_Vs])
        if after_sup is not None:
            after_sup(sup)


SEQ = 8192
NB = SEQ // 128
RATES = (1, 4, 16)
BIG = 1.0e6


def emit_B(S, nc, d, QKl, Vl, Ysrc, b_QKg, b_Vg, b_Ys, seq=SEQ, do_mla=True, do_sb=True, do_dil=True):
    NBk = seq // 128
    NG4 = seq // 512
    dposk = d['dposk']; dposq = d['dposq']; nslope = d['nslope']
    maskc_d = d['maskc']; masks_d = d['masks']; maskd_d = d['maskd']; m1_d = d['m1']; m2_d = d['m2']
    b_ymla = b_Ys; b_ysb = b_Ys; b_ydil = b_Ys
    SHR = 1984; SHV = 896
    Q1 = S.sbuf("Q1", [128, seq], BF16); bQ1 = Buf("Q1")
    K1 = S.sbuf("K1", [128, seq], BF16); bK1 = Buf("K1")
    if do_mla:
        Q2 = S.sbuf("Q2", [64, seq], BF16); bQ2 = Buf("Q2")
        K2 = S.sbuf("K2", [64, seq], BF16); bK2 = Buf("K2")
    V1 = S.sbuf("V1", [128, NBk, 128], BF16); bV1 = Buf("V1")
    two = do_mla or do_sb
    if two:
        Q1b = S.sbuf("Q1b", [128, seq], BF16); bQ1b = Buf("Q1b")
        K1b = S.sbuf("K1b", [128, seq], BF16); bK1b = Buf("K1b")
        V1b = S.sbuf("V1b", [128, NBk, 128], BF16); bV1b = Buf("V1b")
        if do_mla:
            Q2b = S.sbuf("Q2b", [64, seq], BF16); bQ2b = Buf("Q2b")
    NP = 8
    PT = [S.sbuf("PT%d" % i, [128, 512], BF16) for i in range(NP)]; bPT = [Buf("PT%d" % i) for i in range(NP)]
    SP = [S.sbuf("SP%d" % i, [128, 512], BF16) for i in range(NP)]; bSP = [Buf("SP%d" % i) for i in range(NP)]
    EN = [S.sbuf("EN%d" % i, [128, 512], F32) for i in range(4)]; bEN = [Buf("EN%d" % i) for i in range(4)]
    SNt = [S.sbuf("SN%d" % i, [128, 512], F32) for i in range(NP)]; bSN = [Buf("SN%d" % i) for i in range(NP)]
    UU = [S.sbuf("UU%d" % i, [128, 512], F32) for i in range(4)]; bUU = [Buf("UU%d" % i) for i in range(4)]
    YS = [S.sbuf("YS%d" % i, [128, 512], BF16) for i in range(2)]; bYS = [Buf("YS%d" % i) for i in range(2)]
    RD = S.sbuf("RD", [128, 512], F32); bRD = Buf("RD")
    maskc = S.sbuf("maskc_t", [128, 4, 512], BF16); bmaskc = Buf("maskc")
    masks = S.sbuf("masks_t", [128, 4, 512], BF16); bmasks = Buf("masks")
    maskd = S.sbuf("maskd_t", [128, 256], F32); bmaskd = Buf("maskd")
    M1 = S.sbuf("M1", [128, 128], BF16); bM1 = Buf("M1")
    M2 = S.sbuf("M2", [128, 128], BF16); bM2 = Buf("M2")
    ones = S.sbuf("ones", [128, 128], BF16); bones = Buf("ones")
    nsl = S.sbuf("nsl", [128, 3], F32); bnsl = Buf("nsl")
    ps = S.psum("ps", [128, 8, 512], F32); bps = [Buf("ps%d" % i) for i in range(8)]

    S.op("dve", lambda e: e.memset(ones[:], 1.0), writes=[bones])
    S.dma("sp", lambda e: e.dma_start(out=maskc[:], in_=maskc_d[:, :, :]), writes=[bmaskc])
    S.dma("sp", lambda e: e.dma_start(out=masks[:], in_=masks_d[:, :, :]), writes=[bmasks])
    S.dma("sp", lambda e: e.dma_start(out=maskd[:], in_=maskd_d[:, :]), writes=[bmaskd])
    S.dma("sp", lambda e: e.dma_start(out=M1[:], in_=m1_d[:, :]), writes=[bM1])
    S.dma("sp", lambda e: e.dma_start(out=M2[:], in_=m2_d[:, :]), writes=[bM2])
    S.dma("sp", lambda e: e.dma_start(out=nsl[:], in_=nslope[:, :]), writes=[bnsl])

    QK5 = QKl.rearrange("c r (p a) t -> c r p (a t)", a=1) if False else QKl
    Vl4 = Vl
    Vl6 = Vl.rearrange("c r (b p) d -> c r b p d", p=128)

    def load_fm(dst, bdst, R0, rows=128):
        dv4 = dst[0:rows, :].rearrange("p (r u t) -> p r u t", r=4, u=2)
        for u in range(2):
            S.dma("sp", lambda e: e.dma_start(out=dv4[:, :, u, :],
                                              in_=QK5[u * 4 + R0 // 512, :, R0 % 512:R0 % 512 + rows, :].rearrange("r p t -> p r t")),
                  reads=[b_QKg], writes=[bdst])

    def load_v(c0, Vt=None, bVt=None):
        Vt = V1 if Vt is None else Vt
        bVt = bV1 if bVt is None else bVt
        for r in range(4):
            for cp in range(4):
                S.dma("sp", lambda e: e.dma_start(out=Vt[:, r * 16 + cp * 4:r * 16 + cp * 4 + 4, :],
                                                  in_=Vl6[cp, r, :, :, c0:c0 + 128].rearrange("b p d -> p b d")),
                      reads=[b_Vg], writes=[bVt])

    def load_v_perm(c0, rt):
        if rt == 1:
            load_v(c0)
        elif rt == 4:
            for s_ in range(4):
                for r in range(4):
                    S.dma("sp", lambda e: e.dma_start(out=V1[:, s_ * 16 + 4 * r:s_ * 16 + 4 * r + 4, :],
                                                      in_=Vl4[:, r, s_:s_ + 4 * 127 + 1:4, c0:c0 + 128].rearrange("c p d -> p c d")),
                          reads=[b_Vg], writes=[bV1])
        else:
            for s_ in range(16):
                for cp in range(4):
                    S.dma("sp", lambda e: e.dma_start(out=V1[32 * cp:32 * cp + 32, s_ * 4:s_ * 4 + 4, :],
                                                      in_=Vl4[cp, :, s_:s_ + 16 * 31 + 1:16, c0:c0 + 128].rearrange("m p d -> p m d")),
                          reads=[b_Vg], writes=[bV1])

    Ys5 = Ysrc

    def yout(blk, g4):
        return Ys5[g4 // 8, blk, :, (g4 % 8) * 512:(g4 % 8) * 512 + 512]

    def pipeline(steps):
        prev = None
        for s1, s2 in steps:
            s1()
            if prev is not None:
                prev()
            prev = s2
        if prev is not None:
            prev()

    cnt = {"z": 0, "o": 0, "pt": 0, "ys": 0, "en": 0, "uu": 0}

    def interleave(a, b):
        out = []
        for x, y in zip(a, b):
            out.append(x); out.append(y)
        return out

    if do_mla:
        load_fm(K2, bK2, 1920, 64)
        hs = [dict(Q=Q1, bQ=bQ1, Qr=Q2, bQr=bQ2, K=K1, bK=bK1, V=V1, bV=bV1, zb=(0, 1), ob=2, db=3),
              dict(Q=Q1b, bQ=bQ1b, Qr=Q2b, bQr=bQ2b, K=K1b, bK=bK1b, V=V1b, bV=bV1b, zb=(6, 7), ob=4, db=5)]
        allsteps = []
        for h in range(2):
            H = hs[h]
            load_fm(H["Q"], H["bQ"], 1280 + h * 128)
            load_fm(H["Qr"], H["bQr"], 1792 + h * 64, 64)
            load_fm(H["K"], H["bK"], 1536 + h * 128)
            load_v(640 + h * 128, H["V"], H["bV"])
            steps = []
            zc = 0
            for g4 in range(NG4):
                qs = slice(g4 * 512, (g4 + 1) * 512)
                ob = H["ob"]; db = H["db"]
                nst = 4 * g4 + 4
                for j in range(nst):
                    zb = H["zb"][zc % 2]; zc += 1
                    ks = slice(j * 128, (j + 1) * 128)
                    box = {}

                    def s1(qs=qs, j=j, zb=zb, ks=ks, g4=g4, H=H, box=box):
                        pi = cnt["pt"] % NP; cnt["pt"] += 1
                        box["pi"] = pi
                        S.op("pe", lambda e: e.matmul(ps[:, zb, :], lhsT=H["K"][:, ks], rhs=H["Q"][:, qs], start=True, stop=False),
                             reads=[H["bK"], H["bQ"]], writes=[bps[zb]])
                        S.op("pe", lambda e: e.matmul(ps[:, zb, :], lhsT=K2[0:64, ks], rhs=H["Qr"][0:64, qs], start=False, stop=True),
                             reads=[bK2, H["bQr"]], writes=[bps[zb]])
                        S.op("act", lambda e: e.activation(out=PT[pi][:], in_=ps[:, zb, :], func=AF.Exp),
                             reads=[bps[zb]], writes=[bPT[pi]])
                        if j >= 4 * g4:
                            S.op("dve", lambda e: e.tensor_tensor(out=PT[pi][:], in0=PT[pi][:], in1=maskc[:, j - 4 * g4, :], op=ALU.mult),
                                 reads=[bPT[pi], bmaskc], writes=[bPT[pi]])

                    def s2(j=j, ob=ob, db=db, nst=nst, h=h, g4=g4, H=H, box=box):
                        pi = box["pi"]
                        S.op("pe", lambda e: e.matmul(ps[:, ob, :], lhsT=H["V"][:, j, :], rhs=PT[pi][:], start=(j == 0), stop=(j == nst - 1)),
                             reads=[H["bV"], bPT[pi]], writes=[bps[ob]])
                        S.op("pe", lambda e: e.matmul(ps[:, db, :], lhsT=ones[:], rhs=PT[pi][:], start=(j == 0), stop=(j == nst - 1)),
                             reads=[bones, bPT[pi]], writes=[bps[db]])
                        if j == nst - 1:
                            yi = cnt["ys"] % 2; cnt["ys"] += 1
                            ri = cnt["uu"] % 4; cnt["uu"] += 1
                            S.op("dve", lambda e: e.reciprocal(out=UU[ri][:], in_=ps[:, db, :]), reads=[bps[db]], writes=[bUU[ri]])
                            S.op("dve", lambda e: e.tensor_tensor(out=YS[yi][:], in0=ps[:, ob, :], in1=UU[ri][:], op=ALU.mult),
                                 reads=[bps[ob], bUU[ri]], writes=[bYS[yi]])
                            S.dma("sp", lambda e: e.dma_start(out=yout(h, g4), in_=YS[yi][:]), reads=[bYS[yi]], writes=[b_ymla])
                    steps.append((s1, s2))
            allsteps.append(steps)
        pipeline(interleave(allsteps[0], allsteps[1]))

    if do_sb:
        hs = [dict(Q=Q1, bQ=bQ1, K=K1, bK=bK1, V=V1, bV=bV1, zb=(0, 1), ob=2, rb=3),
              dict(Q=Q1b, bQ=bQ1b, K=K1b, bK=bK1b, V=V1b, bV=bV1b, zb=(6, 7), ob=4, rb=5)]
        allsteps = []
        for h in range(2):
            H = hs[h]
            load_fm(H["Q"], H["bQ"], 768 + h * 128)
            load_fm(H["K"], H["bK"], 1024 + h * 128)
            load_v(384 + h * 128, H["V"], H["bV"])
            steps = []
            zc = 0
            for g4 in range(NG4):
                qs = slice(g4 * 512, (g4 + 1) * 512)
                ob = H["ob"]; rb = H["rb"]
                nst = 4 * g4 + 4
                for idx, j in enumerate(reversed(range(nst))):
                    first = idx == 0; last = idx == nst - 1
                    zb = H["zb"][zc % 2]; zc += 1
                    ks = slice(j * 128, (j + 1) * 128)
                    box = {}

                    def t1(qs=qs, j=j, zb=zb, ks=ks, g4=g4, H=H, box=box):
                        pi = cnt["pt"] % NP; cnt["pt"] += 1
                        ei = cnt["en"] % 4; cnt["en"] += 1
                        box["pi"] = pi
                        S.op("pe", lambda e: e.matmul(ps[:, zb, :], lhsT=H["K"][:, ks], rhs=H["Q"][:, qs], start=True, stop=True),
                             reads=[H["bK"], H["bQ"]], writes=[bps[zb]])
                        S.op("act", lambda e: e.activation(out=EN[ei][:], in_=ps[:, zb, :], func=AF.Exp, scale=-1.0),
                             reads=[bps[zb]], writes=[bEN[ei]])
                        S.op("act", lambda e: e.activation(out=SNt[pi][:], in_=EN[ei][:], func=AF.Ln, bias=1.0),
                             reads=[bEN[ei]], writes=[bSN[pi]])
                        S.op("dve", lambda e: e.tensor_tensor(out=SP[pi][:], in0=ps[:, zb, :], in1=SNt[pi][:], op=ALU.add),
                             reads=[bps[zb], bSN[pi]], writes=[bSP[pi]])
                        if j >= 4 * g4:
                            S.op("dve", lambda e: e.tensor_tensor(out=SP[pi][:], in0=SP[pi][:], in1=masks[:, j - 4 * g4, :], op=ALU.mult),
                                 reads=[bSP[pi], bmasks], writes=[bSP[pi]])

                    def t2(j=j, rb=rb, first=first, g4=g4, box=box):
                        pi = box["pi"]
                        ui = cnt["uu"] % 4; cnt["uu"] += 1
                        S.op("pe", lambda e: e.matmul(ps[:, rb, :], lhsT=M1[:], rhs=SP[pi][:], start=first, stop=False, skip_group_check=True),
                             reads=[bM1, bSP[pi]], writes=[bps[rb]])
                        S.op("dve", lambda e: e.tensor_tensor(out=UU[ui][:], in0=SNt[pi][:], in1=ps[:, rb, :], op=ALU.add),
                             reads=[bSN[pi], bps[rb]], writes=[bUU[ui]])
                        S.op("act", lambda e: e.activation(out=PT[pi][:], in_=UU[ui][:], func=AF.Exp, scale=-1.0),
                             reads=[bUU[ui]], writes=[bPT[pi]])
                        if j >= 4 * g4:
                            S.op("dve", lambda e: e.tensor_tensor(out=PT[pi][:], in0=PT[pi][:], in1=masks[:, j - 4 * g4, :], op=ALU.mult),
                                 reads=[bPT[pi], bmasks], writes=[bPT[pi]])

                    def t3(rb=rb, last=last, box=box):
                        pi = box["pi"]
                        S.op("pe", lambda e: e.matmul(ps[:, rb, :], lhsT=M2[:], rhs=SP[pi][:], start=False, stop=last, skip_group_check=True),
                             reads=[bM2, bSP[pi]], writes=[bps[rb]])

                    def t4(j=j, ob=ob, first=first, last=last, h=h, g4=g4, H=H, box=box):
                        pi = box["pi"]
                        S.op("pe", lambda e: e.matmul(ps[:, ob, :], lhsT=H["V"][:, j, :], rhs=PT[pi][:], start=first, stop=last),
                             reads=[H["bV"], bPT[pi]], writes=[bps[ob]])
                        if last:
                            yi = cnt["ys"] % 2; cnt["ys"] += 1
                            S.op("act", lambda e: e.activation(out=YS[yi][:], in_=ps[:, ob, :], func=AF.Copy),
                                 reads=[bps[ob]], writes=[bYS[yi]])
                            S.dma("sp", lambda e: e.dma_start(out=yout(3 + h, g4), in_=YS[yi][:]), reads=[bYS[yi]], writes=[b_ysb])
                    steps.append((t1, t2, t3, t4))
            allsteps.append(steps)
        N_ = len(allsteps[0])
        for X in allsteps:
            X[0][0]()
        for k in range(N_):
            for X in allsteps:
                X[k][1]()
            if k > 0:
                for X in allsteps:
                    X[k - 1][3]()
            if k + 1 < N_:
                for X in allsteps:
                    X[k + 1][0]()
            for X in allsteps:
                X[k][2]()
        for X in allsteps:
            X[N_ - 1][3]()

    if do_dil:
        accn = S.sbuf("accn", [128, seq], F32); baccn = Buf("accn")
        accd = S.sbuf("accd", [128, seq], F32); baccd = Buf("accd")
        posk_i = S.sbuf("posk_i", [128, NBk], I32); bposk_i = Buf("posk_i")
        posk = S.sbuf("posk", [128, NBk], F32); bposk = Buf("posk")
        pq_i = [S.sbuf("pq_i%d" % i, [128, 128], I32) for i in range(2)]; bpq_i = [Buf("pq_i0"), Buf("pq_i1")]
        pq = [S.sbuf("pq%d" % i, [128, 128], F32) for i in range(2)]; bpq = [Buf("pq0"), Buf("pq1")]
        dd = [S.sbuf("dd%d" % i, [128, 256], F32) for i in range(2)]; bdd = [Buf("dd0"), Buf("dd1")]
        zt = [S.sbuf("zt%d" % i, [128, 256], F32) for i in range(2)]; bzt = [Buf("zt0"), Buf("zt1")]
        for g in range(3):
            r = RATES[g]
            L = seq // r
            nb = L // 128
            load_fm(Q1, bQ1, g * 128)
            load_fm(K1, bK1, 384 + g * 128)
            load_v_perm(g * 128, r)
            S.dma("sp", lambda e: e.dma_start(out=posk_i[:], in_=dposk[g, :, :]), writes=[bposk_i])
            S.op("dve", lambda e: e.tensor_copy(out=posk[:], in_=posk_i[:]), reads=[bposk_i], writes=[bposk])
            S.op("dve", lambda e: e.tensor_scalar(out=posk[:], in0=posk[:], scalar1=-1.0, scalar2=None, op0=ALU.mult), reads=[bposk], writes=[bposk])
            dsteps = []
            for i in range(NBk):
                s, m = divmod(i, nb)
                hp = m > 0
                c0 = 0 if hp else 128
                zb = cnt["z"] % 2; cnt["z"] += 1
                ob = 2 + (cnt["o"] % 2); db = 4 + (cnt["o"] % 2); cnt["o"] += 1
                pi = cnt["pt"] % NP; cnt["pt"] += 1
                bi = i % 2
                st0 = s + 128 * r * m
                qsl = slice(st0, st0 + 127 * r + 1, r) if r > 1 else slice(st0, st0 + 128)
                psl = (slice(st0 - 128 * r, st0 - 128 * r + 127 * r + 1, r) if r > 1 else slice(st0 - 128, st0)) if hp else None
                def s1(i=i, hp=hp, c0=c0, zb=zb, pi=pi, bi=bi, qsl=qsl, psl=psl, g=g):
                    if hp:
                        S.op("pe", lambda e: e.matmul(ps[:, zb, 0:128], lhsT=K1[:, psl], rhs=Q1[:, qsl], start=True, stop=True),
                             reads=[bK1, bQ1], writes=[bps[zb]])
                    S.op("pe", lambda e: e.matmul(ps[:, zb, 128:256], lhsT=K1[:, qsl], rhs=Q1[:, qsl], start=True, stop=True),
                         reads=[bK1, bQ1], writes=[bps[zb]])
                    S.dma("sp", lambda e: e.dma_start(out=pq_i[bi][:], in_=dposq[g:g + 1, i * 128:(i + 1) * 128].partition_broadcast(128)),
                          writes=[bpq_i[bi]])
                    S.op("dve", lambda e: e.tensor_copy(out=pq[bi][:], in_=pq_i[bi][:]), reads=[bpq_i[bi]], writes=[bpq[bi]])
                    if hp:
                        S.op("act", lambda e: e.activation(out=dd[bi][:, 0:128], in_=pq[bi][:], func=AF.Abs, bias=posk[:, i - 1:i], scale=1.0),
                             reads=[bpq[bi], bposk], writes=[bdd[bi]])
                    S.op("act", lambda e: e.activation(out=dd[bi][:, 128:256], in_=pq[bi][:], func=AF.Abs, bias=posk[:, i:i + 1], scale=1.0),
                         reads=[bpq[bi], bposk], writes=[bdd[bi]])
                    S.op("dve", lambda e: e.tensor_tensor(out=dd[bi][:, c0:256], in0=dd[bi][:, c0:256], in1=maskd[:, c0:256], op=ALU.add),
                         reads=[bdd[bi], bmaskd], writes=[bdd[bi]])
                    S.op("dve", lambda e: e.scalar_tensor_tensor(out=zt[bi][:, c0:256], in0=dd[bi][:, c0:256], scalar=nsl[:, g:g + 1],
                                                                 in1=ps[:, zb, c0:256], op0=ALU.mult, op1=ALU.add),
                         reads=[bdd[bi], bnsl, bps[zb]], writes=[bzt[bi]])
                    S.op("act", lambda e: e.activation(out=PT[pi][:, c0:256], in_=zt[bi][:, c0:256], func=AF.Exp),
                         reads=[bzt[bi]], writes=[bPT[pi]])

                def s2(i=i, hp=hp, ob=ob, db=db, pi=pi, s=s, m=m, r=r, g=g, st0=st0):
                    if hp:
                        S.op("pe", lambda e: e.matmul(ps[:, ob, 0:128], lhsT=V1[:, i - 1, :], rhs=PT[pi][:, 0:128], start=True, stop=False),
                             reads=[bV1, bPT[pi]], writes=[bps[ob]])
                    S.op("pe", lambda e: e.matmul(ps[:, ob, 0:128], lhsT=V1[:, i, :], rhs=PT[pi][:, 128:256], start=(not hp), stop=True),
                         reads=[bV1, bPT[pi]], writes=[bps[ob]])
                    if hp:
                        S.op("pe", lambda e: e.matmul(ps[:, db, 0:128], lhsT=ones[:], rhs=PT[pi][:, 0:128], start=True, stop=False),
                             reads=[bones, bPT[pi]], writes=[bps[db]])
                    S.op("pe", lambda e: e.matmul(ps[:, db, 0:128], lhsT=ones[:], rhs=PT[pi][:, 128:256], start=(not hp), stop=True),
                         reads=[bones, bPT[pi]], writes=[bps[db]])
                    an = accn[:, st0:st0 + 127 * r + 1:r] if r > 1 else accn[:, st0:st0 + 128]
                    ad = accd[:, st0:st0 + 127 * r + 1:r] if r > 1 else accd[:, st0:st0 + 128]
                    if g == 0:
                        S.op("act", lambda e: e.activation(out=an, in_=ps[:, ob, 0:128], func=AF.Copy), reads=[bps[ob]], writes=[baccn])
                        S.op("dve", lambda e: e.tensor_copy(out=ad, in_=ps[:, db, 0:128]), reads=[bps[db]], writes=[baccd])
                    else:
                        S.op("dve", lambda e: e.tensor_tensor(out=an, in0=an, in1=ps[:, ob, 0:128], op=ALU.add), reads=[baccn, bps[ob]], writes=[baccn])
                        S.op("dve", lambda e: e.tensor_tensor(out=ad, in0=ad, in1=ps[:, db, 0:128], op=ALU.add), reads=[baccd, bps[db]], writes=[baccd])
                dsteps.append((s1, s2))
            pipeline(dsteps)
        for c in range(seq // 512):
            cs = slice(c * 512, (c + 1) * 512)
            yi = cnt["ys"] % 2; cnt["ys"] += 1
            S.op("dve", lambda e: e.reciprocal(out=RD[:], in_=accd[:, cs]), reads=[baccd], writes=[bRD])
            S.op("dve", lambda e: e.tensor_tensor(out=YS[yi][:], in0=accn[:, cs], in1=RD[:], op=ALU.mult), reads=[baccn, bRD], writes=[bYS[yi]])
            S.dma("sp", lambda e: e.dma_start(out=yout(2, c), in_=YS[yi][:]), reads=[bYS[yi]], writes=[b_ydil])


def consts_B():
    import ml_dtypes
    k = np.arange(128)[:, None]
    q = np.arange(512)[None, :]
    maskc = np.stack([(128 * i0 + k <= q) for i0 in range(4)], axis=1).astype(np.float32)
    masks = np.stack([(128 * i0 + k < q) for i0 in range(4)], axis=1).astype(np.float32)
    kl = np.arange(128)[:, None]; ql = np.arange(128)[None, :]
    maskd = np.concatenate([np.where(kl >= ql, 0.0, BIG), np.where(kl <= ql, 0.0, BIG)], axis=1).astype(np.float32)
    p = np.arange(128)[:, None]; m = np.arange(128)[None, :]
    m1 = (p > m).astype(np.float32); m2 = (p <= m).astype(np.float32)
    bf = ml_dtypes.bfloat16
    return dict(maskc=maskc.astype(bf), masks=masks.astype(bf), maskd=maskd, m1=m1.astype(bf), m2=m2.astype(bf))


D = 2048
NK = 16
EPS = 1e-6
NE = 16
BIGR = 1.0e4


def emit_C(S, nc, d, l, Yl, gT, xT, x2T, xfT, b_Yg, b_gT, b_xsrc, b_x2T, b_xfT, ntok=2048):
    NT = ntok // 512
    cT = d['cT']; wada = d['wada'][l]; bada = d['bada_c'][l]; gmoe = d['gmoe'][l]; gfin = d['gfin']
    womla = d['womla'][l]; wodil = d['wodil'][l]; wosb = d['wosb'][l]; wout = d['wout'][l]
    wr = d['wr']; br = d['br']; wg = d['wg'][l]; wu = d['wu'][l]; wd = d['wd'][l]; ident = d['ident']
    DBG = False
    xt = S.sbuf("xt", [128, NK, 512], F32); b_xt = [Buf("xt%d" % k) for k in range(NK)]
    scr = S.sbuf("scr", [128, 20, 512], BF16); b_scr = Buf("scr")
    mg = S.sbuf("mg", [128, NK, 512], BF16); b_mg = Buf("mg")
    a2b = S.sbuf("a2b", [128, NK, 512], BF16); b_a2b = Buf("a2b")
    NW = 5
    W = [S.sbuf("W%d" % i, [128, NK, 512], BF16) for i in range(NW)]; b_W = [Buf("W%d" % i) for i in range(NW)]
    gt = [S.sbuf("gt%d" % i, [128, 3, 512], F32) for i in range(2)]; b_gt = [Buf("gt0"), Buf("gt1")]
    tmp = [S.sbuf("tmp%d" % i, [128, 512], F32) for i in range(4)]; b_tmp = [Buf("tmp%d" % i) for i in range(4)]
    hp = [S.sbuf("hp%d" % i, [128, 4, 512], BF16) for i in range(2)]; b_hp = [Buf("hp0"), Buf("hp1")]
    rstd = S.sbuf("rstd", [128, 512], F32); b_rstd = Buf("rstd")
    lnt = S.sbuf("lnt", [128, 512], F32); b_lnt = Buf("lnt")
    ones = S.sbuf("ones", [128, 128], BF16); b_ones = Buf("ones")
    onesf = S.sbuf("onesf", [128, 128], F32); b_onesf = Buf("onesf")
    idt = S.sbuf("idt", [128, 128], F32); b_idt = Buf("idt")
    Gm = [S.sbuf("Gm%d" % i, [128, 128], F32) for i in range(2)]; b_Gm = [Buf("Gm0"), Buf("Gm1")]
    cin = S.sbuf("cin", [128, NK], F32); b_cin = Buf("cin")
    cact = S.sbuf("cact", [128, NK], BF16); b_cact = Buf("cact")
    badat = S.sbuf("badat", [128, 64], F32); b_badat = Buf("badat")
    gmoet = S.sbuf("gmoet", [128, NK], F32); b_gmoet = Buf("gmoet")
    gfint = S.sbuf("gfint", [128, NK], F32); b_gfint = Buf("gfint")
    modt = S.sbuf("modt", [128, 64], F32); b_modt = Buf("modt")
    s1m = S.sbuf("s1m", [128, NK], F32); b_s1m = Buf("s1m")
    wrt = S.sbuf("wrt", [128, NK, NE], F32); b_wrt = Buf("wrt")
    brt = S.sbuf("brt", [128, NE], F32); b_brt = Buf("brt")
    lt = S.sbuf("lt", [16, 512], F32); b_lt = Buf("lt")
    sc = S.sbuf("sc", [128, 64], F32); b_sc = Buf("sc")
    bi = S.sbuf("bi", [128, 64], F32); b_bi = Buf("bi")
    bi2 = S.sbuf("bi2", [128, 64], F32); b_bi2 = Buf("bi2")
    eq = S.sbuf("eq", [128, 64], F32); b_eq = Buf("eq")
    m1 = S.sbuf("m1", [128, 16], F32); b_m1 = Buf("m1")
    m2 = S.sbuf("m2", [128, 16], F32); b_m2 = Buf("m2")
    gs = S.sbuf("gs", [128, 16], F32); b_gs = Buf("gs")
    gmax = S.sbuf("gmax", [128, 4], F32); b_gmax = Buf("gmax")
    gsel = S.sbuf("gsel", [128, 16], F32); b_gsel = Buf("gsel")
    wsel = S.sbuf("wsel", [128, 64], F32); b_wsel = Buf("wsel")
    den = S.sbuf("den", [128, 4], F32); b_den = Buf("den")
    gates = S.sbuf("gates", [128, 64], F32); b_gates = Buf("gates")
    ps = S.psum("ps", [128, 8, 512], F32); b_ps = [Buf("ps%d" % i) for i in range(8)]
    st = {"bank": 0, "w": 0, "gt": 0, "tmp": 0, "hp": 0, "gm": 0}

    def nbank():
        i = st["bank"]; st["bank"] = (i + 1) % 6
        return i

    def nw():
        i = st["w"]; st["w"] = (i + 1) % NW
        return i

    def ntmp():
        i = st["tmp"]; st["tmp"] = (i + 1) % 4
        return i

    def load_w(wi, src2d, nk, c0, ncols, k_off=0):
        srcv = src2d.rearrange("(k p) c -> p k c", p=128)
        h = max(1, nk // 2)
        for k0 in range(0, nk, h):
            S.dma("pool", lambda e: e.dma_start(out=W[wi][:, k_off + k0:k_off + k0 + h, 0:ncols],
                                                in_=srcv[:, k0:k0 + h, c0:c0 + ncols]), writes=[b_W[wi]])

    S.op("dve", lambda e: e.memset(ones[:], 1.0), writes=[b_ones])
    S.op("dve", lambda e: e.memset(onesf[:], 1.0), writes=[b_onesf])
    for dst, bd, src in ((cin, b_cin, cT), (badat, b_badat, bada), (gmoet, b_gmoet, gmoe), (gfint, b_gfint, gfin),
                         (brt, b_brt, br), (idt, b_idt, ident)):
        S.dma("sp", lambda e: e.dma_start(out=dst[:], in_=src[:, :]), writes=[bd])
    S.dma("sp", lambda e: e.dma_start(out=wrt[:], in_=wr.rearrange("(k p) e -> p k e", p=128)), writes=[b_wrt])
    S.op("act", lambda e: e.activation(out=cact[:], in_=cin[:], func=AF.Silu), reads=[b_cin], writes=[b_cact])

    mb = nbank()
    for g in range(16):
        wi = nw()
        load_w(wi, wada, NK, 4096 + g * 512, 512)
        for m in range(4):
            n = g * 4 + m
            for k in range(NK):
                S.op("pe", lambda e: e.matmul(ps[:, mb, n:n + 1], lhsT=W[wi][:, k, m * 128:(m + 1) * 128], rhs=cact[:, k:k + 1],
                                              start=(k == 0), stop=(k == NK - 1)), reads=[b_W[wi], b_cact], writes=[b_ps[mb]])
    S.op("dve", lambda e: e.tensor_tensor(out=modt[:], in0=ps[:, mb, 0:64], in1=badat[:], op=ALU.add),
         reads=[b_ps[mb], b_badat], writes=[b_modt])
    S.op("dve", lambda e: e.scalar_tensor_tensor(out=s1m[:], in0=modt[:, 32:48], scalar=1.0, in1=gmoet[:], op0=ALU.add, op1=ALU.mult),
         reads=[b_modt, b_gmoet], writes=[b_s1m])

    def rms_rstd(src3, bsrcs):
        S.op("act", lambda e: e.activation(out=scr[:, 0:NK, :], in_=src3, func=AF.Square), reads=bsrcs, writes=[b_scr])
        bk = nbank()
        for k in range(NK):
            S.op("pe", lambda e: e.matmul(ps[:, bk, :], lhsT=ones[:], rhs=scr[:, k, :], start=(k == 0), stop=(k == NK - 1)),
                 reads=[b_scr, b_ones], writes=[b_ps[bk]])
        S.op("act", lambda e: e.activation(out=lnt[:], in_=ps[:, bk, :], func=AF.Ln, scale=1.0 / D, bias=EPS),
             reads=[b_ps[bk]], writes=[b_lnt])
        S.op("act", lambda e: e.activation(out=rstd[:], in_=lnt[:], func=AF.Exp, scale=-0.5), reads=[b_lnt], writes=[b_rstd])

    xTv = xT.rearrange("(k p) t -> p k t", p=128)
    gTv = gT.rearrange("(b n p) t -> p b n t", b=3, p=128)
    x2v = x2T.rearrange("(k p) t -> p k t", p=128) if x2T is not None else None
    xfv = xfT.rearrange("(k p) t -> p k t", p=128) if xfT is not None else None

    for t in range(NT):
        ts = slice(t * 512, (t + 1) * 512)
        for k0 in range(0, NK, 4):
            S.dma("sp", lambda e: e.dma_start(out=xt[:, k0:k0 + 4, :], in_=xTv[:, k0:k0 + 4, ts]), reads=[b_xsrc], writes=b_xt[k0:k0 + 4])
        for i_ in range(2):
            S.dma("sp", lambda e: e.dma_start(out=scr[:, i_:8:2, :], in_=Yl[i_, :, :, ts].rearrange("j p t -> p j t")), reads=[b_Yg], writes=[b_scr])
            S.dma("sp", lambda e: e.dma_start(out=scr[:, 12 + i_:20:2, :], in_=Yl[3 + i_, :, :, ts].rearrange("j p t -> p j t")), reads=[b_Yg], writes=[b_scr])
        S.dma("sp", lambda e: e.dma_start(out=scr[:, 8:12, :], in_=Yl[2, :, :, ts].rearrange("j p t -> p j t")), reads=[b_Yg], writes=[b_scr])
        for ng in range(4):
            wa = nw(); load_w(wa, womla, 8, ng * 512, 512); load_w(wa, wodil, 4, ng * 512, 512, k_off=8)
            wb_ = nw(); load_w(wb_, wosb, 8, ng * 512, 512)
            for m in range(4):
                n = ng * 4 + m
                gi = st["gt"]; st["gt"] ^= 1
                S.dma("sp", lambda e: e.dma_start(out=gt[gi][:], in_=gTv[:, :, n, ts]), reads=[b_gT], writes=[b_gt[gi]])
                banks = []
                for brn, (wi, koff, nk, yoff) in enumerate(((wa, 0, 8, 0), (wa, 8, 4, 8), (wb_, 0, 8, 12))):
                    bk = nbank(); banks.append(bk)
                    for k in range(nk):
                        S.op("pe", lambda e: e.matmul(ps[:, bk, :], lhsT=W[wi][:, koff + k, m * 128:(m + 1) * 128], rhs=scr[:, yoff + k, :],
                                                      start=(k == 0), stop=(k == nk - 1)), reads=[b_W[wi], b_scr], writes=[b_ps[bk]])
                ta = ntmp(); tb = ntmp()
                S.op("dve", lambda e: e.tensor_tensor(out=tmp[ta][:], in0=ps[:, banks[0], :], in1=gt[gi][:, 0, :], op=ALU.mult),
                     reads=[b_ps[banks[0]], b_gt[gi]], writes=[b_tmp[ta]])
                S.op("dve", lambda e: e.tensor_tensor(out=tmp[tb][:], in0=ps[:, banks[1], :], in1=gt[gi][:, 1, :], op=ALU.mult),
                     reads=[b_ps[banks[1]], b_gt[gi]], writes=[b_tmp[tb]])
                S.op("pool", lambda e: e.tensor_tensor(out=tmp[ta][:], in0=tmp[ta][:], in1=tmp[tb][:], op=ALU.add),
                     reads=[b_tmp[ta], b_tmp[tb]], writes=[b_tmp[ta]])
                S.op("dve", lambda e: e.tensor_tensor(out=tmp[tb][:], in0=ps[:, banks[2], :], in1=gt[gi][:, 2, :], op=ALU.mult),
                     reads=[b_ps[banks[2]], b_gt[gi]], writes=[b_tmp[tb]])
                S.op("pool", lambda e: e.tensor_tensor(out=mg[:, n, :], in0=tmp[ta][:], in1=tmp[tb][:], op=ALU.add),
                     reads=[b_tmp[ta], b_tmp[tb]], writes=[b_mg])
        for ng in range(4):
            wi = nw(); load_w(wi, wout, NK, ng * 512, 512)
            for m in range(4):
                n = ng * 4 + m
                bk = nbank()
                for k in range(NK):
                    S.op("pe", lambda e: e.matmul(ps[:, bk, :], lhsT=W[wi][:, k, m * 128:(m + 1) * 128], rhs=mg[:, k, :],
                                                  start=(k == 0), stop=(k == NK - 1)), reads=[b_W[wi], b_mg], writes=[b_ps[bk]])
                S.op("dve", lambda e: e.scalar_tensor_tensor(out=xt[:, n, :], in0=ps[:, bk, :], scalar=modt[:, n:n + 1], in1=xt[:, n, :],
                                                             op0=ALU.mult, op1=ALU.add), reads=[b_ps[bk], b_modt, b_xt[n]], writes=[b_xt[n]])
        if DBG:
            x1v = x1T.rearrange("(k p) t -> p k t", p=128)
            for k0 in range(0, NK, 4):
                S.dma("sp", lambda e: e.dma_start(out=x1v[:, k0:k0 + 4, ts], in_=xt[:, k0:k0 + 4, :]), reads=b_xt[k0:k0 + 4], writes=[b_x1T])
        rms_rstd(xt[:, :, :], b_xt)
        lb = nbank()
        for k in range(NK):
            ta = ntmp(); tb = ntmp()
            S.op("dve", lambda e: e.tensor_tensor(out=tmp[ta][:], in0=xt[:, k, :], in1=rstd[:], op=ALU.mult),
                 reads=[b_xt[k], b_rstd], writes=[b_tmp[ta]])
            S.op("act", lambda e: e.activation(out=tmp[tb][:], in_=tmp[ta][:], func=AF.Identity, scale=s1m[:, k:k + 1], bias=modt[:, 16 + k:17 + k]),
                 reads=[b_tmp[ta], b_s1m, b_modt], writes=[b_tmp[tb]])
            S.op("act", lambda e: e.activation(out=a2b[:, k, :], in_=tmp[ta][:], func=AF.Identity, scale=s1m[:, k:k + 1], bias=modt[:, 16 + k:17 + k]),
                 reads=[b_tmp[ta], b_s1m, b_modt], writes=[b_a2b])
            S.op("pe", lambda e: e.matmul(ps[0:16, lb, :], lhsT=wrt[:, k, :], rhs=tmp[tb][:], start=(k == 0), stop=(k == NK - 1)),
                 reads=[b_wrt, b_tmp[tb]], writes=[b_ps[lb]])
        S.op("act", lambda e: e.activation(out=lt[:], in_=ps[0:16, lb, :], func=AF.Copy), reads=[b_ps[lb]], writes=[b_lt])
        tb_ = nbank()
        for sub in range(4):
            S.op("pe", lambda e: e.matmul(ps[:, tb_, sub * 16:(sub + 1) * 16], lhsT=lt[0:16, sub * 128:(sub + 1) * 128], rhs=idt[0:16, 0:16],
                                          start=True, stop=True), reads=[b_lt, b_idt], writes=[b_ps[tb_]])
        S.op("act", lambda e: e.activation(out=sc[:], in_=ps[:, tb_, 0:64], func=AF.Sigmoid), reads=[b_ps[tb_]], writes=[b_sc])
        for sub in range(4):
            S.op("dve", lambda e: e.tensor_tensor(out=bi[:, sub * 16:(sub + 1) * 16], in0=sc[:, sub * 16:(sub + 1) * 16], in1=brt[:], op=ALU.add),
                 reads=[b_sc, b_brt], writes=[b_bi])
        v3 = lambda tl: tl[:, :].rearrange("p (g e) -> p g e", e=4)
        S.op("dve", lambda e: e.tensor_reduce(out=m1[:], in_=v3(bi), axis=AX.X, op=ALU.max), reads=[b_bi], writes=[b_m1])
        for ee in range(4):
            S.op("dve", lambda e: e.tensor_tensor(out=v3(eq)[:, :, ee], in0=v3(bi)[:, :, ee], in1=m1[:], op=ALU.is_equal),
                 reads=[b_bi, b_m1], writes=[b_eq])
        S.op("dve", lambda e: e.scalar_tensor_tensor(out=bi2[:], in0=eq[:], scalar=-BIGR, in1=bi[:], op0=ALU.mult, op1=ALU.add),
             reads=[b_eq, b_bi], writes=[b_bi2])
        S.op("dve", lambda e: e.tensor_reduce(out=m2[:], in_=v3(bi2), axis=AX.X, op=ALU.max), reads=[b_bi2], writes=[b_m2])
        S.op("dve", lambda e: e.tensor_tensor(out=gs[:], in0=m1[:], in1=m2[:], op=ALU.add), reads=[b_m1, b_m2], writes=[b_gs])
        S.op("dve", lambda e: e.tensor_reduce(out=gmax[:], in_=gs[:, :].rearrange("p (s g) -> p s g", g=4), axis=AX.X, op=ALU.max),
             reads=[b_gs], writes=[b_gmax])
        for g in range(4):
            S.op("dve", lambda e: e.tensor_tensor(out=gsel[:, :].rearrange("p (s g) -> p s g", g=4)[:, :, g],
                                                  in0=gs[:, :].rearrange("p (s g) -> p s g", g=4)[:, :, g], in1=gmax[:], op=ALU.is_equal),
                 reads=[b_gs, b_gmax], writes=[b_gsel])
        for ee in range(4):
            S.op("dve", lambda e: e.tensor_tensor(out=v3(eq)[:, :, ee], in0=v3(bi)[:, :, ee], in1=m2[:], op=ALU.is_ge),
                 reads=[b_bi, b_m2], writes=[b_eq])
            S.op("dve", lambda e: e.tensor_tensor(out=v3(eq)[:, :, ee], in0=v3(eq)[:, :, ee], in1=gsel[:], op=ALU.mult),
                 reads=[b_eq, b_gsel], writes=[b_eq])
        S.op("dve", lambda e: e.tensor_tensor(out=wsel[:], in0=sc[:], in1=eq[:], op=ALU.mult), reads=[b_sc, b_eq], writes=[b_wsel])
        S.op("dve", lambda e: e.tensor_reduce(out=den[:], in_=wsel[:, :].rearrange("p (s e) -> p s e", e=16), axis=AX.X, op=ALU.add),
             reads=[b_wsel], writes=[b_den])
        S.op("dve", lambda e: e.reciprocal(out=den[:], in_=den[:]), reads=[b_den], writes=[b_den])
        for sub in range(4):
            S.op("dve", lambda e: e.tensor_scalar(out=gates[:, sub * 16:(sub + 1) * 16], in0=wsel[:, sub * 16:(sub + 1) * 16],
                                                  scalar1=den[:, sub:sub + 1], scalar2=None, op0=ALU.mult), reads=[b_wsel, b_den], writes=[b_gates])
        if DBG and t == 0:
            for i_, (tl, bb, w_) in enumerate(((sc, b_sc, 64), (bi, b_bi, 64), (m1, b_m1, 16), (m2, b_m2, 16), (gsel, b_gsel, 16), (eq, b_eq, 64), (gates, b_gates, 64), (den, b_den, 4))):
                S.dma("sp", lambda e: e.dma_start(out=dbg[:, i_, 0:w_], in_=tl[:, 0:w_]), reads=[bb], writes=[b_dbg])
            a2v = a2T.rearrange("(k p) t -> p k t", p=128)
            S.dma("sp", lambda e: e.dma_start(out=a2v[:, :, ts], in_=a2b[:, :, :]), reads=[b_a2b], writes=[b_a2T])
        for ex in range(NE):
            w1 = nw(); load_w(w1, wg[ex], NK, 0, 512)
            w2 = nw(); load_w(w2, wu[ex], NK, 0, 512)
            w3 = nw()
            wdv = wd[ex].rearrange("(k p) c -> p k c", p=128)
            W3v = W[w3][:, :, :].rearrange("p (k a) c -> p k (a c)", a=4)
            for k0 in range(0, 4, 2):
                S.dma("pool", lambda e: e.dma_start(out=W3v[:, k0:k0 + 2, :], in_=wdv[:, k0:k0 + 2, :]), writes=[b_W[w3]])
            gb = 6 + (ex % 2)
            for sub in range(4):
                gi = st["gm"]; st["gm"] ^= 1
                S.op("dve", lambda e: e.tensor_scalar(out=Gm[gi][:], in0=onesf[:], scalar1=gates[:, sub * 16 + ex:sub * 16 + ex + 1], scalar2=None,
                                                      op0=ALU.mult), reads=[b_onesf, b_gates], writes=[b_Gm[gi]])
                S.op("pe", lambda e: e.matmul(ps[:, gb, sub * 128:(sub + 1) * 128], lhsT=Gm[gi][:], rhs=idt[:], start=True, stop=True),
                     reads=[b_Gm[gi], b_idt], writes=[b_ps[gb]])
            hi = st["hp"]; st["hp"] ^= 1
            for dc in range(4):
                b1 = nbank()
                for k in range(NK):
                    S.op("pe", lambda e: e.matmul(ps[:, b1, :], lhsT=W[w1][:, k, dc * 128:(dc + 1) * 128], rhs=a2b[:, k, :],
                                                  start=(k == 0), stop=(k == NK - 1)), reads=[b_W[w1], b_a2b], writes=[b_ps[b1]])
                b2 = nbank()
                for k in range(NK):
                    S.op("pe", lambda e: e.matmul(ps[:, b2, :], lhsT=W[w2][:, k, dc * 128:(dc + 1) * 128], rhs=a2b[:, k, :],
                                                  start=(k == 0), stop=(k == NK - 1)), reads=[b_W[w2], b_a2b], writes=[b_ps[b2]])
                ta = ntmp(); tb = ntmp()
                S.op("act", lambda e: e.activation(out=tmp[ta][:], in_=ps[:, b1, :], func=AF.Silu), reads=[b_ps[b1]], writes=[b_tmp[ta]])
                S.op("dve", lambda e: e.tensor_tensor(out=tmp[tb][:], in0=tmp[ta][:], in1=ps[:, b2, :], op=ALU.mult),
                     reads=[b_tmp[ta], b_ps[b2]], writes=[b_tmp[tb]])
                S.op("dve", lambda e: e.tensor_tensor(out=hp[hi][:, dc, :], in0=tmp[tb][:], in1=ps[:, gb, :], op=ALU.mult),
                     reads=[b_tmp[tb], b_ps[gb]], writes=[b_hp[hi]])
            for n in range(NK):
                bk = nbank()
                for dc in range(4):
                    S.op("pe", lambda e: e.matmul(ps[:, bk, :], lhsT=W3v[:, dc, n * 128:(n + 1) * 128], rhs=hp[hi][:, dc, :],
                                                  start=(dc == 0), stop=(dc == 3)), reads=[b_W[w3], b_hp[hi]], writes=[b_ps[bk]])
                S.op("dve", lambda e: e.scalar_tensor_tensor(out=xt[:, n, :], in0=ps[:, bk, :], scalar=modt[:, 48 + n:49 + n], in1=xt[:, n, :],
                                                             op0=ALU.mult, op1=ALU.add), reads=[b_ps[bk], b_modt, b_xt[n]], writes=[b_xt[n]])
        if x2v is not None:
            for k0 in range(0, NK, 4):
                S.dma("sp", lambda e: e.dma_start(out=x2v[:, k0:k0 + 4, ts], in_=xt[:, k0:k0 + 4, :]), reads=b_xt[k0:k0 + 4], writes=[b_x2T])
        if xfv is not None:
            rms_rstd(xt[:, :, :], b_xt)
            for k in range(NK):
                ta = ntmp()
                S.op("dve", lambda e: e.scalar_tensor_tensor(out=tmp[ta][:], in0=xt[:, k, :], scalar=gfint[:, k:k + 1], in1=rstd[:],
                                                             op0=ALU.mult, op1=ALU.mult), reads=[b_xt[k], b_gfint, b_rstd], writes=[b_tmp[ta]])
                S.dma("sp", lambda e: e.dma_start(out=xfv[:, k, ts], in_=tmp[ta][:]), reads=[b_tmp[ta]], writes=[b_xfT])


def build_fused():
    nc = bass.Bass("TRN2", target_bir_lowering=False)
    ext = lambda n, s, dt: nc.dram_tensor(n, s, dt, kind="ExternalInput").ap()
    d = {}
    d['xT'] = ext("xT", [2048, 2048], F32)
    jt = ext("jt", [1, 2], I32)
    d['cT'] = ext("cT", [128, 16], F32)
    d['pos'] = ext("pos", [1, 2048], I32)
    d['invf'] = ext("invf", [64, 1], F32)
    d['sgn'] = ext("sgn", [64, 1], F32)
    d['wada'] = ext("wada", [2, 2048, 12288], F32)
    d['bada_a'] = ext("bada_a", [2, 128, 32], F32)
    d['bada_c'] = ext("bada_c", [2, 128, 64], F32)
    d['gmix'] = ext("gmix", [2, 128, 16], F32)
    d['gq'] = ext("gq", [2, 128, 4], F32)
    d['gkv'] = ext("gkv", [2, 128, 4], F32)
    d['gmoe'] = ext("gmoe", [2, 128, 16], F32)
    d['gfin'] = ext("gfin", [128, 16], F32)
    d['win'] = ext("win", [2, 2048, 14912], F32)
    d['wsw'] = ext("wsw", [2, 2048, 64], F32)
    d['wuq'] = ext("wuq", [2, 512, 1536], F32)
    d['wuqs'] = ext("wuqs", [2, 512, 512], F32)
    d['wukv'] = ext("wukv", [2, 512, 2048], F32)
    d['womla'] = ext("womla", [2, 1024, 2048], F32)
    d['wodil'] = ext("wodil", [2, 512, 2048], F32)
    d['wosb'] = ext("wosb", [2, 1024, 2048], F32)
    d['wout'] = ext("wout", [2, 2048, 2048], F32)
    d['wr'] = ext("wr", [2048, 16], F32)
    d['br'] = ext("br", [128, 16], F32)
    d['wg'] = ext("wg", [2, 16, 2048, 512], F32)
    d['wu'] = ext("wu", [2, 16, 2048, 512], F32)
    d['wd'] = ext("wd", [2, 16, 512, 2048], F32)
    d['ident'] = ext("ident", [128, 128], F32)
    d['maskc'] = ext("maskc", [128, 4, 512], BF16)
    d['masks'] = ext("masks", [128, 4, 512], BF16)
    d['maskd'] = ext("maskd", [128, 256], F32)
    d['m1'] = ext("m1", [128, 128], BF16)
    d['m2'] = ext("m2", [128, 128], BF16)
    d['nslope'] = ext("nslope", [128, 3], F32)
    d['dposk'] = ext("dposk", [3, 128, 64], I32)
    d['dposq'] = ext("dposq", [3, 8192], I32)
    xfT = nc.dram_tensor("xfT", [2048, 2048], F32, kind="ExternalOutput").ap()
    b_xfT = Buf("xfT")
    x2s = nc.dram_tensor("x2scr", [2048, 2048], F32).ap()
    b_x2s = Buf("x2s")
    b_x0 = Buf("x0")
    GROUPS = [[0, 1, 2, 3], [4, 5, 6, 7]]

    S = Sched(nc)
    S.enable_dyn(jt[:, :])
    CH = 128 * 4096
    QKs_h = nc.dram_tensor("QKs", [32, 128, 4096], BF16)
    Vs_h = nc.dram_tensor("Vs", [16, 128, 4096], BF16)
    QKg_h = nc.dram_tensor("QKg", [32, 512, 4096], BF16)
    Vg_h = nc.dram_tensor("Vg", [16, 512, 4096], BF16)
    Ys_h = nc.dram_tensor("Ys", [10, 128, 4096], BF16)
    Yg_h = nc.dram_tensor("Yg", [10, 512, 4096], BF16)
    gsc = nc.dram_tensor("gscr", [6144, 2048], F32).ap()
    QKl_h = nc.dram_tensor("QKl", [4096, 4096], BF16)
    Vl_h = nc.dram_tensor("Vl", [2048, 4096], BF16)
    Yl_h = nc.dram_tensor("Yl", [2560, 2048], BF16)
    for l in range(2):
        b_QKs, b_Vs, b_QKg, b_Vg, b_Ys, b_Yg, b_g = (Buf(n) for n in ("QKs", "Vs", "QKg", "Vg", "Ys", "Yg", "g"))
        b_QKl, b_Vl, b_Yl = Buf("QKl"), Buf("Vl"), Buf("Yl")
        xsrc = d['xT'] if l == 0 else x2s
        b_xsrc = b_x0 if l == 0 else b_x2s
        QKs_v = QKs_h.ap().rearrange("c p (a t) -> (c p a) t", a=4).rearrange("(s u r) t -> s u r t", s=4, u=2)
        Vs_v = Vs_h.ap().rearrange("c p (a d) -> (c p a) d", a=4).rearrange("(s t) d -> s t d", s=4)
        S.begin_phase()
        b_QKs2 = [Buf("QKs0"), Buf("QKs1")]
        b_Vs2 = [Buf("Vs0"), Buf("Vs1")]

        pending = []

        def after_sup(sup):
            for sh in range(4):
                for q in range(4):
                    pending.append((QKs_h, QKg_h, sh * 8 + sup * 4 + q, b_QKs2[sup], b_QKg))
            for sh in range(4):
                for q in range(2):
                    pending.append((Vs_h, Vg_h, sh * 4 + sup * 2 + q, b_Vs2[sup], b_Vg))
            if sup == 1:
                while pending:
                    tick()

        def tick():
            if pending:
                sh_, gh_, c, br_, bw_ = pending.pop(0)
                S.cc(lambda e: e.collective_compute("AllGather", ALU.bypass, replica_groups=GROUPS, ins=[sh_.ap()[c].opt()],
                                                    outs=[gh_.ap()[c].opt()]), reads=[br_], writes=[bw_])
        emit_A(S, nc, d, l, xsrc, QKs_v, Vs_v, gsc, b_QKs2, b_Vs2, b_g, after_sup=after_sup, tick=tick)
        S.end_phase()
        S.begin_phase()
        for hh in range(2):
            S.dma_dyn(QKl_h.ap()[hh * 2048:(hh + 1) * 2048, :], QKg_h, 8 * 4 * CH, hh * 2048 * 4096, [[4096, 2048], [1, 4096]],
                      reads=[b_QKg], writes=[b_QKl])
        S.dma_dyn(Vl_h.ap()[:, :], Vg_h, 4 * 4 * CH, 0, [[4096, 2048], [1, 4096]], reads=[b_Vg], writes=[b_Vl])
        QKl = QKl_h.ap().rearrange("(c r p) (a t) -> c r (p a) t", c=8, r=4, a=4)
        Vl = Vl_h.ap().rearrange("(c r p) (a d) -> c r (p a) d", c=4, r=4, a=4)
        Ys_v = Ys_h.ap().rearrange("(th b) p t -> th b p t", th=2)
        S.barrier()
        emit_B(S, nc, d, QKl, Vl, Ys_v, b_QKl, b_Vl, b_Ys, do_mla=True, do_sb=False, do_dil=False)
        S.end_phase()
        S.begin_phase()
        emit_B(S, nc, d, QKl, Vl, Ys_v, b_QKl, b_Vl, b_Ys, do_mla=False, do_sb=True, do_dil=False)
        for c in (0, 1, 3, 4, 5, 6, 8, 9):
            S.cc(lambda e: e.collective_compute("AllGather", ALU.bypass, replica_groups=GROUPS, ins=[Ys_h.ap()[c].opt()], outs=[Yg_h.ap()[c].opt()]),
                 reads=[b_Ys], writes=[b_Yg])
        S.end_phase(wait_cc=False)
        S.begin_phase()
        emit_B(S, nc, d, QKl, Vl, Ys_v, b_QKl, b_Vl, b_Ys, do_mla=False, do_sb=False, do_dil=True)
        for c in (2, 7):
            S.cc(lambda e: e.collective_compute("AllGather", ALU.bypass, replica_groups=GROUPS, ins=[Ys_h.ap()[c].opt()], outs=[Yg_h.ap()[c].opt()]),
                 reads=[b_Ys], writes=[b_Yg])
        S.end_phase()
        S.begin_phase()
        for b0, nb_ in ((0, 3), (3, 2)):
            S.dma_dyn(Yl_h.ap()[b0 * 512:(b0 + nb_) * 512, :], Yg_h, 1, b0 * 4 * CH, [[4 * CH, nb_], [4096, 512], [1, 2048]],
                      reads=[b_Yg], writes=[b_Yl], which=1)
        Yl = Yl_h.ap().rearrange("(b j p) t -> b j p t", b=5, j=4)
        emit_C(S, nc, d, l, Yl, gsc, xsrc, x2s if l == 0 else None, xfT if l == 1 else None,
               b_Yl, b_g, b_xsrc, b_x2s, b_xfT)
        S.end_phase()
    S.close()
    return nc


_PROG = {}


def _f(a):
    return np.ascontiguousarray(a)


def kernel(**inputs):
    inp = {k: np.asarray(v) for k, v in inputs.items()}
    B_, S_ = inp['x'].shape[:2]
    cores = list(range(8))
    pos = inp['positions'].astype(np.int32)
    perm = [np.concatenate([np.arange(s, S_, r) for s in range(r)]) for r in RATES]
    slopes = (np.float32(2.0) ** (np.float32(-8.0) * np.arange(1, 13, dtype=np.float32) / np.float32(12))).reshape(3, 4)
    half = 32
    invf = (np.float32(10000.0) ** (-(np.arange(half, dtype=np.float32)) / np.float32(half))).astype(np.float32)
    wu_ = inp['w_uq']
    wuqs = np.stack([np.concatenate([np.concatenate([wu_[l][:, h * 192 + 160:h * 192 + 192], wu_[l][:, h * 192 + 128:h * 192 + 160]], axis=1)
                                     for h in range(8)], axis=1) for l in range(2)])
    wi_ = inp['w_in']
    lay = lambda a, n: _f(a.reshape(a.shape[0], n, 128).transpose(0, 2, 1))
    shared = {
        'invf': _f(np.concatenate([invf, invf])[:, None]),
        'sgn': _f(np.concatenate([-np.ones(32, np.float32), np.ones(32, np.float32)])[:, None]),
        'wada': _f(inp['w_ada']),
        'bada_a': lay(inp['b_ada'][:, :4096], 32),
        'bada_c': lay(inp['b_ada'][:, 4096:], 64),
        'gmix': lay(inp['g_mix'], 16), 'gq': lay(inp['g_q'], 4), 'gkv': lay(inp['g_kv'], 4), 'gmoe': lay(inp['g_moe'], 16),
        'gfin': _f(inp['g_final'].reshape(16, 128).T),
        'win': _f(wi_), 'wsw': _f(np.concatenate([wi_[:, :, 1056:1088], wi_[:, :, 1024:1056]], axis=2)),
        'wuq': _f(wu_), 'wuqs': _f(wuqs), 'wukv': _f(inp['w_ukv']),
        'womla': _f(inp['w_o_mla']), 'wodil': _f(inp['w_o_dil']), 'wosb': _f(inp['w_o_sb']), 'wout': _f(inp['w_out']),
        'wr': _f(inp['w_router']), 'br': _f(np.broadcast_to(inp['b_router'][None, :], (128, 16))),
        'wg': _f(inp['w_gate']), 'wu': _f(inp['w_up']), 'wd': _f(inp['w_down']), 'ident': np.eye(128, dtype=np.float32),
    }
    shared.update(consts_B())
    maps = []
    for c in cores:
        b, j = c // 4, c % 4
        m = dict(shared)
        m['xT'] = _f(inp['x'][b, j * 2048:(j + 1) * 2048, :].T)
        m['jt'] = np.array([[j, (j // 2) * 5 * 4 * 128 * 4096 + (j % 2) * 2048]], np.int32)
        m['cT'] = _f(inp['c'][b].reshape(16, 128).T)
        m['pos'] = _f(pos[b, j * 2048:(j + 1) * 2048][None, :])
        pp = np.stack([pos[b][perm[g]] for g in range(3)]).astype(np.int32)
        m['dposq'] = _f(pp)
        m['dposk'] = _f(pp.reshape(3, S_ // 128, 128).transpose(0, 2, 1))
        m['nslope'] = _f(np.broadcast_to(-slopes[:, j][None, :], (128, 3)).astype(np.float32))
        maps.append(m)
    if "f" not in _PROG:
        _PROG["f"] = build_fused()
    res = run_bass_kernel_spmd(_PROG["f"], maps, core_ids=cores).results
    out = np.empty(inp['x'].shape, dtype=np.float32)
    for c in cores:
        b, j = c // 4, c % 4
        out[b, j * 2048:(j + 1) * 2048, :] = np.asarray(res[c]['xfT']).T
    return out
```

```python
import math
import numpy as np
import concourse.bass as bass
import concourse.mybir as mybir
from concourse.bass_utils import run_bass_kernel_spmd

F32 = mybir.dt.float32
BF16 = mybir.dt.bfloat16
I32 = mybir.dt.int32
AF = mybir.ActivationFunctionType
ALU = mybir.AluOpType
AX = mybir.AxisListType


class Buf:
    __slots__ = ("name", "w", "r")

    def __init__(self, name):
        self.name = name
        self.w = None
        self.r = {}


class _Rec:
    def __init__(self):
        self.call = None

    def __getattr__(self, name):
        def f(*a, **kw):
            self.call = (name, a, kw)
            return self
        return f


def _record(fn):
    r = _Rec()
    fn(r)
    assert r.call is not None
    return r.call


class Sched:
    ENGS = ("pe", "act", "dve", "pool", "sp")
    NDMA = 12

    def __init__(self, nc):
        self.nc = nc
        self.streams = {e: [] for e in self.ENGS}
        self.cnt = {e: 0 for e in self.ENGS}
        self.seen = {e: {} for e in self.ENGS}
        self.dma_i = {"sp": 0, "pool": 0, "act": 0}
        self.dma_val = {}
        self.sems = {}
        self._ctx = []
        self._perm = []
        self.cc_val = {}
        self._phase_mark = 0
        self.uses_dyn = False
        self._phase_no = 0
        self.jt_ap = None
        self.jsb = None
        self.jt_cnt = 0
        for e in ("pe", "act", "dve", "pool"):
            self._mk_sem("E_" + e)
        for q in ("sp", "pool", "act"):
            for k in range(self.NDMA):
                self._mk_sem("D_%s%d" % (q, k))
                self.dma_val["D_%s%d" % (q, k)] = 0

    def _mk_sem(self, key):
        cm = self.nc.semaphore(key)
        self.sems[key] = cm.__enter__()
        self._perm.append(cm)

    _mk_sem_perm = _mk_sem

    def enable_dyn(self, jt_ap):
        self.jt_ap = jt_ap
        self._mk_sem("JT")
        cm = self.nc.sbuf_tensor("jsb", [1, 2], I32)
        self.jsb = cm.__enter__()
        self._perm.append(cm)

    def sbuf(self, name, shape, dtype):
        cm = self.nc.sbuf_tensor("%s_p%d" % (name, self._phase_no), shape, dtype)
        t = cm.__enter__()
        self._ctx.append(cm)
        return t

    def psum(self, name, shape, dtype):
        cm = self.nc.psum_tensor("%s_p%d" % (name, self._phase_no), shape, dtype)
        t = cm.__enter__()
        self._ctx.append(cm)
        return t

    def _deps(self, eng, reads, writes):
        deps = {}

        def add(tok):
            if tok is None:
                return
            k, v = tok
            if deps.get(k, -1) < v:
                deps[k] = v
        for b in reads:
            add(b.w)
        for b in writes:
            add(b.w)
            for k, v in b.r.items():
                add((k, v))
        out = []
        seen = self.seen[eng]
        for k, v in deps.items():
            if eng == "pe" and k == "E_pe":
                continue
            if seen.get(k, -1) >= v:
                continue
            seen[k] = v
            out.append((k, v))
        return out

    def _mark(self, tok, reads, writes):
        k, v = tok
        for b in reads:
            if b.r.get(k, -1) < v:
                b.r[k] = v
        for b in writes:
            b.w = tok
            b.r = {}

    def op(self, eng, fn, reads=(), writes=()):
        waits = self._deps(eng, reads, writes)
        self.cnt[eng] += 1
        tok = ("E_" + eng, self.cnt[eng])
        self.streams[eng].append((waits, _record(fn), tok[0], 1))
        self._mark(tok, reads, writes)

    def dma(self, q, fn, reads=(), writes=()):
        i = self.dma_i[q]
        self.dma_i[q] += 1
        key = "D_%s%d" % (q, i % self.NDMA)
        waits = self._deps(q, reads, writes)
        prev = self.dma_val[key]
        if prev > 0 and self.seen[q].get(key, -1) < prev:
            self.seen[q][key] = prev
            waits.append((key, prev))
        self.dma_val[key] = prev + 16
        tok = (key, prev + 16)
        self.streams[q].append((waits, _record(fn), key, 16))
        self._mark(tok, reads, writes)

    def begin_phase(self):
        self._phase_mark = len(self._ctx)
        self._phase_no += 1

    def barrier(self, wait_cc=True):
        allv = {}
        for e in ("pe", "act", "dve", "pool"):
            if self.cnt[e] > 0:
                allv["E_" + e] = self.cnt[e]
        for k, v in self.dma_val.items():
            if v > 0:
                allv[k] = v
        for k, v in self.cc_val.items():
            if v > 0 and wait_cc:
                allv[k] = v
        for eng in self.ENGS:
            waits = []
            for k, v in allv.items():
                if self.seen[eng].get(k, -1) < v:
                    self.seen[eng][k] = v
                    waits.append((k, v))
            self.streams[eng].append((waits, None, None, 0))

    def end_phase(self, wait_cc=True):
        self.barrier(wait_cc)
        self.emit()
        self.streams = {e: [] for e in self.ENGS}
        while len(self._ctx) > self._phase_mark:
            self._ctx.pop().__exit__(None, None, None)

    def cc(self, fn, reads=(), writes=()):
        key = "CC"
        if key not in self.sems:
            self._mk_sem_perm(key)
            self.cc_val[key] = 0
        n = self.cc_val[key] + 1
        self.cc_val[key] = n
        waits = self._deps("pool", reads, writes)
        self.streams["pool"].append((waits, _record(fn), key, None))
        self._mark((key, n), reads, writes)

    def dma_dyn(self, out_ap, tensor, jmul, const, ap_list, reads=(), writes=(), which=0):
        q = "sp"
        i = self.dma_i[q]
        self.dma_i[q] += 1
        key = "D_%s%d" % (q, i % self.NDMA)
        waits = self._deps(q, reads, writes)
        prev = self.dma_val[key]
        if prev > 0 and self.seen[q].get(key, -1) < prev:
            self.seen[q][key] = prev
            waits.append((key, prev))
        self.dma_val[key] = prev + 16
        tok = (key, prev + 16)
        self.streams[q].append((waits, ("__dyn__", (out_ap, tensor, int(jmul), int(const), [list(x) for x in ap_list], which), {}), key, 16))
        self._mark(tok, reads, writes)
        self.uses_dyn = True

    def final_wait(self, eng, bufs):
        waits = self._deps(eng, bufs, ())
        self.streams[eng].append((waits, None, None, 0))

    def emit(self):
        nc = self.nc
        sems = self.sems
        streams = self.streams

        def run(engine, lst, regs=None):
            for waits, fn, key, inc in lst:
                for k, v in waits:
                    engine.wait_ge(sems[k], v)
                if fn is not None:
                    name, a, kw = fn
                    if name == "__dyn__":
                        out_ap, tensor, jmul, const, ap_list, which = a
                        rj, ro = regs[which], regs[2]
                        engine.reg_mul(ro, rj, jmul)
                        engine.reg_add(ro, ro, const)
                        ins = engine.dma_start(out=out_ap, in_=bass.AP(tensor, ro, ap_list))
                    else:
                        ins = getattr(engine, name)(*a, **kw)
                    if inc is None:
                        ins.then_inc(sems[key])
                    else:
                        ins.then_inc(sems[key], inc)

        with nc.Block() as block:
            @block.tensor
            def _(e):
                run(e, streams["pe"])

            @block.scalar
            def _(e):
                run(e, streams["act"])

            @block.vector
            def _(e):
                run(e, streams["dve"])

            @block.gpsimd
            def _(e):
                run(e, streams["pool"])

            @block.sync
            def _(e):
                if any(fn is not None and fn[0] == "__dyn__" for _, fn, _, _ in streams["sp"]):
                    self.jt_cnt += 1
                    with e.register("rj%d" % self.jt_cnt) as rj, e.register("ry%d" % self.jt_cnt) as ry, e.register("ro%d" % self.jt_cnt) as ro:
                        e.dma_start(out=self.jsb[:, :], in_=self.jt_ap).then_inc(sems["JT"], 16)
                        e.wait_ge(sems["JT"], 16 * self.jt_cnt)
                        e.reg_load(rj, self.jsb[0:1, 0:1])
                        e.reg_load(ry, self.jsb[0:1, 1:2])
                        run(e, streams["sp"], (rj, ry, ro))
                else:
                    run(e, streams["sp"])

    def close(self):
        for cm in reversed(self._ctx):
            cm.__exit__(None, None, None)
        self._ctx = []
        for cm in reversed(self._perm):
            cm.__exit__(None, None, None)
        self._perm = []


D = 2048
NK = 16
TS = 1024
TP = 128
EPS = 1e-6
TWO_PI = 2.0 * math.pi
C1 = 6.28125
C2 = TWO_PI - C1


def emit_A(S, nc, d, l, xT, QKs, Vs, gT, b_QKs_l, b_Vs_l, b_gT, ntok=2048, after_sup=None, tick=None):
    cT = d['cT']; wada = d['wada'][l]; bada = d['bada_a'][l]; gmix = d['gmix'][l]; win = d['win'][l]; wsw = d['wsw'][l]
    gq = d['gq'][l]; gkv = d['gkv'][l]; wuq = d['wuq'][l]; wuqs = d['wuqs'][l]; wukv = d['wukv'][l]
    pos = d['pos']; invf = d['invf']; sgn = d['sgn']
    b_QKs = b_QKs_l[0]; b_Vs = b_Vs_l[0]
    b_projT = b_QKs; b_mlaq = b_QKs; b_mlakv = b_QKs; b_mlakr = b_QKs
    aT = S.sbuf("aT", [128, NK, TS], BF16); b_aT = [Buf("aT%d" % i) for i in range(TS // TP)]
    wb = [S.sbuf("wb%d" % i, [128, NK, 512], BF16) for i in range(2)]; b_wb = [Buf("wb0"), Buf("wb1")]
    xt = S.sbuf("xt", [128, NK, TP], F32); b_xt = Buf("xt")
    sq = S.sbuf("sq", [128, NK, TP], BF16); b_sq = Buf("sq")
    rstd = S.sbuf("rstd", [128, 512], F32); b_rstd = Buf("rstd")
    lnt = S.sbuf("lnt", [128, 512], F32); b_lnt = Buf("lnt")
    tmp = [S.sbuf("tmp%d" % i, [128, 512], F32) for i in range(2)]; b_tmp = [Buf("tmp0"), Buf("tmp1")]
    ones = S.sbuf("ones", [128, 128], BF16); b_ones = Buf("ones")
    cin = S.sbuf("cin", [128, NK], F32); b_cin = Buf("cin")
    cact = S.sbuf("cact", [128, NK], BF16); b_cact = Buf("cact")
    badat = S.sbuf("badat", [128, 32], F32); b_badat = Buf("badat")
    gmixt = S.sbuf("gmixt", [128, NK], F32); b_gmixt = Buf("gmixt")
    modt = S.sbuf("modt", [128, 32], F32); b_modt = Buf("modt")
    s1 = S.sbuf("s1", [128, NK], F32); b_s1 = Buf("s1")
    cq = S.sbuf("cq", [128, 4, TS], F32); b_cq = Buf("cq")
    ckv = S.sbuf("ckv", [128, 4, TS], F32); b_ckv = Buf("ckv")
    kr = S.sbuf("kr", [64, TS], F32); b_kr = Buf("kr")
    krs = S.sbuf("krs", [64, TS], F32); b_krs = Buf("krs")
    wswb = S.sbuf("wswb", [128, NK, 64], BF16); b_wswb = Buf("wswb")
    wuqb = S.sbuf("wuqb", [128, 4, 1536], BF16); b_wuqb = Buf("wuqb")
    wuqsb = S.sbuf("wuqsb", [128, 4, 512], BF16); b_wuqsb = Buf("wuqsb")
    wukvb = S.sbuf("wukvb", [128, 4, 2048], BF16); b_wukvb = Buf("wukvb")
    gqt = S.sbuf("gqt", [128, 4], F32); b_gqt = Buf("gqt")
    gkvt = S.sbuf("gkvt", [128, 4], F32); b_gkvt = Buf("gkvt")
    ob = [S.sbuf("ob%d" % i, [128, TS], BF16) for i in range(2)]; b_ob = [Buf("ob0"), Buf("ob1")]
    of = [S.sbuf("of%d" % i, [128, TS], F32) for i in range(2)]; b_of = [Buf("of0"), Buf("of1")]
    lat = S.sbuf("lat", [128, 4, 512], BF16); b_lat = Buf("lat")
    posi = S.sbuf("posi", [64, 512], I32); b_posi = Buf("posi")
    ang = S.sbuf("ang", [64, 512], F32); b_ang = Buf("ang")
    kf = S.sbuf("kf", [64, 512], F32); b_kf = Buf("kf")
    ki = posi; b_ki = b_posi
    rr = S.sbuf("rr", [64, 512], F32); b_rr = Buf("rr")
    rc = S.sbuf("rc", [64, 512], F32); b_rc = Buf("rc")
    mm = S.sbuf("mm", [64, 512], F32); b_mm = Buf("mm")
    CS = S.sbuf("CS", [64, 512], F32); b_CS = Buf("CS")
    SN = S.sbuf("SN", [64, 512], F32); b_SN = Buf("SN")
    invft = S.sbuf("invft", [64, 1], F32); b_invft = Buf("invft")
    sgnt = S.sbuf("sgnt", [64, 1], F32); b_sgnt = Buf("sgnt")
    t1 = ang; b_t1 = b_ang
    t2 = kf; b_t2 = b_kf
    ps = S.psum("ps", [128, 8, 512], F32); b_ps = [Buf("ps%d" % i) for i in range(8)]
    st = {"bank": 0, "w": 0, "ob": 0, "of": 0, "tmp": 0, "ev": 0}

    def nbank():
        i = st["bank"]; st["bank"] = (i + 1) % 8
        return i

    def load_w(dst, bdst, src, nk, c0, ncols):
        srcv = src.rearrange("(k p) c -> p k c", p=128)
        h = max(1, nk // 2)
        for k0 in range(0, nk, h):
            S.dma("pool", lambda e, k0=k0: e.dma_start(out=dst[:, k0:k0 + h, 0:ncols],
                                                      in_=srcv[:, k0:k0 + h, c0:c0 + ncols]),
                  writes=[bdst])

    S.op("dve", lambda e: e.memset(ones[:], 1.0), writes=[b_ones])
    S.dma("sp", lambda e: e.dma_start(out=cin[:], in_=cT[:, :]), writes=[b_cin])
    S.dma("sp", lambda e: e.dma_start(out=badat[:], in_=bada[:, :]), writes=[b_badat])
    S.dma("sp", lambda e: e.dma_start(out=gmixt[:], in_=gmix[:, :]), writes=[b_gmixt])
    S.dma("sp", lambda e: e.dma_start(out=gqt[:], in_=gq[:, :]), writes=[b_gqt])
    S.dma("sp", lambda e: e.dma_start(out=gkvt[:], in_=gkv[:, :]), writes=[b_gkvt])
    S.dma("sp", lambda e: e.dma_start(out=invft[:], in_=invf[:, :]), writes=[b_invft])
    S.dma("sp", lambda e: e.dma_start(out=sgnt[:], in_=sgn[:, :]), writes=[b_sgnt])
    S.op("act", lambda e: e.activation(out=cact[:], in_=cin[:], func=AF.Silu), reads=[b_cin], writes=[b_cact])

    mb = nbank()
    for g in range(8):
        wi = st["w"]; st["w"] ^= 1
        load_w(wb[wi], b_wb[wi], wada, NK, g * 512, 512)
        for m in range(4):
            n = g * 4 + m
            for k in range(NK):
                S.op("pe", lambda e, wi=wi, m=m, k=k, n=n: e.matmul(
                    ps[:, mb, n:n + 1], lhsT=wb[wi][:, k, m * 128:(m + 1) * 128], rhs=cact[:, k:k + 1],
                    start=(k == 0), stop=(k == NK - 1)), reads=[b_wb[wi], b_cact], writes=[b_ps[mb]])
    S.op("dve", lambda e: e.tensor_tensor(out=modt[:], in0=ps[:, mb, 0:32], in1=badat[:], op=ALU.add),
         reads=[b_ps[mb], b_badat], writes=[b_modt])
    S.op("dve", lambda e: e.scalar_tensor_tensor(out=s1[:], in0=modt[:, 16:32], scalar=1.0, in1=gmixt[:],
                                                 op0=ALU.add, op1=ALU.mult),
         reads=[b_modt, b_gmixt], writes=[b_s1])

    load_w(wswb, b_wswb, wsw, NK, 0, 64)
    load_w(wuqb, b_wuqb, wuq, 4, 0, 1536)
    load_w(wuqsb, b_wuqsb, wuqs, 4, 0, 512)
    load_w(wukvb, b_wukvb, wukv, 4, 0, 2048)

    def rms_rstd(src_sq, bsrc, nk, width, dim):
        bk = nbank()
        for k in range(nk):
            S.op("pe", lambda e, k=k: e.matmul(ps[:, bk, 0:width], lhsT=ones[:], rhs=src_sq[:, k, 0:width],
                                               start=(k == 0), stop=(k == nk - 1)),
                 reads=[bsrc, b_ones], writes=[b_ps[bk]])
        S.op("act", lambda e: e.activation(out=lnt[:, 0:width], in_=ps[:, bk, 0:width], func=AF.Ln,
                                           scale=1.0 / dim, bias=EPS), reads=[b_ps[bk]], writes=[b_lnt])
        S.op("act", lambda e: e.activation(out=rstd[:, 0:width], in_=lnt[:, 0:width], func=AF.Exp, scale=-0.5),
             reads=[b_lnt], writes=[b_rstd])

    xTv = xT.rearrange("(k p) t -> p k t", p=128)
    for sup in range(ntok // TS):
        t0s = sup * TS
        b_QKs = b_QKs_l[sup]; b_Vs = b_Vs_l[sup]
        for pt in range(TS // TP):
            tok0 = t0s + pt * TP
            for k0 in (0, 8):
                S.dma("sp", lambda e, k0=k0, tok0=tok0: e.dma_start(out=xt[:, k0:k0 + 8, :],
                                                                    in_=xTv[:, k0:k0 + 8, tok0:tok0 + TP]),
                      writes=[b_xt])
            S.op("act", lambda e: e.activation(out=sq[:], in_=xt[:], func=AF.Square), reads=[b_xt], writes=[b_sq])
            rms_rstd(sq, b_sq, NK, TP, float(D))
            for k in range(NK):
                ti = st["tmp"]; st["tmp"] ^= 1
                S.op("dve", lambda e, k=k, ti=ti: e.tensor_tensor(out=tmp[ti][:, 0:TP], in0=xt[:, k, :],
                                                                  in1=rstd[:, 0:TP], op=ALU.mult),
                     reads=[b_xt, b_rstd], writes=[b_tmp[ti]])
                S.op("act", lambda e, k=k, ti=ti, pt=pt: e.activation(
                    out=aT[:, k, pt * TP:(pt + 1) * TP], in_=tmp[ti][:, 0:TP], func=AF.Identity,
                    scale=s1[:, k:k + 1], bias=modt[:, k:k + 1]),
                    reads=[b_tmp[ti], b_s1, b_modt], writes=[b_aT[pt]])

        def gemm_group(src, c0, ncols, epilogue):
            wi = st["w"]; st["w"] ^= 1
            load_w(wb[wi], b_wb[wi], src, NK, c0, ncols)
            if tick is not None:
                tick()
            for m in range((ncols + 127) // 128):
                mc = min(128, ncols - m * 128)
                for t in range(TS // 512):
                    bk = nbank()
                    for k in range(NK):
                        S.op("pe", lambda e, wi=wi, m=m, mc=mc, t=t, k=k, bk=bk: e.matmul(
                            ps[0:mc, bk, :], lhsT=wb[wi][:, k, m * 128:m * 128 + mc],
                            rhs=aT[:, k, t * 512:(t + 1) * 512], start=(k == 0), stop=(k == NK - 1)),
                            reads=[b_wb[wi]] + b_aT[4 * t:4 * t + 4], writes=[b_ps[bk]])
                    epilogue(m, mc, t, bk)

        def evac(out_ap, bout, bk, mc, scale=1.0, func=None):
            st["ev"] ^= 1
            if func is not None or st["ev"]:
                f = func if func is not None else AF.Copy
                S.op("act", lambda e: e.activation(out=out_ap, in_=ps[0:mc, bk, :], func=f, scale=scale),
                     reads=[b_ps[bk]], writes=[bout])
            else:
                S.op("dve", lambda e: e.tensor_scalar(out=out_ap, in0=ps[0:mc, bk, :], scalar1=scale, scalar2=None,
                                                      op0=ALU.mult), reads=[b_ps[bk]], writes=[bout])

        gemm_group(win, 0, 512, lambda m, mc, t, bk: evac(cq[:, m, t * 512:(t + 1) * 512], b_cq, bk, mc))
        gemm_group(win, 512, 512, lambda m, mc, t, bk: evac(ckv[:, m, t * 512:(t + 1) * 512], b_ckv, bk, mc))
        gemm_group(win, 1024, 64, lambda m, mc, t, bk: evac(kr[:, t * 512:(t + 1) * 512], b_kr, bk, mc))
        gemm_group(wsw, 0, 64, lambda m, mc, t, bk: evac(krs[:, t * 512:(t + 1) * 512], b_krs, bk, mc))

        def out_bf(dst, bdst, row0, scale):
            cur = {}

            def ep(m, mc, t, bk):
                if t == 0:
                    cur["i"] = st["ob"]; st["ob"] ^= 1
                i = cur["i"]
                evac(ob[i][:, t * 512:(t + 1) * 512], b_ob[i], bk, mc, scale=scale)
                if t == TS // 512 - 1:
                    r = row0 + m * 128
                    S.dma("sp", lambda e, i=i, r=r: e.dma_start(out=dst[r:r + 128, t0s:t0s + TS], in_=ob[i][:, :]),
                          reads=[b_ob[i]], writes=[bdst])
            return ep

        def out_f32(dst, bdst, row0, func):
            cur = {}

            def ep(m, mc, t, bk):
                if t == 0:
                    cur["i"] = st["of"]; st["of"] ^= 1
                i = cur["i"]
                evac(of[i][:, t * 512:(t + 1) * 512], b_of[i], bk, mc, func=func)
                if t == TS // 512 - 1:
                    r = row0 + m * 128
                    S.dma("sp", lambda e, i=i, r=r: e.dma_start(out=dst[r:r + 128, t0s:t0s + TS], in_=of[i][:, :]),
                          reads=[b_of[i]], writes=[bdst])
            return ep

        sc = 128.0 ** -0.5
        def out_qk(rowfn, scale):
            cur = {}

            def ep(m, mc, t, bk):
                if t == 0:
                    cur["i"] = st["ob"]; st["ob"] ^= 1
                i = cur["i"]
                evac(ob[i][:, t * 512:(t + 1) * 512], b_ob[i], bk, mc, scale=scale)
                if t == TS // 512 - 1:
                    sh, r0 = rowfn(m)
                    S.dma("sp", lambda e: e.dma_start(out=QKs[sh, sup, r0:r0 + 128, :], in_=ob[i][:, :]),
                          reads=[b_ob[i]], writes=[b_QKs])
            return ep

        def gemm_group_tm(c0, store):
            wi = st["w"]; st["w"] ^= 1
            load_w(wb[wi], b_wb[wi], win, NK, c0, 512)
            if tick is not None:
                tick()
            for s_ in range(TS // 128):
                bk = nbank()
                for k in range(NK):
                    S.op("pe", lambda e: e.matmul(ps[:, bk, :], lhsT=aT[:, k, s_ * 128:(s_ + 1) * 128], rhs=wb[wi][:, k, 0:512],
                                                  start=(k == 0), stop=(k == NK - 1)), reads=[b_wb[wi], b_aT[s_]], writes=[b_ps[bk]])
                oi = st["ob"]; st["ob"] ^= 1
                evac(ob[oi][:, 0:512], b_ob[oi], bk, 128)
                store(s_, oi)

        for g in range(3):
            gemm_group(win, 1088 + g * 512, 512, out_qk(lambda m, g=g: (m, g * 128), sc))
        for g in range(3):
            gemm_group(win, 1088 + 1536 + g * 512, 512, out_qk(lambda m, g=g: (m, 384 + g * 128), 1.0))
        for g in range(3):
            def st_dv(s_, oi, g=g):
                tk = t0s + s_ * 128
                S.dma("sp", lambda e: e.dma_start(out=Vs[:, tk:tk + 128, g * 128:(g + 1) * 128].rearrange("h p c -> p h c"),
                                                  in_=ob[oi][:, 0:512].rearrange("p (h c) -> p h c", c=128)),
                      reads=[b_ob[oi]], writes=[b_Vs])
            gemm_group_tm(1088 + 3072 + g * 512, st_dv)
        for gi in range(2):
            gemm_group(win, 5696 + gi * 512, 512, out_qk(lambda m, gi=gi: ((4 * gi + m) // 2, 768 + (m % 2) * 128), sc))
        for gi in range(2):
            gemm_group(win, 5696 + 1024 + gi * 512, 512, out_qk(lambda m, gi=gi: ((4 * gi + m) // 2, 1024 + (m % 2) * 128), 1.0))
        for gi in range(2):
            def st_sv(s_, oi, gi=gi):
                tk = t0s + s_ * 128
                S.dma("sp", lambda e: e.dma_start(out=Vs[2 * gi:2 * gi + 2, tk:tk + 128, 384:640].rearrange("j p c -> p j c"),
                                                  in_=ob[oi][:, 0:512].rearrange("p (j c) -> p j c", c=256)),
                      reads=[b_ob[oi]], writes=[b_Vs])
            gemm_group_tm(5696 + 2048 + gi * 512, st_sv)

        scm = 192.0 ** -0.5
        for tt in range(TS // 512):
            tok0 = t0s + tt * 512
            tsl = slice(tt * 512, (tt + 1) * 512)
            S.dma("sp", lambda e, tok0=tok0: e.dma_start(out=posi[:], in_=pos[0:1, tok0:tok0 + 512].partition_broadcast(64)),
                  writes=[b_posi])
            S.op("dve", lambda e: e.tensor_copy(out=ang[:], in_=posi[:]), reads=[b_posi], writes=[b_ang])
            S.op("dve", lambda e: e.tensor_scalar(out=ang[:], in0=ang[:], scalar1=invft[:, 0:1], scalar2=None, op0=ALU.mult),
                 reads=[b_ang, b_invft], writes=[b_ang])
            S.op("dve", lambda e: e.tensor_scalar(out=kf[:], in0=ang[:], scalar1=1.0 / TWO_PI, scalar2=None, op0=ALU.mult),
                 reads=[b_ang], writes=[b_kf])
            S.op("dve", lambda e: e.tensor_copy(out=ki[:], in_=kf[:]), reads=[b_kf], writes=[b_ki])
            S.op("dve", lambda e: e.tensor_copy(out=kf[:], in_=ki[:]), reads=[b_ki], writes=[b_kf])
            S.op("dve", lambda e: e.scalar_tensor_tensor(out=rr[:], in0=kf[:], scalar=-C1, in1=ang[:], op0=ALU.mult, op1=ALU.add),
                 reads=[b_kf, b_ang], writes=[b_rr])
            S.op("dve", lambda e: e.scalar_tensor_tensor(out=rr[:], in0=kf[:], scalar=-C2, in1=rr[:], op0=ALU.mult, op1=ALU.add),
                 reads=[b_kf, b_rr], writes=[b_rr])

            def wrap(r, br):
                S.op("dve", lambda e: e.tensor_scalar(out=mm[:], in0=r[:], scalar1=math.pi, scalar2=-TWO_PI, op0=ALU.is_gt, op1=ALU.mult),
                     reads=[br], writes=[b_mm])
                S.op("dve", lambda e: e.tensor_tensor(out=r[:], in0=r[:], in1=mm[:], op=ALU.add), reads=[br, b_mm], writes=[br])
                S.op("dve", lambda e: e.tensor_scalar(out=mm[:], in0=r[:], scalar1=-math.pi, scalar2=TWO_PI, op0=ALU.is_lt, op1=ALU.mult),
                     reads=[br], writes=[b_mm])
                S.op("dve", lambda e: e.tensor_tensor(out=r[:], in0=r[:], in1=mm[:], op=ALU.add), reads=[br, b_mm], writes=[br])
                S.op("dve", lambda e: e.tensor_scalar(out=r[:], in0=r[:], scalar1=3.1415925, scalar2=-3.1415925, op0=ALU.min, op1=ALU.max),
                     reads=[br], writes=[br])
            wrap(rr, b_rr)
            S.op("dve", lambda e: e.tensor_scalar(out=rc[:], in0=rr[:], scalar1=math.pi / 2, scalar2=None, op0=ALU.add),
                 reads=[b_rr], writes=[b_rc])
            wrap(rc, b_rc)
            S.op("act", lambda e: e.activation(out=CS[:], in_=rc[:], func=AF.Sin), reads=[b_rc], writes=[b_CS])
            S.op("act", lambda e: e.activation(out=SN[:], in_=rr[:], func=AF.Sin, scale=sgnt[:, 0:1]), reads=[b_rr, b_sgnt], writes=[b_SN])

            def rope_out(src_r, bsr, src_s, bss, scale, dst_ap, bdst):
                S.op("dve", lambda e: e.scalar_tensor_tensor(out=t1[:], in0=src_r, scalar=scale, in1=CS[:], op0=ALU.mult, op1=ALU.mult),
                     reads=[bsr, b_CS], writes=[b_t1])
                S.op("dve", lambda e: e.scalar_tensor_tensor(out=t2[:], in0=src_s, scalar=scale, in1=SN[:], op0=ALU.mult, op1=ALU.mult),
                     reads=[bss, b_SN], writes=[b_t2])
                S.op("dve", lambda e: e.tensor_tensor(out=dst_ap, in0=t1[:], in1=t2[:], op=ALU.add),
                     reads=[b_t1, b_t2], writes=[bdst])

            oi = st["ob"]; st["ob"] ^= 1
            rope_out(kr[:, tsl], b_kr, krs[:, tsl], b_krs, 1.0, ob[oi][0:64, 0:512], b_ob[oi])
            for sh in range(4):
                S.dma("sp", lambda e: e.dma_start(out=QKs[sh, sup, 1920:1984, tok0 - t0s:tok0 - t0s + 512], in_=ob[oi][0:64, 0:512]),
                      reads=[b_ob[oi]], writes=[b_QKs])

            def latent_norm(src, bsrc, gt, bgt):
                sqv = sq[:, :, :].rearrange("p (k a) t -> p k (a t)", a=4)
                S.op("act", lambda e: e.activation(out=sqv, in_=src[:, :, tsl], func=AF.Square), reads=[bsrc], writes=[b_sq])
                rms_rstd(sqv, b_sq, 4, 512, 512.0)
                for k in range(4):
                    S.op("dve", lambda e, k=k: e.scalar_tensor_tensor(out=lat[:, k, :], in0=src[:, k, tsl], scalar=gt[:, k:k + 1],
                                                                      in1=rstd[:, :], op0=ALU.mult, op1=ALU.mult),
                         reads=[bsrc, bgt, b_rstd], writes=[b_lat])

            latent_norm(cq, b_cq, gqt, b_gqt)
            for h in range(8):
                bk = nbank()
                for k in range(4):
                    S.op("pe", lambda e, h=h, k=k, bk=bk: e.matmul(ps[:, bk, :], lhsT=wuqb[:, k, h * 192:h * 192 + 128], rhs=lat[:, k, :],
                                                                   start=(k == 0), stop=(k == 3)), reads=[b_wuqb, b_lat], writes=[b_ps[bk]])
                oi = st["ob"]; st["ob"] ^= 1
                evac(ob[oi][:, 0:512], b_ob[oi], bk, 128, scale=scm)
                S.dma("sp", lambda e, oi=oi, h=h, tok0=tok0: e.dma_start(out=QKs[h // 2, sup, 1280 + (h % 2) * 128:1280 + (h % 2) * 128 + 128, tok0 - t0s:tok0 - t0s + 512], in_=ob[oi][:, 0:512]),
                      reads=[b_ob[oi]], writes=[b_mlaq])
                bk1 = nbank(); bk2 = nbank()
                for k in range(4):
                    S.op("pe", lambda e, h=h, k=k, bk1=bk1: e.matmul(ps[0:64, bk1, :], lhsT=wuqb[:, k, h * 192 + 128:h * 192 + 192], rhs=lat[:, k, :],
                                                                     start=(k == 0), stop=(k == 3)), reads=[b_wuqb, b_lat], writes=[b_ps[bk1]])
                for k in range(4):
                    S.op("pe", lambda e, h=h, k=k, bk2=bk2: e.matmul(ps[0:64, bk2, :], lhsT=wuqsb[:, k, h * 64:(h + 1) * 64], rhs=lat[:, k, :],
                                                                     start=(k == 0), stop=(k == 3)), reads=[b_wuqsb, b_lat], writes=[b_ps[bk2]])
                oi = st["ob"]; st["ob"] ^= 1
                rope_out(ps[0:64, bk1, :], b_ps[bk1], ps[0:64, bk2, :], b_ps[bk2], scm, ob[oi][0:64, 0:512], b_ob[oi])
                S.dma("sp", lambda e, oi=oi, h=h, tok0=tok0: e.dma_start(out=QKs[h // 2, sup, 1792 + (h % 2) * 64:1792 + (h % 2) * 64 + 64, tok0 - t0s:tok0 - t0s + 512], in_=ob[oi][0:64, 0:512]),
                      reads=[b_ob[oi]], writes=[b_mlaq])
            latent_norm(ckv, b_ckv, gkvt, b_gkvt)
            for h in range(8):
                bk = nbank()
                for k in range(4):
                    S.op("pe", lambda e: e.matmul(ps[:, bk, :], lhsT=wukvb[:, k, h * 256:h * 256 + 128], rhs=lat[:, k, :],
                                                  start=(k == 0), stop=(k == 3)), reads=[b_wukvb, b_lat], writes=[b_ps[bk]])
                oi = st["ob"]; st["ob"] ^= 1
                evac(ob[oi][:, 0:512], b_ob[oi], bk, 128)
                S.dma("sp", lambda e: e.dma_start(out=QKs[h // 2, sup, 1536 + (h % 2) * 128:1536 + (h % 2) * 128 + 128, tok0 - t0s:tok0 - t0s + 512], in_=ob[oi][:, 0:512]),
                      reads=[b_ob[oi]], writes=[b_QKs])
            wv = wukvb[:, :, :].rearrange("p k (h c) -> p k h c", c=256)
            for s4 in range(4):
                for hg in range(2):
                    bk = nbank()
                    for k in range(4):
                        S.op("pe", lambda e: e.matmul(ps[:, bk, :].rearrange("p (h c) -> p h c", c=128), lhsT=lat[:, k, s4 * 128:(s4 + 1) * 128],
                                                      rhs=wv[:, k, hg * 4:(hg + 1) * 4, 128:256], start=(k == 0), stop=(k == 3)),
                             reads=[b_wukvb, b_lat], writes=[b_ps[bk]])
                    oi = st["ob"]; st["ob"] ^= 1
                    evac(ob[oi][:, 0:512], b_ob[oi], bk, 128)
                    tk = tok0 + s4 * 128
                    S.dma("sp", lambda e: e.dma_start(out=Vs[2 * hg:2 * hg + 2, tk:tk + 128, 640:896].rearrange("j p c -> p j c"),
                                                      in_=ob[oi][:, 0:512].rearrange("p (j c) -> p j c", c=256)),
                          reads=[b_ob[oi]], writes=[b_Vs])
        if after_sup is not None:
            after_sup(sup)
        for gi in range(12):
            gemm_group(win, 8768 + gi * 512, 512, out_f32(gT, b_gT, gi * 512, AF.Sigmoid))


SEQ = 8192
NB = SEQ // 128
RATES = (1, 4, 16)
BIG = 1.0e6


def emit_B(S, nc, d, QKl, Vl, Ysrc, b_QKg, b_Vg, b_Ys, seq=SEQ, do_mla=True, do_sb=True, do_dil=True):
    NBk = seq // 128
    NG4 = seq // 512
    dposk = d['dposk']; dposq = d['dposq']; nslope = d['nslope']
    maskc_d = d['maskc']; masks_d = d['masks']; maskd_d = d['maskd']; m1_d = d['m1']; m2_d = d['m2']
    b_ymla = b_Ys; b_ysb = b_Ys; b_ydil = b_Ys
    SHR = 1984; SHV = 896
    Q1 = S.sbuf("Q1", [128, seq], BF16); bQ1 = Buf("Q1")
    K1 = S.sbuf("K1", [128, seq], BF16); bK1 = Buf("K1")
    if do_mla:
        Q2 = S.sbuf("Q2", [64, seq], BF16); bQ2 = Buf("Q2")
        K2 = S.sbuf("K2", [64, seq], BF16); bK2 = Buf("K2")
    V1 = S.sbuf("V1", [128, NBk, 128], BF16); bV1 = Buf("V1")
    two = do_mla or do_sb
    if two:
        Q1b = S.sbuf("Q1b", [128, seq], BF16); bQ1b = Buf("Q1b")
        K1b = S.sbuf("K1b", [128, seq], BF16); bK1b = Buf("K1b")
        V1b = S.sbuf("V1b", [128, NBk, 128], BF16); bV1b = Buf("V1b")
        if do_mla:
            Q2b = S.sbuf("Q2b", [64, seq], BF16); bQ2b = Buf("Q2b")
    NP = 8
    PT = [S.sbuf("PT%d" % i, [128, 512], BF16) for i in range(NP)]; bPT = [Buf("PT%d" % i) for i in range(NP)]
    SP = [S.sbuf("SP%d" % i, [128, 512], BF16) for i in range(NP)]; bSP = [Buf("SP%d" % i) for i in range(NP)]
    EN = [S.sbuf("EN%d" % i, [128, 512], F32) for i in range(4)]; bEN = [Buf("EN%d" % i) for i in range(4)]
    SNt = [S.sbuf("SN%d" % i, [128, 512], F32) for i in range(NP)]; bSN = [Buf("SN%d" % i) for i in range(NP)]
    UU = [S.sbuf("UU%d" % i, [128, 512], F32) for i in range(4)]; bUU = [Buf("UU%d" % i) for i in range(4)]
    YS = [S.sbuf("YS%d" % i, [128, 512], BF16) for i in range(2)]; bYS = [Buf("YS%d" % i) for i in range(2)]
    RD = S.sbuf("RD", [128, 512], F32); bRD = Buf("RD")
    maskc = S.sbuf("maskc_t", [128, 4, 512], BF16); bmaskc = Buf("maskc")
    masks = S.sbuf("masks_t", [128, 4, 512], BF16); bmasks = Buf("masks")
    maskd = S.sbuf("maskd_t", [128, 256], F32); bmaskd = Buf("maskd")
    M1 = S.sbuf("M1", [128, 128], BF16); bM1 = Buf("M1")
    M2 = S.sbuf("M2", [128, 128], BF16); bM2 = Buf("M2")
    ones = S.sbuf("ones", [128, 128], BF16); bones = Buf("ones")
    nsl = S.sbuf("nsl", [128, 3], F32); bnsl = Buf("nsl")
    ps = S.psum("ps", [128, 8, 512], F32); bps = [Buf("ps%d" % i) for i in range(8)]

    S.op("dve", lambda e: e.memset(ones[:], 1.0), writes=[bones])
    S.dma("sp", lambda e: e.dma_start(out=maskc[:], in_=maskc_d[:, :, :]), writes=[bmaskc])
    S.dma("sp", lambda e: e.dma_start(out=masks[:], in_=masks_d[:, :, :]), writes=[bmasks])
    S.dma("sp", lambda e: e.dma_start(out=maskd[:], in_=maskd_d[:, :]), writes=[bmaskd])
    S.dma("sp", lambda e: e.dma_start(out=M1[:], in_=m1_d[:, :]), writes=[bM1])
    S.dma("sp", lambda e: e.dma_start(out=M2[:], in_=m2_d[:, :]), writes=[bM2])
    S.dma("sp", lambda e: e.dma_start(out=nsl[:], in_=nslope[:, :]), writes=[bnsl])

    QK5 = QKl.rearrange("c r (p a) t -> c r p (a t)", a=1) if False else QKl
    Vl4 = Vl
    Vl6 = Vl.rearrange("c r (b p) d -> c r b p d", p=128)

    def load_fm(dst, bdst, R0, rows=128):
        dv4 = dst[0:rows, :].rearrange("p (r u t) -> p r u t", r=4, u=2)
        for u in range(2):
            S.dma("sp", lambda e: e.dma_start(out=dv4[:, :, u, :],
                                              in_=QK5[u * 4 + R0 // 512, :, R0 % 512:R0 % 512 + rows, :].rearrange("r p t -> p r t")),
                  reads=[b_QKg], writes=[bdst])

    def load_v(c0, Vt=None, bVt=None):
        Vt = V1 if Vt is None else Vt
        bVt = bV1 if bVt is None else bVt
        for r in range(4):
            for cp in range(4):
                S.dma("sp", lambda e: e.dma_start(out=Vt[:, r * 16 + cp * 4:r * 16 + cp * 4 + 4, :],
                                                  in_=Vl6[cp, r, :, :, c0:c0 + 128].rearrange("b p d -> p b d")),
                      reads=[b_Vg], writes=[bVt])

    def load_v_perm(c0, rt):
        if rt == 1:
            load_v(c0)
        elif rt == 4:
            for s_ in range(4):
                for r in range(4):
                    S.dma("sp", lambda e: e.dma_start(out=V1[:, s_ * 16 + 4 * r:s_ * 16 + 4 * r + 4, :],
                                                      in_=Vl4[:, r, s_:s_ + 4 * 127 + 1:4, c0:c0 + 128].rearrange("c p d -> p c d")),
                          reads=[b_Vg], writes=[bV1])
        else:
            for s_ in range(16):
                for cp in range(4):
                    S.dma("sp", lambda e: e.dma_start(out=V1[32 * cp:32 * cp + 32, s_ * 4:s_ * 4 + 4, :],
                                                      in_=Vl4[cp, :, s_:s_ + 16 * 31 + 1:16, c0:c0 + 128].rearrange("m p d -> p m d")),
                          reads=[b_Vg], writes=[bV1])

    Ys5 = Ysrc

    def yout(blk, g4):
        return Ys5[g4 // 8, blk, :, (g4 % 8) * 512:(g4 % 8) * 512 + 512]

    def pipeline(steps):
        prev = None
        for s1, s2 in steps:
            s1()
            if prev is not None:
                prev()
            prev = s2
        if prev is not None:
            prev()

    cnt = {"z": 0, "o": 0, "pt": 0, "ys": 0, "en": 0, "uu": 0}

    def interleave(a, b):
        out = []
        for x, y in zip(a, b):
            out.append(x); out.append(y)
        return out

    if do_mla:
        load_fm(K2, bK2, 1920, 64)
        hs = [dict(Q=Q1, bQ=bQ1, Qr=Q2, bQr=bQ2, K=K1, bK=bK1, V=V1, bV=bV1, zb=(0, 1), ob=2, db=3),
              dict(Q=Q1b, bQ=bQ1b, Qr=Q2b, bQr=bQ2b, K=K1b, bK=bK1b, V=V1b, bV=bV1b, zb=(6, 7), ob=4, db=5)]
        allsteps = []
        for h in range(2):
            H = hs[h]
            load_fm(H["Q"], H["bQ"], 1280 + h * 128)
            load_fm(H["Qr"], H["bQr"], 1792 + h * 64, 64)
            load_fm(H["K"], H["bK"], 1536 + h * 128)
            load_v(640 + h * 128, H["V"], H["bV"])
            steps = []
            zc = 0
            for g4 in range(NG4):
                qs = slice(g4 * 512, (g4 + 1) * 512)
                ob = H["ob"]; db = H["db"]
                nst = 4 * g4 + 4
                for j in range(nst):
                    zb = H["zb"][zc % 2]; zc += 1
                    ks = slice(j * 128, (j + 1) * 128)
                    box = {}

                    def s1(qs=qs, j=j, zb=zb, ks=ks, g4=g4, H=H, box=box):
                        pi = cnt["pt"] % NP; cnt["pt"] += 1
                        box["pi"] = pi
                        S.op("pe", lambda e: e.matmul(ps[:, zb, :], lhsT=H["K"][:, ks], rhs=H["Q"][:, qs], start=True, stop=False),
                             reads=[H["bK"], H["bQ"]], writes=[bps[zb]])
                        S.op("pe", lambda e: e.matmul(ps[:, zb, :], lhsT=K2[0:64, ks], rhs=H["Qr"][0:64, qs], start=False, stop=True),
                             reads=[bK2, H["bQr"]], writes=[bps[zb]])
                        S.op("act", lambda e: e.activation(out=PT[pi][:], in_=ps[:, zb, :], func=AF.Exp),
                             reads=[bps[zb]], writes=[bPT[pi]])
                        if j >= 4 * g4:
                            S.op("dve", lambda e: e.tensor_tensor(out=PT[pi][:], in0=PT[pi][:], in1=maskc[:, j - 4 * g4, :], op=ALU.mult),
                                 reads=[bPT[pi], bmaskc], writes=[bPT[pi]])

                    def s2(j=j, ob=ob, db=db, nst=nst, h=h, g4=g4, H=H, box=box):
                        pi = box["pi"]
                        S.op("pe", lambda e: e.matmul(ps[:, ob, :], lhsT=H["V"][:, j, :], rhs=PT[pi][:], start=(j == 0), stop=(j == nst - 1)),
                             reads=[H["bV"], bPT[pi]], writes=[bps[ob]])
                        S.op("pe", lambda e: e.matmul(ps[:, db, :], lhsT=ones[:], rhs=PT[pi][:], start=(j == 0), stop=(j == nst - 1)),
                             reads=[bones, bPT[pi]], writes=[bps[db]])
                        if j == nst - 1:
                            yi = cnt["ys"] % 2; cnt["ys"] += 1
                            ri = cnt["uu"] % 4; cnt["uu"] += 1
                            S.op("dve", lambda e: e.reciprocal(out=UU[ri][:], in_=ps[:, db, :]), reads=[bps[db]], writes=[bUU[ri]])
                            S.op("dve", lambda e: e.tensor_tensor(out=YS[yi][:], in0=ps[:, ob, :], in1=UU[ri][:], op=ALU.mult),
                                 reads=[bps[ob], bUU[ri]], writes=[bYS[yi]])
                            S.dma("sp", lambda e: e.dma_start(out=yout(h, g4), in_=YS[yi][:]), reads=[bYS[yi]], writes=[b_ymla])
                    steps.append((s1, s2))
            allsteps.append(steps)
        pipeline(interleave(allsteps[0], allsteps[1]))

    if do_sb:
        hs = [dict(Q=Q1, bQ=bQ1, K=K1, bK=bK1, V=V1, bV=bV1, zb=(0, 1), ob=2, rb=3),
              dict(Q=Q1b, bQ=bQ1b, K=K1b, bK=bK1b, V=V1b, bV=bV1b, zb=(6, 7), ob=4, rb=5)]
        allsteps = []
        for h in range(2):
            H = hs[h]
            load_fm(H["Q"], H["bQ"], 768 + h * 128)
            load_fm(H["K"], H["bK"], 1024 + h * 128)
            load_v(384 + h * 128, H["V"], H["bV"])
            steps = []
            zc = 0
            for g4 in range(NG4):
                qs = slice(g4 * 512, (g4 + 1) * 512)
                ob = H["ob"]; rb = H["rb"]
                nst = 4 * g4 + 4
                for idx, j in enumerate(reversed(range(nst))):
                    first = idx == 0; last = idx == nst - 1
                    zb = H["zb"][zc % 2]; zc += 1
                    ks = slice(j * 128, (j + 1) * 128)
                    box = {}

                    def t1(qs=qs, j=j, zb=zb, ks=ks, g4=g4, H=H, box=box):
                        pi = cnt["pt"] % NP; cnt["pt"] += 1
                        ei = cnt["en"] % 4; cnt["en"] += 1
                        box["pi"] = pi
                        S.op("pe", lambda e: e.matmul(ps[:, zb, :], lhsT=H["K"][:, ks], rhs=H["Q"][:, qs], start=True, stop=True),
                             reads=[H["bK"], H["bQ"]], writes=[bps[zb]])
                        S.op("act", lambda e: e.activation(out=EN[ei][:], in_=ps[:, zb, :], func=AF.Exp, scale=-1.0),
                             reads=[bps[zb]], writes=[bEN[ei]])
                        S.op("act", lambda e: e.activation(out=SNt[pi][:], in_=EN[ei][:], func=AF.Ln, bias=1.0),
                             reads=[bEN[ei]], writes=[bSN[pi]])
                        S.op("dve", lambda e: e.tensor_tensor(out=SP[pi][:], in0=ps[:, zb, :], in1=SNt[pi][:], op=ALU.add),
                             reads=[bps[zb], bSN[pi]], writes=[bSP[pi]])
                        if j >= 4 * g4:
                            S.op("dve", lambda e: e.tensor_tensor(out=SP[pi][:], in0=SP[pi][:], in1=masks[:, j - 4 * g4, :], op=ALU.mult),
                                 reads=[bSP[pi], bmasks], writes=[bSP[pi]])

                    def t2(j=j, rb=rb, first=first, g4=g4, box=box):
                        pi = box["pi"]
                        ui = cnt["uu"] % 4; cnt["uu"] += 1
                        S.op("pe", lambda e: e.matmul(ps[:, rb, :], lhsT=M1[:], rhs=SP[pi][:], start=first, stop=False, skip_group_check=True),
                             reads=[bM1, bSP[pi]], writes=[bps[rb]])
                        S.op("dve", lambda e: e.tensor_tensor(out=UU[ui][:], in0=SNt[pi][:], in1=ps[:, rb, :], op=ALU.add),
                             reads=[bSN[pi], bps[rb]], writes=[bUU[ui]])
                        S.op("act", lambda e: e.activation(out=PT[pi][:], in_=UU[ui][:], func=AF.Exp, scale=-1.0),
                             reads=[bUU[ui]], writes=[bPT[pi]])
                        if j >= 4 * g4:
                            S.op("dve", lambda e: e.tensor_tensor(out=PT[pi][:], in0=PT[pi][:], in1=masks[:, j - 4 * g4, :], op=ALU.mult),
                                 reads=[bPT[pi], bmasks], writes=[bPT[pi]])

                    def t3(rb=rb, last=last, box=box):
                        pi = box["pi"]
                        S.op("pe", lambda e: e.matmul(ps[:, rb, :], lhsT=M2[:], rhs=SP[pi][:], start=False, stop=last, skip_group_check=True),
                             reads=[bM2, bSP[pi]], writes=[bps[rb]])

                    def t4(j=j, ob=ob, first=first, last=last, h=h, g4=g4, H=H, box=box):
                        pi = box["pi"]
                        S.op("pe", lambda e: e.matmul(ps[:, ob, :], lhsT=H["V"][:, j, :], rhs=PT[pi][:], start=first, stop=last),
                             reads=[H["bV"], bPT[pi]], writes=[bps[ob]])
                        if last:
                            yi = cnt["ys"] % 2; cnt["ys"] += 1
                            S.op("act", lambda e: e.activation(out=YS[yi][:], in_=ps[:, ob, :], func=AF.Copy),
                                 reads=[bps[ob]], writes=[bYS[yi]])
                            S.dma("sp", lambda e: e.dma_start(out=yout(3 + h, g4), in_=YS[yi][:]), reads=[bYS[yi]], writes=[b_ysb])
                    steps.append((t1, t2, t3, t4))
            allsteps.append(steps)
        N_ = len(allsteps[0])
        for X in allsteps:
            X[0][0]()
        for k in range(N_):
            for X in allsteps:
                X[k][1]()
            if k > 0:
                for X in allsteps:
                    X[k - 1][3]()
            if k + 1 < N_:
                for X in allsteps:
                    X[k + 1][0]()
            for X in allsteps:
                X[k][2]()
        for X in allsteps:
            X[N_ - 1][3]()

    if do_dil:
        accn = S.sbuf("accn", [128, seq], F32); baccn = Buf("accn")
        accd = S.sbuf("accd", [128, seq], F32); baccd = Buf("accd")
        posk_i = S.sbuf("posk_i", [128, NBk], I32); bposk_i = Buf("posk_i")
        posk = S.sbuf("posk", [128, NBk], F32); bposk = Buf("posk")
        pq_i = [S.sbuf("pq_i%d" % i, [128, 128], I32) for i in range(2)]; bpq_i = [Buf("pq_i0"), Buf("pq_i1")]
        pq = [S.sbuf("pq%d" % i, [128, 128], F32) for i in range(2)]; bpq = [Buf("pq0"), Buf("pq1")]
        dd = [S.sbuf("dd%d" % i, [128, 256], F32) for i in range(2)]; bdd = [Buf("dd0"), Buf("dd1")]
        zt = [S.sbuf("zt%d" % i, [128, 256], F32) for i in range(2)]; bzt = [Buf("zt0"), Buf("zt1")]
        for g in range(3):
            r = RATES[g]
            L = seq // r
            nb = L // 128
            load_fm(Q1, bQ1, g * 128)
            load_fm(K1, bK1, 384 + g * 128)
            load_v_perm(g * 128, r)
            S.dma("sp", lambda e: e.dma_start(out=posk_i[:], in_=dposk[g, :, :]), writes=[bposk_i])
            S.op("dve", lambda e: e.tensor_copy(out=posk[:], in_=posk_i[:]), reads=[bposk_i], writes=[bposk])
            S.op("dve", lambda e: e.tensor_scalar(out=posk[:], in0=posk[:], scalar1=-1.0, scalar2=None, op0=ALU.mult), reads=[bposk], writes=[bposk])
            dsteps = []
            for i in range(NBk):
                s, m = divmod(i, nb)
                hp = m > 0
                c0 = 0 if hp else 128
                zb = cnt["z"] % 2; cnt["z"] += 1
                ob = 2 + (cnt["o"] % 2); db = 4 + (cnt["o"] % 2); cnt["o"] += 1
                pi = cnt["pt"] % NP; cnt["pt"] += 1
                bi = i % 2
                st0 = s + 128 * r * m
                qsl = slice(st0, st0 + 127 * r + 1, r) if r > 1 else slice(st0, st0 + 128)
                psl = (slice(st0 - 128 * r, st0 - 128 * r + 127 * r + 1, r) if r > 1 else slice(st0 - 128, st0)) if hp else None
                def s1(i=i, hp=hp, c0=c0, zb=zb, pi=pi, bi=bi, qsl=qsl, psl=psl, g=g):
                    if hp:
                        S.op("pe", lambda e: e.matmul(ps[:, zb, 0:128], lhsT=K1[:, psl], rhs=Q1[:, qsl], start=True, stop=True),
                             reads=[bK1, bQ1], writes=[bps[zb]])
                    S.op("pe", lambda e: e.matmul(ps[:, zb, 128:256], lhsT=K1[:, qsl], rhs=Q1[:, qsl], start=True, stop=True),
                         reads=[bK1, bQ1], writes=[bps[zb]])
                    S.dma("sp", lambda e: e.dma_start(out=pq_i[bi][:], in_=dposq[g:g + 1, i * 128:(i + 1) * 128].partition_broadcast(128)),
                          writes=[bpq_i[bi]])
                    S.op("dve", lambda e: e.tensor_copy(out=pq[bi][:], in_=pq_i[bi][:]), reads=[bpq_i[bi]], writes=[bpq[bi]])
                    if hp:
                        S.op("act", lambda e: e.activation(out=dd[bi][:, 0:128], in_=pq[bi][:], func=AF.Abs, bias=posk[:, i - 1:i], scale=1.0),
                             reads=[bpq[bi], bposk], writes=[bdd[bi]])
                    S.op("act", lambda e: e.activation(out=dd[bi][:, 128:256], in_=pq[bi][:], func=AF.Abs, bias=posk[:, i:i + 1], scale=1.0),
                         reads=[bpq[bi], bposk], writes=[bdd[bi]])
                    S.op("dve", lambda e: e.tensor_tensor(out=dd[bi][:, c0:256], in0=dd[bi][:, c0:256], in1=maskd[:, c0:256], op=ALU.add),
                         reads=[bdd[bi], bmaskd], writes=[bdd[bi]])
                    S.op("dve", lambda e: e.scalar_tensor_tensor(out=zt[bi][:, c0:256], in0=dd[bi][:, c0:256], scalar=nsl[:, g:g + 1],
                                                                 in1=ps[:, zb, c0:256], op0=ALU.mult, op1=ALU.add),
                         reads=[bdd[bi], bnsl, bps[zb]], writes=[bzt[bi]])
                    S.op("act", lambda e: e.activation(out=PT[pi][:, c0:256], in_=zt[bi][:, c0:256], func=AF.Exp),
                         reads=[bzt[bi]], writes=[bPT[pi]])

                def s2(i=i, hp=hp, ob=ob, db=db, pi=pi, s=s, m=m, r=r, g=g, st0=st0):
                    if hp:
                        S.op("pe", lambda e: e.matmul(ps[:, ob, 0:128], lhsT=V1[:, i - 1, :], rhs=PT[pi][:, 0:128], start=True, stop=False),
                             reads=[bV1, bPT[pi]], writes=[bps[ob]])
                    S.op("pe", lambda e: e.matmul(ps[:, ob, 0:128], lhsT=V1[:, i, :], rhs=PT[pi][:, 128:256], start=(not hp), stop=True),
                         reads=[bV1, bPT[pi]], writes=[bps[ob]])
                    if hp:
                        S.op("pe", lambda e: e.matmul(ps[:, db, 0:128], lhsT=ones[:], rhs=PT[pi][:, 0:128], start=True, stop=False),
                             reads=[bones, bPT[pi]], writes=[bps[db]])
                    S.op("pe", lambda e: e.matmul(ps[:, db, 0:128], lhsT=ones[:], rhs=PT[pi][:, 128:256], start=(not hp), stop=True),
                         reads=[bones, bPT[pi]], writes=[bps[db]])
                    an = accn[:, st0:st0 + 127 * r + 1:r] if r > 1 else accn[:, st0:st0 + 128]
                    ad = accd[:, st0:st0 + 127 * r + 1:r] if r > 1 else accd[:, st0:st0 + 128]
                    if g == 0:
                        S.op("act", lambda e: e.activation(out=an, in_=ps[:, ob, 0:128], func=AF.Copy), reads=[bps[ob]], writes=[baccn])
                        S.op("dve", lambda e: e.tensor_copy(out=ad, in_=ps[:, db, 0:128]), reads=[bps[db]], writes=[baccd])
                    else:
                        S.op("dve", lambda e: e.tensor_tensor(out=an, in0=an, in1=ps[:, ob, 0:128], op=ALU.add), reads=[baccn, bps[ob]], writes=[baccn])
                        S.op("dve", lambda e: e.tensor_tensor(out=ad, in0=ad, in1=ps[:, db, 0:128], op=ALU.add), reads=[baccd, bps[db]], writes=[baccd])
                dsteps.append((s1, s2))
            pipeline(dsteps)
        for c in range(seq // 512):
            cs = slice(c * 512, (c + 1) * 512)
            yi = cnt["ys"] % 2; cnt["ys"] += 1
            S.op("dve", lambda e: e.reciprocal(out=RD[:], in_=accd[:, cs]), reads=[baccd], writes=[bRD])
            S.op("dve", lambda e: e.tensor_tensor(out=YS[yi][:], in0=accn[:, cs], in1=RD[:], op=ALU.mult), reads=[baccn, bRD], writes=[bYS[yi]])
            S.dma("sp", lambda e: e.dma_start(out=yout(2, c), in_=YS[yi][:]), reads=[bYS[yi]], writes=[b_ydil])


def consts_B():
    import ml_dtypes
    k = np.arange(128)[:, None]
    q = np.arange(512)[None, :]
    maskc = np.stack([(128 * i0 + k <= q) for i0 in range(4)], axis=1).astype(np.float32)
    masks = np.stack([(128 * i0 + k < q) for i0 in range(4)], axis=1).astype(np.float32)
    kl = np.arange(128)[:, None]; ql = np.arange(128)[None, :]
    maskd = np.concatenate([np.where(kl >= ql, 0.0, BIG), np.where(kl <= ql, 0.0, BIG)], axis=1).astype(np.float32)
    p = np.arange(128)[:, None]; m = np.arange(128)[None, :]
    m1 = (p > m).astype(np.float32); m2 = (p <= m).astype(np.float32)
    bf = ml_dtypes.bfloat16
    return dict(maskc=maskc.astype(bf), masks=masks.astype(bf), maskd=maskd, m1=m1.astype(bf), m2=m2.astype(bf))


D = 2048
NK = 16
EPS = 1e-6
NE = 16
BIGR = 1.0e4


def emit_C(S, nc, d, l, Yl, gT, xT, x2T, xfT, b_Yg, b_gT, b_xsrc, b_x2T, b_xfT, ntok=2048):
    NT = ntok // 512
    cT = d['cT']; wada = d['wada'][l]; bada = d['bada_c'][l]; gmoe = d['gmoe'][l]; gfin = d['gfin']
    womla = d['womla'][l]; wodil = d['wodil'][l]; wosb = d['wosb'][l]; wout = d['wout'][l]
    wr = d['wr']; br = d['br']; wg = d['wg'][l]; wu = d['wu'][l]; wd = d['wd'][l]; ident = d['ident']
    DBG = False
    xt = S.sbuf("xt", [128, NK, 512], F32); b_xt = [Buf("xt%d" % k) for k in range(NK)]
    scr = S.sbuf("scr", [128, 20, 512], BF16); b_scr = Buf("scr")
    mg = S.sbuf("mg", [128, NK, 512], BF16); b_mg = Buf("mg")
    a2b = S.sbuf("a2b", [128, NK, 512], BF16); b_a2b = Buf("a2b")
    NW = 5
    W = [S.sbuf("W%d" % i, [128, NK, 512], BF16) for i in range(NW)]; b_W = [Buf("W%d" % i) for i in range(NW)]
    gt = [S.sbuf("gt%d" % i, [128, 3, 512], F32) for i in range(2)]; b_gt = [Buf("gt0"), Buf("gt1")]
    tmp = [S.sbuf("tmp%d" % i, [128, 512], F32) for i in range(4)]; b_tmp = [Buf("tmp%d" % i) for i in range(4)]
    hp = [S.sbuf("hp%d" % i, [128, 4, 512], BF16) for i in range(2)]; b_hp = [Buf("hp0"), Buf("hp1")]
    rstd = S.sbuf("rstd", [128, 512], F32); b_rstd = Buf("rstd")
    lnt = S.sbuf("lnt", [128, 512], F32); b_lnt = Buf("lnt")
    ones = S.sbuf("ones", [128, 128], BF16); b_ones = Buf("ones")
    onesf = S.sbuf("onesf", [128, 128], F32); b_onesf = Buf("onesf")
    idt = S.sbuf("idt", [128, 128], F32); b_idt = Buf("idt")
    Gm = [S.sbuf("Gm%d" % i, [128, 128], F32) for i in range(2)]; b_Gm = [Buf("Gm0"), Buf("Gm1")]
    cin = S.sbuf("cin", [128, NK], F32); b_cin = Buf("cin")
    cact = S.sbuf("cact", [128, NK], BF16); b_cact = Buf("cact")
    badat = S.sbuf("badat", [128, 64], F32); b_badat = Buf("badat")
    gmoet = S.sbuf("gmoet", [128, NK], F32); b_gmoet = Buf("gmoet")
    gfint = S.sbuf("gfint", [128, NK], F32); b_gfint = Buf("gfint")
    modt = S.sbuf("modt", [128, 64], F32); b_modt = Buf("modt")
    s1m = S.sbuf("s1m", [128, NK], F32); b_s1m = Buf("s1m")
    wrt = S.sbuf("wrt", [128, NK, NE], F32); b_wrt = Buf("wrt")
    brt = S.sbuf("brt", [128, NE], F32); b_brt = Buf("brt")
    lt = S.sbuf("lt", [16, 512], F32); b_lt = Buf("lt")
    sc = S.sbuf("sc", [128, 64], F32); b_sc = Buf("sc")
    bi = S.sbuf("bi", [128, 64], F32); b_bi = Buf("bi")
    bi2 = S.sbuf("bi2", [128, 64], F32); b_bi2 = Buf("bi2")
    eq = S.sbuf("eq", [128, 64], F32); b_eq = Buf("eq")
    m1 = S.sbuf("m1", [128, 16], F32); b_m1 = Buf("m1")
    m2 = S.sbuf("m2", [128, 16], F32); b_m2 = Buf("m2")
    gs = S.sbuf("gs", [128, 16], F32); b_gs = Buf("gs")
    gmax = S.sbuf("gmax", [128, 4], F32); b_gmax = Buf("gmax")
    gsel = S.sbuf("gsel", [128, 16], F32); b_gsel = Buf("gsel")
    wsel = S.sbuf("wsel", [128, 64], F32); b_wsel = Buf("wsel")
    den = S.sbuf("den", [128, 4], F32); b_den = Buf("den")
    gates = S.sbuf("gates", [128, 64], F32); b_gates = Buf("gates")
    ps = S.psum("ps", [128, 8, 512], F32); b_ps = [Buf("ps%d" % i) for i in range(8)]
    st = {"bank": 0, "w": 0, "gt": 0, "tmp": 0, "hp": 0, "gm": 0}

    def nbank():
        i = st["bank"]; st["bank"] = (i + 1) % 6
        return i

    def nw():
        i = st["w"]; st["w"] = (i + 1) % NW
        return i

    def ntmp():
        i = st["tmp"]; st["tmp"] = (i + 1) % 4
        return i

    def load_w(wi, src2d, nk, c0, ncols, k_off=0):
        srcv = src2d.rearrange("(k p) c -> p k c", p=128)
        h = max(1, nk // 2)
        for k0 in range(0, nk, h):
            S.dma("pool", lambda e: e.dma_start(out=W[wi][:, k_off + k0:k_off + k0 + h, 0:ncols],
                                                in_=srcv[:, k0:k0 + h, c0:c0 + ncols]), writes=[b_W[wi]])

    S.op("dve", lambda e: e.memset(ones[:], 1.0), writes=[b_ones])
    S.op("dve", lambda e: e.memset(onesf[:], 1.0), writes=[b_onesf])
    for dst, bd, src in ((cin, b_cin, cT), (badat, b_badat, bada), (gmoet, b_gmoet, gmoe), (gfint, b_gfint, gfin),
                         (brt, b_brt, br), (idt, b_idt, ident)):
        S.dma("sp", lambda e: e.dma_start(out=dst[:], in_=src[:, :]), writes=[bd])
    S.dma("sp", lambda e: e.dma_start(out=wrt[:], in_=wr.rearrange("(k p) e -> p k e", p=128)), writes=[b_wrt])
    S.op("act", lambda e: e.activation(out=cact[:], in_=cin[:], func=AF.Silu), reads=[b_cin], writes=[b_cact])

    mb = nbank()
    for g in range(16):
        wi = nw()
        load_w(wi, wada, NK, 4096 + g * 512, 512)
        for m in range(4):
            n = g * 4 + m
            for k in range(NK):
                S.op("pe", lambda e: e.matmul(ps[:, mb, n:n + 1], lhsT=W[wi][:, k, m * 128:(m + 1) * 128], rhs=cact[:, k:k + 1],
                                              start=(k == 0), stop=(k == NK - 1)), reads=[b_W[wi], b_cact], writes=[b_ps[mb]])
    S.op("dve", lambda e: e.tensor_tensor(out=modt[:], in0=ps[:, mb, 0:64], in1=badat[:], op=ALU.add),
         reads=[b_ps[mb], b_badat], writes=[b_modt])
    S.op("dve", lambda e: e.scalar_tensor_tensor(out=s1m[:], in0=modt[:, 32:48], scalar=1.0, in1=gmoet[:], op0=ALU.add, op1=ALU.mult),
         reads=[b_modt, b_gmoet], writes=[b_s1m])

    def rms_rstd(src3, bsrcs):
        S.op("act", lambda e: e.activation(out=scr[:, 0:NK, :], in_=src3, func=AF.Square), reads=bsrcs, writes=[b_scr])
        bk = nbank()
        for k in range(NK):
            S.op("pe", lambda e: e.matmul(ps[:, bk, :], lhsT=ones[:], rhs=scr[:, k, :], start=(k == 0), stop=(k == NK - 1)),
                 reads=[b_scr, b_ones], writes=[b_ps[bk]])
        S.op("act", lambda e: e.activation(out=lnt[:], in_=ps[:, bk, :], func=AF.Ln, scale=1.0 / D, bias=EPS),
             reads=[b_ps[bk]], writes=[b_lnt])
        S.op("act", lambda e: e.activation(out=rstd[:], in_=lnt[:], func=AF.Exp, scale=-0.5), reads=[b_lnt], writes=[b_rstd])

    xTv = xT.rearrange("(k p) t -> p k t", p=128)
    gTv = gT.rearrange("(b n p) t -> p b n t", b=3, p=128)
    x2v = x2T.rearrange("(k p) t -> p k t", p=128) if x2T is not None else None
    xfv = xfT.rearrange("(k p) t -> p k t", p=128) if xfT is not None else None

    for t in range(NT):
        ts = slice(t * 512, (t + 1) * 512)
        for k0 in range(0, NK, 4):
            S.dma("sp", lambda e: e.dma_start(out=xt[:, k0:k0 + 4, :], in_=xTv[:, k0:k0 + 4, ts]), reads=[b_xsrc], writes=b_xt[k0:k0 + 4])
        for i_ in range(2):
            S.dma("sp", lambda e: e.dma_start(out=scr[:, i_:8:2, :], in_=Yl[i_, :, :, ts].rearrange("j p t -> p j t")), reads=[b_Yg], writes=[b_scr])
            S.dma("sp", lambda e: e.dma_start(out=scr[:, 12 + i_:20:2, :], in_=Yl[3 + i_, :, :, ts].rearrange("j p t -> p j t")), reads=[b_Yg], writes=[b_scr])
        S.dma("sp", lambda e: e.dma_start(out=scr[:, 8:12, :], in_=Yl[2, :, :, ts].rearrange("j p t -> p j t")), reads=[b_Yg], writes=[b_scr])
        for ng in range(4):
            wa = nw(); load_w(wa, womla, 8, ng * 512, 512); load_w(wa, wodil, 4, ng * 512, 512, k_off=8)
            wb_ = nw(); load_w(wb_, wosb, 8, ng * 512, 512)
            for m in range(4):
                n = ng * 4 + m
                gi = st["gt"]; st["gt"] ^= 1
                S.dma("sp", lambda e: e.dma_start(out=gt[gi][:], in_=gTv[:, :, n, ts]), reads=[b_gT], writes=[b_gt[gi]])
                banks = []
                for brn, (wi, koff, nk, yoff) in enumerate(((wa, 0, 8, 0), (wa, 8, 4, 8), (wb_, 0, 8, 12))):
                    bk = nbank(); banks.append(bk)
                    for k in range(nk):
                        S.op("pe", lambda e: e.matmul(ps[:, bk, :], lhsT=W[wi][:, koff + k, m * 128:(m + 1) * 128], rhs=scr[:, yoff + k, :],
                                                      start=(k == 0), stop=(k == nk - 1)), reads=[b_W[wi], b_scr], writes=[b_ps[bk]])
                ta = ntmp(); tb = ntmp()
                S.op("dve", lambda e: e.tensor_tensor(out=tmp[ta][:], in0=ps[:, banks[0], :], in1=gt[gi][:, 0, :], op=ALU.mult),
                     reads=[b_ps[banks[0]], b_gt[gi]], writes=[b_tmp[ta]])
                S.op("dve", lambda e: e.tensor_tensor(out=tmp[tb][:], in0=ps[:, banks[1], :], in1=gt[gi][:, 1, :], op=ALU.mult),
                     reads=[b_ps[banks[1]], b_gt[gi]], writes=[b_tmp[tb]])
                S.op("pool", lambda e: e.tensor_tensor(out=tmp[ta][:], in0=tmp[ta][:], in1=tmp[tb][:], op=ALU.add),
                     reads=[b_tmp[ta], b_tmp[tb]], writes=[b_tmp[ta]])
                S.op("dve", lambda e: e.tensor_tensor(out=tmp[tb][:], in0=ps[:, banks[2], :], in1=gt[gi][:, 2, :], op=ALU.mult),
                     reads=[b_ps[banks[2]], b_gt[gi]], writes=[b_tmp[tb]])
                S.op("pool", lambda e: e.tensor_tensor(out=mg[:, n, :], in0=tmp[ta][:], in1=tmp[tb][:], op=ALU.add),
                     reads=[b_tmp[ta], b_tmp[tb]], writes=[b_mg])
        for ng in range(4):
            wi = nw(); load_w(wi, wout, NK, ng * 512, 512)
            for m in range(4):
                n = ng * 4 + m
                bk = nbank()
                for k in range(NK):
                    S.op("pe", lambda e: e.matmul(ps[:, bk, :], lhsT=W[wi][:, k, m * 128:(m + 1) * 128], rhs=mg[:, k, :],
                                                  start=(k == 0), stop=(k == NK - 1)), reads=[b_W[wi], b_mg], writes=[b_ps[bk]])
                S.op("dve", lambda e: e.scalar_tensor_tensor(out=xt[:, n, :], in0=ps[:, bk, :], scalar=modt[:, n:n + 1], in1=xt[:, n, :],
                                                             op0=ALU.mult, op1=ALU.add), reads=[b_ps[bk], b_modt, b_xt[n]], writes=[b_xt[n]])
        if DBG:
            x1v = x1T.rearrange("(k p) t -> p k t", p=128)
            for k0 in range(0, NK, 4):
                S.dma("sp", lambda e: e.dma_start(out=x1v[:, k0:k0 + 4, ts], in_=xt[:, k0:k0 + 4, :]), reads=b_xt[k0:k0 + 4], writes=[b_x1T])
        rms_rstd(xt[:, :, :], b_xt)
        lb = nbank()
        for k in range(NK):
            ta = ntmp(); tb = ntmp()
            S.op("dve", lambda e: e.tensor_tensor(out=tmp[ta][:], in0=xt[:, k, :], in1=rstd[:], op=ALU.mult),
                 reads=[b_xt[k], b_rstd], writes=[b_tmp[ta]])
            S.op("act", lambda e: e.activation(out=tmp[tb][:], in_=tmp[ta][:], func=AF.Identity, scale=s1m[:, k:k + 1], bias=modt[:, 16 + k:17 + k]),
                 reads=[b_tmp[ta], b_s1m, b_modt], writes=[b_tmp[tb]])
            S.op("act", lambda e: e.activation(out=a2b[:, k, :], in_=tmp[ta][:], func=AF.Identity, scale=s1m[:, k:k + 1], bias=modt[:, 16 + k:17 + k]),
                 reads=[b_tmp[ta], b_s1m, b_modt], writes=[b_a2b])
            S.op("pe", lambda e: e.matmul(ps[0:16, lb, :], lhsT=wrt[:, k, :], rhs=tmp[tb][:], start=(k == 0), stop=(k == NK - 1)),
                 reads=[b_wrt, b_tmp[tb]], writes=[b_ps[lb]])
        S.op("act", lambda e: e.activation(out=lt[:], in_=ps[0:16, lb, :], func=AF.Copy), reads=[b_ps[lb]], writes=[b_lt])
        tb_ = nbank()
        for sub in range(4):
            S.op("pe", lambda e: e.matmul(ps[:, tb_, sub * 16:(sub + 1) * 16], lhsT=lt[0:16, sub * 128:(sub + 1) * 128], rhs=idt[0:16, 0:16],
                                          start=True, stop=True), reads=[b_lt, b_idt], writes=[b_ps[tb_]])
        S.op("act", lambda e: e.activation(out=sc[:], in_=ps[:, tb_, 0:64], func=AF.Sigmoid), reads=[b_ps[tb_]], writes=[b_sc])
        for sub in range(4):
            S.op("dve", lambda e: e.tensor_tensor(out=bi[:, sub * 16:(sub + 1) * 16], in0=sc[:, sub * 16:(sub + 1) * 16], in1=brt[:], op=ALU.add),
                 reads=[b_sc, b_brt], writes=[b_bi])
        v3 = lambda tl: tl[:, :].rearrange("p (g e) -> p g e", e=4)
        S.op("dve", lambda e: e.tensor_reduce(out=m1[:], in_=v3(bi), axis=AX.X, op=ALU.max), reads=[b_bi], writes=[b_m1])
        for ee in range(4):
            S.op("dve", lambda e: e.tensor_tensor(out=v3(eq)[:, :, ee], in0=v3(bi)[:, :, ee], in1=m1[:], op=ALU.is_equal),
                 reads=[b_bi, b_m1], writes=[b_eq])
        S.op("dve", lambda e: e.scalar_tensor_tensor(out=bi2[:], in0=eq[:], scalar=-BIGR, in1=bi[:], op0=ALU.mult, op1=ALU.add),
             reads=[b_eq, b_bi], writes=[b_bi2])
        S.op("dve", lambda e: e.tensor_reduce(out=m2[:], in_=v3(bi2), axis=AX.X, op=ALU.max), reads=[b_bi2], writes=[b_m2])
        S.op("dve", lambda e: e.tensor_tensor(out=gs[:], in0=m1[:], in1=m2[:], op=ALU.add), reads=[b_m1, b_m2], writes=[b_gs])
        S.op("dve", lambda e: e.tensor_reduce(out=gmax[:], in_=gs[:, :].rearrange("p (s g) -> p s g", g=4), axis=AX.X, op=ALU.max),
             reads=[b_gs], writes=[b_gmax])
        for g in range(4):
            S.op("dve", lambda e: e.tensor_tensor(out=gsel[:, :].rearrange("p (s g) -> p s g", g=4)[:, :, g],
                                                  in0=gs[:, :].rearrange("p (s g) -> p s g", g=4)[:, :, g], in1=gmax[:], op=ALU.is_equal),
                 reads=[b_gs, b_gmax], writes=[b_gsel])
        for ee in range(4):
            S.op("dve", lambda e: e.tensor_tensor(out=v3(eq)[:, :, ee], in0=v3(bi)[:, :, ee], in1=m2[:], op=ALU.is_ge),
                 reads=[b_bi, b_m2], writes=[b_eq])
            S.op("dve", lambda e: e.tensor_tensor(out=v3(eq)[:, :, ee], in0=v3(eq)[:, :, ee], in1=gsel[:], op=ALU.mult),
                 reads=[b_eq, b_gsel], writes=[b_eq])
        S.op("dve", lambda e: e.tensor_tensor(out=wsel[:], in0=sc[:], in1=eq[:], op=ALU.mult), reads=[b_sc, b_eq], writes=[b_wsel])
        S.op("dve", lambda e: e.tensor_reduce(out=den[:], in_=wsel[:, :].rearrange("p (s e) -> p s e", e=16), axis=AX.X, op=ALU.add),
             reads=[b_wsel], writes=[b_den])
        S.op("dve", lambda e: e.reciprocal(out=den[:], in_=den[:]), reads=[b_den], writes=[b_den])
        for sub in range(4):
            S.op("dve", lambda e: e.tensor_scalar(out=gates[:, sub * 16:(sub + 1) * 16], in0=wsel[:, sub * 16:(sub + 1) * 16],
                                                  scalar1=den[:, sub:sub + 1], scalar2=None, op0=ALU.mult), reads=[b_wsel, b_den], writes=[b_gates])
        if DBG and t == 0:
            for i_, (tl, bb, w_) in enumerate(((sc, b_sc, 64), (bi, b_bi, 64), (m1, b_m1, 16), (m2, b_m2, 16), (gsel, b_gsel, 16), (eq, b_eq, 64), (gates, b_gates, 64), (den, b_den, 4))):
                S.dma("sp", lambda e: e.dma_start(out=dbg[:, i_, 0:w_], in_=tl[:, 0:w_]), reads=[bb], writes=[b_dbg])
            a2v = a2T.rearrange("(k p) t -> p k t", p=128)
            S.dma("sp", lambda e: e.dma_start(out=a2v[:, :, ts], in_=a2b[:, :, :]), reads=[b_a2b], writes=[b_a2T])
        for ex in range(NE):
            w1 = nw(); load_w(w1, wg[ex], NK, 0, 512)
            w2 = nw(); load_w(w2, wu[ex], NK, 0, 512)
            w3 = nw()
            wdv = wd[ex].rearrange("(k p) c -> p k c", p=128)
            W3v = W[w3][:, :, :].rearrange("p (k a) c -> p k (a c)", a=4)
            for k0 in range(0, 4, 2):
                S.dma("pool", lambda e: e.dma_start(out=W3v[:, k0:k0 + 2, :], in_=wdv[:, k0:k0 + 2, :]), writes=[b_W[w3]])
            gb = 6 + (ex % 2)
            for sub in range(4):
                gi = st["gm"]; st["gm"] ^= 1
                S.op("dve", lambda e: e.tensor_scalar(out=Gm[gi][:], in0=onesf[:], scalar1=gates[:, sub * 16 + ex:sub * 16 + ex + 1], scalar2=None,
                                                      op0=ALU.mult), reads=[b_onesf, b_gates], writes=[b_Gm[gi]])
                S.op("pe", lambda e: e.matmul(ps[:, gb, sub * 128:(sub + 1) * 128], lhsT=Gm[gi][:], rhs=idt[:], start=True, stop=True),
                     reads=[b_Gm[gi], b_idt], writes=[b_ps[gb]])
            hi = st["hp"]; st["hp"] ^= 1
            for dc in range(4):
                b1 = nbank()
                for k in range(NK):
                    S.op("pe", lambda e: e.matmul(ps[:, b1, :], lhsT=W[w1][:, k, dc * 128:(dc + 1) * 128], rhs=a2b[:, k, :],
                                                  start=(k == 0), stop=(k == NK - 1)), reads=[b_W[w1], b_a2b], writes=[b_ps[b1]])
                b2 = nbank()
                for k in range(NK):
                    S.op("pe", lambda e: e.matmul(ps[:, b2, :], lhsT=W[w2][:, k, dc * 128:(dc + 1) * 128], rhs=a2b[:, k, :],
                                                  start=(k == 0), stop=(k == NK - 1)), reads=[b_W[w2], b_a2b], writes=[b_ps[b2]])
                ta = ntmp(); tb = ntmp()
                S.op("act", lambda e: e.activation(out=tmp[ta][:], in_=ps[:, b1, :], func=AF.Silu), reads=[b_ps[b1]], writes=[b_tmp[ta]])
                S.op("dve", lambda e: e.tensor_tensor(out=tmp[tb][:], in0=tmp[ta][:], in1=ps[:, b2, :], op=ALU.mult),
                     reads=[b_tmp[ta], b_ps[b2]], writes=[b_tmp[tb]])
                S.op("dve", lambda e: e.tensor_tensor(out=hp[hi][:, dc, :], in0=tmp[tb][:], in1=ps[:, gb, :], op=ALU.mult),
                     reads=[b_tmp[tb], b_ps[gb]], writes=[b_hp[hi]])
            for n in range(NK):
                bk = nbank()
                for dc in range(4):
                    S.op("pe", lambda e: e.matmul(ps[:, bk, :], lhsT=W3v[:, dc, n * 128:(n + 1) * 128], rhs=hp[hi][:, dc, :],
                                                  start=(dc == 0), stop=(dc == 3)), reads=[b_W[w3], b_hp[hi]], writes=[b_ps[bk]])
                S.op("dve", lambda e: e.scalar_tensor_tensor(out=xt[:, n, :], in0=ps[:, bk, :], scalar=modt[:, 48 + n:49 + n], in1=xt[:, n, :],
                                                             op0=ALU.mult, op1=ALU.add), reads=[b_ps[bk], b_modt, b_xt[n]], writes=[b_xt[n]])
        if x2v is not None:
            for k0 in range(0, NK, 4):
                S.dma("sp", lambda e: e.dma_start(out=x2v[:, k0:k0 + 4, ts], in_=xt[:, k0:k0 + 4, :]), reads=b_xt[k0:k0 + 4], writes=[b_x2T])
        if xfv is not None:
            rms_rstd(xt[:, :, :], b_xt)
            for k in range(NK):
                ta = ntmp()
                S.op("dve", lambda e: e.scalar_tensor_tensor(out=tmp[ta][:], in0=xt[:, k, :], scalar=gfint[:, k:k + 1], in1=rstd[:],
                                                             op0=ALU.mult, op1=ALU.mult), reads=[b_xt[k], b_gfint, b_rstd], writes=[b_tmp[ta]])
                S.dma("sp", lambda e: e.dma_start(out=xfv[:, k, ts], in_=tmp[ta][:]), reads=[b_tmp[ta]], writes=[b_xfT])


def build_fused():
    nc = bass.Bass("TRN2", target_bir_lowering=False)
    ext = lambda n, s, dt: nc.dram_tensor(n, s, dt, kind="ExternalInput").ap()
    d = {}
    d['xT'] = ext("xT", [2048, 2048], F32)
    jt = ext("jt", [1, 2], I32)
    d['cT'] = ext("cT", [128, 16], F32)
    d['pos'] = ext("pos", [1, 2048], I32)
    d['invf'] = ext("invf", [64, 1], F32)
    d['sgn'] = ext("sgn", [64, 1], F32)
    d['wada'] = ext("wada", [2, 2048, 12288], F32)
    d['bada_a'] = ext("bada_a", [2, 128, 32], F32)
    d['bada_c'] = ext("bada_c", [2, 128, 64], F32)
    d['gmix'] = ext("gmix", [2, 128, 16], F32)
    d['gq'] = ext("gq", [2, 128, 4], F32)
    d['gkv'] = ext("gkv", [2, 128, 4], F32)
    d['gmoe'] = ext("gmoe", [2, 128, 16], F32)
    d['gfin'] = ext("gfin", [128, 16], F32)
    d['win'] = ext("win", [2, 2048, 14912], F32)
    d['wsw'] = ext("wsw", [2, 2048, 64], F32)
    d['wuq'] = ext("wuq", [2, 512, 1536], F32)
    d['wuqs'] = ext("wuqs", [2, 512, 512], F32)
    d['wukv'] = ext("wukv", [2, 512, 2048], F32)
    d['womla'] = ext("womla", [2, 1024, 2048], F32)
    d['wodil'] = ext("wodil", [2, 512, 2048], F32)
    d['wosb'] = ext("wosb", [2, 1024, 2048], F32)
    d['wout'] = ext("wout", [2, 2048, 2048], F32)
    d['wr'] = ext("wr", [2048, 16], F32)
    d['br'] = ext("br", [128, 16], F32)
    d['wg'] = ext("wg", [2, 16, 2048, 512], F32)
    d['wu'] = ext("wu", [2, 16, 2048, 512], F32)
    d['wd'] = ext("wd", [2, 16, 512, 2048], F32)
    d['ident'] = ext("ident", [128, 128], F32)
    d['maskc'] = ext("maskc", [128, 4, 512], BF16)
    d['masks'] = ext("masks", [128, 4, 512], BF16)
    d['maskd'] = ext("maskd", [128, 256], F32)
    d['m1'] = ext("m1", [128, 128], BF16)
    d['m2'] = ext("m2", [128, 128], BF16)
    d['nslope'] = ext("nslope", [128, 3], F32)
    d['dposk'] = ext("dposk", [3, 128, 64], I32)
    d['dposq'] = ext("dposq", [3, 8192], I32)
    xfT = nc.dram_tensor("xfT", [2048, 2048], F32, kind="ExternalOutput").ap()
    b_xfT = Buf("xfT")
    x2s = nc.dram_tensor("x2scr", [2048, 2048], F32).ap()
    b_x2s = Buf("x2s")
    b_x0 = Buf("x0")
    GROUPS = [[0, 1, 2, 3], [4, 5, 6, 7]]

    S = Sched(nc)
    S.enable_dyn(jt[:, :])
    CH = 128 * 4096
    QKs_h = nc.dram_tensor("QKs", [32, 128, 4096], BF16)
    Vs_h = nc.dram_tensor("Vs", [16, 128, 4096], BF16)
    QKg_h = nc.dram_tensor("QKg", [32, 512, 4096], BF16)
    Vg_h = nc.dram_tensor("Vg", [16, 512, 4096], BF16)
    Ys_h = nc.dram_tensor("Ys", [10, 128, 4096], BF16)
    Yg_h = nc.dram_tensor("Yg", [10, 512, 4096], BF16)
    gsc = nc.dram_tensor("gscr", [6144, 2048], F32).ap()
    QKl_h = nc.dram_tensor("QKl", [4096, 4096], BF16)
    Vl_h = nc.dram_tensor("Vl", [2048, 4096], BF16)
    Yl_h = nc.dram_tensor("Yl", [2560, 2048], BF16)
    for l in range(2):
        b_QKs, b_Vs, b_QKg, b_Vg, b_Ys, b_Yg, b_g = (Buf(n) for n in ("QKs", "Vs", "QKg", "Vg", "Ys", "Yg", "g"))
        b_QKl, b_Vl, b_Yl = Buf("QKl"), Buf("Vl"), Buf("Yl")
        xsrc = d['xT'] if l == 0 else x2s
        b_xsrc = b_x0 if l == 0 else b_x2s
        QKs_v = QKs_h.ap().rearrange("c p (a t) -> (c p a) t", a=4).rearrange("(s u r) t -> s u r t", s=4, u=2)
        Vs_v = Vs_h.ap().rearrange("c p (a d) -> (c p a) d", a=4).rearrange("(s t) d -> s t d", s=4)
        S.begin_phase()
        b_QKs2 = [Buf("QKs0"), Buf("QKs1")]
        b_Vs2 = [Buf("Vs0"), Buf("Vs1")]

        pending = []
        rate = [1]

        def after_sup(sup):
            for sh in range(4):
                for q in range(4):
                    pending.append((QKs_h, QKg_h, sh * 8 + sup * 4 + q, b_QKs2[sup], b_QKg))
            for sh in range(4):
                for q in range(2):
                    pending.append((Vs_h, Vg_h, sh * 4 + sup * 2 + q, b_Vs2[sup], b_Vg))
            if sup == 1:
                rate[0] = 2

        def tick():
            for _ in range(rate[0]):
                if not pending:
                    break
                sh_, gh_, c, br_, bw_ = pending.pop(0)
                S.cc(lambda e: e.collective_compute("AllGather", ALU.bypass, replica_groups=GROUPS, ins=[sh_.ap()[c].opt()],
                                                    outs=[gh_.ap()[c].opt()]), reads=[br_], writes=[bw_])
        emit_A(S, nc, d, l, xsrc, QKs_v, Vs_v, gsc, b_QKs2, b_Vs2, b_g, after_sup=after_sup, tick=tick)
        while pending:
            tick()
        S.end_phase()
        S.begin_phase()
        for hh in range(2):
            S.dma_dyn(QKl_h.ap()[hh * 2048:(hh + 1) * 2048, :], QKg_h, 8 * 4 * CH, hh * 2048 * 4096, [[4096, 2048], [1, 4096]],
                      reads=[b_QKg], writes=[b_QKl])
        S.dma_dyn(Vl_h.ap()[:, :], Vg_h, 4 * 4 * CH, 0, [[4096, 2048], [1, 4096]], reads=[b_Vg], writes=[b_Vl])
        QKl = QKl_h.ap().rearrange("(c r p) (a t) -> c r (p a) t", c=8, r=4, a=4)
        Vl = Vl_h.ap().rearrange("(c r p) (a d) -> c r (p a) d", c=4, r=4, a=4)
        Ys_v = Ys_h.ap().rearrange("(th b) p t -> th b p t", th=2)
        S.barrier()
        emit_B(S, nc, d, QKl, Vl, Ys_v, b_QKl, b_Vl, b_Ys, do_mla=True, do_sb=False, do_dil=False)
        S.end_phase()
        S.begin_phase()
        emit_B(S, nc, d, QKl, Vl, Ys_v, b_QKl, b_Vl, b_Ys, do_mla=False, do_sb=True, do_dil=False)
        for c in (0, 1, 3, 4, 5, 6, 8, 9):
            S.cc(lambda e: e.collective_compute("AllGather", ALU.bypass, replica_groups=GROUPS, ins=[Ys_h.ap()[c].opt()], outs=[Yg_h.ap()[c].opt()]),
                 reads=[b_Ys], writes=[b_Yg])
        S.end_phase(wait_cc=False)
        S.begin_phase()
        emit_B(S, nc, d, QKl, Vl, Ys_v, b_QKl, b_Vl, b_Ys, do_mla=False, do_sb=False, do_dil=True)
        for c in (2, 7):
            S.cc(lambda e: e.collective_compute("AllGather", ALU.bypass, replica_groups=GROUPS, ins=[Ys_h.ap()[c].opt()], outs=[Yg_h.ap()[c].opt()]),
                 reads=[b_Ys], writes=[b_Yg])
        S.end_phase()
        S.begin_phase()
        for b0, nb_ in ((0, 3), (3, 2)):
            S.dma_dyn(Yl_h.ap()[b0 * 512:(b0 + nb_) * 512, :], Yg_h, 1, b0 * 4 * CH, [[4 * CH, nb_], [4096, 512], [1, 2048]],
                      reads=[b_Yg], writes=[b_Yl], which=1)
        Yl = Yl_h.ap().rearrange("(b j p) t -> b j p t", b=5, j=4)
        emit_C(S, nc, d, l, Yl, gsc, xsrc, x2s if l == 0 else None, xfT if l == 1 else None,
               b_Yl, b_g, b_xsrc, b_x2s, b_xfT)
        S.end_phase()
    S.close()
    return nc


_PROG = {}


def _f(a):
    return np.ascontiguousarray(a)


def kernel(**inputs):
    inp = {k: np.asarray(v) for k, v in inputs.items()}
    B_, S_ = inp['x'].shape[:2]
    cores = list(range(8))
    pos = inp['positions'].astype(np.int32)
    perm = [np.concatenate([np.arange(s, S_, r) for s in range(r)]) for r in RATES]
    slopes = (np.float32(2.0) ** (np.float32(-8.0) * np.arange(1, 13, dtype=np.float32) / np.float32(12))).reshape(3, 4)
    half = 32
    invf = (np.float32(10000.0) ** (-(np.arange(half, dtype=np.float32)) / np.float32(half))).astype(np.float32)
    wu_ = inp['w_uq']
    wuqs = np.stack([np.concatenate([np.concatenate([wu_[l][:, h * 192 + 160:h * 192 + 192], wu_[l][:, h * 192 + 128:h * 192 + 160]], axis=1)
                                     for h in range(8)], axis=1) for l in range(2)])
    wi_ = inp['w_in']
    lay = lambda a, n: _f(a.reshape(a.shape[0], n, 128).transpose(0, 2, 1))
    shared = {
        'invf': _f(np.concatenate([invf, invf])[:, None]),
        'sgn': _f(np.concatenate([-np.ones(32, np.float32), np.ones(32, np.float32)])[:, None]),
        'wada': _f(inp['w_ada']),
        'bada_a': lay(inp['b_ada'][:, :4096], 32),
        'bada_c': lay(inp['b_ada'][:, 4096:], 64),
        'gmix': lay(inp['g_mix'], 16), 'gq': lay(inp['g_q'], 4), 'gkv': lay(inp['g_kv'], 4), 'gmoe': lay(inp['g_moe'], 16),
        'gfin': _f(inp['g_final'].reshape(16, 128).T),
        'win': _f(wi_), 'wsw': _f(np.concatenate([wi_[:, :, 1056:1088], wi_[:, :, 1024:1056]], axis=2)),
        'wuq': _f(wu_), 'wuqs': _f(wuqs), 'wukv': _f(inp['w_ukv']),
        'womla': _f(inp['w_o_mla']), 'wodil': _f(inp['w_o_dil']), 'wosb': _f(inp['w_o_sb']), 'wout': _f(inp['w_out']),
        'wr': _f(inp['w_router']), 'br': _f(np.broadcast_to(inp['b_router'][None, :], (128, 16))),
        'wg': _f(inp['w_gate']), 'wu': _f(inp['w_up']), 'wd': _f(inp['w_down']), 'ident': np.eye(128, dtype=np.float32),
    }
    shared.update(consts_B())
    maps = []
    for c in cores:
        b, j = c // 4, c % 4
        m = dict(shared)
        m['xT'] = _f(inp['x'][b, j * 2048:(j + 1) * 2048, :].T)
        m['jt'] = np.array([[j, (j // 2) * 5 * 4 * 128 * 4096 + (j % 2) * 2048]], np.int32)
        m['cT'] = _f(inp['c'][b].reshape(16, 128).T)
        m['pos'] = _f(pos[b, j * 2048:(j + 1) * 2048][None, :])
        pp = np.stack([pos[b][perm[g]] for g in range(3)]).astype(np.int32)
        m['dposq'] = _f(pp)
        m['dposk'] = _f(pp.reshape(3, S_ // 128, 128).transpose(0, 2, 1))
        m['nslope'] = _f(np.broadcast_to(-slopes[:, j][None, :], (128, 3)).astype(np.float32))
        maps.append(m)
    if "f" not in _PROG:
        _PROG["f"] = build_fused()
    res = run_bass_kernel_spmd(_PROG["f"], maps, core_ids=cores).results
    out = np.empty(inp['x'].shape, dtype=np.float32)
    for c in cores:
        b, j = c // 4, c % 4
        out[b, j * 2048:(j + 1) * 2048, :] = np.asarray(res[c]['xfT']).T
    return out
```

```python
import math
import numpy as np
import concourse.bass as bass
import concourse.mybir as mybir
from concourse.bass_utils import run_bass_kernel_spmd

F32 = mybir.dt.float32
BF16 = mybir.dt.bfloat16
I32 = mybir.dt.int32
AF = mybir.ActivationFunctionType
ALU = mybir.AluOpType
AX = mybir.AxisListType


class Buf:
    __slots__ = ("name", "w", "r")

    def __init__(self, name):
        self.name = name
        self.w = None
        self.r = {}


class _Rec:
    def __init__(self):
        self.call = None

    def __getattr__(self, name):
        def f(*a, **kw):
            self.call = (name, a, kw)
            return self
        return f


def _record(fn):
    r = _Rec()
    fn(r)
    assert r.call is not None
    return r.call


class Sched:
    ENGS = ("pe", "act", "dve", "pool", "sp")
    NDMA = 24

    def __init__(self, nc):
        self.nc = nc
        self.streams = {e: [] for e in self.ENGS}
        self.cnt = {e: 0 for e in self.ENGS}
        self.seen = {e: {} for e in self.ENGS}
        self.dma_i = {"sp": 0, "pool": 0, "act": 0}
        self.dma_val = {}
        self.sems = {}
        self._ctx = []
        self._perm = []
        self.cc_val = {}
        self._phase_mark = 0
        self.uses_dyn = False
        self._phase_no = 0
        self.jt_ap = None
        self.jsb = None
        self.jt_cnt = 0
        for e in ("pe", "act", "dve", "pool"):
            self._mk_sem("E_" + e)
        for q in ("sp", "pool", "act"):
            for k in range(self.NDMA):
                self._mk_sem("D_%s%d" % (q, k))
                self.dma_val["D_%s%d" % (q, k)] = 0

    def _mk_sem(self, key):
        cm = self.nc.semaphore(key)
        self.sems[key] = cm.__enter__()
        self._perm.append(cm)

    _mk_sem_perm = _mk_sem

    def enable_dyn(self, jt_ap):
        self.jt_ap = jt_ap
        self._mk_sem("JT")
        cm = self.nc.sbuf_tensor("jsb", [1, 2], I32)
        self.jsb = cm.__enter__()
        self._perm.append(cm)

    def sbuf(self, name, shape, dtype):
        cm = self.nc.sbuf_tensor("%s_p%d" % (name, self._phase_no), shape, dtype)
        t = cm.__enter__()
        self._ctx.append(cm)
        return t

    def psum(self, name, shape, dtype):
        cm = self.nc.psum_tensor("%s_p%d" % (name, self._phase_no), shape, dtype)
        t = cm.__enter__()
        self._ctx.append(cm)
        return t

    def _deps(self, eng, reads, writes):
        deps = {}

        def add(tok):
            if tok is None:
                return
            k, v = tok
            if deps.get(k, -1) < v:
                deps[k] = v
        for b in reads:
            add(b.w)
        for b in writes:
            add(b.w)
            for k, v in b.r.items():
                add((k, v))
        out = []
        seen = self.seen[eng]
        for k, v in deps.items():
            if eng == "pe" and k == "E_pe":
                continue
            if seen.get(k, -1) >= v:
                continue
            seen[k] = v
            out.append((k, v))
        return out

    def _mark(self, tok, reads, writes):
        k, v = tok
        for b in reads:
            if b.r.get(k, -1) < v:
                b.r[k] = v
        for b in writes:
            b.w = tok
            b.r = {}

    def op(self, eng, fn, reads=(), writes=()):
        waits = self._deps(eng, reads, writes)
        self.cnt[eng] += 1
        tok = ("E_" + eng, self.cnt[eng])
        self.streams[eng].append((waits, _record(fn), tok[0], 1))
        self._mark(tok, reads, writes)

    def dma(self, q, fn, reads=(), writes=()):
        i = self.dma_i[q]
        self.dma_i[q] += 1
        key = "D_%s%d" % (q, i % self.NDMA)
        waits = self._deps(q, reads, writes)
        prev = self.dma_val[key]
        if prev > 0 and self.seen[q].get(key, -1) < prev:
            self.seen[q][key] = prev
            waits.append((key, prev))
        self.dma_val[key] = prev + 16
        tok = (key, prev + 16)
        self.streams[q].append((waits, _record(fn), key, 16))
        self._mark(tok, reads, writes)

    def begin_phase(self):
        self._phase_mark = len(self._ctx)
        self._phase_no += 1

    def barrier(self, wait_cc=True):
        allv = {}
        for e in ("pe", "act", "dve", "pool"):
            if self.cnt[e] > 0:
                allv["E_" + e] = self.cnt[e]
        for k, v in self.dma_val.items():
            if v > 0:
                allv[k] = v
        for k, v in self.cc_val.items():
            if v > 0 and wait_cc:
                allv[k] = v
        for eng in self.ENGS:
            waits = []
            for k, v in allv.items():
                if self.seen[eng].get(k, -1) < v:
                    self.seen[eng][k] = v
                    waits.append((k, v))
            self.streams[eng].append((waits, None, None, 0))

    def end_phase(self, wait_cc=True):
        self.barrier(wait_cc)
        self.emit()
        self.streams = {e: [] for e in self.ENGS}
        while len(self._ctx) > self._phase_mark:
            self._ctx.pop().__exit__(None, None, None)

    def cc(self, fn, reads=(), writes=()):
        key = "CC"
        if key not in self.sems:
            self._mk_sem_perm(key)
            self.cc_val[key] = 0
        n = self.cc_val[key] + 1
        self.cc_val[key] = n
        waits = self._deps("pool", reads, writes)
        self.streams["pool"].append((waits, _record(fn), key, None))
        self._mark((key, n), reads, writes)

    def dma_dyn(self, out_ap, tensor, jmul, const, ap_list, reads=(), writes=(), which=0):
        q = "sp"
        i = self.dma_i[q]
        self.dma_i[q] += 1
        key = "D_%s%d" % (q, i % self.NDMA)
        waits = self._deps(q, reads, writes)
        prev = self.dma_val[key]
        if prev > 0 and self.seen[q].get(key, -1) < prev:
            self.seen[q][key] = prev
            waits.append((key, prev))
        self.dma_val[key] = prev + 16
        tok = (key, prev + 16)
        self.streams[q].append((waits, ("__dyn__", (out_ap, tensor, int(jmul), int(const), [list(x) for x in ap_list], which), {}), key, 16))
        self._mark(tok, reads, writes)
        self.uses_dyn = True

    def final_wait(self, eng, bufs):
        waits = self._deps(eng, bufs, ())
        self.streams[eng].append((waits, None, None, 0))

    def emit(self):
        nc = self.nc
        sems = self.sems
        streams = self.streams

        def run(engine, lst, regs=None):
            for waits, fn, key, inc in lst:
                for k, v in waits:
                    engine.wait_ge(sems[k], v)
                if fn is not None:
                    name, a, kw = fn
                    if name == "__dyn__":
                        out_ap, tensor, jmul, const, ap_list, which = a
                        rj, ro = regs[which], regs[2]
                        engine.reg_mul(ro, rj, jmul)
                        engine.reg_add(ro, ro, const)
                        ins = engine.dma_start(out=out_ap, in_=bass.AP(tensor, ro, ap_list))
                    else:
                        ins = getattr(engine, name)(*a, **kw)
                    if inc is None:
                        ins.then_inc(sems[key])
                    else:
                        ins.then_inc(sems[key], inc)

        with nc.Block() as block:
            @block.tensor
            def _(e):
                run(e, streams["pe"])

            @block.scalar
            def _(e):
                run(e, streams["act"])

            @block.vector
            def _(e):
                run(e, streams["dve"])

            @block.gpsimd
            def _(e):
                run(e, streams["pool"])

            @block.sync
            def _(e):
                if any(fn is not None and fn[0] == "__dyn__" for _, fn, _, _ in streams["sp"]):
                    self.jt_cnt += 1
                    with e.register("rj%d" % self.jt_cnt) as rj, e.register("ry%d" % self.jt_cnt) as ry, e.register("ro%d" % self.jt_cnt) as ro:
                        e.dma_start(out=self.jsb[:, :], in_=self.jt_ap).then_inc(sems["JT"], 16)
                        e.wait_ge(sems["JT"], 16 * self.jt_cnt)
                        e.reg_load(rj, self.jsb[0:1, 0:1])
                        e.reg_load(ry, self.jsb[0:1, 1:2])
                        run(e, streams["sp"], (rj, ry, ro))
                else:
                    run(e, streams["sp"])

    def close(self):
        for cm in reversed(self._ctx):
            cm.__exit__(None, None, None)
        self._ctx = []
        for cm in reversed(self._perm):
            cm.__exit__(None, None, None)
        self._perm = []


D = 2048
NK = 16
TS = 1024
TP = 128
EPS = 1e-6
TWO_PI = 2.0 * math.pi
C1 = 6.28125
C2 = TWO_PI - C1


def emit_A(S, nc, d, l, xT, QKs, Vs, gT, b_QKs_l, b_Vs_l, b_gT, ntok=2048, after_sup=None, tick=None):
    cT = d['cT']; wada = d['wada'][l]; bada = d['bada_a'][l]; gmix = d['gmix'][l]; win = d['win'][l]; wsw = d['wsw'][l]
    gq = d['gq'][l]; gkv = d['gkv'][l]; wuq = d['wuq'][l]; wuqs = d['wuqs'][l]; wukv = d['wukv'][l]
    pos = d['pos']; invf = d['invf']; sgn = d['sgn']
    b_QKs = b_QKs_l[0]; b_Vs = b_Vs_l[0]
    b_projT = b_QKs; b_mlaq = b_QKs; b_mlakv = b_QKs; b_mlakr = b_QKs
    aT = S.sbuf("aT", [128, NK, TS], BF16); b_aT = [Buf("aT%d" % i) for i in range(TS // TP)]
    wb = [S.sbuf("wb%d" % i, [128, NK, 512], BF16) for i in range(2)]; b_wb = [Buf("wb0"), Buf("wb1")]
    xt = S.sbuf("xt", [128, NK, TP], F32); b_xt = Buf("xt")
    sq = S.sbuf("sq", [128, NK, TP], BF16); b_sq = Buf("sq")
    rstd = S.sbuf("rstd", [128, 512], F32); b_rstd = Buf("rstd")
    lnt = S.sbuf("lnt", [128, 512], F32); b_lnt = Buf("lnt")
    tmp = [S.sbuf("tmp%d" % i, [128, 512], F32) for i in range(2)]; b_tmp = [Buf("tmp0"), Buf("tmp1")]
    ones = S.sbuf("ones", [128, 128], BF16); b_ones = Buf("ones")
    cin = S.sbuf("cin", [128, NK], F32); b_cin = Buf("cin")
    cact = S.sbuf("cact", [128, NK], BF16); b_cact = Buf("cact")
    badat = S.sbuf("badat", [128, 32], F32); b_badat = Buf("badat")
    gmixt = S.sbuf("gmixt", [128, NK], F32); b_gmixt = Buf("gmixt")
    modt = S.sbuf("modt", [128, 32], F32); b_modt = Buf("modt")
    s1 = S.sbuf("s1", [128, NK], F32); b_s1 = Buf("s1")
    cq = S.sbuf("cq", [128, 4, TS], F32); b_cq = Buf("cq")
    ckv = S.sbuf("ckv", [128, 4, TS], F32); b_ckv = Buf("ckv")
    kr = S.sbuf("kr", [64, TS], F32); b_kr = Buf("kr")
    krs = S.sbuf("krs", [64, TS], F32); b_krs = Buf("krs")
    wswb = S.sbuf("wswb", [128, NK, 64], BF16); b_wswb = Buf("wswb")
    wuqb = S.sbuf("wuqb", [128, 4, 1536], BF16); b_wuqb = Buf("wuqb")
    wuqsb = S.sbuf("wuqsb", [128, 4, 512], BF16); b_wuqsb = Buf("wuqsb")
    wukvb = S.sbuf("wukvb", [128, 4, 2048], BF16); b_wukvb = Buf("wukvb")
    gqt = S.sbuf("gqt", [128, 4], F32); b_gqt = Buf("gqt")
    gkvt = S.sbuf("gkvt", [128, 4], F32); b_gkvt = Buf("gkvt")
    ob = [S.sbuf("ob%d" % i, [128, TS], BF16) for i in range(2)]; b_ob = [Buf("ob0"), Buf("ob1")]
    of = [S.sbuf("of%d" % i, [128, TS], F32) for i in range(2)]; b_of = [Buf("of0"), Buf("of1")]
    lat = S.sbuf("lat", [128, 4, 512], BF16); b_lat = Buf("lat")
    posi = S.sbuf("posi", [64, 512], I32); b_posi = Buf("posi")
    ang = S.sbuf("ang", [64, 512], F32); b_ang = Buf("ang")
    kf = S.sbuf("kf", [64, 512], F32); b_kf = Buf("kf")
    ki = posi; b_ki = b_posi
    rr = S.sbuf("rr", [64, 512], F32); b_rr = Buf("rr")
    rc = S.sbuf("rc", [64, 512], F32); b_rc = Buf("rc")
    mm = S.sbuf("mm", [64, 512], F32); b_mm = Buf("mm")
    CS = S.sbuf("CS", [64, 512], F32); b_CS = Buf("CS")
    SN = S.sbuf("SN", [64, 512], F32); b_SN = Buf("SN")
    invft = S.sbuf("invft", [64, 1], F32); b_invft = Buf("invft")
    sgnt = S.sbuf("sgnt", [64, 1], F32); b_sgnt = Buf("sgnt")
    t1 = ang; b_t1 = b_ang
    t2 = kf; b_t2 = b_kf
    ps = S.psum("ps", [128, 8, 512], F32); b_ps = [Buf("ps%d" % i) for i in range(8)]
    st = {"bank": 0, "w": 0, "ob": 0, "of": 0, "tmp": 0, "ev": 0}

    def nbank():
        i = st["bank"]; st["bank"] = (i + 1) % 8
        return i

    def load_w(dst, bdst, src, nk, c0, ncols):
        srcv = src.rearrange("(k p) c -> p k c", p=128)
        h = max(1, nk // 2)
        for k0 in range(0, nk, h):
            S.dma("pool", lambda e, k0=k0: e.dma_start(out=dst[:, k0:k0 + h, 0:ncols],
                                                      in_=srcv[:, k0:k0 + h, c0:c0 + ncols]),
                  writes=[bdst])

    S.op("dve", lambda e: e.memset(ones[:], 1.0), writes=[b_ones])
    S.dma("sp", lambda e: e.dma_start(out=cin[:], in_=cT[:, :]), writes=[b_cin])
    S.dma("sp", lambda e: e.dma_start(out=badat[:], in_=bada[:, :]), writes=[b_badat])
    S.dma("sp", lambda e: e.dma_start(out=gmixt[:], in_=gmix[:, :]), writes=[b_gmixt])
    S.dma("sp", lambda e: e.dma_start(out=gqt[:], in_=gq[:, :]), writes=[b_gqt])
    S.dma("sp", lambda e: e.dma_start(out=gkvt[:], in_=gkv[:, :]), writes=[b_gkvt])
    S.dma("sp", lambda e: e.dma_start(out=invft[:], in_=invf[:, :]), writes=[b_invft])
    S.dma("sp", lambda e: e.dma_start(out=sgnt[:], in_=sgn[:, :]), writes=[b_sgnt])
    S.op("act", lambda e: e.activation(out=cact[:], in_=cin[:], func=AF.Silu), reads=[b_cin], writes=[b_cact])

    mb = nbank()
    for g in range(8):
        wi = st["w"]; st["w"] ^= 1
        load_w(wb[wi], b_wb[wi], wada, NK, g * 512, 512)
        for m in range(4):
            n = g * 4 + m
            for k in range(NK):
                S.op("pe", lambda e, wi=wi, m=m, k=k, n=n: e.matmul(
                    ps[:, mb, n:n + 1], lhsT=wb[wi][:, k, m * 128:(m + 1) * 128], rhs=cact[:, k:k + 1],
                    start=(k == 0), stop=(k == NK - 1)), reads=[b_wb[wi], b_cact], writes=[b_ps[mb]])
    S.op("dve", lambda e: e.tensor_tensor(out=modt[:], in0=ps[:, mb, 0:32], in1=badat[:], op=ALU.add),
         reads=[b_ps[mb], b_badat], writes=[b_modt])
    S.op("dve", lambda e: e.scalar_tensor_tensor(out=s1[:], in0=modt[:, 16:32], scalar=1.0, in1=gmixt[:],
                                                 op0=ALU.add, op1=ALU.mult),
         reads=[b_modt, b_gmixt], writes=[b_s1])

    load_w(wswb, b_wswb, wsw, NK, 0, 64)
    load_w(wuqb, b_wuqb, wuq, 4, 0, 1536)
    load_w(wuqsb, b_wuqsb, wuqs, 4, 0, 512)
    load_w(wukvb, b_wukvb, wukv, 4, 0, 2048)

    def rms_rstd(src_sq, bsrc, nk, width, dim):
        bk = nbank()
        for k in range(nk):
            S.op("pe", lambda e, k=k: e.matmul(ps[:, bk, 0:width], lhsT=ones[:], rhs=src_sq[:, k, 0:width],
                                               start=(k == 0), stop=(k == nk - 1)),
                 reads=[bsrc, b_ones], writes=[b_ps[bk]])
        S.op("act", lambda e: e.activation(out=lnt[:, 0:width], in_=ps[:, bk, 0:width], func=AF.Ln,
                                           scale=1.0 / dim, bias=EPS), reads=[b_ps[bk]], writes=[b_lnt])
        S.op("act", lambda e: e.activation(out=rstd[:, 0:width], in_=lnt[:, 0:width], func=AF.Exp, scale=-0.5),
             reads=[b_lnt], writes=[b_rstd])

    xTv = xT.rearrange("(k p) t -> p k t", p=128)
    for sup in range(ntok // TS):
        t0s = sup * TS
        b_QKs = b_QKs_l[sup]; b_Vs = b_Vs_l[sup]
        for pt in range(TS // TP):
            tok0 = t0s + pt * TP
            for k0 in (0, 8):
                S.dma("sp", lambda e, k0=k0, tok0=tok0: e.dma_start(out=xt[:, k0:k0 + 8, :],
                                                                    in_=xTv[:, k0:k0 + 8, tok0:tok0 + TP]),
                      writes=[b_xt])
            S.op("act", lambda e: e.activation(out=sq[:], in_=xt[:], func=AF.Square), reads=[b_xt], writes=[b_sq])
            rms_rstd(sq, b_sq, NK, TP, float(D))
            for k in range(NK):
                ti = st["tmp"]; st["tmp"] ^= 1
                S.op("dve", lambda e, k=k, ti=ti: e.tensor_tensor(out=tmp[ti][:, 0:TP], in0=xt[:, k, :],
                                                                  in1=rstd[:, 0:TP], op=ALU.mult),
                     reads=[b_xt, b_rstd], writes=[b_tmp[ti]])
                S.op("act", lambda e, k=k, ti=ti, pt=pt: e.activation(
                    out=aT[:, k, pt * TP:(pt + 1) * TP], in_=tmp[ti][:, 0:TP], func=AF.Identity,
                    scale=s1[:, k:k + 1], bias=modt[:, k:k + 1]),
                    reads=[b_tmp[ti], b_s1, b_modt], writes=[b_aT[pt]])

        def gemm_group(src, c0, ncols, epilogue):
            wi = st["w"]; st["w"] ^= 1
            load_w(wb[wi], b_wb[wi], src, NK, c0, ncols)
            if tick is not None:
                tick()
            for m in range((ncols + 127) // 128):
                mc = min(128, ncols - m * 128)
                for t in range(TS // 512):
                    bk = nbank()
                    for k in range(NK):
                        S.op("pe", lambda e, wi=wi, m=m, mc=mc, t=t, k=k, bk=bk: e.matmul(
                            ps[0:mc, bk, :], lhsT=wb[wi][:, k, m * 128:m * 128 + mc],
                            rhs=aT[:, k, t * 512:(t + 1) * 512], start=(k == 0), stop=(k == NK - 1)),
                            reads=[b_wb[wi]] + b_aT[4 * t:4 * t + 4], writes=[b_ps[bk]])
                    epilogue(m, mc, t, bk)

        def evac(out_ap, bout, bk, mc, scale=1.0, func=None):
            st["ev"] ^= 1
            if func is not None or st["ev"]:
                f = func if func is not None else AF.Copy
                S.op("act", lambda e: e.activation(out=out_ap, in_=ps[0:mc, bk, :], func=f, scale=scale),
                     reads=[b_ps[bk]], writes=[bout])
            else:
                S.op("dve", lambda e: e.tensor_scalar(out=out_ap, in0=ps[0:mc, bk, :], scalar1=scale, scalar2=None,
                                                      op0=ALU.mult), reads=[b_ps[bk]], writes=[bout])

        gemm_group(win, 0, 512, lambda m, mc, t, bk: evac(cq[:, m, t * 512:(t + 1) * 512], b_cq, bk, mc))
        gemm_group(win, 512, 512, lambda m, mc, t, bk: evac(ckv[:, m, t * 512:(t + 1) * 512], b_ckv, bk, mc))
        gemm_group(win, 1024, 64, lambda m, mc, t, bk: evac(kr[:, t * 512:(t + 1) * 512], b_kr, bk, mc))
        gemm_group(wsw, 0, 64, lambda m, mc, t, bk: evac(krs[:, t * 512:(t + 1) * 512], b_krs, bk, mc))

        def out_bf(dst, bdst, row0, scale):
            cur = {}

            def ep(m, mc, t, bk):
                if t == 0:
                    cur["i"] = st["ob"]; st["ob"] ^= 1
                i = cur["i"]
                evac(ob[i][:, t * 512:(t + 1) * 512], b_ob[i], bk, mc, scale=scale)
                if t == TS // 512 - 1:
                    r = row0 + m * 128
                    S.dma("sp", lambda e, i=i, r=r: e.dma_start(out=dst[r:r + 128, t0s:t0s + TS], in_=ob[i][:, :]),
                          reads=[b_ob[i]], writes=[bdst])
            return ep

        def out_f32(dst, bdst, row0, func):
            cur = {}

            def ep(m, mc, t, bk):
                if t == 0:
                    cur["i"] = st["of"]; st["of"] ^= 1
                i = cur["i"]
                evac(of[i][:, t * 512:(t + 1) * 512], b_of[i], bk, mc, func=func)
                if t == TS // 512 - 1:
                    r = row0 + m * 128
                    S.dma("sp", lambda e, i=i, r=r: e.dma_start(out=dst[r:r + 128, t0s:t0s + TS], in_=of[i][:, :]),
                          reads=[b_of[i]], writes=[bdst])
            return ep

        sc = 128.0 ** -0.5
        def out_qk(rowfn, scale):
            cur = {}

            def ep(m, mc, t, bk):
                if t == 0:
                    cur["i"] = st["ob"]; st["ob"] ^= 1
                i = cur["i"]
                evac(ob[i][:, t * 512:(t + 1) * 512], b_ob[i], bk, mc, scale=scale)
                if t == TS // 512 - 1:
                    sh, r0 = rowfn(m)
                    S.dma("sp", lambda e: e.dma_start(out=QKs[sh, sup, r0:r0 + 128, :], in_=ob[i][:, :]),
                          reads=[b_ob[i]], writes=[b_QKs])
            return ep

        def gemm_group_tm(c0, store):
            wi = st["w"]; st["w"] ^= 1
            load_w(wb[wi], b_wb[wi], win, NK, c0, 512)
            if tick is not None:
                tick()
            for s_ in range(TS // 128):
                bk = nbank()
                for k in range(NK):
                    S.op("pe", lambda e: e.matmul(ps[:, bk, :], lhsT=aT[:, k, s_ * 128:(s_ + 1) * 128], rhs=wb[wi][:, k, 0:512],
                                                  start=(k == 0), stop=(k == NK - 1)), reads=[b_wb[wi], b_aT[s_]], writes=[b_ps[bk]])
                oi = st["ob"]; st["ob"] ^= 1
                evac(ob[oi][:, 0:512], b_ob[oi], bk, 128)
                store(s_, oi)

        for g in range(3):
            gemm_group(win, 1088 + g * 512, 512, out_qk(lambda m, g=g: (m, g * 128), sc))
        for g in range(3):
            gemm_group(win, 1088 + 1536 + g * 512, 512, out_qk(lambda m, g=g: (m, 384 + g * 128), 1.0))
        for g in range(3):
            def st_dv(s_, oi, g=g):
                tk = t0s + s_ * 128
                S.dma("sp", lambda e: e.dma_start(out=Vs[:, tk:tk + 128, g * 128:(g + 1) * 128].rearrange("h p c -> p h c"),
                                                  in_=ob[oi][:, 0:512].rearrange("p (h c) -> p h c", c=128)),
                      reads=[b_ob[oi]], writes=[b_Vs])
            gemm_group_tm(1088 + 3072 + g * 512, st_dv)
        for gi in range(2):
            gemm_group(win, 5696 + gi * 512, 512, out_qk(lambda m, gi=gi: ((4 * gi + m) // 2, 768 + (m % 2) * 128), sc))
        for gi in range(2):
            gemm_group(win, 5696 + 1024 + gi * 512, 512, out_qk(lambda m, gi=gi: ((4 * gi + m) // 2, 1024 + (m % 2) * 128), 1.0))
        for gi in range(2):
            def st_sv(s_, oi, gi=gi):
                tk = t0s + s_ * 128
                S.dma("sp", lambda e: e.dma_start(out=Vs[2 * gi:2 * gi + 2, tk:tk + 128, 384:640].rearrange("j p c -> p j c"),
                                                  in_=ob[oi][:, 0:512].rearrange("p (j c) -> p j c", c=256)),
                      reads=[b_ob[oi]], writes=[b_Vs])
            gemm_group_tm(5696 + 2048 + gi * 512, st_sv)

        scm = 192.0 ** -0.5
        for tt in range(TS // 512):
            tok0 = t0s + tt * 512
            tsl = slice(tt * 512, (tt + 1) * 512)
            S.dma("sp", lambda e, tok0=tok0: e.dma_start(out=posi[:], in_=pos[0:1, tok0:tok0 + 512].partition_broadcast(64)),
                  writes=[b_posi])
            S.op("dve", lambda e: e.tensor_copy(out=ang[:], in_=posi[:]), reads=[b_posi], writes=[b_ang])
            S.op("dve", lambda e: e.tensor_scalar(out=ang[:], in0=ang[:], scalar1=invft[:, 0:1], scalar2=None, op0=ALU.mult),
                 reads=[b_ang, b_invft], writes=[b_ang])
            S.op("dve", lambda e: e.tensor_scalar(out=kf[:], in0=ang[:], scalar1=1.0 / TWO_PI, scalar2=None, op0=ALU.mult),
                 reads=[b_ang], writes=[b_kf])
            S.op("dve", lambda e: e.tensor_copy(out=ki[:], in_=kf[:]), reads=[b_kf], writes=[b_ki])
            S.op("dve", lambda e: e.tensor_copy(out=kf[:], in_=ki[:]), reads=[b_ki], writes=[b_kf])
            S.op("dve", lambda e: e.scalar_tensor_tensor(out=rr[:], in0=kf[:], scalar=-C1, in1=ang[:], op0=ALU.mult, op1=ALU.add),
                 reads=[b_kf, b_ang], writes=[b_rr])
            S.op("dve", lambda e: e.scalar_tensor_tensor(out=rr[:], in0=kf[:], scalar=-C2, in1=rr[:], op0=ALU.mult, op1=ALU.add),
                 reads=[b_kf, b_rr], writes=[b_rr])

            def wrap(r, br):
                S.op("dve", lambda e: e.tensor_scalar(out=mm[:], in0=r[:], scalar1=math.pi, scalar2=-TWO_PI, op0=ALU.is_gt, op1=ALU.mult),
                     reads=[br], writes=[b_mm])
                S.op("dve", lambda e: e.tensor_tensor(out=r[:], in0=r[:], in1=mm[:], op=ALU.add), reads=[br, b_mm], writes=[br])
                S.op("dve", lambda e: e.tensor_scalar(out=mm[:], in0=r[:], scalar1=-math.pi, scalar2=TWO_PI, op0=ALU.is_lt, op1=ALU.mult),
                     reads=[br], writes=[b_mm])
                S.op("dve", lambda e: e.tensor_tensor(out=r[:], in0=r[:], in1=mm[:], op=ALU.add), reads=[br, b_mm], writes=[br])
                S.op("dve", lambda e: e.tensor_scalar(out=r[:], in0=r[:], scalar1=3.1415925, scalar2=-3.1415925, op0=ALU.min, op1=ALU.max),
                     reads=[br], writes=[br])
            wrap(rr, b_rr)
            S.op("dve", lambda e: e.tensor_scalar(out=rc[:], in0=rr[:], scalar1=math.pi / 2, scalar2=None, op0=ALU.add),
                 reads=[b_rr], writes=[b_rc])
            wrap(rc, b_rc)
            S.op("act", lambda e: e.activation(out=CS[:], in_=rc[:], func=AF.Sin), reads=[b_rc], writes=[b_CS])
            S.op("act", lambda e: e.activation(out=SN[:], in_=rr[:], func=AF.Sin, scale=sgnt[:, 0:1]), reads=[b_rr, b_sgnt], writes=[b_SN])

            def rope_out(src_r, bsr, src_s, bss, scale, dst_ap, bdst):
                S.op("dve", lambda e: e.scalar_tensor_tensor(out=t1[:], in0=src_r, scalar=scale, in1=CS[:], op0=ALU.mult, op1=ALU.mult),
                     reads=[bsr, b_CS], writes=[b_t1])
                S.op("dve", lambda e: e.scalar_tensor_tensor(out=t2[:], in0=src_s, scalar=scale, in1=SN[:], op0=ALU.mult, op1=ALU.mult),
                     reads=[bss, b_SN], writes=[b_t2])
                S.op("dve", lambda e: e.tensor_tensor(out=dst_ap, in0=t1[:], in1=t2[:], op=ALU.add),
                     reads=[b_t1, b_t2], writes=[bdst])

            oi = st["ob"]; st["ob"] ^= 1
            rope_out(kr[:, tsl], b_kr, krs[:, tsl], b_krs, 1.0, ob[oi][0:64, 0:512], b_ob[oi])
            for sh in range(4):
                S.dma("sp", lambda e: e.dma_start(out=QKs[sh, sup, 1920:1984, tok0 - t0s:tok0 - t0s + 512], in_=ob[oi][0:64, 0:512]),
                      reads=[b_ob[oi]], writes=[b_QKs])

            def latent_norm(src, bsrc, gt, bgt):
                sqv = sq[:, :, :].rearrange("p (k a) t -> p k (a t)", a=4)
                S.op("act", lambda e: e.activation(out=sqv, in_=src[:, :, tsl], func=AF.Square), reads=[bsrc], writes=[b_sq])
                rms_rstd(sqv, b_sq, 4, 512, 512.0)
                for k in range(4):
                    S.op("dve", lambda e, k=k: e.scalar_tensor_tensor(out=lat[:, k, :], in0=src[:, k, tsl], scalar=gt[:, k:k + 1],
                                                                      in1=rstd[:, :], op0=ALU.mult, op1=ALU.mult),
                         reads=[bsrc, bgt, b_rstd], writes=[b_lat])

            latent_norm(cq, b_cq, gqt, b_gqt)
            for h in range(8):
                bk = nbank()
                for k in range(4):
                    S.op("pe", lambda e, h=h, k=k, bk=bk: e.matmul(ps[:, bk, :], lhsT=wuqb[:, k, h * 192:h * 192 + 128], rhs=lat[:, k, :],
                                                                   start=(k == 0), stop=(k == 3)), reads=[b_wuqb, b_lat], writes=[b_ps[bk]])
                oi = st["ob"]; st["ob"] ^= 1
                evac(ob[oi][:, 0:512], b_ob[oi], bk, 128, scale=scm)
                S.dma("sp", lambda e, oi=oi, h=h, tok0=tok0: e.dma_start(out=QKs[h // 2, sup, 1280 + (h % 2) * 128:1280 + (h % 2) * 128 + 128, tok0 - t0s:tok0 - t0s + 512], in_=ob[oi][:, 0:512]),
                      reads=[b_ob[oi]], writes=[b_mlaq])
                bk1 = nbank(); bk2 = nbank()
                for k in range(4):
                    S.op("pe", lambda e, h=h, k=k, bk1=bk1: e.matmul(ps[0:64, bk1, :], lhsT=wuqb[:, k, h * 192 + 128:h * 192 + 192], rhs=lat[:, k, :],
                                                                     start=(k == 0), stop=(k == 3)), reads=[b_wuqb, b_lat], writes=[b_ps[bk1]])
                for k in range(4):
                    S.op("pe", lambda e, h=h, k=k, bk2=bk2: e.matmul(ps[0:64, bk2, :], lhsT=wuqsb[:, k, h * 64:(h + 1) * 64], rhs=lat[:, k, :],
                                                                     start=(k == 0), stop=(k == 3)), reads=[b_wuqsb, b_lat], writes=[b_ps[bk2]])
                oi = st["ob"]; st["ob"] ^= 1
                rope_out(ps[0:64, bk1, :], b_ps[bk1], ps[0:64, bk2, :], b_ps[bk2], scm, ob[oi][0:64, 0:512], b_ob[oi])
                S.dma("sp", lambda e, oi=oi, h=h, tok0=tok0: e.dma_start(out=QKs[h // 2, sup, 1792 + (h % 2) * 64:1792 + (h % 2) * 64 + 64, tok0 - t0s:tok0 - t0s + 512], in_=ob[oi][0:64, 0:512]),
                      reads=[b_ob[oi]], writes=[b_mlaq])
            latent_norm(ckv, b_ckv, gkvt, b_gkvt)
            for h in range(8):
                bk = nbank()
                for k in range(4):
                    S.op("pe", lambda e: e.matmul(ps[:, bk, :], lhsT=wukvb[:, k, h * 256:h * 256 + 128], rhs=lat[:, k, :],
                                                  start=(k == 0), stop=(k == 3)), reads=[b_wukvb, b_lat], writes=[b_ps[bk]])
                oi = st["ob"]; st["ob"] ^= 1
                evac(ob[oi][:, 0:512], b_ob[oi], bk, 128)
                S.dma("sp", lambda e: e.dma_start(out=QKs[h // 2, sup, 1536 + (h % 2) * 128:1536 + (h % 2) * 128 + 128, tok0 - t0s:tok0 - t0s + 512], in_=ob[oi][:, 0:512]),
                      reads=[b_ob[oi]], writes=[b_QKs])
            wv = wukvb[:, :, :].rearrange("p k (h c) -> p k h c", c=256)
            for s4 in range(4):
                for hg in range(2):
                    bk = nbank()
                    for k in range(4):
                        S.op("pe", lambda e: e.matmul(ps[:, bk, :].rearrange("p (h c) -> p h c", c=128), lhsT=lat[:, k, s4 * 128:(s4 + 1) * 128],
                                                      rhs=wv[:, k, hg * 4:(hg + 1) * 4, 128:256], start=(k == 0), stop=(k == 3)),
                             reads=[b_wukvb, b_lat], writes=[b_ps[bk]])
                    oi = st["ob"]; st["ob"] ^= 1
                    evac(ob[oi][:, 0:512], b_ob[oi], bk, 128)
                    tk = tok0 + s4 * 128
                    S.dma("sp", lambda e: e.dma_start(out=Vs[2 * hg:2 * hg + 2, tk:tk + 128, 640:896].rearrange("j p c -> p j c"),
                                                      in_=ob[oi][:, 0:512].rearrange("p (j c) -> p j c", c=256)),
                          reads=[b_ob[oi]], writes=[b_Vs])
        if after_sup is not None:
            after_sup(sup)
        for gi in range(12):
            gemm_group(win, 8768 + gi * 512, 512, out_f32(gT, b_gT, gi * 512, AF.Sigmoid))


SEQ = 8192
NB = SEQ // 128
RATES = (1, 4, 16)
BIG = 1.0e6


def emit_B(S, nc, d, QKl, Vl, Ysrc, b_QKg, b_Vg, b_Ys, seq=SEQ, do_mla=True, do_sb=True, do_dil=True):
    NBk = seq // 128
    NG4 = seq // 512
    dposk = d['dposk']; dposq = d['dposq']; nslope = d['nslope']
    maskc_d = d['maskc']; masks_d = d['masks']; maskd_d = d['maskd']; m1_d = d['m1']; m2_d = d['m2']
    b_ymla = b_Ys; b_ysb = b_Ys; b_ydil = b_Ys
    SHR = 1984; SHV = 896
    Q1 = S.sbuf("Q1", [128, seq], BF16); bQ1 = Buf("Q1")
    K1 = S.sbuf("K1", [128, seq], BF16); bK1 = Buf("K1")
    if do_mla:
        Q2 = S.sbuf("Q2", [64, seq], BF16); bQ2 = Buf("Q2")
        K2 = S.sbuf("K2", [64, seq], BF16); bK2 = Buf("K2")
    V1 = S.sbuf("V1", [128, NBk, 128], BF16); bV1 = Buf("V1")
    two = do_mla or do_sb
    if two:
        Q1b = S.sbuf("Q1b", [128, seq], BF16); bQ1b = Buf("Q1b")
        K1b = S.sbuf("K1b", [128, seq], BF16); bK1b = Buf("K1b")
        V1b = S.sbuf("V1b", [128, NBk, 128], BF16); bV1b = Buf("V1b")
        if do_mla:
            Q2b = S.sbuf("Q2b", [64, seq], BF16); bQ2b = Buf("Q2b")
    NP = 8
    PT = [S.sbuf("PT%d" % i, [128, 512], BF16) for i in range(NP)]; bPT = [Buf("PT%d" % i) for i in range(NP)]
    SP = [S.sbuf("SP%d" % i, [128, 512], BF16) for i in range(NP)]; bSP = [Buf("SP%d" % i) for i in range(NP)]
    EN = [S.sbuf("EN%d" % i, [128, 512], F32) for i in range(4)]; bEN = [Buf("EN%d" % i) for i in range(4)]
    SNt = [S.sbuf("SN%d" % i, [128, 512], F32) for i in range(NP)]; bSN = [Buf("SN%d" % i) for i in range(NP)]
    UU = [S.sbuf("UU%d" % i, [128, 512], F32) for i in range(4)]; bUU = [Buf("UU%d" % i) for i in range(4)]
    YS = [S.sbuf("YS%d" % i, [128, 512], BF16) for i in range(2)]; bYS = [Buf("YS%d" % i) for i in range(2)]
    RD = S.sbuf("RD", [128, 512], F32); bRD = Buf("RD")
    maskc = S.sbuf("maskc_t", [128, 4, 512], BF16); bmaskc = Buf("maskc")
    masks = S.sbuf("masks_t", [128, 4, 512], BF16); bmasks = Buf("masks")
    maskd = S.sbuf("maskd_t", [128, 256], F32); bmaskd = Buf("maskd")
    M1 = S.sbuf("M1", [128, 128], BF16); bM1 = Buf("M1")
    M2 = S.sbuf("M2", [128, 128], BF16); bM2 = Buf("M2")
    ones = S.sbuf("ones", [128, 128], BF16); bones = Buf("ones")
    nsl = S.sbuf("nsl", [128, 3], F32); bnsl = Buf("nsl")
    ps = S.psum("ps", [128, 8, 512], F32); bps = [Buf("ps%d" % i) for i in range(8)]

    S.op("dve", lambda e: e.memset(ones[:], 1.0), writes=[bones])
    S.dma("sp", lambda e: e.dma_start(out=maskc[:], in_=maskc_d[:, :, :]), writes=[bmaskc])
    S.dma("sp", lambda e: e.dma_start(out=masks[:], in_=masks_d[:, :, :]), writes=[bmasks])
    S.dma("sp", lambda e: e.dma_start(out=maskd[:], in_=maskd_d[:, :]), writes=[bmaskd])
    S.dma("sp", lambda e: e.dma_start(out=M1[:], in_=m1_d[:, :]), writes=[bM1])
    S.dma("sp", lambda e: e.dma_start(out=M2[:], in_=m2_d[:, :]), writes=[bM2])
    S.dma("sp", lambda e: e.dma_start(out=nsl[:], in_=nslope[:, :]), writes=[bnsl])

    QK5 = QKl.rearrange("c r (p a) t -> c r p (a t)", a=1) if False else QKl
    Vl4 = Vl
    Vl6 = Vl.rearrange("c r (b p) d -> c r b p d", p=128)

    def load_fm(dst, bdst, R0, rows=128):
        dv4 = dst[0:rows, :].rearrange("p (r u t) -> p r u t", r=4, u=2)
        for u in range(2):
            S.dma("sp", lambda e: e.dma_start(out=dv4[:, :, u, :],
                                              in_=QK5[u * 4 + R0 // 512, :, R0 % 512:R0 % 512 + rows, :].rearrange("r p t -> p r t")),
                  reads=[b_QKg], writes=[bdst])

    def load_v(c0, Vt=None, bVt=None):
        Vt = V1 if Vt is None else Vt
        bVt = bV1 if bVt is None else bVt
        for r in range(4):
            for cp in range(4):
                S.dma("sp", lambda e: e.dma_start(out=Vt[:, r * 16 + cp * 4:r * 16 + cp * 4 + 4, :],
                                                  in_=Vl6[cp, r, :, :, c0:c0 + 128].rearrange("b p d -> p b d")),
                      reads=[b_Vg], writes=[bVt])

    def load_v_perm(c0, rt):
        if rt == 1:
            load_v(c0)
        elif rt == 4:
            for s_ in range(4):
                for r in range(4):
                    S.dma("sp", lambda e: e.dma_start(out=V1[:, s_ * 16 + 4 * r:s_ * 16 + 4 * r + 4, :],
                                                      in_=Vl4[:, r, s_:s_ + 4 * 127 + 1:4, c0:c0 + 128].rearrange("c p d -> p c d")),
                          reads=[b_Vg], writes=[bV1])
        else:
            for s_ in range(16):
                for cp in range(4):
                    S.dma("sp", lambda e: e.dma_start(out=V1[32 * cp:32 * cp + 32, s_ * 4:s_ * 4 + 4, :],
                                                      in_=Vl4[cp, :, s_:s_ + 16 * 31 + 1:16, c0:c0 + 128].rearrange("m p d -> p m d")),
                          reads=[b_Vg], writes=[bV1])

    Ys5 = Ysrc

    def yout(blk, g4):
        return Ys5[g4 // 8, blk, :, (g4 % 8) * 512:(g4 % 8) * 512 + 512]

    def pipeline(steps):
        prev = None
        for s1, s2 in steps:
            s1()
            if prev is not None:
                prev()
            prev = s2
        if prev is not None:
            prev()

    cnt = {"z": 0, "o": 0, "pt": 0, "ys": 0, "en": 0, "uu": 0}

    def interleave(a, b):
        out = []
        for x, y in zip(a, b):
            out.append(x); out.append(y)
        return out

    if do_mla:
        load_fm(K2, bK2, 1920, 64)
        hs = [dict(Q=Q1, bQ=bQ1, Qr=Q2, bQr=bQ2, K=K1, bK=bK1, V=V1, bV=bV1, zb=(0, 1), ob=2, db=3),
              dict(Q=Q1b, bQ=bQ1b, Qr=Q2b, bQr=bQ2b, K=K1b, bK=bK1b, V=V1b, bV=bV1b, zb=(6, 7), ob=4, db=5)]
        allsteps = []
        for h in range(2):
            H = hs[h]
            load_fm(H["Q"], H["bQ"], 1280 + h * 128)
            load_fm(H["Qr"], H["bQr"], 1792 + h * 64, 64)
            load_fm(H["K"], H["bK"], 1536 + h * 128)
            load_v(640 + h * 128, H["V"], H["bV"])
            steps = []
            zc = 0
            for g4 in range(NG4):
                qs = slice(g4 * 512, (g4 + 1) * 512)
                ob = H["ob"]; db = H["db"]
                nst = 4 * g4 + 4
                for j in range(nst):
                    zb = H["zb"][zc % 2]; zc += 1
                    ks = slice(j * 128, (j + 1) * 128)
                    box = {}

                    def s1(qs=qs, j=j, zb=zb, ks=ks, g4=g4, H=H, box=box):
                        pi = cnt["pt"] % NP; cnt["pt"] += 1
                        box["pi"] = pi
                        S.op("pe", lambda e: e.matmul(ps[:, zb, :], lhsT=H["K"][:, ks], rhs=H["Q"][:, qs], start=True, stop=False),
                             reads=[H["bK"], H["bQ"]], writes=[bps[zb]])
                        S.op("pe", lambda e: e.matmul(ps[:, zb, :], lhsT=K2[0:64, ks], rhs=H["Qr"][0:64, qs], start=False, stop=True),
                             reads=[bK2, H["bQr"]], writes=[bps[zb]])
                        S.op("act", lambda e: e.activation(out=PT[pi][:], in_=ps[:, zb, :], func=AF.Exp),
                             reads=[bps[zb]], writes=[bPT[pi]])
                        if j >= 4 * g4:
                            S.op("dve", lambda e: e.tensor_tensor(out=PT[pi][:], in0=PT[pi][:], in1=maskc[:, j - 4 * g4, :], op=ALU.mult),
                                 reads=[bPT[pi], bmaskc], writes=[bPT[pi]])

                    def s2(j=j, ob=ob, db=db, nst=nst, h=h, g4=g4, H=H, box=box):
                        pi = box["pi"]
                        S.op("pe", lambda e: e.matmul(ps[:, ob, :], lhsT=H["V"][:, j, :], rhs=PT[pi][:], start=(j == 0), stop=(j == nst - 1)),
                             reads=[H["bV"], bPT[pi]], writes=[bps[ob]])
                        S.op("pe", lambda e: e.matmul(ps[:, db, :], lhsT=ones[:], rhs=PT[pi][:], start=(j == 0), stop=(j == nst - 1)),
                             reads=[bones, bPT[pi]], writes=[bps[db]])
                        if j == nst - 1:
                            yi = cnt["ys"] % 2; cnt["ys"] += 1
                            ri = cnt["uu"] % 4; cnt["uu"] += 1
                            S.op("dve", lambda e: e.reciprocal(out=UU[ri][:], in_=ps[:, db, :]), reads=[bps[db]], writes=[bUU[ri]])
                            S.op("dve", lambda e: e.tensor_tensor(out=YS[yi][:], in0=ps[:, ob, :], in1=UU[ri][:], op=ALU.mult),
                                 reads=[bps[ob], bUU[ri]], writes=[bYS[yi]])
                            S.dma("sp", lambda e: e.dma_start(out=yout(h, g4), in_=YS[yi][:]), reads=[bYS[yi]], writes=[b_ymla])
                    steps.append((s1, s2))
            allsteps.append(steps)
        pipeline(interleave(allsteps[0], allsteps[1]))

    if do_sb:
        hs = [dict(Q=Q1, bQ=bQ1, K=K1, bK=bK1, V=V1, bV=bV1, zb=(0, 1), ob=2, rb=3),
              dict(Q=Q1b, bQ=bQ1b, K=K1b, bK=bK1b, V=V1b, bV=bV1b, zb=(6, 7), ob=4, rb=5)]
        allsteps = []
        for h in range(2):
            H = hs[h]
            load_fm(H["Q"], H["bQ"], 768 + h * 128)
            load_fm(H["K"], H["bK"], 1024 + h * 128)
            load_v(384 + h * 128, H["V"], H["bV"])
            steps = []
            zc = 0
            for g4 in range(NG4):
                qs = slice(g4 * 512, (g4 + 1) * 512)
                ob = H["ob"]; rb = H["rb"]
                nst = 4 * g4 + 4
                for idx, j in enumerate(reversed(range(nst))):
                    first = idx == 0; last = idx == nst - 1
                    zb = H["zb"][zc % 2]; zc += 1
                    ks = slice(j * 128, (j + 1) * 128)
                    box = {}

                    def t1(qs=qs, j=j, zb=zb, ks=ks, g4=g4, H=H, box=box):
                        pi = cnt["pt"] % NP; cnt["pt"] += 1
                        ei = cnt["en"] % 4; cnt["en"] += 1
                        box["pi"] = pi
                        S.op("pe", lambda e: e.matmul(ps[:, zb, :], lhsT=H["K"][:, ks], rhs=H["Q"][:, qs], start=True, stop=True),
                             reads=[H["bK"], H["bQ"]], writes=[bps[zb]])
                        S.op("act", lambda e: e.activation(out=EN[ei][:], in_=ps[:, zb, :], func=AF.Exp, scale=-1.0),
                             reads=[bps[zb]], writes=[bEN[ei]])
                        S.op("act", lambda e: e.activation(out=SNt[pi][:], in_=EN[ei][:], func=AF.Ln, bias=1.0),
                             reads=[bEN[ei]], writes=[bSN[pi]])
                        S.op("dve", lambda e: e.tensor_tensor(out=SP[pi][:], in0=ps[:, zb, :], in1=SNt[pi][:], op=ALU.add),
                             reads=[bps[zb], bSN[pi]], writes=[bSP[pi]])
                        if j >= 4 * g4:
                            S.op("dve", lambda e: e.tensor_tensor(out=SP[pi][:], in0=SP[pi][:], in1=masks[:, j - 4 * g4, :], op=ALU.mult),
                                 reads=[bSP[pi], bmasks], writes=[bSP[pi]])

                    def t2(j=j, rb=rb, first=first, g4=g4, box=box):
                        pi = box["pi"]
                        ui = cnt["uu"] % 4; cnt["uu"] += 1
                        S.op("pe", lambda e: e.matmul(ps[:, rb, :], lhsT=M1[:], rhs=SP[pi][:], start=first, stop=False, skip_group_check=True),
                             reads=[bM1, bSP[pi]], writes=[bps[rb]])
                        S.op("dve", lambda e: e.tensor_tensor(out=UU[ui][:], in0=SNt[pi][:], in1=ps[:, rb, :], op=ALU.add),
                             reads=[bSN[pi], bps[rb]], writes=[bUU[ui]])
                        S.op("act", lambda e: e.activation(out=PT[pi][:], in_=UU[ui][:], func=AF.Exp, scale=-1.0),
                             reads=[bUU[ui]], writes=[bPT[pi]])
                        if j >= 4 * g4:
                            S.op("dve", lambda e: e.tensor_tensor(out=PT[pi][:], in0=PT[pi][:], in1=masks[:, j - 4 * g4, :], op=ALU.mult),
                                 reads=[bPT[pi], bmasks], writes=[bPT[pi]])

                    def t3(rb=rb, last=last, box=box):
                        pi = box["pi"]
                        S.op("pe", lambda e: e.matmul(ps[:, rb, :], lhsT=M2[:], rhs=SP[pi][:], start=False, stop=last, skip_group_check=True),
                             reads=[bM2, bSP[pi]], writes=[bps[rb]])

                    def t4(j=j, ob=ob, first=first, last=last, h=h, g4=g4, H=H, box=box):
                        pi = box["pi"]
                        S.op("pe", lambda e: e.matmul(ps[:, ob, :], lhsT=H["V"][:, j, :], rhs=PT[pi][:], start=first, stop=last),
                             reads=[H["bV"], bPT[pi]], writes=[bps[ob]])
                        if last:
                            yi = cnt["ys"] % 2; cnt["ys"] += 1
                            S.op("act", lambda e: e.activation(out=YS[yi][:], in_=ps[:, ob, :], func=AF.Copy),
                                 reads=[bps[ob]], writes=[bYS[yi]])
                            S.dma("sp", lambda e: e.dma_start(out=yout(3 + h, g4), in_=YS[yi][:]), reads=[bYS[yi]], writes=[b_ysb])
                    steps.append((t1, t2, t3, t4))
            allsteps.append(steps)
        N_ = len(allsteps[0])
        for X in allsteps:
            X[0][0]()
        for k in range(N_):
            for X in allsteps:
                X[k][1]()
            if k > 0:
                for X in allsteps:
                    X[k - 1][3]()
            if k + 1 < N_:
                for X in allsteps:
                    X[k + 1][0]()
            for X in allsteps:
                X[k][2]()
        for X in allsteps:
            X[N_ - 1][3]()

    if do_dil:
        accn = S.sbuf("accn", [128, seq], F32); baccn = Buf("accn")
        accd = S.sbuf("accd", [128, seq], F32); baccd = Buf("accd")
        posk_i = S.sbuf("posk_i", [128, NBk], I32); bposk_i = Buf("posk_i")
        posk = S.sbuf("posk", [128, NBk], F32); bposk = Buf("posk")
        pq_i = [S.sbuf("pq_i%d" % i, [128, 128], I32) for i in range(2)]; bpq_i = [Buf("pq_i0"), Buf("pq_i1")]
        pq = [S.sbuf("pq%d" % i, [128, 128], F32) for i in range(2)]; bpq = [Buf("pq0"), Buf("pq1")]
        dd = [S.sbuf("dd%d" % i, [128, 256], F32) for i in range(2)]; bdd = [Buf("dd0"), Buf("dd1")]
        zt = [S.sbuf("zt%d" % i, [128, 256], F32) for i in range(2)]; bzt = [Buf("zt0"), Buf("zt1")]
        for g in range(3):
            r = RATES[g]
            L = seq // r
            nb = L // 128
            load_fm(Q1, bQ1, g * 128)
            load_fm(K1, bK1, 384 + g * 128)
            load_v_perm(g * 128, r)
            S.dma("sp", lambda e: e.dma_start(out=posk_i[:], in_=dposk[g, :, :]), writes=[bposk_i])
            S.op("dve", lambda e: e.tensor_copy(out=posk[:], in_=posk_i[:]), reads=[bposk_i], writes=[bposk])
            S.op("dve", lambda e: e.tensor_scalar(out=posk[:], in0=posk[:], scalar1=-1.0, scalar2=None, op0=ALU.mult), reads=[bposk], writes=[bposk])
            dsteps = []
            for i in range(NBk):
                s, m = divmod(i, nb)
                hp = m > 0
                c0 = 0 if hp else 128
                zb = cnt["z"] % 2; cnt["z"] += 1
                ob = 2 + (cnt["o"] % 2); db = 4 + (cnt["o"] % 2); cnt["o"] += 1
                pi = cnt["pt"] % NP; cnt["pt"] += 1
                bi = i % 2
                st0 = s + 128 * r * m
                qsl = slice(st0, st0 + 127 * r + 1, r) if r > 1 else slice(st0, st0 + 128)
                psl = (slice(st0 - 128 * r, st0 - 128 * r + 127 * r + 1, r) if r > 1 else slice(st0 - 128, st0)) if hp else None
                def s1(i=i, hp=hp, c0=c0, zb=zb, pi=pi, bi=bi, qsl=qsl, psl=psl, g=g):
                    if hp:
                        S.op("pe", lambda e: e.matmul(ps[:, zb, 0:128], lhsT=K1[:, psl], rhs=Q1[:, qsl], start=True, stop=True),
                             reads=[bK1, bQ1], writes=[bps[zb]])
                    S.op("pe", lambda e: e.matmul(ps[:, zb, 128:256], lhsT=K1[:, qsl], rhs=Q1[:, qsl], start=True, stop=True),
                         reads=[bK1, bQ1], writes=[bps[zb]])
                    S.dma("sp", lambda e: e.dma_start(out=pq_i[bi][:], in_=dposq[g:g + 1, i * 128:(i + 1) * 128].partition_broadcast(128)),
                          writes=[bpq_i[bi]])
                    S.op("dve", lambda e: e.tensor_copy(out=pq[bi][:], in_=pq_i[bi][:]), reads=[bpq_i[bi]], writes=[bpq[bi]])
                    if hp:
                        S.op("act", lambda e: e.activation(out=dd[bi][:, 0:128], in_=pq[bi][:], func=AF.Abs, bias=posk[:, i - 1:i], scale=1.0),
                             reads=[bpq[bi], bposk], writes=[bdd[bi]])
                    S.op("act", lambda e: e.activation(out=dd[bi][:, 128:256], in_=pq[bi][:], func=AF.Abs, bias=posk[:, i:i + 1], scale=1.0),
                         reads=[bpq[bi], bposk], writes=[bdd[bi]])
                    S.op("dve", lambda e: e.tensor_tensor(out=dd[bi][:, c0:256], in0=dd[bi][:, c0:256], in1=maskd[:, c0:256], op=ALU.add),
                         reads=[bdd[bi], bmaskd], writes=[bdd[bi]])
                    S.op("dve", lambda e: e.scalar_tensor_tensor(out=zt[bi][:, c0:256], in0=dd[bi][:, c0:256], scalar=nsl[:, g:g + 1],
                                                                 in1=ps[:, zb, c0:256], op0=ALU.mult, op1=ALU.add),
                         reads=[bdd[bi], bnsl, bps[zb]], writes=[bzt[bi]])
                    S.op("act", lambda e: e.activation(out=PT[pi][:, c0:256], in_=zt[bi][:, c0:256], func=AF.Exp),
                         reads=[bzt[bi]], writes=[bPT[pi]])

                def s2(i=i, hp=hp, ob=ob, db=db, pi=pi, s=s, m=m, r=r, g=g, st0=st0):
                    if hp:
                        S.op("pe", lambda e: e.matmul(ps[:, ob, 0:128], lhsT=V1[:, i - 1, :], rhs=PT[pi][:, 0:128], start=True, stop=False),
                             reads=[bV1, bPT[pi]], writes=[bps[ob]])
                    S.op("pe", lambda e: e.matmul(ps[:, ob, 0:128], lhsT=V1[:, i, :], rhs=PT[pi][:, 128:256], start=(not hp), stop=True),
                         reads=[bV1, bPT[pi]], writes=[bps[ob]])
                    if hp:
                        S.op("pe", lambda e: e.matmul(ps[:, db, 0:128], lhsT=ones[:], rhs=PT[pi][:, 0:128], start=True, stop=False),
                             reads=[bones, bPT[pi]], writes=[bps[db]])
                    S.op("pe", lambda e: e.matmul(ps[:, db, 0:128], lhsT=ones[:], rhs=PT[pi][:, 128:256], start=(not hp), stop=True),
                         reads=[bones, bPT[pi]], writes=[bps[db]])
                    an = accn[:, st0:st0 + 127 * r + 1:r] if r > 1 else accn[:, st0:st0 + 128]
                    ad = accd[:, st0:st0 + 127 * r + 1:r] if r > 1 else accd[:, st0:st0 + 128]
                    if g == 0:
                        S.op("act", lambda e: e.activation(out=an, in_=ps[:, ob, 0:128], func=AF.Copy), reads=[bps[ob]], writes=[baccn])
                        S.op("dve", lambda e: e.tensor_copy(out=ad, in_=ps[:, db, 0:128]), reads=[bps[db]], writes=[baccd])
                    else:
                        S.op("dve", lambda e: e.tensor_tensor(out=an, in0=an, in1=ps[:, ob, 0:128], op=ALU.add), reads=[baccn, bps[ob]], writes=[baccn])
                        S.op("dve", lambda e: e.tensor_tensor(out=ad, in0=ad, in1=ps[:, db, 0:128], op=ALU.add), reads=[baccd, bps[db]], writes=[baccd])
                dsteps.append((s1, s2))
            pipeline(dsteps)
        for c in range(seq // 512):
            cs = slice(c * 512, (c + 1) * 512)
            yi = cnt["ys"] % 2; cnt["ys"] += 1
            S.op("dve", lambda e: e.reciprocal(out=RD[:], in_=accd[:, cs]), reads=[baccd], writes=[bRD])
            S.op("dve", lambda e: e.tensor_tensor(out=YS[yi][:], in0=accn[:, cs], in1=RD[:], op=ALU.mult), reads=[baccn, bRD], writes=[bYS[yi]])
            S.dma("sp", lambda e: e.dma_start(out=yout(2, c), in_=YS[yi][:]), reads=[bYS[yi]], writes=[b_ydil])


def consts_B():
    import ml_dtypes
    k = np.arange(128)[:, None]
    q = np.arange(512)[None, :]
    maskc = np.stack([(128 * i0 + k <= q) for i0 in range(4)], axis=1).astype(np.float32)
    masks = np.stack([(128 * i0 + k < q) for i0 in range(4)], axis=1).astype(np.float32)
    kl = np.arange(128)[:, None]; ql = np.arange(128)[None, :]
    maskd = np.concatenate([np.where(kl >= ql, 0.0, BIG), np.where(kl <= ql, 0.0, BIG)], axis=1).astype(np.float32)
    p = np.arange(128)[:, None]; m = np.arange(128)[None, :]
    m1 = (p > m).astype(np.float32); m2 = (p <= m).astype(np.float32)
    bf = ml_dtypes.bfloat16
    return dict(maskc=maskc.astype(bf), masks=masks.astype(bf), maskd=maskd, m1=m1.astype(bf), m2=m2.astype(bf))


D = 2048
NK = 16
EPS = 1e-6
NE = 16
BIGR = 1.0e4


def emit_C(S, nc, d, l, Yl, gT, xT, x2T, xfT, b_Yg, b_gT, b_xsrc, b_x2T, b_xfT, ntok=2048):
    NT = ntok // 512
    cT = d['cT']; wada = d['wada'][l]; bada = d['bada_c'][l]; gmoe = d['gmoe'][l]; gfin = d['gfin']
    womla = d['womla'][l]; wodil = d['wodil'][l]; wosb = d['wosb'][l]; wout = d['wout'][l]
    wr = d['wr']; br = d['br']; wg = d['wg'][l]; wu = d['wu'][l]; wd = d['wd'][l]; ident = d['ident']
    DBG = False
    xt = S.sbuf("xt", [128, NK, 512], F32); b_xt = [Buf("xt%d" % k) for k in range(NK)]
    scr = S.sbuf("scr", [128, 20, 512], BF16); b_scr = Buf("scr")
    mg = S.sbuf("mg", [128, NK, 512], BF16); b_mg = Buf("mg")
    a2b = S.sbuf("a2b", [128, NK, 512], BF16); b_a2b = Buf("a2b")
    NW = 5
    W = [S.sbuf("W%d" % i, [128, NK, 512], BF16) for i in range(NW)]; b_W = [Buf("W%d" % i) for i in range(NW)]
    gt = [S.sbuf("gt%d" % i, [128, 3, 512], F32) for i in range(2)]; b_gt = [Buf("gt0"), Buf("gt1")]
    tmp = [S.sbuf("tmp%d" % i, [128, 512], F32) for i in range(4)]; b_tmp = [Buf("tmp%d" % i) for i in range(4)]
    hp = [S.sbuf("hp%d" % i, [128, 4, 512], BF16) for i in range(2)]; b_hp = [Buf("hp0"), Buf("hp1")]
    rstd = S.sbuf("rstd", [128, 512], F32); b_rstd = Buf("rstd")
    lnt = S.sbuf("lnt", [128, 512], F32); b_lnt = Buf("lnt")
    ones = S.sbuf("ones", [128, 128], BF16); b_ones = Buf("ones")
    onesf = S.sbuf("onesf", [128, 128], F32); b_onesf = Buf("onesf")
    idt = S.sbuf("idt", [128, 128], F32); b_idt = Buf("idt")
    Gm = [S.sbuf("Gm%d" % i, [128, 128], F32) for i in range(2)]; b_Gm = [Buf("Gm0"), Buf("Gm1")]
    cin = S.sbuf("cin", [128, NK], F32); b_cin = Buf("cin")
    cact = S.sbuf("cact", [128, NK], BF16); b_cact = Buf("cact")
    badat = S.sbuf("badat", [128, 64], F32); b_badat = Buf("badat")
    gmoet = S.sbuf("gmoet", [128, NK], F32); b_gmoet = Buf("gmoet")
    gfint = S.sbuf("gfint", [128, NK], F32); b_gfint = Buf("gfint")
    modt = S.sbuf("modt", [128, 64], F32); b_modt = Buf("modt")
    s1m = S.sbuf("s1m", [128, NK], F32); b_s1m = Buf("s1m")
    wrt = S.sbuf("wrt", [128, NK, NE], F32); b_wrt = Buf("wrt")
    brt = S.sbuf("brt", [128, NE], F32); b_brt = Buf("brt")
    lt = S.sbuf("lt", [16, 512], F32); b_lt = Buf("lt")
    sc = S.sbuf("sc", [128, 64], F32); b_sc = Buf("sc")
    bi = S.sbuf("bi", [128, 64], F32); b_bi = Buf("bi")
    bi2 = S.sbuf("bi2", [128, 64], F32); b_bi2 = Buf("bi2")
    eq = S.sbuf("eq", [128, 64], F32); b_eq = Buf("eq")
    m1 = S.sbuf("m1", [128, 16], F32); b_m1 = Buf("m1")
    m2 = S.sbuf("m2", [128, 16], F32); b_m2 = Buf("m2")
    gs = S.sbuf("gs", [128, 16], F32); b_gs = Buf("gs")
    gmax = S.sbuf("gmax", [128, 4], F32); b_gmax = Buf("gmax")
    gsel = S.sbuf("gsel", [128, 16], F32); b_gsel = Buf("gsel")
    wsel = S.sbuf("wsel", [128, 64], F32); b_wsel = Buf("wsel")
    den = S.sbuf("den", [128, 4], F32); b_den = Buf("den")
    gates = S.sbuf("gates", [128, 64], F32); b_gates = Buf("gates")
    ps = S.psum("ps", [128, 8, 512], F32); b_ps = [Buf("ps%d" % i) for i in range(8)]
    st = {"bank": 0, "w": 0, "gt": 0, "tmp": 0, "hp": 0, "gm": 0}

    def nbank():
        i = st["bank"]; st["bank"] = (i + 1) % 6
        return i

    def nw():
        i = st["w"]; st["w"] = (i + 1) % NW
        return i

    def ntmp():
        i = st["tmp"]; st["tmp"] = (i + 1) % 4
        return i

    def load_w(wi, src2d, nk, c0, ncols, k_off=0):
        srcv = src2d.rearrange("(k p) c -> p k c", p=128)
        h = max(1, nk // 2)
        for k0 in range(0, nk, h):
            S.dma("pool", lambda e: e.dma_start(out=W[wi][:, k_off + k0:k_off + k0 + h, 0:ncols],
                                                in_=srcv[:, k0:k0 + h, c0:c0 + ncols]), writes=[b_W[wi]])

    S.op("dve", lambda e: e.memset(ones[:], 1.0), writes=[b_ones])
    S.op("dve", lambda e: e.memset(onesf[:], 1.0), writes=[b_onesf])
    for dst, bd, src in ((cin, b_cin, cT), (badat, b_badat, bada), (gmoet, b_gmoet, gmoe), (gfint, b_gfint, gfin),
                         (brt, b_brt, br), (idt, b_idt, ident)):
        S.dma("sp", lambda e: e.dma_start(out=dst[:], in_=src[:, :]), writes=[bd])
    S.dma("sp", lambda e: e.dma_start(out=wrt[:], in_=wr.rearrange("(k p) e -> p k e", p=128)), writes=[b_wrt])
    S.op("act", lambda e: e.activation(out=cact[:], in_=cin[:], func=AF.Silu), reads=[b_cin], writes=[b_cact])

    mb = nbank()
    for g in range(16):
        wi = nw()
        load_w(wi, wada, NK, 4096 + g * 512, 512)
        for m in range(4):
            n = g * 4 + m
            for k in range(NK):
                S.op("pe", lambda e: e.matmul(ps[:, mb, n:n + 1], lhsT=W[wi][:, k, m * 128:(m + 1) * 128], rhs=cact[:, k:k + 1],
                                              start=(k == 0), stop=(k == NK - 1)), reads=[b_W[wi], b_cact], writes=[b_ps[mb]])
    S.op("dve", lambda e: e.tensor_tensor(out=modt[:], in0=ps[:, mb, 0:64], in1=badat[:], op=ALU.add),
         reads=[b_ps[mb], b_badat], writes=[b_modt])
    S.op("dve", lambda e: e.scalar_tensor_tensor(out=s1m[:], in0=modt[:, 32:48], scalar=1.0, in1=gmoet[:], op0=ALU.add, op1=ALU.mult),
         reads=[b_modt, b_gmoet], writes=[b_s1m])

    def rms_rstd(src3, bsrcs):
        S.op("act", lambda e: e.activation(out=scr[:, 0:NK, :], in_=src3, func=AF.Square), reads=bsrcs, writes=[b_scr])
        bk = nbank()
        for k in range(NK):
            S.op("pe", lambda e: e.matmul(ps[:, bk, :], lhsT=ones[:], rhs=scr[:, k, :], start=(k == 0), stop=(k == NK - 1)),
                 reads=[b_scr, b_ones], writes=[b_ps[bk]])
        S.op("act", lambda e: e.activation(out=lnt[:], in_=ps[:, bk, :], func=AF.Ln, scale=1.0 / D, bias=EPS),
             reads=[b_ps[bk]], writes=[b_lnt])
        S.op("act", lambda e: e.activation(out=rstd[:], in_=lnt[:], func=AF.Exp, scale=-0.5), reads=[b_lnt], writes=[b_rstd])

    xTv = xT.rearrange("(k p) t -> p k t", p=128)
    gTv = gT.rearrange("(b n p) t -> p b n t", b=3, p=128)
    x2v = x2T.rearrange("(k p) t -> p k t", p=128) if x2T is not None else None
    xfv = xfT.rearrange("(k p) t -> p k t", p=128) if xfT is not None else None

    for t in range(NT):
        ts = slice(t * 512, (t + 1) * 512)
        for k0 in range(0, NK, 4):
            S.dma("sp", lambda e: e.dma_start(out=xt[:, k0:k0 + 4, :], in_=xTv[:, k0:k0 + 4, ts]), reads=[b_xsrc], writes=b_xt[k0:k0 + 4])
        for i_ in range(2):
            S.dma("sp", lambda e: e.dma_start(out=scr[:, i_:8:2, :], in_=Yl[i_, :, :, ts].rearrange("j p t -> p j t")), reads=[b_Yg], writes=[b_scr])
            S.dma("sp", lambda e: e.dma_start(out=scr[:, 12 + i_:20:2, :], in_=Yl[3 + i_, :, :, ts].rearrange("j p t -> p j t")), reads=[b_Yg], writes=[b_scr])
        S.dma("sp", lambda e: e.dma_start(out=scr[:, 8:12, :], in_=Yl[2, :, :, ts].rearrange("j p t -> p j t")), reads=[b_Yg], writes=[b_scr])
        for ng in range(4):
            wa = nw(); load_w(wa, womla, 8, ng * 512, 512); load_w(wa, wodil, 4, ng * 512, 512, k_off=8)
            wb_ = nw(); load_w(wb_, wosb, 8, ng * 512, 512)
            for m in range(4):
                n = ng * 4 + m
                gi = st["gt"]; st["gt"] ^= 1
                S.dma("sp", lambda e: e.dma_start(out=gt[gi][:], in_=gTv[:, :, n, ts]), reads=[b_gT], writes=[b_gt[gi]])
                banks = []
                for brn, (wi, koff, nk, yoff) in enumerate(((wa, 0, 8, 0), (wa, 8, 4, 8), (wb_, 0, 8, 12))):
                    bk = nbank(); banks.append(bk)
                    for k in range(nk):
                        S.op("pe", lambda e: e.matmul(ps[:, bk, :], lhsT=W[wi][:, koff + k, m * 128:(m + 1) * 128], rhs=scr[:, yoff + k, :],
                                                      start=(k == 0), stop=(k == nk - 1)), reads=[b_W[wi], b_scr], writes=[b_ps[bk]])
                ta = ntmp(); tb = ntmp()
                S.op("dve", lambda e: e.tensor_tensor(out=tmp[ta][:], in0=ps[:, banks[0], :], in1=gt[gi][:, 0, :], op=ALU.mult),
                     reads=[b_ps[banks[0]], b_gt[gi]], writes=[b_tmp[ta]])
                S.op("dve", lambda e: e.tensor_tensor(out=tmp[tb][:], in0=ps[:, banks[1], :], in1=gt[gi][:, 1, :], op=ALU.mult),
                     reads=[b_ps[banks[1]], b_gt[gi]], writes=[b_tmp[tb]])
                S.op("pool", lambda e: e.tensor_tensor(out=tmp[ta][:], in0=tmp[ta][:], in1=tmp[tb][:], op=ALU.add),
                     reads=[b_tmp[ta], b_tmp[tb]], writes=[b_tmp[ta]])
                S.op("dve", lambda e: e.tensor_tensor(out=tmp[tb][:], in0=ps[:, banks[2], :], in1=gt[gi][:, 2, :], op=ALU.mult),
                     reads=[b_ps[banks[2]], b_gt[gi]], writes=[b_tmp[tb]])
                S.op("pool", lambda e: e.tensor_tensor(out=mg[:, n, :], in0=tmp[ta][:], in1=tmp[tb][:], op=ALU.add),
                     reads=[b_tmp[ta], b_tmp[tb]], writes=[b_mg])
        for ng in range(4):
            wi = nw(); load_w(wi, wout, NK, ng * 512, 512)
            for m in range(4):
                n = ng * 4 + m
                bk = nbank()
                for k in range(NK):
                    S.op("pe", lambda e: e.matmul(ps[:, bk, :], lhsT=W[wi][:, k, m * 128:(m + 1) * 128], rhs=mg[:, k, :],
                                                  start=(k == 0), stop=(k == NK - 1)), reads=[b_W[wi], b_mg], writes=[b_ps[bk]])
                S.op("dve", lambda e: e.scalar_tensor_tensor(out=xt[:, n, :], in0=ps[:, bk, :], scalar=modt[:, n:n + 1], in1=xt[:, n, :],
                                                             op0=ALU.mult, op1=ALU.add), reads=[b_ps[bk], b_modt, b_xt[n]], writes=[b_xt[n]])
        if DBG:
            x1v = x1T.rearrange("(k p) t -> p k t", p=128)
            for k0 in range(0, NK, 4):
                S.dma("sp", lambda e: e.dma_start(out=x1v[:, k0:k0 + 4, ts], in_=xt[:, k0:k0 + 4, :]), reads=b_xt[k0:k0 + 4], writes=[b_x1T])
        rms_rstd(xt[:, :, :], b_xt)
        lb = nbank()
        for k in range(NK):
            ta = ntmp(); tb = ntmp()
            S.op("dve", lambda e: e.tensor_tensor(out=tmp[ta][:], in0=xt[:, k, :], in1=rstd[:], op=ALU.mult),
                 reads=[b_xt[k], b_rstd], writes=[b_tmp[ta]])
            S.op("act", lambda e: e.activation(out=tmp[tb][:], in_=tmp[ta][:], func=AF.Identity, scale=s1m[:, k:k + 1], bias=modt[:, 16 + k:17 + k]),
                 reads=[b_tmp[ta], b_s1m, b_modt], writes=[b_tmp[tb]])
            S.op("act", lambda e: e.activation(out=a2b[:, k, :], in_=tmp[ta][:], func=AF.Identity, scale=s1m[:, k:k + 1], bias=modt[:, 16 + k:17 + k]),
                 reads=[b_tmp[ta], b_s1m, b_modt], writes=[b_a2b])
            S.op("pe", lambda e: e.matmul(ps[0:16, lb, :], lhsT=wrt[:, k, :], rhs=tmp[tb][:], start=(k == 0), stop=(k == NK - 1)),
                 reads=[b_wrt, b_tmp[tb]], writes=[b_ps[lb]])
        S.op("act", lambda e: e.activation(out=lt[:], in_=ps[0:16, lb, :], func=AF.Copy), reads=[b_ps[lb]], writes=[b_lt])
        tb_ = nbank()
        for sub in range(4):
            S.op("pe", lambda e: e.matmul(ps[:, tb_, sub * 16:(sub + 1) * 16], lhsT=lt[0:16, sub * 128:(sub + 1) * 128], rhs=idt[0:16, 0:16],
                                          start=True, stop=True), reads=[b_lt, b_idt], writes=[b_ps[tb_]])
        S.op("act", lambda e: e.activation(out=sc[:], in_=ps[:, tb_, 0:64], func=AF.Sigmoid), reads=[b_ps[tb_]], writes=[b_sc])
        for sub in range(4):
            S.op("dve", lambda e: e.tensor_tensor(out=bi[:, sub * 16:(sub + 1) * 16], in0=sc[:, sub * 16:(sub + 1) * 16], in1=brt[:], op=ALU.add),
                 reads=[b_sc, b_brt], writes=[b_bi])
        v3 = lambda tl: tl[:, :].rearrange("p (g e) -> p g e", e=4)
        S.op("dve", lambda e: e.tensor_reduce(out=m1[:], in_=v3(bi), axis=AX.X, op=ALU.max), reads=[b_bi], writes=[b_m1])
        for ee in range(4):
            S.op("dve", lambda e: e.tensor_tensor(out=v3(eq)[:, :, ee], in0=v3(bi)[:, :, ee], in1=m1[:], op=ALU.is_equal),
                 reads=[b_bi, b_m1], writes=[b_eq])
        S.op("dve", lambda e: e.scalar_tensor_tensor(out=bi2[:], in0=eq[:], scalar=-BIGR, in1=bi[:], op0=ALU.mult, op1=ALU.add),
             reads=[b_eq, b_bi], writes=[b_bi2])
        S.op("dve", lambda e: e.tensor_reduce(out=m2[:], in_=v3(bi2), axis=AX.X, op=ALU.max), reads=[b_bi2], writes=[b_m2])
        S.op("dve", lambda e: e.tensor_tensor(out=gs[:], in0=m1[:], in1=m2[:], op=ALU.add), reads=[b_m1, b_m2], writes=[b_gs])
        S.op("dve", lambda e: e.tensor_reduce(out=gmax[:], in_=gs[:, :].rearrange("p (s g) -> p s g", g=4), axis=AX.X, op=ALU.max),
             reads=[b_gs], writes=[b_gmax])
        for g in range(4):
            S.op("dve", lambda e: e.tensor_tensor(out=gsel[:, :].rearrange("p (s g) -> p s g", g=4)[:, :, g],
                                                  in0=gs[:, :].rearrange("p (s g) -> p s g", g=4)[:, :, g], in1=gmax[:], op=ALU.is_equal),
                 reads=[b_gs, b_gmax], writes=[b_gsel])
        for ee in range(4):
            S.op("dve", lambda e: e.tensor_tensor(out=v3(eq)[:, :, ee], in0=v3(bi)[:, :, ee], in1=m2[:], op=ALU.is_ge),
                 reads=[b_bi, b_m2], writes=[b_eq])
            S.op("dve", lambda e: e.tensor_tensor(out=v3(eq)[:, :, ee], in0=v3(eq)[:, :, ee], in1=gsel[:], op=ALU.mult),
                 reads=[b_eq, b_gsel], writes=[b_eq])
        S.op("dve", lambda e: e.tensor_tensor(out=wsel[:], in0=sc[:], in1=eq[:], op=ALU.mult), reads=[b_sc, b_eq], writes=[b_wsel])
        S.op("dve", lambda e: e.tensor_reduce(out=den[:], in_=wsel[:, :].rearrange("p (s e) -> p s e", e=16), axis=AX.X, op=ALU.add),
             reads=[b_wsel], writes=[b_den])
        S.op("dve", lambda e: e.reciprocal(out=den[:], in_=den[:]), reads=[b_den], writes=[b_den])
        for sub in range(4):
            S.op("dve", lambda e: e.tensor_scalar(out=gates[:, sub * 16:(sub + 1) * 16], in0=wsel[:, sub * 16:(sub + 1) * 16],
                                                  scalar1=den[:, sub:sub + 1], scalar2=None, op0=ALU.mult), reads=[b_wsel, b_den], writes=[b_gates])
        if DBG and t == 0:
            for i_, (tl, bb, w_) in enumerate(((sc, b_sc, 64), (bi, b_bi, 64), (m1, b_m1, 16), (m2, b_m2, 16), (gsel, b_gsel, 16), (eq, b_eq, 64), (gates, b_gates, 64), (den, b_den, 4))):
                S.dma("sp", lambda e: e.dma_start(out=dbg[:, i_, 0:w_], in_=tl[:, 0:w_]), reads=[bb], writes=[b_dbg])
            a2v = a2T.rearrange("(k p) t -> p k t", p=128)
            S.dma("sp", lambda e: e.dma_start(out=a2v[:, :, ts], in_=a2b[:, :, :]), reads=[b_a2b], writes=[b_a2T])
        for ex in range(NE):
            w1 = nw(); load_w(w1, wg[ex], NK, 0, 512)
            w2 = nw(); load_w(w2, wu[ex], NK, 0, 512)
            w3 = nw()
            wdv = wd[ex].rearrange("(k p) c -> p k c", p=128)
            W3v = W[w3][:, :, :].rearrange("p (k a) c -> p k (a c)", a=4)
            for k0 in range(0, 4, 2):
                S.dma("pool", lambda e: e.dma_start(out=W3v[:, k0:k0 + 2, :], in_=wdv[:, k0:k0 + 2, :]), writes=[b_W[w3]])
            gb = 6 + (ex % 2)
            for sub in range(4):
                gi = st["gm"]; st["gm"] ^= 1
                S.op("dve", lambda e: e.tensor_scalar(out=Gm[gi][:], in0=onesf[:], scalar1=gates[:, sub * 16 + ex:sub * 16 + ex + 1], scalar2=None,
                                                      op0=ALU.mult), reads=[b_onesf, b_gates], writes=[b_Gm[gi]])
                S.op("pe", lambda e: e.matmul(ps[:, gb, sub * 128:(sub + 1) * 128], lhsT=Gm[gi][:], rhs=idt[:], start=True, stop=True),
                     reads=[b_Gm[gi], b_idt], writes=[b_ps[gb]])
            hi = st["hp"]; st["hp"] ^= 1
            for dc in range(4):
                b1 = nbank()
                for k in range(NK):
                    S.op("pe", lambda e: e.matmul(ps[:, b1, :], lhsT=W[w1][:, k, dc * 128:(dc + 1) * 128], rhs=a2b[:, k, :],
                                                  start=(k == 0), stop=(k == NK - 1)), reads=[b_W[w1], b_a2b], writes=[b_ps[b1]])
                b2 = nbank()
                for k in range(NK):
                    S.op("pe", lambda e: e.matmul(ps[:, b2, :], lhsT=W[w2][:, k, dc * 128:(dc + 1) * 128], rhs=a2b[:, k, :],
                                                  start=(k == 0), stop=(k == NK - 1)), reads=[b_W[w2], b_a2b], writes=[b_ps[b2]])
                ta = ntmp(); tb = ntmp()
                S.op("act", lambda e: e.activation(out=tmp[ta][:], in_=ps[:, b1, :], func=AF.Silu), reads=[b_ps[b1]], writes=[b_tmp[ta]])
                S.op("dve", lambda e: e.tensor_tensor(out=tmp[tb][:], in0=tmp[ta][:], in1=ps[:, b2, :], op=ALU.mult),
                     reads=[b_tmp[ta], b_ps[b2]], writes=[b_tmp[tb]])
                S.op("dve", lambda e: e.tensor_tensor(out=hp[hi][:, dc, :], in0=tmp[tb][:], in1=ps[:, gb, :], op=ALU.mult),
                     reads=[b_tmp[tb], b_ps[gb]], writes=[b_hp[hi]])
            for n in range(NK):
                bk = nbank()
                for dc in range(4):
                    S.op("pe", lambda e: e.matmul(ps[:, bk, :], lhsT=W3v[:, dc, n * 128:(n + 1) * 128], rhs=hp[hi][:, dc, :],
                                                  start=(dc == 0), stop=(dc == 3)), reads=[b_W[w3], b_hp[hi]], writes=[b_ps[bk]])
                S.op("dve", lambda e: e.scalar_tensor_tensor(out=xt[:, n, :], in0=ps[:, bk, :], scalar=modt[:, 48 + n:49 + n], in1=xt[:, n, :],
                                                             op0=ALU.mult, op1=ALU.add), reads=[b_ps[bk], b_modt, b_xt[n]], writes=[b_xt[n]])
        if x2v is not None:
            for k0 in range(0, NK, 4):
                S.dma("sp", lambda e: e.dma_start(out=x2v[:, k0:k0 + 4, ts], in_=xt[:, k0:k0 + 4, :]), reads=b_xt[k0:k0 + 4], writes=[b_x2T])
        if xfv is not None:
            rms_rstd(xt[:, :, :], b_xt)
            for k in range(NK):
                ta = ntmp()
                S.op("dve", lambda e: e.scalar_tensor_tensor(out=tmp[ta][:], in0=xt[:, k, :], scalar=gfint[:, k:k + 1], in1=rstd[:],
                                                             op0=ALU.mult, op1=ALU.mult), reads=[b_xt[k], b_gfint, b_rstd], writes=[b_tmp[ta]])
                S.dma("sp", lambda e: e.dma_start(out=xfv[:, k, ts], in_=tmp[ta][:]), reads=[b_tmp[ta]], writes=[b_xfT])


def build_fused():
    nc = bass.Bass("TRN2", target_bir_lowering=False)
    ext = lambda n, s, dt: nc.dram_tensor(n, s, dt, kind="ExternalInput").ap()
    d = {}
    d['xT'] = ext("xT", [2048, 2048], F32)
    jt = ext("jt", [1, 2], I32)
    d['cT'] = ext("cT", [128, 16], F32)
    d['pos'] = ext("pos", [1, 2048], I32)
    d['invf'] = ext("invf", [64, 1], F32)
    d['sgn'] = ext("sgn", [64, 1], F32)
    d['wada'] = ext("wada", [2, 2048, 12288], F32)
    d['bada_a'] = ext("bada_a", [2, 128, 32], F32)
    d['bada_c'] = ext("bada_c", [2, 128, 64], F32)
    d['gmix'] = ext("gmix", [2, 128, 16], F32)
    d['gq'] = ext("gq", [2, 128, 4], F32)
    d['gkv'] = ext("gkv", [2, 128, 4], F32)
    d['gmoe'] = ext("gmoe", [2, 128, 16], F32)
    d['gfin'] = ext("gfin", [128, 16], F32)
    d['win'] = ext("win", [2, 2048, 14912], F32)
    d['wsw'] = ext("wsw", [2, 2048, 64], F32)
    d['wuq'] = ext("wuq", [2, 512, 1536], F32)
    d['wuqs'] = ext("wuqs", [2, 512, 512], F32)
    d['wukv'] = ext("wukv", [2, 512, 2048], F32)
    d['womla'] = ext("womla", [2, 1024, 2048], F32)
    d['wodil'] = ext("wodil", [2, 512, 2048], F32)
    d['wosb'] = ext("wosb", [2, 1024, 2048], F32)
    d['wout'] = ext("wout", [2, 2048, 2048], F32)
    d['wr'] = ext("wr", [2048, 16], F32)
    d['br'] = ext("br", [128, 16], F32)
    d['wg'] = ext("wg", [2, 16, 2048, 512], F32)
    d['wu'] = ext("wu", [2, 16, 2048, 512], F32)
    d['wd'] = ext("wd", [2, 16, 512, 2048], F32)
    d['ident'] = ext("ident", [128, 128], F32)
    d['maskc'] = ext("maskc", [128, 4, 512], BF16)
    d['masks'] = ext("masks", [128, 4, 512], BF16)
    d['maskd'] = ext("maskd", [128, 256], F32)
    d['m1'] = ext("m1", [128, 128], BF16)
    d['m2'] = ext("m2", [128, 128], BF16)
    d['nslope'] = ext("nslope", [128, 3], F32)
    d['dposk'] = ext("dposk", [3, 128, 64], I32)
    d['dposq'] = ext("dposq", [3, 8192], I32)
    xfT = nc.dram_tensor("xfT", [2048, 2048], F32, kind="ExternalOutput").ap()
    b_xfT = Buf("xfT")
    x2s = nc.dram_tensor("x2scr", [2048, 2048], F32).ap()
    b_x2s = Buf("x2s")
    b_x0 = Buf("x0")
    GROUPS = [[0, 1, 2, 3], [4, 5, 6, 7]]

    S = Sched(nc)
    S.enable_dyn(jt[:, :])
    CH = 128 * 4096
    QKs_h = nc.dram_tensor("QKs", [32, 128, 4096], BF16)
    Vs_h = nc.dram_tensor("Vs", [16, 128, 4096], BF16)
    QKg_h = nc.dram_tensor("QKg", [32, 512, 4096], BF16)
    Vg_h = nc.dram_tensor("Vg", [16, 512, 4096], BF16)
    Ys_h = nc.dram_tensor("Ys", [10, 128, 4096], BF16)
    Yg_h = nc.dram_tensor("Yg", [10, 512, 4096], BF16)
    gsc = nc.dram_tensor("gscr", [6144, 2048], F32).ap()
    QKl_h = nc.dram_tensor("QKl", [4096, 4096], BF16)
    Vl_h = nc.dram_tensor("Vl", [2048, 4096], BF16)
    Yl_h = nc.dram_tensor("Yl", [2560, 2048], BF16)
    for l in range(2):
        b_QKs, b_Vs, b_QKg, b_Vg, b_Ys, b_Yg, b_g = (Buf(n) for n in ("QKs", "Vs", "QKg", "Vg", "Ys", "Yg", "g"))
        b_QKl, b_Vl, b_Yl = Buf("QKl"), Buf("Vl"), Buf("Yl")
        xsrc = d['xT'] if l == 0 else x2s
        b_xsrc = b_x0 if l == 0 else b_x2s
        QKs_v = QKs_h.ap().rearrange("c p (a t) -> (c p a) t", a=4).rearrange("(s u r) t -> s u r t", s=4, u=2)
        Vs_v = Vs_h.ap().rearrange("c p (a d) -> (c p a) d", a=4).rearrange("(s t) d -> s t d", s=4)
        S.begin_phase()
        b_QKs2 = [Buf("QKs0"), Buf("QKs1")]
        b_Vs2 = [Buf("Vs0"), Buf("Vs1")]

        pending = []
        rate = [1]

        def after_sup(sup):
            for sh in range(4):
                for q in range(4):
                    pending.append((QKs_h, QKg_h, sh * 8 + sup * 4 + q, b_QKs2[sup], b_QKg))
            for sh in range(4):
                for q in range(2):
                    pending.append((Vs_h, Vg_h, sh * 4 + sup * 2 + q, b_Vs2[sup], b_Vg))
            if sup == 1:
                rate[0] = 2

        def tick():
            for _ in range(rate[0]):
                if not pending:
                    break
                sh_, gh_, c, br_, bw_ = pending.pop(0)
                S.cc(lambda e: e.collective_compute("AllGather", ALU.bypass, replica_groups=GROUPS, ins=[sh_.ap()[c].opt()],
                                                    outs=[gh_.ap()[c].opt()]), reads=[br_], writes=[bw_])
        emit_A(S, nc, d, l, xsrc, QKs_v, Vs_v, gsc, b_QKs2, b_Vs2, b_g, after_sup=after_sup, tick=tick)
        while pending:
            tick()
        S.end_phase()
        S.begin_phase()
        for hh in range(2):
            S.dma_dyn(QKl_h.ap()[hh * 2048:(hh + 1) * 2048, :], QKg_h, 8 * 4 * CH, hh * 2048 * 4096, [[4096, 2048], [1, 4096]],
                      reads=[b_QKg], writes=[b_QKl])
        S.dma_dyn(Vl_h.ap()[:, :], Vg_h, 4 * 4 * CH, 0, [[4096, 2048], [1, 4096]], reads=[b_Vg], writes=[b_Vl])
        QKl = QKl_h.ap().rearrange("(c r p) (a t) -> c r (p a) t", c=8, r=4, a=4)
        Vl = Vl_h.ap().rearrange("(c r p) (a d) -> c r (p a) d", c=4, r=4, a=4)
        Ys_v = Ys_h.ap().rearrange("(th b) p t -> th b p t", th=2)
        S.barrier()
        emit_B(S, nc, d, QKl, Vl, Ys_v, b_QKl, b_Vl, b_Ys, do_mla=True, do_sb=False, do_dil=False)
        S.end_phase()
        S.begin_phase()
        emit_B(S, nc, d, QKl, Vl, Ys_v, b_QKl, b_Vl, b_Ys, do_mla=False, do_sb=True, do_dil=False)
        for c in (0, 1, 3, 4, 5, 6, 8, 9):
            S.cc(lambda e: e.collective_compute("AllGather", ALU.bypass, replica_groups=GROUPS, ins=[Ys_h.ap()[c].opt()], outs=[Yg_h.ap()[c].opt()]),
                 reads=[b_Ys], writes=[b_Yg])
        S.end_phase(wait_cc=False)
        S.begin_phase()
        emit_B(S, nc, d, QKl, Vl, Ys_v, b_QKl, b_Vl, b_Ys, do_mla=False, do_sb=False, do_dil=True)
        for c in (2, 7):
            S.cc(lambda e: e.collective_compute("AllGather", ALU.bypass, replica_groups=GROUPS, ins=[Ys_h.ap()[c].opt()], outs=[Yg_h.ap()[c].opt()]),
                 reads=[b_Ys], writes=[b_Yg])
        S.end_phase()
        S.begin_phase()
        for b0, nb_ in ((0, 3), (3, 2)):
            S.dma_dyn(Yl_h.ap()[b0 * 512:(b0 + nb_) * 512, :], Yg_h, 1, b0 * 4 * CH, [[4 * CH, nb_], [4096, 512], [1, 2048]],
                      reads=[b_Yg], writes=[b_Yl], which=1)
        Yl = Yl_h.ap().rearrange("(b j p) t -> b j p t", b=5, j=4)
        emit_C(S, nc, d, l, Yl, gsc, xsrc, x2s if l == 0 else None, xfT if l == 1 else None,
               b_Yl, b_g, b_xsrc, b_x2s, b_xfT)
        S.end_phase()
    S.close()
    return nc


_PROG = {}


def _f(a):
    return np.ascontiguousarray(a)


def kernel(**inputs):
    inp = {k: np.asarray(v) for k, v in inputs.items()}
    B_, S_ = inp['x'].shape[:2]
    cores = list(range(8))
    pos = inp['positions'].astype(np.int32)
    perm = [np.concatenate([np.arange(s, S_, r) for s in range(r)]) for r in RATES]
    slopes = (np.float32(2.0) ** (np.float32(-8.0) * np.arange(1, 13, dtype=np.float32) / np.float32(12))).reshape(3, 4)
    half = 32
    invf = (np.float32(10000.0) ** (-(np.arange(half, dtype=np.float32)) / np.float32(half))).astype(np.float32)
    wu_ = inp['w_uq']
    wuqs = np.stack([np.concatenate([np.concatenate([wu_[l][:, h * 192 + 160:h * 192 + 192], wu_[l][:, h * 192 + 128:h * 192 + 160]], axis=1)
                                     for h in range(8)], axis=1) for l in range(2)])
    wi_ = inp['w_in']
    lay = lambda a, n: _f(a.reshape(a.shape[0], n, 128).transpose(0, 2, 1))
    shared = {
        'invf': _f(np.concatenate([invf, invf])[:, None]),
        'sgn': _f(np.concatenate([-np.ones(32, np.float32), np.ones(32, np.float32)])[:, None]),
        'wada': _f(inp['w_ada']),
        'bada_a': lay(inp['b_ada'][:, :4096], 32),
        'bada_c': lay(inp['b_ada'][:, 4096:], 64),
        'gmix': lay(inp['g_mix'], 16), 'gq': lay(inp['g_q'], 4), 'gkv': lay(inp['g_kv'], 4), 'gmoe': lay(inp['g_moe'], 16),
        'gfin': _f(inp['g_final'].reshape(16, 128).T),
        'win': _f(wi_), 'wsw': _f(np.concatenate([wi_[:, :, 1056:1088], wi_[:, :, 1024:1056]], axis=2)),
        'wuq': _f(wu_), 'wuqs': _f(wuqs), 'wukv': _f(inp['w_ukv']),
        'womla': _f(inp['w_o_mla']), 'wodil': _f(inp['w_o_dil']), 'wosb': _f(inp['w_o_sb']), 'wout': _f(inp['w_out']),
        'wr': _f(inp['w_router']), 'br': _f(np.broadcast_to(inp['b_router'][None, :], (128, 16))),
        'wg': _f(inp['w_gate']), 'wu': _f(inp['w_up']), 'wd': _f(inp['w_down']), 'ident': np.eye(128, dtype=np.float32),
    }
    shared.update(consts_B())
    maps = []
    for c in cores:
        b, j = c // 4, c % 4
        m = dict(shared)
        m['xT'] = _f(inp['x'][b, j * 2048:(j + 1) * 2048, :].T)
        m['jt'] = np.array([[j, (j // 2) * 5 * 4 * 128 * 4096 + (j % 2) * 2048]], np.int32)
        m['cT'] = _f(inp['c'][b].reshape(16, 128).T)
        m['pos'] = _f(pos[b, j * 2048:(j + 1) * 2048][None, :])
        pp = np.stack([pos[b][perm[g]] for g in range(3)]).astype(np.int32)
        m['dposq'] = _f(pp)
        m['dposk'] = _f(pp.reshape(3, S_ // 128, 128).transpose(0, 2, 1))
        m['nslope'] = _f(np.broadcast_to(-slopes[:, j][None, :], (128, 3)).astype(np.float32))
        maps.append(m)
    if "f" not in _PROG:
        _PROG["f"] = build_fused()
    res = run_bass_kernel_spmd(_PROG["f"], maps, core_ids=cores).results
    out = np.empty(inp['x'].shape, dtype=np.float32)
    for c in cores:
        b, j = c // 4, c % 4
        out[b, j * 2048:(j + 1) * 2048, :] = np.asarray(res[c]['xfT']).T
    return out
```

```python
import math
import numpy as np
import concourse.bass as bass
import concourse.mybir as mybir
from concourse.bass_utils import run_bass_kernel_spmd

F32 = mybir.dt.float32
BF16 = mybir.dt.bfloat16
I32 = mybir.dt.int32
AF = mybir.ActivationFunctionType
ALU = mybir.AluOpType
AX = mybir.AxisListType


class Buf:
    __slots__ = ("name", "w", "r")

    def __init__(self, name):
        self.name = name
        self.w = None
        self.r = {}


class _Rec:
    def __init__(self):
        self.call = None

    def __getattr__(self, name):
        def f(*a, **kw):
            self.call = (name, a, kw)
            return self
        return f


def _record(fn):
    r = _Rec()
    fn(r)
    assert r.call is not None
    return r.call


class Sched:
    ENGS = ("pe", "act", "dve", "pool", "sp")
    NDMA = 12

    def __init__(self, nc):
        self.nc = nc
        self.streams = {e: [] for e in self.ENGS}
        self.cnt = {e: 0 for e in self.ENGS}
        self.seen = {e: {} for e in self.ENGS}
        self.dma_i = {"sp": 0, "pool": 0, "act": 0}
        self.dma_val = {}
        self.sems = {}
        self._ctx = []
        self._perm = []
        self.cc_val = {}
        self._phase_mark = 0
        self.uses_dyn = False
        self._phase_no = 0
        self.jt_ap = None
        self.jsb = None
        self.jt_cnt = 0
        for e in ("pe", "act", "dve", "pool"):
            self._mk_sem("E_" + e)
        for q in ("sp", "pool", "act"):
            for k in range(self.NDMA):
                self._mk_sem("D_%s%d" % (q, k))
                self.dma_val["D_%s%d" % (q, k)] = 0

    def _mk_sem(self, key):
        cm = self.nc.semaphore(key)
        self.sems[key] = cm.__enter__()
        self._perm.append(cm)

    _mk_sem_perm = _mk_sem

    def enable_dyn(self, jt_ap):
        self.jt_ap = jt_ap
        self._mk_sem("JT")
        cm = self.nc.sbuf_tensor("jsb", [1, 2], I32)
        self.jsb = cm.__enter__()
        self._perm.append(cm)

    def sbuf(self, name, shape, dtype):
        cm = self.nc.sbuf_tensor("%s_p%d" % (name, self._phase_no), shape, dtype)
        t = cm.__enter__()
        self._ctx.append(cm)
        return t

    def psum(self, name, shape, dtype):
        cm = self.nc.psum_tensor("%s_p%d" % (name, self._phase_no), shape, dtype)
        t = cm.__enter__()
        self._ctx.append(cm)
        return t

    def _deps(self, eng, reads, writes):
        deps = {}

        def add(tok):
            if tok is None:
                return
            k, v = tok
            if deps.get(k, -1) < v:
                deps[k] = v
        for b in reads:
            add(b.w)
        for b in writes:
            add(b.w)
            for k, v in b.r.items():
                add((k, v))
        out = []
        seen = self.seen[eng]
        for k, v in deps.items():
            if eng == "pe" and k == "E_pe":
                continue
            if seen.get(k, -1) >= v:
                continue
            seen[k] = v
            out.append((k, v))
        return out

    def _mark(self, tok, reads, writes):
        k, v = tok
        for b in reads:
            if b.r.get(k, -1) < v:
                b.r[k] = v
        for b in writes:
            b.w = tok
            b.r = {}

    def op(self, eng, fn, reads=(), writes=()):
        waits = self._deps(eng, reads, writes)
        self.cnt[eng] += 1
        tok = ("E_" + eng, self.cnt[eng])
        self.streams[eng].append((waits, _record(fn), tok[0], 1))
        self._mark(tok, reads, writes)

    def dma(self, q, fn, reads=(), writes=()):
        i = self.dma_i[q]
        self.dma_i[q] += 1
        key = "D_%s%d" % (q, i % self.NDMA)
        waits = self._deps(q, reads, writes)
        prev = self.dma_val[key]
        if prev > 0 and self.seen[q].get(key, -1) < prev:
            self.seen[q][key] = prev
            waits.append((key, prev))
        self.dma_val[key] = prev + 16
        tok = (key, prev + 16)
        self.streams[q].append((waits, _record(fn), key, 16))
        self._mark(tok, reads, writes)

    def begin_phase(self):
        self._phase_mark = len(self._ctx)
        self._phase_no += 1

    def barrier(self, wait_cc=True):
        allv = {}
        for e in ("pe", "act", "dve", "pool"):
            if self.cnt[e] > 0:
                allv["E_" + e] = self.cnt[e]
        for k, v in self.dma_val.items():
            if v > 0:
                allv[k] = v
        for k, v in self.cc_val.items():
            if v > 0 and wait_cc:
                allv[k] = v
        for eng in self.ENGS:
            waits = []
            for k, v in allv.items():
                if self.seen[eng].get(k, -1) < v:
                    self.seen[eng][k] = v
                    waits.append((k, v))
            self.streams[eng].append((waits, None, None, 0))

    def end_phase(self, wait_cc=True):
        self.barrier(wait_cc)
        self.emit()
        self.streams = {e: [] for e in self.ENGS}
        while len(self._ctx) > self._phase_mark:
            self._ctx.pop().__exit__(None, None, None)

    def cc(self, fn, reads=(), writes=()):
        key = "CC"
        if key not in self.sems:
            self._mk_sem_perm(key)
            self.cc_val[key] = 0
        n = self.cc_val[key] + 1
        self.cc_val[key] = n
        waits = self._deps("pool", reads, writes)
        self.streams["pool"].append((waits, _record(fn), key, None))
        self._mark((key, n), reads, writes)

    def dma_dyn(self, out_ap, tensor, jmul, const, ap_list, reads=(), writes=(), which=0):
        q = "sp"
        i = self.dma_i[q]
        self.dma_i[q] += 1
        key = "D_%s%d" % (q, i % self.NDMA)
        waits = self._deps(q, reads, writes)
        prev = self.dma_val[key]
        if prev > 0 and self.seen[q].get(key, -1) < prev:
            self.seen[q][key] = prev
            waits.append((key, prev))
        self.dma_val[key] = prev + 16
        tok = (key, prev + 16)
        self.streams[q].append((waits, ("__dyn__", (out_ap, tensor, int(jmul), int(const), [list(x) for x in ap_list], which), {}), key, 16))
        self._mark(tok, reads, writes)
        self.uses_dyn = True

    def final_wait(self, eng, bufs):
        waits = self._deps(eng, bufs, ())
        self.streams[eng].append((waits, None, None, 0))

    def emit(self):
        nc = self.nc
        sems = self.sems
        streams = self.streams

        def run(engine, lst, regs=None):
            for waits, fn, key, inc in lst:
                for k, v in waits:
                    engine.wait_ge(sems[k], v)
                if fn is not None:
                    name, a, kw = fn
                    if name == "__dyn__":
                        out_ap, tensor, jmul, const, ap_list, which = a
                        rj, ro = regs[which], regs[2]
                        engine.reg_mul(ro, rj, jmul)
                        engine.reg_add(ro, ro, const)
                        ins = engine.dma_start(out=out_ap, in_=bass.AP(tensor, ro, ap_list))
                    else:
                        ins = getattr(engine, name)(*a, **kw)
                    if inc is None:
                        ins.then_inc(sems[key])
                    else:
                        ins.then_inc(sems[key], inc)

        with nc.Block() as block:
            @block.tensor
            def _(e):
                run(e, streams["pe"])

            @block.scalar
            def _(e):
                run(e, streams["act"])

            @block.vector
            def _(e):
                run(e, streams["dve"])

            @block.gpsimd
            def _(e):
                run(e, streams["pool"])

            @block.sync
            def _(e):
                if any(fn is not None and fn[0] == "__dyn__" for _, fn, _, _ in streams["sp"]):
                    self.jt_cnt += 1
                    with e.register("rj%d" % self.jt_cnt) as rj, e.register("ry%d" % self.jt_cnt) as ry, e.register("ro%d" % self.jt_cnt) as ro:
                        e.dma_start(out=self.jsb[:, :], in_=self.jt_ap).then_inc(sems["JT"], 16)
                        e.wait_ge(sems["JT"], 16 * self.jt_cnt)
                        e.reg_load(rj, self.jsb[0:1, 0:1])
                        e.reg_load(ry, self.jsb[0:1, 1:2])
                        run(e, streams["sp"], (rj, ry, ro))
                else:
                    run(e, streams["sp"])

    def close(self):
        for cm in reversed(self._ctx):
            cm.__exit__(None, None, None)
        self._ctx = []
        for cm in reversed(self._perm):
            cm.__exit__(None, None, None)
        self._perm = []


D = 2048
NK = 16
TS = 1024
TP = 128
EPS = 1e-6
TWO_PI = 2.0 * math.pi
C1 = 6.28125
C2 = TWO_PI - C1


def emit_A(S, nc, d, l, xT, QKs, Vs, gT, b_QKs_l, b_Vs_l, b_gT, ntok=2048, after_sup=None, tick=None):
    cT = d['cT']; wada = d['wada'][l]; bada = d['bada_a'][l]; gmix = d['gmix'][l]; win = d['win'][l]; wsw = d['wsw'][l]
    gq = d['gq'][l]; gkv = d['gkv'][l]; wuq = d['wuq'][l]; wuqs = d['wuqs'][l]; wukv = d['wukv'][l]
    pos = d['pos']; invf = d['invf']; sgn = d['sgn']
    b_QKs = b_QKs_l[0]; b_Vs = b_Vs_l[0]
    b_projT = b_QKs; b_mlaq = b_QKs; b_mlakv = b_QKs; b_mlakr = b_QKs
    aT = S.sbuf("aT", [128, NK, TS], BF16); b_aT = [Buf("aT%d" % i) for i in range(TS // TP)]
    wb = [S.sbuf("wb%d" % i, [128, NK, 512], BF16) for i in range(2)]; b_wb = [Buf("wb0"), Buf("wb1")]
    xt = S.sbuf("xt", [128, NK, TP], F32); b_xt = Buf("xt")
    sq = S.sbuf("sq", [128, NK, TP], BF16); b_sq = Buf("sq")
    rstd = S.sbuf("rstd", [128, 512], F32); b_rstd = Buf("rstd")
    lnt = S.sbuf("lnt", [128, 512], F32); b_lnt = Buf("lnt")
    tmp = [S.sbuf("tmp%d" % i, [128, 512], F32) for i in range(2)]; b_tmp = [Buf("tmp0"), Buf("tmp1")]
    ones = S.sbuf("ones", [128, 128], BF16); b_ones = Buf("ones")
    cin = S.sbuf("cin", [128, NK], F32); b_cin = Buf("cin")
    cact = S.sbuf("cact", [128, NK], BF16); b_cact = Buf("cact")
    badat = S.sbuf("badat", [128, 32], F32); b_badat = Buf("badat")
    gmixt = S.sbuf("gmixt", [128, NK], F32); b_gmixt = Buf("gmixt")
    modt = S.sbuf("modt", [128, 32], F32); b_modt = Buf("modt")
    s1 = S.sbuf("s1", [128, NK], F32); b_s1 = Buf("s1")
    cq = S.sbuf("cq", [128, 4, TS], F32); b_cq = Buf("cq")
    ckv = S.sbuf("ckv", [128, 4, TS], F32); b_ckv = Buf("ckv")
    kr = S.sbuf("kr", [64, TS], F32); b_kr = Buf("kr")
    krs = S.sbuf("krs", [64, TS], F32); b_krs = Buf("krs")
    wswb = S.sbuf("wswb", [128, NK, 64], BF16); b_wswb = Buf("wswb")
    wuqb = S.sbuf("wuqb", [128, 4, 1536], BF16); b_wuqb = Buf("wuqb")
    wuqsb = S.sbuf("wuqsb", [128, 4, 512], BF16); b_wuqsb = Buf("wuqsb")
    wukvb = S.sbuf("wukvb", [128, 4, 2048], BF16); b_wukvb = Buf("wukvb")
    gqt = S.sbuf("gqt", [128, 4], F32); b_gqt = Buf("gqt")
    gkvt = S.sbuf("gkvt", [128, 4], F32); b_gkvt = Buf("gkvt")
    ob = [S.sbuf("ob%d" % i, [128, TS], BF16) for i in range(2)]; b_ob = [Buf("ob0"), Buf("ob1")]
    of = [S.sbuf("of%d" % i, [128, TS], F32) for i in range(2)]; b_of = [Buf("of0"), Buf("of1")]
    lat = S.sbuf("lat", [128, 4, 512], BF16); b_lat = Buf("lat")
    posi = S.sbuf("posi", [64, 512], I32); b_posi = Buf("posi")
    ang = S.sbuf("ang", [64, 512], F32); b_ang = Buf("ang")
    kf = S.sbuf("kf", [64, 512], F32); b_kf = Buf("kf")
    ki = posi; b_ki = b_posi
    rr = S.sbuf("rr", [64, 512], F32); b_rr = Buf("rr")
    rc = S.sbuf("rc", [64, 512], F32); b_rc = Buf("rc")
    mm = S.sbuf("mm", [64, 512], F32); b_mm = Buf("mm")
    CS = S.sbuf("CS", [64, 512], F32); b_CS = Buf("CS")
    SN = S.sbuf("SN", [64, 512], F32); b_SN = Buf("SN")
    invft = S.sbuf("invft", [64, 1], F32); b_invft = Buf("invft")
    sgnt = S.sbuf("sgnt", [64, 1], F32); b_sgnt = Buf("sgnt")
    t1 = ang; b_t1 = b_ang
    t2 = kf; b_t2 = b_kf
    ps = S.psum("ps", [128, 8, 512], F32); b_ps = [Buf("ps%d" % i) for i in range(8)]
    st = {"bank": 0, "w": 0, "ob": 0, "of": 0, "tmp": 0, "ev": 0}

    def nbank():
        i = st["bank"]; st["bank"] = (i + 1) % 8
        return i

    def load_w(dst, bdst, src, nk, c0, ncols):
        srcv = src.rearrange("(k p) c -> p k c", p=128)
        h = max(1, nk // 2)
        for k0 in range(0, nk, h):
            S.dma("pool", lambda e, k0=k0: e.dma_start(out=dst[:, k0:k0 + h, 0:ncols],
                                                      in_=srcv[:, k0:k0 + h, c0:c0 + ncols]),
                  writes=[bdst])

    S.op("dve", lambda e: e.memset(ones[:], 1.0), writes=[b_ones])
    S.dma("sp", lambda e: e.dma_start(out=cin[:], in_=cT[:, :]), writes=[b_cin])
    S.dma("sp", lambda e: e.dma_start(out=badat[:], in_=bada[:, :]), writes=[b_badat])
    S.dma("sp", lambda e: e.dma_start(out=gmixt[:], in_=gmix[:, :]), writes=[b_gmixt])
    S.dma("sp", lambda e: e.dma_start(out=gqt[:], in_=gq[:, :]), writes=[b_gqt])
    S.dma("sp", lambda e: e.dma_start(out=gkvt[:], in_=gkv[:, :]), writes=[b_gkvt])
    S.dma("sp", lambda e: e.dma_start(out=invft[:], in_=invf[:, :]), writes=[b_invft])
    S.dma("sp", lambda e: e.dma_start(out=sgnt[:], in_=sgn[:, :]), writes=[b_sgnt])
    S.op("act", lambda e: e.activation(out=cact[:], in_=cin[:], func=AF.Silu), reads=[b_cin], writes=[b_cact])

    mb = nbank()
    for g in range(8):
        wi = st["w"]; st["w"] ^= 1
        load_w(wb[wi], b_wb[wi], wada, NK, g * 512, 512)
        for m in range(4):
            n = g * 4 + m
            for k in range(NK):
                S.op("pe", lambda e, wi=wi, m=m, k=k, n=n: e.matmul(
                    ps[:, mb, n:n + 1], lhsT=wb[wi][:, k, m * 128:(m + 1) * 128], rhs=cact[:, k:k + 1],
                    start=(k == 0), stop=(k == NK - 1)), reads=[b_wb[wi], b_cact], writes=[b_ps[mb]])
    S.op("dve", lambda e: e.tensor_tensor(out=modt[:], in0=ps[:, mb, 0:32], in1=badat[:], op=ALU.add),
         reads=[b_ps[mb], b_badat], writes=[b_modt])
    S.op("dve", lambda e: e.scalar_tensor_tensor(out=s1[:], in0=modt[:, 16:32], scalar=1.0, in1=gmixt[:],
                                                 op0=ALU.add, op1=ALU.mult),
         reads=[b_modt, b_gmixt], writes=[b_s1])

    load_w(wswb, b_wswb, wsw, NK, 0, 64)
    load_w(wuqb, b_wuqb, wuq, 4, 0, 1536)
    load_w(wuqsb, b_wuqsb, wuqs, 4, 0, 512)
    load_w(wukvb, b_wukvb, wukv, 4, 0, 2048)

    def rms_rstd(src_sq, bsrc, nk, width, dim):
        bk = nbank()
        for k in range(nk):
            S.op("pe", lambda e, k=k: e.matmul(ps[:, bk, 0:width], lhsT=ones[:], rhs=src_sq[:, k, 0:width],
                                               start=(k == 0), stop=(k == nk - 1)),
                 reads=[bsrc, b_ones], writes=[b_ps[bk]])
        S.op("act", lambda e: e.activation(out=lnt[:, 0:width], in_=ps[:, bk, 0:width], func=AF.Ln,
                                           scale=1.0 / dim, bias=EPS), reads=[b_ps[bk]], writes=[b_lnt])
        S.op("act", lambda e: e.activation(out=rstd[:, 0:width], in_=lnt[:, 0:width], func=AF.Exp, scale=-0.5),
             reads=[b_lnt], writes=[b_rstd])

    xTv = xT.rearrange("(k p) t -> p k t", p=128)
    for sup in range(ntok // TS):
        t0s = sup * TS
        b_QKs = b_QKs_l[sup]; b_Vs = b_Vs_l[sup]
        for pt in range(TS // TP):
            tok0 = t0s + pt * TP
            for k0 in (0, 8):
                S.dma("sp", lambda e, k0=k0, tok0=tok0: e.dma_start(out=xt[:, k0:k0 + 8, :],
                                                                    in_=xTv[:, k0:k0 + 8, tok0:tok0 + TP]),
                      writes=[b_xt])
            S.op("act", lambda e: e.activation(out=sq[:], in_=xt[:], func=AF.Square), reads=[b_xt], writes=[b_sq])
            rms_rstd(sq, b_sq, NK, TP, float(D))
            for k in range(NK):
                ti = st["tmp"]; st["tmp"] ^= 1
                S.op("dve", lambda e, k=k, ti=ti: e.tensor_tensor(out=tmp[ti][:, 0:TP], in0=xt[:, k, :],
                                                                  in1=rstd[:, 0:TP], op=ALU.mult),
                     reads=[b_xt, b_rstd], writes=[b_tmp[ti]])
                S.op("act", lambda e, k=k, ti=ti, pt=pt: e.activation(
                    out=aT[:, k, pt * TP:(pt + 1) * TP], in_=tmp[ti][:, 0:TP], func=AF.Identity,
                    scale=s1[:, k:k + 1], bias=modt[:, k:k + 1]),
                    reads=[b_tmp[ti], b_s1, b_modt], writes=[b_aT[pt]])

        def gemm_group(src, c0, ncols, epilogue):
            wi = st["w"]; st["w"] ^= 1
            load_w(wb[wi], b_wb[wi], src, NK, c0, ncols)
            if tick is not None:
                tick()
            for m in range((ncols + 127) // 128):
                mc = min(128, ncols - m * 128)
                for t in range(TS // 512):
                    bk = nbank()
                    for k in range(NK):
                        S.op("pe", lambda e, wi=wi, m=m, mc=mc, t=t, k=k, bk=bk: e.matmul(
                            ps[0:mc, bk, :], lhsT=wb[wi][:, k, m * 128:m * 128 + mc],
                            rhs=aT[:, k, t * 512:(t + 1) * 512], start=(k == 0), stop=(k == NK - 1)),
                            reads=[b_wb[wi]] + b_aT[4 * t:4 * t + 4], writes=[b_ps[bk]])
                    epilogue(m, mc, t, bk)

        def evac(out_ap, bout, bk, mc, scale=1.0, func=None):
            st["ev"] ^= 1
            if func is not None or st["ev"]:
                f = func if func is not None else AF.Copy
                S.op("act", lambda e: e.activation(out=out_ap, in_=ps[0:mc, bk, :], func=f, scale=scale),
                     reads=[b_ps[bk]], writes=[bout])
            else:
                S.op("dve", lambda e: e.tensor_scalar(out=out_ap, in0=ps[0:mc, bk, :], scalar1=scale, scalar2=None,
                                                      op0=ALU.mult), reads=[b_ps[bk]], writes=[bout])

        gemm_group(win, 0, 512, lambda m, mc, t, bk: evac(cq[:, m, t * 512:(t + 1) * 512], b_cq, bk, mc))
        gemm_group(win, 512, 512, lambda m, mc, t, bk: evac(ckv[:, m, t * 512:(t + 1) * 512], b_ckv, bk, mc))
        gemm_group(win, 1024, 64, lambda m, mc, t, bk: evac(kr[:, t * 512:(t + 1) * 512], b_kr, bk, mc))
        gemm_group(wsw, 0, 64, lambda m, mc, t, bk: evac(krs[:, t * 512:(t + 1) * 512], b_krs, bk, mc))

        def out_bf(dst, bdst, row0, scale):
            cur = {}

            def ep(m, mc, t, bk):
                if t == 0:
                    cur["i"] = st["ob"]; st["ob"] ^= 1
                i = cur["i"]
                evac(ob[i][:, t * 512:(t + 1) * 512], b_ob[i], bk, mc, scale=scale)
                if t == TS // 512 - 1:
                    r = row0 + m * 128
                    S.dma("sp", lambda e, i=i, r=r: e.dma_start(out=dst[r:r + 128, t0s:t0s + TS], in_=ob[i][:, :]),
                          reads=[b_ob[i]], writes=[bdst])
            return ep

        def out_f32(dst, bdst, row0, func):
            cur = {}

            def ep(m, mc, t, bk):
                if t == 0:
                    cur["i"] = st["of"]; st["of"] ^= 1
                i = cur["i"]
                evac(of[i][:, t * 512:(t + 1) * 512], b_of[i], bk, mc, func=func)
                if t == TS // 512 - 1:
                    r = row0 + m * 128
                    S.dma("sp", lambda e, i=i, r=r: e.dma_start(out=dst[r:r + 128, t0s:t0s + TS], in_=of[i][:, :]),
                          reads=[b_of[i]], writes=[bdst])
            return ep

        sc = 128.0 ** -0.5
        def out_qk(rowfn, scale):
            cur = {}

            def ep(m, mc, t, bk):
                if t == 0:
                    cur["i"] = st["ob"]; st["ob"] ^= 1
                i = cur["i"]
                evac(ob[i][:, t * 512:(t + 1) * 512], b_ob[i], bk, mc, scale=scale)
                if t == TS // 512 - 1:
                    sh, r0 = rowfn(m)
                    S.dma("sp", lambda e: e.dma_start(out=QKs[sh, sup, r0:r0 + 128, :], in_=ob[i][:, :]),
                          reads=[b_ob[i]], writes=[b_QKs])
            return ep

        def gemm_group_tm(c0, store):
            wi = st["w"]; st["w"] ^= 1
            load_w(wb[wi], b_wb[wi], win, NK, c0, 512)
            if tick is not None:
                tick()
            for s_ in range(TS // 128):
                bk = nbank()
                for k in range(NK):
                    S.op("pe", lambda e: e.matmul(ps[:, bk, :], lhsT=aT[:, k, s_ * 128:(s_ + 1) * 128], rhs=wb[wi][:, k, 0:512],
                                                  start=(k == 0), stop=(k == NK - 1)), reads=[b_wb[wi], b_aT[s_]], writes=[b_ps[bk]])
                oi = st["ob"]; st["ob"] ^= 1
                evac(ob[oi][:, 0:512], b_ob[oi], bk, 128)
                store(s_, oi)

        for g in range(3):
            gemm_group(win, 1088 + g * 512, 512, out_qk(lambda m, g=g: (m, g * 128), sc))
        for g in range(3):
            gemm_group(win, 1088 + 1536 + g * 512, 512, out_qk(lambda m, g=g: (m, 384 + g * 128), 1.0))
        for g in range(3):
            def st_dv(s_, oi, g=g):
                tk = t0s + s_ * 128
                S.dma("sp", lambda e: e.dma_start(out=Vs[:, tk:tk + 128, g * 128:(g + 1) * 128].rearrange("h p c -> p h c"),
                                                  in_=ob[oi][:, 0:512].rearrange("p (h c) -> p h c", c=128)),
                      reads=[b_ob[oi]], writes=[b_Vs])
            gemm_group_tm(1088 + 3072 + g * 512, st_dv)
        for gi in range(2):
            gemm_group(win, 5696 + gi * 512, 512, out_qk(lambda m, gi=gi: ((4 * gi + m) // 2, 768 + (m % 2) * 128), sc))
        for gi in range(2):
            gemm_group(win, 5696 + 1024 + gi * 512, 512, out_qk(lambda m, gi=gi: ((4 * gi + m) // 2, 1024 + (m % 2) * 128), 1.0))
        for gi in range(2):
            def st_sv(s_, oi, gi=gi):
                tk = t0s + s_ * 128
                S.dma("sp", lambda e: e.dma_start(out=Vs[2 * gi:2 * gi + 2, tk:tk + 128, 384:640].rearrange("j p c -> p j c"),
                                                  in_=ob[oi][:, 0:512].rearrange("p (j c) -> p j c", c=256)),
                      reads=[b_ob[oi]], writes=[b_Vs])
            gemm_group_tm(5696 + 2048 + gi * 512, st_sv)

        scm = 192.0 ** -0.5
        for tt in range(TS // 512):
            tok0 = t0s + tt * 512
            tsl = slice(tt * 512, (tt + 1) * 512)
            S.dma("sp", lambda e, tok0=tok0: e.dma_start(out=posi[:], in_=pos[0:1, tok0:tok0 + 512].partition_broadcast(64)),
                  writes=[b_posi])
            S.op("dve", lambda e: e.tensor_copy(out=ang[:], in_=posi[:]), reads=[b_posi], writes=[b_ang])
            S.op("dve", lambda e: e.tensor_scalar(out=ang[:], in0=ang[:], scalar1=invft[:, 0:1], scalar2=None, op0=ALU.mult),
                 reads=[b_ang, b_invft], writes=[b_ang])
            S.op("dve", lambda e: e.tensor_scalar(out=kf[:], in0=ang[:], scalar1=1.0 / TWO_PI, scalar2=None, op0=ALU.mult),
                 reads=[b_ang], writes=[b_kf])
            S.op("dve", lambda e: e.tensor_copy(out=ki[:], in_=kf[:]), reads=[b_kf], writes=[b_ki])
            S.op("dve", lambda e: e.tensor_copy(out=kf[:], in_=ki[:]), reads=[b_ki], writes=[b_kf])
            S.op("dve", lambda e: e.scalar_tensor_tensor(out=rr[:], in0=kf[:], scalar=-C1, in1=ang[:], op0=ALU.mult, op1=ALU.add),
                 reads=[b_kf, b_ang], writes=[b_rr])
            S.op("dve", lambda e: e.scalar_tensor_tensor(out=rr[:], in0=kf[:], scalar=-C2, in1=rr[:], op0=ALU.mult, op1=ALU.add),
                 reads=[b_kf, b_rr], writes=[b_rr])

            def wrap(r, br):
                S.op("dve", lambda e: e.tensor_scalar(out=mm[:], in0=r[:], scalar1=math.pi, scalar2=-TWO_PI, op0=ALU.is_gt, op1=ALU.mult),
                     reads=[br], writes=[b_mm])
                S.op("dve", lambda e: e.tensor_tensor(out=r[:], in0=r[:], in1=mm[:], op=ALU.add), reads=[br, b_mm], writes=[br])
                S.op("dve", lambda e: e.tensor_scalar(out=mm[:], in0=r[:], scalar1=-math.pi, scalar2=TWO_PI, op0=ALU.is_lt, op1=ALU.mult),
                     reads=[br], writes=[b_mm])
                S.op("dve", lambda e: e.tensor_tensor(out=r[:], in0=r[:], in1=mm[:], op=ALU.add), reads=[br, b_mm], writes=[br])
                S.op("dve", lambda e: e.tensor_scalar(out=r[:], in0=r[:], scalar1=3.1415925, scalar2=-3.1415925, op0=ALU.min, op1=ALU.max),
                     reads=[br], writes=[br])
            wrap(rr, b_rr)
            S.op("dve", lambda e: e.tensor_scalar(out=rc[:], in0=rr[:], scalar1=math.pi / 2, scalar2=None, op0=ALU.add),
                 reads=[b_rr], writes=[b_rc])
            wrap(rc, b_rc)
            S.op("act", lambda e: e.activation(out=CS[:], in_=rc[:], func=AF.Sin), reads=[b_rc], writes=[b_CS])
            S.op("act", lambda e: e.activation(out=SN[:], in_=rr[:], func=AF.Sin, scale=sgnt[:, 0:1]), reads=[b_rr, b_sgnt], writes=[b_SN])

            def rope_out(src_r, bsr, src_s, bss, scale, dst_ap, bdst):
                S.op("dve", lambda e: e.scalar_tensor_tensor(out=t1[:], in0=src_r, scalar=scale, in1=CS[:], op0=ALU.mult, op1=ALU.mult),
                     reads=[bsr, b_CS], writes=[b_t1])
                S.op("dve", lambda e: e.scalar_tensor_tensor(out=t2[:], in0=src_s, scalar=scale, in1=SN[:], op0=ALU.mult, op1=ALU.mult),
                     reads=[bss, b_SN], writes=[b_t2])
                S.op("dve", lambda e: e.tensor_tensor(out=dst_ap, in0=t1[:], in1=t2[:], op=ALU.add),
                     reads=[b_t1, b_t2], writes=[bdst])

            oi = st["ob"]; st["ob"] ^= 1
            rope_out(kr[:, tsl], b_kr, krs[:, tsl], b_krs, 1.0, ob[oi][0:64, 0:512], b_ob[oi])
            for sh in range(4):
                S.dma("sp", lambda e: e.dma_start(out=QKs[sh, sup, 1920:1984, tok0 - t0s:tok0 - t0s + 512], in_=ob[oi][0:64, 0:512]),
                      reads=[b_ob[oi]], writes=[b_QKs])

            def latent_norm(src, bsrc, gt, bgt):
                sqv = sq[:, :, :].rearrange("p (k a) t -> p k (a t)", a=4)
                S.op("act", lambda e: e.activation(out=sqv, in_=src[:, :, tsl], func=AF.Square), reads=[bsrc], writes=[b_sq])
                rms_rstd(sqv, b_sq, 4, 512, 512.0)
                for k in range(4):
                    S.op("dve", lambda e, k=k: e.scalar_tensor_tensor(out=lat[:, k, :], in0=src[:, k, tsl], scalar=gt[:, k:k + 1],
                                                                      in1=rstd[:, :], op0=ALU.mult, op1=ALU.mult),
                         reads=[bsrc, bgt, b_rstd], writes=[b_lat])

            latent_norm(cq, b_cq, gqt, b_gqt)
            for h in range(8):
                bk = nbank()
                for k in range(4):
                    S.op("pe", lambda e, h=h, k=k, bk=bk: e.matmul(ps[:, bk, :], lhsT=wuqb[:, k, h * 192:h * 192 + 128], rhs=lat[:, k, :],
                                                                   start=(k == 0), stop=(k == 3)), reads=[b_wuqb, b_lat], writes=[b_ps[bk]])
                oi = st["ob"]; st["ob"] ^= 1
                evac(ob[oi][:, 0:512], b_ob[oi], bk, 128, scale=scm)
                S.dma("sp", lambda e, oi=oi, h=h, tok0=tok0: e.dma_start(out=QKs[h // 2, sup, 1280 + (h % 2) * 128:1280 + (h % 2) * 128 + 128, tok0 - t0s:tok0 - t0s + 512], in_=ob[oi][:, 0:512]),
                      reads=[b_ob[oi]], writes=[b_mlaq])
                bk1 = nbank(); bk2 = nbank()
                for k in range(4):
                    S.op("pe", lambda e, h=h, k=k, bk1=bk1: e.matmul(ps[0:64, bk1, :], lhsT=wuqb[:, k, h * 192 + 128:h * 192 + 192], rhs=lat[:, k, :],
                                                                     start=(k == 0), stop=(k == 3)), reads=[b_wuqb, b_lat], writes=[b_ps[bk1]])
                for k in range(4):
                    S.op("pe", lambda e, h=h, k=k, bk2=bk2: e.matmul(ps[0:64, bk2, :], lhsT=wuqsb[:, k, h * 64:(h + 1) * 64], rhs=lat[:, k, :],
                                                                     start=(k == 0), stop=(k == 3)), reads=[b_wuqsb, b_lat], writes=[b_ps[bk2]])
                oi = st["ob"]; st["ob"] ^= 1
                rope_out(ps[0:64, bk1, :], b_ps[bk1], ps[0:64, bk2, :], b_ps[bk2], scm, ob[oi][0:64, 0:512], b_ob[oi])
                S.dma("sp", lambda e, oi=oi, h=h, tok0=tok0: e.dma_start(out=QKs[h // 2, sup, 1792 + (h % 2) * 64:1792 + (h % 2) * 64 + 64, tok0 - t0s:tok0 - t0s + 512], in_=ob[oi][0:64, 0:512]),
                      reads=[b_ob[oi]], writes=[b_mlaq])
            latent_norm(ckv, b_ckv, gkvt, b_gkvt)
            for h in range(8):
                bk = nbank()
                for k in range(4):
                    S.op("pe", lambda e: e.matmul(ps[:, bk, :], lhsT=wukvb[:, k, h * 256:h * 256 + 128], rhs=lat[:, k, :],
                                                  start=(k == 0), stop=(k == 3)), reads=[b_wukvb, b_lat], writes=[b_ps[bk]])
                oi = st["ob"]; st["ob"] ^= 1
                evac(ob[oi][:, 0:512], b_ob[oi], bk, 128)
                S.dma("sp", lambda e: e.dma_start(out=QKs[h // 2, sup, 1536 + (h % 2) * 128:1536 + (h % 2) * 128 + 128, tok0 - t0s:tok0 - t0s + 512], in_=ob[oi][:, 0:512]),
                      reads=[b_ob[oi]], writes=[b_QKs])
            wv = wukvb[:, :, :].rearrange("p k (h c) -> p k h c", c=256)
            for s4 in range(4):
                for hg in range(2):
                    bk = nbank()
                    for k in range(4):
                        S.op("pe", lambda e: e.matmul(ps[:, bk, :].rearrange("p (h c) -> p h c", c=128), lhsT=lat[:, k, s4 * 128:(s4 + 1) * 128],
                                                      rhs=wv[:, k, hg * 4:(hg + 1) * 4, 128:256], start=(k == 0), stop=(k == 3)),
                             reads=[b_wukvb, b_lat], writes=[b_ps[bk]])
                    oi = st["ob"]; st["ob"] ^= 1
                    evac(ob[oi][:, 0:512], b_ob[oi], bk, 128)
                    tk = tok0 + s4 * 128
                    S.dma("sp", lambda e: e.dma_start(out=Vs[2 * hg:2 * hg + 2, tk:tk + 128, 640:896].rearrange("j p c -> p j c"),
                                                      in_=ob[oi][:, 0:512].rearrange("p (j c) -> p j c", c=256)),
                          reads=[b_ob[oi]], writes=[b_Vs])
        if after_sup is not None:
            after_sup(sup)
        for gi in range(12):
            gemm_group(win, 8768 + gi * 512, 512, out_f32(gT, b_gT, gi * 512, AF.Sigmoid))


SEQ = 8192
NB = SEQ // 128
RATES = (1, 4, 16)
BIG = 1.0e6


def emit_B(S, nc, d, QKl, Vl, Ysrc, b_QKg, b_Vg, b_Ys, seq=SEQ, do_mla=True, do_sb=True, do_dil=True):
    NBk = seq // 128
    NG4 = seq // 512
    dposk = d['dposk']; dposq = d['dposq']; nslope = d['nslope']
    maskc_d = d['maskc']; masks_d = d['masks']; maskd_d = d['maskd']; m1_d = d['m1']; m2_d = d['m2']
    b_ymla = b_Ys; b_ysb = b_Ys; b_ydil = b_Ys
    SHR = 1984; SHV = 896
    Q1 = S.sbuf("Q1", [128, seq], BF16); bQ1 = Buf("Q1")
    K1 = S.sbuf("K1", [128, seq], BF16); bK1 = Buf("K1")
    if do_mla:
        Q2 = S.sbuf("Q2", [64, seq], BF16); bQ2 = Buf("Q2")
        K2 = S.sbuf("K2", [64, seq], BF16); bK2 = Buf("K2")
    V1 = S.sbuf("V1", [128, NBk, 128], BF16); bV1 = Buf("V1")
    two = do_mla or do_sb
    if two:
        Q1b = S.sbuf("Q1b", [128, seq], BF16); bQ1b = Buf("Q1b")
        K1b = S.sbuf("K1b", [128, seq], BF16); bK1b = Buf("K1b")
        V1b = S.sbuf("V1b", [128, NBk, 128], BF16); bV1b = Buf("V1b")
        if do_mla:
            Q2b = S.sbuf("Q2b", [64, seq], BF16); bQ2b = Buf("Q2b")
    NP = 8
    PT = [S.sbuf("PT%d" % i, [128, 512], BF16) for i in range(NP)]; bPT = [Buf("PT%d" % i) for i in range(NP)]
    SP = [S.sbuf("SP%d" % i, [128, 512], BF16) for i in range(NP)]; bSP = [Buf("SP%d" % i) for i in range(NP)]
    EN = [S.sbuf("EN%d" % i, [128, 512], F32) for i in range(4)]; bEN = [Buf("EN%d" % i) for i in range(4)]
    SNt = [S.sbuf("SN%d" % i, [128, 512], F32) for i in range(NP)]; bSN = [Buf("SN%d" % i) for i in range(NP)]
    UU = [S.sbuf("UU%d" % i, [128, 512], F32) for i in range(4)]; bUU = [Buf("UU%d" % i) for i in range(4)]
    YS = [S.sbuf("YS%d" % i, [128, 512], BF16) for i in range(2)]; bYS = [Buf("YS%d" % i) for i in range(2)]
    RD = S.sbuf("RD", [128, 512], F32); bRD = Buf("RD")
    maskc = S.sbuf("maskc_t", [128, 4, 512], BF16); bmaskc = Buf("maskc")
    masks = S.sbuf("masks_t", [128, 4, 512], BF16); bmasks = Buf("masks")
    maskd = S.sbuf("maskd_t", [128, 256], F32); bmaskd = Buf("maskd")
    M1 = S.sbuf("M1", [128, 128], BF16); bM1 = Buf("M1")
    M2 = S.sbuf("M2", [128, 128], BF16); bM2 = Buf("M2")
    ones = S.sbuf("ones", [128, 128], BF16); bones = Buf("ones")
    nsl = S.sbuf("nsl", [128, 3], F32); bnsl = Buf("nsl")
    ps = S.psum("ps", [128, 8, 512], F32); bps = [Buf("ps%d" % i) for i in range(8)]

    S.op("dve", lambda e: e.memset(ones[:], 1.0), writes=[bones])
    S.dma("sp", lambda e: e.dma_start(out=maskc[:], in_=maskc_d[:, :, :]), writes=[bmaskc])
    S.dma("sp", lambda e: e.dma_start(out=masks[:], in_=masks_d[:, :, :]), writes=[bmasks])
    S.dma("sp", lambda e: e.dma_start(out=maskd[:], in_=maskd_d[:, :]), writes=[bmaskd])
    S.dma("sp", lambda e: e.dma_start(out=M1[:], in_=m1_d[:, :]), writes=[bM1])
    S.dma("sp", lambda e: e.dma_start(out=M2[:], in_=m2_d[:, :]), writes=[bM2])
    S.dma("sp", lambda e: e.dma_start(out=nsl[:], in_=nslope[:, :]), writes=[bnsl])

    QK5 = QKl.rearrange("c r (p a) t -> c r p (a t)", a=1) if False else QKl
    Vl4 = Vl
    Vl6 = Vl.rearrange("c r (b p) d -> c r b p d", p=128)

    def load_fm(dst, bdst, R0, rows=128):
        dv4 = dst[0:rows, :].rearrange("p (r u t) -> p r u t", r=4, u=2)
        for u in range(2):
            S.dma("sp", lambda e: e.dma_start(out=dv4[:, :, u, :],
                                              in_=QK5[u * 4 + R0 // 512, :, R0 % 512:R0 % 512 + rows, :].rearrange("r p t -> p r t")),
                  reads=[b_QKg], writes=[bdst])

    def load_v(c0, Vt=None, bVt=None):
        Vt = V1 if Vt is None else Vt
        bVt = bV1 if bVt is None else bVt
        for r in range(4):
            for cp in range(4):
                S.dma("sp", lambda e: e.dma_start(out=Vt[:, r * 16 + cp * 4:r * 16 + cp * 4 + 4, :],
                                                  in_=Vl6[cp, r, :, :, c0:c0 + 128].rearrange("b p d -> p b d")),
                      reads=[b_Vg], writes=[bVt])

    def load_v_perm(c0, rt):
        if rt == 1:
            load_v(c0)
        elif rt == 4:
            for s_ in range(4):
                for r in range(4):
                    S.dma("sp", lambda e: e.dma_start(out=V1[:, s_ * 16 + 4 * r:s_ * 16 + 4 * r + 4, :],
                                                      in_=Vl4[:, r, s_:s_ + 4 * 127 + 1:4, c0:c0 + 128].rearrange("c p d -> p c d")),
                          reads=[b_Vg], writes=[bV1])
        else:
            for s_ in range(16):
                for cp in range(4):
                    S.dma("sp", lambda e: e.dma_start(out=V1[32 * cp:32 * cp + 32, s_ * 4:s_ * 4 + 4, :],
                                                      in_=Vl4[cp, :, s_:s_ + 16 * 31 + 1:16, c0:c0 + 128].rearrange("m p d -> p m d")),
                          reads=[b_Vg], writes=[bV1])

    Ys5 = Ysrc

    def yout(blk, g4):
        return Ys5[g4 // 8, blk, :, (g4 % 8) * 512:(g4 % 8) * 512 + 512]

    def pipeline(steps):
        prev = None
        for s1, s2 in steps:
            s1()
            if prev is not None:
                prev()
            prev = s2
        if prev is not None:
            prev()

    cnt = {"z": 0, "o": 0, "pt": 0, "ys": 0, "en": 0, "uu": 0}

    def interleave(a, b):
        out = []
        for x, y in zip(a, b):
            out.append(x); out.append(y)
        return out

    if do_mla:
        load_fm(K2, bK2, 1920, 64)
        hs = [dict(Q=Q1, bQ=bQ1, Qr=Q2, bQr=bQ2, K=K1, bK=bK1, V=V1, bV=bV1, zb=(0, 1), ob=2, db=3),
              dict(Q=Q1b, bQ=bQ1b, Qr=Q2b, bQr=bQ2b, K=K1b, bK=bK1b, V=V1b, bV=bV1b, zb=(6, 7), ob=4, db=5)]
        allsteps = []
        for h in range(2):
            H = hs[h]
            load_fm(H["Q"], H["bQ"], 1280 + h * 128)
            load_fm(H["Qr"], H["bQr"], 1792 + h * 64, 64)
            load_fm(H["K"], H["bK"], 1536 + h * 128)
            load_v(640 + h * 128, H["V"], H["bV"])
            steps = []
            zc = 0
            for g4 in range(NG4):
                qs = slice(g4 * 512, (g4 + 1) * 512)
                ob = H["ob"]; db = H["db"]
                nst = 4 * g4 + 4
                for j in range(nst):
                    zb = H["zb"][zc % 2]; zc += 1
                    ks = slice(j * 128, (j + 1) * 128)
                    box = {}

                    def s1(qs=qs, j=j, zb=zb, ks=ks, g4=g4, H=H, box=box):
                        pi = cnt["pt"] % NP; cnt["pt"] += 1
                        box["pi"] = pi
                        S.op("pe", lambda e: e.matmul(ps[:, zb, :], lhsT=H["K"][:, ks], rhs=H["Q"][:, qs], start=True, stop=False),
                             reads=[H["bK"], H["bQ"]], writes=[bps[zb]])
                        S.op("pe", lambda e: e.matmul(ps[:, zb, :], lhsT=K2[0:64, ks], rhs=H["Qr"][0:64, qs], start=False, stop=True),
                             reads=[bK2, H["bQr"]], writes=[bps[zb]])
                        S.op("act", lambda e: e.activation(out=PT[pi][:], in_=ps[:, zb, :], func=AF.Exp),
                             reads=[bps[zb]], writes=[bPT[pi]])
                        if j >= 4 * g4:
                            S.op("dve", lambda e: e.tensor_tensor(out=PT[pi][:], in0=PT[pi][:], in1=maskc[:, j - 4 * g4, :], op=ALU.mult),
                                 reads=[bPT[pi], bmaskc], writes=[bPT[pi]])

                    def s2(j=j, ob=ob, db=db, nst=nst, h=h, g4=g4, H=H, box=box):
                        pi = box["pi"]
                        S.op("pe", lambda e: e.matmul(ps[:, ob, :], lhsT=H["V"][:, j, :], rhs=PT[pi][:], start=(j == 0), stop=(j == nst - 1)),
                             reads=[H["bV"], bPT[pi]], writes=[bps[ob]])
                        S.op("pe", lambda e: e.matmul(ps[:, db, :], lhsT=ones[:], rhs=PT[pi][:], start=(j == 0), stop=(j == nst - 1)),
                             reads=[bones, bPT[pi]], writes=[bps[db]])
                        if j == nst - 1:
                            yi = cnt["ys"] % 2; cnt["ys"] += 1
                            ri = cnt["uu"] % 4; cnt["uu"] += 1
                            S.op("dve", lambda e: e.reciprocal(out=UU[ri][:], in_=ps[:, db, :]), reads=[bps[db]], writes=[bUU[ri]])
                            S.op("dve", lambda e: e.tensor_tensor(out=YS[yi][:], in0=ps[:, ob, :], in1=UU[ri][:], op=ALU.mult),
                                 reads=[bps[ob], bUU[ri]], writes=[bYS[yi]])
                            S.dma("sp", lambda e: e.dma_start(out=yout(h, g4), in_=YS[yi][:]), reads=[bYS[yi]], writes=[b_ymla])
                    steps.append((s1, s2))
            allsteps.append(steps)
        pipeline(interleave(allsteps[0], allsteps[1]))

    if do_sb:
        hs = [dict(Q=Q1, bQ=bQ1, K=K1, bK=bK1, V=V1, bV=bV1, zb=(0, 1), ob=2, rb=3),
              dict(Q=Q1b, bQ=bQ1b, K=K1b, bK=bK1b, V=V1b, bV=bV1b, zb=(6, 7), ob=4, rb=5)]
        allsteps = []
        for h in range(2):
            H = hs[h]
            load_fm(H["Q"], H["bQ"], 768 + h * 128)
            load_fm(H["K"], H["bK"], 1024 + h * 128)
            load_v(384 + h * 128, H["V"], H["bV"])
            steps = []
            zc = 0
            for g4 in range(NG4):
                qs = slice(g4 * 512, (g4 + 1) * 512)
                ob = H["ob"]; rb = H["rb"]
                nst = 4 * g4 + 4
                for idx, j in enumerate(reversed(range(nst))):
                    first = idx == 0; last = idx == nst - 1
                    zb = H["zb"][zc % 2]; zc += 1
                    ks = slice(j * 128, (j + 1) * 128)
                    box = {}

                    def t1(qs=qs, j=j, zb=zb, ks=ks, g4=g4, H=H, box=box):
                        pi = cnt["pt"] % NP; cnt["pt"] += 1
                        ei = cnt["en"] % 4; cnt["en"] += 1
                        box["pi"] = pi
                        S.op("pe", lambda e: e.matmul(ps[:, zb, :], lhsT=H["K"][:, ks], rhs=H["Q"][:, qs], start=True, stop=True),
                             reads=[H["bK"], H["bQ"]], writes=[bps[zb]])
                        S.op("act", lambda e: e.activation(out=EN[ei][:], in_=ps[:, zb, :], func=AF.Exp, scale=-1.0),
                             reads=[bps[zb]], writes=[bEN[ei]])
                        S.op("act", lambda e: e.activation(out=SNt[pi][:], in_=EN[ei][:], func=AF.Ln, bias=1.0),
                             reads=[bEN[ei]], writes=[bSN[pi]])
                        S.op("dve", lambda e: e.tensor_tensor(out=SP[pi][:], in0=ps[:, zb, :], in1=SNt[pi][:], op=ALU.add),
                             reads=[bps[zb], bSN[pi]], writes=[bSP[pi]])
                        if j >= 4 * g4:
                            S.op("dve", lambda e: e.tensor_tensor(out=SP[pi][:], in0=SP[pi][:], in1=masks[:, j - 4 * g4, :], op=ALU.mult),
                                 reads=[bSP[pi], bmasks], writes=[bSP[pi]])

                    def t2(j=j, rb=rb, first=first, g4=g4, box=box):
                        pi = box["pi"]
                        ui = cnt["uu"] % 4; cnt["uu"] += 1
                        S.op("pe", lambda e: e.matmul(ps[:, rb, :], lhsT=M1[:], rhs=SP[pi][:], start=first, stop=False, skip_group_check=True),
                             reads=[bM1, bSP[pi]], writes=[bps[rb]])
                        S.op("dve", lambda e: e.tensor_tensor(out=UU[ui][:], in0=SNt[pi][:], in1=ps[:, rb, :], op=ALU.add),
                             reads=[bSN[pi], bps[rb]], writes=[bUU[ui]])
                        S.op("act", lambda e: e.activation(out=PT[pi][:], in_=UU[ui][:], func=AF.Exp, scale=-1.0),
                             reads=[bUU[ui]], writes=[bPT[pi]])
                        if j >= 4 * g4:
                            S.op("dve", lambda e: e.tensor_tensor(out=PT[pi][:], in0=PT[pi][:], in1=masks[:, j - 4 * g4, :], op=ALU.mult),
                                 reads=[bPT[pi], bmasks], writes=[bPT[pi]])

                    def t3(rb=rb, last=last, box=box):
                        pi = box["pi"]
                        S.op("pe", lambda e: e.matmul(ps[:, rb, :], lhsT=M2[:], rhs=SP[pi][:], start=False, stop=last, skip_group_check=True),
                             reads=[bM2, bSP[pi]], writes=[bps[rb]])

                    def t4(j=j, ob=ob, first=first, last=last, h=h, g4=g4, H=H, box=box):
                        pi = box["pi"]
                        S.op("pe", lambda e: e.matmul(ps[:, ob, :], lhsT=H["V"][:, j, :], rhs=PT[pi][:], start=first, stop=last),
                             reads=[H["bV"], bPT[pi]], writes=[bps[ob]])
                        if last:
                            yi = cnt["ys"] % 2; cnt["ys"] += 1
                            S.op("act", lambda e: e.activation(out=YS[yi][:], in_=ps[:, ob, :], func=AF.Copy),
                                 reads=[bps[ob]], writes=[bYS[yi]])
                            S.dma("sp", lambda e: e.dma_start(out=yout(3 + h, g4), in_=YS[yi][:]), reads=[bYS[yi]], writes=[b_ysb])
                    steps.append((t1, t2, t3, t4))
            allsteps.append(steps)
        N_ = len(allsteps[0])
        for X in allsteps:
            X[0][0]()
        for k in range(N_):
            for X in allsteps:
                X[k][1]()
            if k > 0:
                for X in allsteps:
                    X[k - 1][3]()
            if k + 1 < N_:
                for X in allsteps:
                    X[k + 1][0]()
            for X in allsteps:
                X[k][2]()
        for X in allsteps:
            X[N_ - 1][3]()

    if do_dil:
        accn = S.sbuf("accn", [128, seq], F32); baccn = Buf("accn")
        accd = S.sbuf("accd", [128, seq], F32); baccd = Buf("accd")
        posk_i = S.sbuf("posk_i", [128, NBk], I32); bposk_i = Buf("posk_i")
        posk = S.sbuf("posk", [128, NBk], F32); bposk = Buf("posk")
        pq_i = [S.sbuf("pq_i%d" % i, [128, 128], I32) for i in range(3)]; bpq_i = [Buf("pq_i%d" % i) for i in range(3)]
        pq = [S.sbuf("pq%d" % i, [128, 128], F32) for i in range(3)]; bpq = [Buf("pq%d" % i) for i in range(3)]
        dd = [S.sbuf("dd%d" % i, [128, 256], F32) for i in range(3)]; bdd = [Buf("dd%d" % i) for i in range(3)]
        zt = [S.sbuf("zt%d" % i, [128, 256], F32) for i in range(3)]; bzt = [Buf("zt%d" % i) for i in range(3)]
        for g in range(3):
            r = RATES[g]
            L = seq // r
            nb = L // 128
            load_fm(Q1, bQ1, g * 128)
            load_fm(K1, bK1, 384 + g * 128)
            load_v_perm(g * 128, r)
            S.dma("sp", lambda e: e.dma_start(out=posk_i[:], in_=dposk[g, :, :]), writes=[bposk_i])
            S.op("dve", lambda e: e.tensor_copy(out=posk[:], in_=posk_i[:]), reads=[bposk_i], writes=[bposk])
            S.op("dve", lambda e: e.tensor_scalar(out=posk[:], in0=posk[:], scalar1=-1.0, scalar2=None, op0=ALU.mult), reads=[bposk], writes=[bposk])
            dsteps = []
            for i in range(NBk):
                s, m = divmod(i, nb)
                hp = m > 0
                c0 = 0 if hp else 128
                zb = cnt["z"] % 2; cnt["z"] += 1
                ob = 2 + (cnt["o"] % 2); db = 4 + (cnt["o"] % 2); cnt["o"] += 1
                pi = cnt["pt"] % NP; cnt["pt"] += 1
                bi = i % 3
                st0 = s + 128 * r * m
                qsl = slice(st0, st0 + 127 * r + 1, r) if r > 1 else slice(st0, st0 + 128)
                psl = (slice(st0 - 128 * r, st0 - 128 * r + 127 * r + 1, r) if r > 1 else slice(st0 - 128, st0)) if hp else None
                def s1(i=i, hp=hp, c0=c0, zb=zb, pi=pi, bi=bi, qsl=qsl, psl=psl, g=g):
                    if hp:
                        S.op("pe", lambda e: e.matmul(ps[:, zb, 0:128], lhsT=K1[:, psl], rhs=Q1[:, qsl], start=True, stop=True),
                             reads=[bK1, bQ1], writes=[bps[zb]])
                    S.op("pe", lambda e: e.matmul(ps[:, zb, 128:256], lhsT=K1[:, qsl], rhs=Q1[:, qsl], start=True, stop=True),
                         reads=[bK1, bQ1], writes=[bps[zb]])
                    S.dma("sp", lambda e: e.dma_start(out=pq_i[bi][:], in_=dposq[g:g + 1, i * 128:(i + 1) * 128].partition_broadcast(128)),
                          writes=[bpq_i[bi]])
                    S.op("dve", lambda e: e.tensor_copy(out=pq[bi][:], in_=pq_i[bi][:]), reads=[bpq_i[bi]], writes=[bpq[bi]])
                    if hp:
                        S.op("act", lambda e: e.activation(out=dd[bi][:, 0:128], in_=pq[bi][:], func=AF.Abs, bias=posk[:, i - 1:i], scale=1.0),
                             reads=[bpq[bi], bposk], writes=[bdd[bi]])
                    S.op("act", lambda e: e.activation(out=dd[bi][:, 128:256], in_=pq[bi][:], func=AF.Abs, bias=posk[:, i:i + 1], scale=1.0),
                         reads=[bpq[bi], bposk], writes=[bdd[bi]])

                def s1b(i=i, hp=hp, c0=c0, zb=zb, pi=pi, bi=bi, g=g):
                    S.op("dve", lambda e: e.tensor_tensor(out=dd[bi][:, c0:256], in0=dd[bi][:, c0:256], in1=maskd[:, c0:256], op=ALU.add),
                         reads=[bdd[bi], bmaskd], writes=[bdd[bi]])
                    S.op("dve", lambda e: e.scalar_tensor_tensor(out=zt[bi][:, c0:256], in0=dd[bi][:, c0:256], scalar=nsl[:, g:g + 1],
                                                                 in1=ps[:, zb, c0:256], op0=ALU.mult, op1=ALU.add),
                         reads=[bdd[bi], bnsl, bps[zb]], writes=[bzt[bi]])
                    S.op("act", lambda e: e.activation(out=PT[pi][:, c0:256], in_=zt[bi][:, c0:256], func=AF.Exp),
                         reads=[bzt[bi]], writes=[bPT[pi]])

                def s2(i=i, hp=hp, ob=ob, db=db, pi=pi, s=s, m=m, r=r, g=g, st0=st0):
                    if hp:
                        S.op("pe", lambda e: e.matmul(ps[:, ob, 0:128], lhsT=V1[:, i - 1, :], rhs=PT[pi][:, 0:128], start=True, stop=False),
                             reads=[bV1, bPT[pi]], writes=[bps[ob]])
                    S.op("pe", lambda e: e.matmul(ps[:, ob, 0:128], lhsT=V1[:, i, :], rhs=PT[pi][:, 128:256], start=(not hp), stop=True),
                         reads=[bV1, bPT[pi]], writes=[bps[ob]])
                    if hp:
                        S.op("pe", lambda e: e.matmul(ps[:, db, 0:128], lhsT=ones[:], rhs=PT[pi][:, 0:128], start=True, stop=False),
                             reads=[bones, bPT[pi]], writes=[bps[db]])
                    S.op("pe", lambda e: e.matmul(ps[:, db, 0:128], lhsT=ones[:], rhs=PT[pi][:, 128:256], start=(not hp), stop=True),
                         reads=[bones, bPT[pi]], writes=[bps[db]])
                    an = accn[:, st0:st0 + 127 * r + 1:r] if r > 1 else accn[:, st0:st0 + 128]
                    ad = accd[:, st0:st0 + 127 * r + 1:r] if r > 1 else accd[:, st0:st0 + 128]
                    if g == 0:
                        S.op("act", lambda e: e.activation(out=an, in_=ps[:, ob, 0:128], func=AF.Copy), reads=[bps[ob]], writes=[baccn])
                        S.op("dve", lambda e: e.tensor_copy(out=ad, in_=ps[:, db, 0:128]), reads=[bps[db]], writes=[baccd])
                    else:
                        S.op("dve", lambda e: e.tensor_tensor(out=an, in0=an, in1=ps[:, ob, 0:128], op=ALU.add), reads=[baccn, bps[ob]], writes=[baccn])
                        S.op("dve", lambda e: e.tensor_tensor(out=ad, in0=ad, in1=ps[:, db, 0:128], op=ALU.add), reads=[baccd, bps[db]], writes=[baccd])
                dsteps.append((s1, s1b, s2))
            nd = len(dsteps)
            for k in range(-2, nd):
                if k + 2 < nd:
                    dsteps[k + 2][0]()
                if 0 <= k + 1 < nd:
                    dsteps[k + 1][1]()
                if k >= 0:
                    dsteps[k][2]()
        for c in range(seq // 512):
            cs = slice(c * 512, (c + 1) * 512)
            yi = cnt["ys"] % 2; cnt["ys"] += 1
            S.op("dve", lambda e: e.reciprocal(out=RD[:], in_=accd[:, cs]), reads=[baccd], writes=[bRD])
            S.op("dve", lambda e: e.tensor_tensor(out=YS[yi][:], in0=accn[:, cs], in1=RD[:], op=ALU.mult), reads=[baccn, bRD], writes=[bYS[yi]])
            S.dma("sp", lambda e: e.dma_start(out=yout(2, c), in_=YS[yi][:]), reads=[bYS[yi]], writes=[b_ydil])


def consts_B():
    import ml_dtypes
    k = np.arange(128)[:, None]
    q = np.arange(512)[None, :]
    maskc = np.stack([(128 * i0 + k <= q) for i0 in range(4)], axis=1).astype(np.float32)
    masks = np.stack([(128 * i0 + k < q) for i0 in range(4)], axis=1).astype(np.float32)
    kl = np.arange(128)[:, None]; ql = np.arange(128)[None, :]
    maskd = np.concatenate([np.where(kl >= ql, 0.0, BIG), np.where(kl <= ql, 0.0, BIG)], axis=1).astype(np.float32)
    p = np.arange(128)[:, None]; m = np.arange(128)[None, :]
    m1 = (p > m).astype(np.float32); m2 = (p <= m).astype(np.float32)
    bf = ml_dtypes.bfloat16
    return dict(maskc=maskc.astype(bf), masks=masks.astype(bf), maskd=maskd, m1=m1.astype(bf), m2=m2.astype(bf))


D = 2048
NK = 16
EPS = 1e-6
NE = 16
BIGR = 1.0e4


def emit_C(S, nc, d, l, Yl, gT, xT, x2T, xfT, b_Yg, b_gT, b_xsrc, b_x2T, b_xfT, ntok=2048):
    NT = ntok // 512
    cT = d['cT']; wada = d['wada'][l]; bada = d['bada_c'][l]; gmoe = d['gmoe'][l]; gfin = d['gfin']
    womla = d['womla'][l]; wodil = d['wodil'][l]; wosb = d['wosb'][l]; wout = d['wout'][l]
    wr = d['wr']; br = d['br']; wg = d['wg'][l]; wu = d['wu'][l]; wd = d['wd'][l]; ident = d['ident']
    DBG = False
    xt = S.sbuf("xt", [128, NK, 512], F32); b_xt = [Buf("xt%d" % k) for k in range(NK)]
    scr = S.sbuf("scr", [128, 20, 512], BF16); b_scr = Buf("scr")
    mg = S.sbuf("mg", [128, NK, 512], BF16); b_mg = Buf("mg")
    a2b = S.sbuf("a2b", [128, NK, 512], BF16); b_a2b = Buf("a2b")
    NW = 5
    W = [S.sbuf("W%d" % i, [128, NK, 512], BF16) for i in range(NW)]; b_W = [Buf("W%d" % i) for i in range(NW)]
    gt = [S.sbuf("gt%d" % i, [128, 3, 512], F32) for i in range(2)]; b_gt = [Buf("gt0"), Buf("gt1")]
    tmp = [S.sbuf("tmp%d" % i, [128, 512], F32) for i in range(4)]; b_tmp = [Buf("tmp%d" % i) for i in range(4)]
    hp = [S.sbuf("hp%d" % i, [128, 4, 512], BF16) for i in range(2)]; b_hp = [Buf("hp0"), Buf("hp1")]
    rstd = S.sbuf("rstd", [128, 512], F32); b_rstd = Buf("rstd")
    lnt = S.sbuf("lnt", [128, 512], F32); b_lnt = Buf("lnt")
    ones = S.sbuf("ones", [128, 128], BF16); b_ones = Buf("ones")
    onesf = S.sbuf("onesf", [128, 128], F32); b_onesf = Buf("onesf")
    idt = S.sbuf("idt", [128, 128], F32); b_idt = Buf("idt")
    Gm = [S.sbuf("Gm%d" % i, [128, 128], F32) for i in range(2)]; b_Gm = [Buf("Gm0"), Buf("Gm1")]
    cin = S.sbuf("cin", [128, NK], F32); b_cin = Buf("cin")
    cact = S.sbuf("cact", [128, NK], BF16); b_cact = Buf("cact")
    badat = S.sbuf("badat", [128, 64], F32); b_badat = Buf("badat")
    gmoet = S.sbuf("gmoet", [128, NK], F32); b_gmoet = Buf("gmoet")
    gfint = S.sbuf("gfint", [128, NK], F32); b_gfint = Buf("gfint")
    modt = S.sbuf("modt", [128, 64], F32); b_modt = Buf("modt")
    s1m = S.sbuf("s1m", [128, NK], F32); b_s1m = Buf("s1m")
    wrt = S.sbuf("wrt", [128, NK, NE], F32); b_wrt = Buf("wrt")
    brt = S.sbuf("brt", [128, NE], F32); b_brt = Buf("brt")
    lt = S.sbuf("lt", [16, 512], F32); b_lt = Buf("lt")
    sc = S.sbuf("sc", [128, 64], F32); b_sc = Buf("sc")
    bi = S.sbuf("bi", [128, 64], F32); b_bi = Buf("bi")
    bi2 = S.sbuf("bi2", [128, 64], F32); b_bi2 = Buf("bi2")
    eq = S.sbuf("eq", [128, 64], F32); b_eq = Buf("eq")
    m1 = S.sbuf("m1", [128, 16], F32); b_m1 = Buf("m1")
    m2 = S.sbuf("m2", [128, 16], F32); b_m2 = Buf("m2")
    gs = S.sbuf("gs", [128, 16], F32); b_gs = Buf("gs")
    gmax = S.sbuf("gmax", [128, 4], F32); b_gmax = Buf("gmax")
    gsel = S.sbuf("gsel", [128, 16], F32); b_gsel = Buf("gsel")
    wsel = S.sbuf("wsel", [128, 64], F32); b_wsel = Buf("wsel")
    den = S.sbuf("den", [128, 4], F32); b_den = Buf("den")
    gates = S.sbuf("gates", [128, 64], F32); b_gates = Buf("gates")
    ps = S.psum("ps", [128, 8, 512], F32); b_ps = [Buf("ps%d" % i) for i in range(8)]
    st = {"bank": 0, "w": 0, "gt": 0, "tmp": 0, "hp": 0, "gm": 0}

    def nbank():
        i = st["bank"]; st["bank"] = (i + 1) % 6
        return i

    def nw():
        i = st["w"]; st["w"] = (i + 1) % NW
        return i

    def ntmp():
        i = st["tmp"]; st["tmp"] = (i + 1) % 4
        return i

    def load_w(wi, src2d, nk, c0, ncols, k_off=0):
        srcv = src2d.rearrange("(k p) c -> p k c", p=128)
        h = max(1, nk // 2)
        for k0 in range(0, nk, h):
            S.dma("pool", lambda e: e.dma_start(out=W[wi][:, k_off + k0:k_off + k0 + h, 0:ncols],
                                                in_=srcv[:, k0:k0 + h, c0:c0 + ncols]), writes=[b_W[wi]])

    S.op("dve", lambda e: e.memset(ones[:], 1.0), writes=[b_ones])
    S.op("dve", lambda e: e.memset(onesf[:], 1.0), writes=[b_onesf])
    for dst, bd, src in ((cin, b_cin, cT), (badat, b_badat, bada), (gmoet, b_gmoet, gmoe), (gfint, b_gfint, gfin),
                         (brt, b_brt, br), (idt, b_idt, ident)):
        S.dma("sp", lambda e: e.dma_start(out=dst[:], in_=src[:, :]), writes=[bd])
    S.dma("sp", lambda e: e.dma_start(out=wrt[:], in_=wr.rearrange("(k p) e -> p k e", p=128)), writes=[b_wrt])
    S.op("act", lambda e: e.activation(out=cact[:], in_=cin[:], func=AF.Silu), reads=[b_cin], writes=[b_cact])

    mb = nbank()
    for g in range(16):
        wi = nw()
        load_w(wi, wada, NK, 4096 + g * 512, 512)
        for m in range(4):
            n = g * 4 + m
            for k in range(NK):
                S.op("pe", lambda e: e.matmul(ps[:, mb, n:n + 1], lhsT=W[wi][:, k, m * 128:(m + 1) * 128], rhs=cact[:, k:k + 1],
                                              start=(k == 0), stop=(k == NK - 1)), reads=[b_W[wi], b_cact], writes=[b_ps[mb]])
    S.op("dve", lambda e: e.tensor_tensor(out=modt[:], in0=ps[:, mb, 0:64], in1=badat[:], op=ALU.add),
         reads=[b_ps[mb], b_badat], writes=[b_modt])
    S.op("dve", lambda e: e.scalar_tensor_tensor(out=s1m[:], in0=modt[:, 32:48], scalar=1.0, in1=gmoet[:], op0=ALU.add, op1=ALU.mult),
         reads=[b_modt, b_gmoet], writes=[b_s1m])

    def rms_rstd(src3, bsrcs):
        S.op("act", lambda e: e.activation(out=scr[:, 0:NK, :], in_=src3, func=AF.Square), reads=bsrcs, writes=[b_scr])
        bk = nbank()
        for k in range(NK):
            S.op("pe", lambda e: e.matmul(ps[:, bk, :], lhsT=ones[:], rhs=scr[:, k, :], start=(k == 0), stop=(k == NK - 1)),
                 reads=[b_scr, b_ones], writes=[b_ps[bk]])
        S.op("act", lambda e: e.activation(out=lnt[:], in_=ps[:, bk, :], func=AF.Ln, scale=1.0 / D, bias=EPS),
             reads=[b_ps[bk]], writes=[b_lnt])
        S.op("act", lambda e: e.activation(out=rstd[:], in_=lnt[:], func=AF.Exp, scale=-0.5), reads=[b_lnt], writes=[b_rstd])

    xTv = xT.rearrange("(k p) t -> p k t", p=128)
    gTv = gT.rearrange("(b n p) t -> p b n t", b=3, p=128)
    x2v = x2T.rearrange("(k p) t -> p k t", p=128) if x2T is not None else None
    xfv = xfT.rearrange("(k p) t -> p k t", p=128) if xfT is not None else None

    for t in range(NT):
        ts = slice(t * 512, (t + 1) * 512)
        for k0 in range(0, NK, 4):
            S.dma("sp", lambda e: e.dma_start(out=xt[:, k0:k0 + 4, :], in_=xTv[:, k0:k0 + 4, ts]), reads=[b_xsrc], writes=b_xt[k0:k0 + 4])
        for i_ in range(2):
            S.dma("sp", lambda e: e.dma_start(out=scr[:, i_:8:2, :], in_=Yl[i_, :, :, ts].rearrange("j p t -> p j t")), reads=[b_Yg], writes=[b_scr])
            S.dma("sp", lambda e: e.dma_start(out=scr[:, 12 + i_:20:2, :], in_=Yl[3 + i_, :, :, ts].rearrange("j p t -> p j t")), reads=[b_Yg], writes=[b_scr])
        S.dma("sp", lambda e: e.dma_start(out=scr[:, 8:12, :], in_=Yl[2, :, :, ts].rearrange("j p t -> p j t")), reads=[b_Yg], writes=[b_scr])
        for ng in range(4):
            wa = nw(); load_w(wa, womla, 8, ng * 512, 512); load_w(wa, wodil, 4, ng * 512, 512, k_off=8)
            wb_ = nw(); load_w(wb_, wosb, 8, ng * 512, 512)
            for m in range(4):
                n = ng * 4 + m
                gi = st["gt"]; st["gt"] ^= 1
                S.dma("sp", lambda e: e.dma_start(out=gt[gi][:], in_=gTv[:, :, n, ts]), reads=[b_gT], writes=[b_gt[gi]])
                banks = []
                for brn, (wi, koff, nk, yoff) in enumerate(((wa, 0, 8, 0), (wa, 8, 4, 8), (wb_, 0, 8, 12))):
                    bk = nbank(); banks.append(bk)
                    for k in range(nk):
                        S.op("pe", lambda e: e.matmul(ps[:, bk, :], lhsT=W[wi][:, koff + k, m * 128:(m + 1) * 128], rhs=scr[:, yoff + k, :],
                                                      start=(k == 0), stop=(k == nk - 1)), reads=[b_W[wi], b_scr], writes=[b_ps[bk]])
                ta = ntmp(); tb = ntmp()
                S.op("dve", lambda e: e.tensor_tensor(out=tmp[ta][:], in0=ps[:, banks[0], :], in1=gt[gi][:, 0, :], op=ALU.mult),
                     reads=[b_ps[banks[0]], b_gt[gi]], writes=[b_tmp[ta]])
                S.op("dve", lambda e: e.tensor_tensor(out=tmp[tb][:], in0=ps[:, banks[1], :], in1=gt[gi][:, 1, :], op=ALU.mult),
                     reads=[b_ps[banks[1]], b_gt[gi]], writes=[b_tmp[tb]])
                S.op("pool", lambda e: e.tensor_tensor(out=tmp[ta][:], in0=tmp[ta][:], in1=tmp[tb][:], op=ALU.add),
                     reads=[b_tmp[ta], b_tmp[tb]], writes=[b_tmp[ta]])
                S.op("dve", lambda e: e.tensor_tensor(out=tmp[tb][:], in0=ps[:, banks[2], :], in1=gt[gi][:, 2, :], op=ALU.mult),
                     reads=[b_ps[banks[2]], b_gt[gi]], writes=[b_tmp[tb]])
                S.op("pool", lambda e: e.tensor_tensor(out=mg[:, n, :], in0=tmp[ta][:], in1=tmp[tb][:], op=ALU.add),
                     reads=[b_tmp[ta], b_tmp[tb]], writes=[b_mg])
        for ng in range(4):
            wi = nw(); load_w(wi, wout, NK, ng * 512, 512)
            for m in range(4):
                n = ng * 4 + m
                bk = nbank()
                for k in range(NK):
                    S.op("pe", lambda e: e.matmul(ps[:, bk, :], lhsT=W[wi][:, k, m * 128:(m + 1) * 128], rhs=mg[:, k, :],
                                                  start=(k == 0), stop=(k == NK - 1)), reads=[b_W[wi], b_mg], writes=[b_ps[bk]])
                S.op("dve", lambda e: e.scalar_tensor_tensor(out=xt[:, n, :], in0=ps[:, bk, :], scalar=modt[:, n:n + 1], in1=xt[:, n, :],
                                                             op0=ALU.mult, op1=ALU.add), reads=[b_ps[bk], b_modt, b_xt[n]], writes=[b_xt[n]])
        if DBG:
            x1v = x1T.rearrange("(k p) t -> p k t", p=128)
            for k0 in range(0, NK, 4):
                S.dma("sp", lambda e: e.dma_start(out=x1v[:, k0:k0 + 4, ts], in_=xt[:, k0:k0 + 4, :]), reads=b_xt[k0:k0 + 4], writes=[b_x1T])
        rms_rstd(xt[:, :, :], b_xt)
        lb = nbank()
        for k in range(NK):
            ta = ntmp(); tb = ntmp()
            S.op("dve", lambda e: e.tensor_tensor(out=tmp[ta][:], in0=xt[:, k, :], in1=rstd[:], op=ALU.mult),
                 reads=[b_xt[k], b_rstd], writes=[b_tmp[ta]])
            S.op("act", lambda e: e.activation(out=tmp[tb][:], in_=tmp[ta][:], func=AF.Identity, scale=s1m[:, k:k + 1], bias=modt[:, 16 + k:17 + k]),
                 reads=[b_tmp[ta], b_s1m, b_modt], writes=[b_tmp[tb]])
            S.op("act", lambda e: e.activation(out=a2b[:, k, :], in_=tmp[ta][:], func=AF.Identity, scale=s1m[:, k:k + 1], bias=modt[:, 16 + k:17 + k]),
                 reads=[b_tmp[ta], b_s1m, b_modt], writes=[b_a2b])
            S.op("pe", lambda e: e.matmul(ps[0:16, lb, :], lhsT=wrt[:, k, :], rhs=tmp[tb][:], start=(k == 0), stop=(k == NK - 1)),
                 reads=[b_wrt, b_tmp[tb]], writes=[b_ps[lb]])
        S.op("act", lambda e: e.activation(out=lt[:], in_=ps[0:16, lb, :], func=AF.Copy), reads=[b_ps[lb]], writes=[b_lt])
        tb_ = nbank()
        for sub in range(4):
            S.op("pe", lambda e: e.matmul(ps[:, tb_, sub * 16:(sub + 1) * 16], lhsT=lt[0:16, sub * 128:(sub + 1) * 128], rhs=idt[0:16, 0:16],
                                          start=True, stop=True), reads=[b_lt, b_idt], writes=[b_ps[tb_]])
        S.op("act", lambda e: e.activation(out=sc[:], in_=ps[:, tb_, 0:64], func=AF.Sigmoid), reads=[b_ps[tb_]], writes=[b_sc])
        for sub in range(4):
            S.op("dve", lambda e: e.tensor_tensor(out=bi[:, sub * 16:(sub + 1) * 16], in0=sc[:, sub * 16:(sub + 1) * 16], in1=brt[:], op=ALU.add),
                 reads=[b_sc, b_brt], writes=[b_bi])
        v3 = lambda tl: tl[:, :].rearrange("p (g e) -> p g e", e=4)
        S.op("dve", lambda e: e.tensor_reduce(out=m1[:], in_=v3(bi), axis=AX.X, op=ALU.max), reads=[b_bi], writes=[b_m1])
        for ee in range(4):
            S.op("dve", lambda e: e.tensor_tensor(out=v3(eq)[:, :, ee], in0=v3(bi)[:, :, ee], in1=m1[:], op=ALU.is_equal),
                 reads=[b_bi, b_m1], writes=[b_eq])
        S.op("dve", lambda e: e.scalar_tensor_tensor(out=bi2[:], in0=eq[:], scalar=-BIGR, in1=bi[:], op0=ALU.mult, op1=ALU.add),
             reads=[b_eq, b_bi], writes=[b_bi2])
        S.op("dve", lambda e: e.tensor_reduce(out=m2[:], in_=v3(bi2), axis=AX.X, op=ALU.max), reads=[b_bi2], writes=[b_m2])
        S.op("dve", lambda e: e.tensor_tensor(out=gs[:], in0=m1[:], in1=m2[:], op=ALU.add), reads=[b_m1, b_m2], writes=[b_gs])
        S.op("dve", lambda e: e.tensor_reduce(out=gmax[:], in_=gs[:, :].rearrange("p (s g) -> p s g", g=4), axis=AX.X, op=ALU.max),
             reads=[b_gs], writes=[b_gmax])
        for g in range(4):
            S.op("dve", lambda e: e.tensor_tensor(out=gsel[:, :].rearrange("p (s g) -> p s g", g=4)[:, :, g],
                                                  in0=gs[:, :].rearrange("p (s g) -> p s g", g=4)[:, :, g], in1=gmax[:], op=ALU.is_equal),
                 reads=[b_gs, b_gmax], writes=[b_gsel])
        for ee in range(4):
            S.op("dve", lambda e: e.tensor_tensor(out=v3(eq)[:, :, ee], in0=v3(bi)[:, :, ee], in1=m2[:], op=ALU.is_ge),
                 reads=[b_bi, b_m2], writes=[b_eq])
            S.op("dve", lambda e: e.tensor_tensor(out=v3(eq)[:, :, ee], in0=v3(eq)[:, :, ee], in1=gsel[:], op=ALU.mult),
                 reads=[b_eq, b_gsel], writes=[b_eq])
        S.op("dve", lambda e: e.tensor_tensor(out=wsel[:], in0=sc[:], in1=eq[:], op=ALU.mult), reads=[b_sc, b_eq], writes=[b_wsel])
        S.op("dve", lambda e: e.tensor_reduce(out=den[:], in_=wsel[:, :].rearrange("p (s e) -> p s e", e=16), axis=AX.X, op=ALU.add),
             reads=[b_wsel], writes=[b_den])
        S.op("dve", lambda e: e.reciprocal(out=den[:], in_=den[:]), reads=[b_den], writes=[b_den])
        for sub in range(4):
            S.op("dve", lambda e: e.tensor_scalar(out=gates[:, sub * 16:(sub + 1) * 16], in0=wsel[:, sub * 16:(sub + 1) * 16],
                                                  scalar1=den[:, sub:sub + 1], scalar2=None, op0=ALU.mult), reads=[b_wsel, b_den], writes=[b_gates])
        if DBG and t == 0:
            for i_, (tl, bb, w_) in enumerate(((sc, b_sc, 64), (bi, b_bi, 64), (m1, b_m1, 16), (m2, b_m2, 16), (gsel, b_gsel, 16), (eq, b_eq, 64), (gates, b_gates, 64), (den, b_den, 4))):
                S.dma("sp", lambda e: e.dma_start(out=dbg[:, i_, 0:w_], in_=tl[:, 0:w_]), reads=[bb], writes=[b_dbg])
            a2v = a2T.rearrange("(k p) t -> p k t", p=128)
            S.dma("sp", lambda e: e.dma_start(out=a2v[:, :, ts], in_=a2b[:, :, :]), reads=[b_a2b], writes=[b_a2T])
        for ex in range(NE):
            w1 = nw(); load_w(w1, wg[ex], NK, 0, 512)
            w2 = nw(); load_w(w2, wu[ex], NK, 0, 512)
            w3 = nw()
            wdv = wd[ex].rearrange("(k p) c -> p k c", p=128)
            W3v = W[w3][:, :, :].rearrange("p (k a) c -> p k (a c)", a=4)
            for k0 in range(0, 4, 2):
                S.dma("pool", lambda e: e.dma_start(out=W3v[:, k0:k0 + 2, :], in_=wdv[:, k0:k0 + 2, :]), writes=[b_W[w3]])
            gb = 6 + (ex % 2)
            for sub in range(4):
                gi = st["gm"]; st["gm"] ^= 1
                S.op("dve", lambda e: e.tensor_scalar(out=Gm[gi][:], in0=onesf[:], scalar1=gates[:, sub * 16 + ex:sub * 16 + ex + 1], scalar2=None,
                                                      op0=ALU.mult), reads=[b_onesf, b_gates], writes=[b_Gm[gi]])
                S.op("pe", lambda e: e.matmul(ps[:, gb, sub * 128:(sub + 1) * 128], lhsT=Gm[gi][:], rhs=idt[:], start=True, stop=True),
                     reads=[b_Gm[gi], b_idt], writes=[b_ps[gb]])
            hi = st["hp"]; st["hp"] ^= 1
            for dc in range(4):
                b1 = nbank()
                for k in range(NK):
                    S.op("pe", lambda e: e.matmul(ps[:, b1, :], lhsT=W[w1][:, k, dc * 128:(dc + 1) * 128], rhs=a2b[:, k, :],
                                                  start=(k == 0), stop=(k == NK - 1)), reads=[b_W[w1], b_a2b], writes=[b_ps[b1]])
                b2 = nbank()
                for k in range(NK):
                    S.op("pe", lambda e: e.matmul(ps[:, b2, :], lhsT=W[w2][:, k, dc * 128:(dc + 1) * 128], rhs=a2b[:, k, :],
                                                  start=(k == 0), stop=(k == NK - 1)), reads=[b_W[w2], b_a2b], writes=[b_ps[b2]])
                ta = ntmp(); tb = ntmp()
                S.op("act", lambda e: e.activation(out=tmp[ta][:], in_=ps[:, b1, :], func=AF.Silu), reads=[b_ps[b1]], writes=[b_tmp[ta]])
                S.op("dve", lambda e: e.tensor_tensor(out=tmp[tb][:], in0=tmp[ta][:], in1=ps[:, b2, :], op=ALU.mult),
                     reads=[b_tmp[ta], b_ps[b2]], writes=[b_tmp[tb]])
                S.op("dve", lambda e: e.tensor_tensor(out=hp[hi][:, dc, :], in0=tmp[tb][:], in1=ps[:, gb, :], op=ALU.mult),
                     reads=[b_tmp[tb], b_ps[gb]], writes=[b_hp[hi]])
            for n in range(NK):
                bk = nbank()
                for dc in range(4):
                    S.op("pe", lambda e: e.matmul(ps[:, bk, :], lhsT=W3v[:, dc, n * 128:(n + 1) * 128], rhs=hp[hi][:, dc, :],
                                                  start=(dc == 0), stop=(dc == 3)), reads=[b_W[w3], b_hp[hi]], writes=[b_ps[bk]])
                S.op("dve", lambda e: e.scalar_tensor_tensor(out=xt[:, n, :], in0=ps[:, bk, :], scalar=modt[:, 48 + n:49 + n], in1=xt[:, n, :],
                                                             op0=ALU.mult, op1=ALU.add), reads=[b_ps[bk], b_modt, b_xt[n]], writes=[b_xt[n]])
        if x2v is not None:
            for k0 in range(0, NK, 4):
                S.dma("sp", lambda e: e.dma_start(out=x2v[:, k0:k0 + 4, ts], in_=xt[:, k0:k0 + 4, :]), reads=b_xt[k0:k0 + 4], writes=[b_x2T])
        if xfv is not None:
            rms_rstd(xt[:, :, :], b_xt)
            for k in range(NK):
                ta = ntmp()
                S.op("dve", lambda e: e.scalar_tensor_tensor(out=tmp[ta][:], in0=xt[:, k, :], scalar=gfint[:, k:k + 1], in1=rstd[:],
                                                             op0=ALU.mult, op1=ALU.mult), reads=[b_xt[k], b_gfint, b_rstd], writes=[b_tmp[ta]])
                S.dma("sp", lambda e: e.dma_start(out=xfv[:, k, ts], in_=tmp[ta][:]), reads=[b_tmp[ta]], writes=[b_xfT])


def build_fused():
    nc = bass.Bass("TRN2", target_bir_lowering=False)
    ext = lambda n, s, dt: nc.dram_tensor(n, s, dt, kind="ExternalInput").ap()
    d = {}
    d['xT'] = ext("xT", [2048, 2048], F32)
    jt = ext("jt", [1, 2], I32)
    d['cT'] = ext("cT", [128, 16], F32)
    d['pos'] = ext("pos", [1, 2048], I32)
    d['invf'] = ext("invf", [64, 1], F32)
    d['sgn'] = ext("sgn", [64, 1], F32)
    d['wada'] = ext("wada", [2, 2048, 12288], F32)
    d['bada_a'] = ext("bada_a", [2, 128, 32], F32)
    d['bada_c'] = ext("bada_c", [2, 128, 64], F32)
    d['gmix'] = ext("gmix", [2, 128, 16], F32)
    d['gq'] = ext("gq", [2, 128, 4], F32)
    d['gkv'] = ext("gkv", [2, 128, 4], F32)
    d['gmoe'] = ext("gmoe", [2, 128, 16], F32)
    d['gfin'] = ext("gfin", [128, 16], F32)
    d['win'] = ext("win", [2, 2048, 14912], F32)
    d['wsw'] = ext("wsw", [2, 2048, 64], F32)
    d['wuq'] = ext("wuq", [2, 512, 1536], F32)
    d['wuqs'] = ext("wuqs", [2, 512, 512], F32)
    d['wukv'] = ext("wukv", [2, 512, 2048], F32)
    d['womla'] = ext("womla", [2, 1024, 2048], F32)
    d['wodil'] = ext("wodil", [2, 512, 2048], F32)
    d['wosb'] = ext("wosb", [2, 1024, 2048], F32)
    d['wout'] = ext("wout", [2, 2048, 2048], F32)
    d['wr'] = ext("wr", [2048, 16], F32)
    d['br'] = ext("br", [128, 16], F32)
    d['wg'] = ext("wg", [2, 16, 2048, 512], F32)
    d['wu'] = ext("wu", [2, 16, 2048, 512], F32)
    d['wd'] = ext("wd", [2, 16, 512, 2048], F32)
    d['ident'] = ext("ident", [128, 128], F32)
    d['maskc'] = ext("maskc", [128, 4, 512], BF16)
    d['masks'] = ext("masks", [128, 4, 512], BF16)
    d['maskd'] = ext("maskd", [128, 256], F32)
    d['m1'] = ext("m1", [128, 128], BF16)
    d['m2'] = ext("m2", [128, 128], BF16)
    d['nslope'] = ext("nslope", [128, 3], F32)
    d['dposk'] = ext("dposk", [3, 128, 64], I32)
    d['dposq'] = ext("dposq", [3, 8192], I32)
    xfT = nc.dram_tensor("xfT", [2048, 2048], F32, kind="ExternalOutput").ap()
    b_xfT = Buf("xfT")
    x2s = nc.dram_tensor("x2scr", [2048, 2048], F32).ap()
    b_x2s = Buf("x2s")
    b_x0 = Buf("x0")
    GROUPS = [[0, 1, 2, 3], [4, 5, 6, 7]]

    S = Sched(nc)
    S.enable_dyn(jt[:, :])
    CH = 128 * 4096
    QKs_h = nc.dram_tensor("QKs", [32, 128, 4096], BF16)
    Vs_h = nc.dram_tensor("Vs", [16, 128, 4096], BF16)
    QKg_h = nc.dram_tensor("QKg", [32, 512, 4096], BF16)
    Vg_h = nc.dram_tensor("Vg", [16, 512, 4096], BF16)
    Ys_h = nc.dram_tensor("Ys", [10, 128, 4096], BF16)
    Yg_h = nc.dram_tensor("Yg", [10, 512, 4096], BF16)
    gsc = nc.dram_tensor("gscr", [6144, 2048], F32).ap()
    QKl_h = nc.dram_tensor("QKl", [4096, 4096], BF16)
    Vl_h = nc.dram_tensor("Vl", [2048, 4096], BF16)
    Yl_h = nc.dram_tensor("Yl", [2560, 2048], BF16)
    for l in range(2):
        b_QKs, b_Vs, b_QKg, b_Vg, b_Ys, b_Yg, b_g = (Buf(n) for n in ("QKs", "Vs", "QKg", "Vg", "Ys", "Yg", "g"))
        b_QKl, b_Vl, b_Yl = Buf("QKl"), Buf("Vl"), Buf("Yl")
        xsrc = d['xT'] if l == 0 else x2s
        b_xsrc = b_x0 if l == 0 else b_x2s
        QKs_v = QKs_h.ap().rearrange("c p (a t) -> (c p a) t", a=4).rearrange("(s u r) t -> s u r t", s=4, u=2)
        Vs_v = Vs_h.ap().rearrange("c p (a d) -> (c p a) d", a=4).rearrange("(s t) d -> s t d", s=4)
        S.begin_phase()
        b_QKs2 = [Buf("QKs0"), Buf("QKs1")]
        b_Vs2 = [Buf("Vs0"), Buf("Vs1")]

        pending = []
        rate = [1]

        def after_sup(sup):
            for sh in range(4):
                for q in range(4):
                    pending.append((QKs_h, QKg_h, sh * 8 + sup * 4 + q, b_QKs2[sup], b_QKg))
            for sh in range(4):
                for q in range(2):
                    pending.append((Vs_h, Vg_h, sh * 4 + sup * 2 + q, b_Vs2[sup], b_Vg))
            if sup == 1:
                rate[0] = 2

        def tick():
            for _ in range(rate[0]):
                if not pending:
                    break
                sh_, gh_, c, br_, bw_ = pending.pop(0)
                S.cc(lambda e: e.collective_compute("AllGather", ALU.bypass, replica_groups=GROUPS, ins=[sh_.ap()[c].opt()],
                                                    outs=[gh_.ap()[c].opt()]), reads=[br_], writes=[bw_])
        emit_A(S, nc, d, l, xsrc, QKs_v, Vs_v, gsc, b_QKs2, b_Vs2, b_g, after_sup=after_sup, tick=tick)
        while pending:
            tick()
        S.end_phase()
        S.begin_phase()
        for hh in range(2):
            S.dma_dyn(QKl_h.ap()[hh * 2048:(hh + 1) * 2048, :], QKg_h, 8 * 4 * CH, hh * 2048 * 4096, [[4096, 2048], [1, 4096]],
                      reads=[b_QKg], writes=[b_QKl])
        S.dma_dyn(Vl_h.ap()[:, :], Vg_h, 4 * 4 * CH, 0, [[4096, 2048], [1, 4096]], reads=[b_Vg], writes=[b_Vl])
        QKl = QKl_h.ap().rearrange("(c r p) (a t) -> c r (p a) t", c=8, r=4, a=4)
        Vl = Vl_h.ap().rearrange("(c r p) (a d) -> c r (p a) d", c=4, r=4, a=4)
        Ys_v = Ys_h.ap().rearrange("(th b) p t -> th b p t", th=2)
        S.barrier()
        emit_B(S, nc, d, QKl, Vl, Ys_v, b_QKl, b_Vl, b_Ys, do_mla=True, do_sb=False, do_dil=False)
        S.end_phase()
        S.begin_phase()
        emit_B(S, nc, d, QKl, Vl, Ys_v, b_QKl, b_Vl, b_Ys, do_mla=False, do_sb=True, do_dil=False)
        for c in (0, 1, 3, 4, 5, 6, 8, 9):
            S.cc(lambda e: e.collective_compute("AllGather", ALU.bypass, replica_groups=GROUPS, ins=[Ys_h.ap()[c].opt()], outs=[Yg_h.ap()[c].opt()]),
                 reads=[b_Ys], writes=[b_Yg])
        S.end_phase(wait_cc=False)
        S.begin_phase()
        emit_B(S, nc, d, QKl, Vl, Ys_v, b_QKl, b_Vl, b_Ys, do_mla=False, do_sb=False, do_dil=True)
        for c in (2, 7):
            S.cc(lambda e: e.collective_compute("AllGather", ALU.bypass, replica_groups=GROUPS, ins=[Ys_h.ap()[c].opt()], outs=[Yg_h.ap()[c].opt()]),
                 reads=[b_Ys], writes=[b_Yg])
        S.end_phase()
        S.begin_phase()
        for b0, nb_ in ((0, 3), (3, 2)):
            S.dma_dyn(Yl_h.ap()[b0 * 512:(b0 + nb_) * 512, :], Yg_h, 1, b0 * 4 * CH, [[4 * CH, nb_], [4096, 512], [1, 2048]],
                      reads=[b_Yg], writes=[b_Yl], which=1)
        Yl = Yl_h.ap().rearrange("(b j p) t -> b j p t", b=5, j=4)
        emit_C(S, nc, d, l, Yl, gsc, xsrc, x2s if l == 0 else None, xfT if l == 1 else None,
               b_Yl, b_g, b_xsrc, b_x2s, b_xfT)
        S.end_phase()
    S.close()
    return nc


_PROG = {}


def _f(a):
    return np.ascontiguousarray(a)


def kernel(**inputs):
    inp = {k: np.asarray(v) for k, v in inputs.items()}
    B_, S_ = inp['x'].shape[:2]
    cores = list(range(8))
    pos = inp['positions'].astype(np.int32)
    perm = [np.concatenate([np.arange(s, S_, r) for s in range(r)]) for r in RATES]
    slopes = (np.float32(2.0) ** (np.float32(-8.0) * np.arange(1, 13, dtype=np.float32) / np.float32(12))).reshape(3, 4)
    half = 32
    invf = (np.float32(10000.0) ** (-(np.arange(half, dtype=np.float32)) / np.float32(half))).astype(np.float32)
    wu_ = inp['w_uq']
    wuqs = np.stack([np.concatenate([np.concatenate([wu_[l][:, h * 192 + 160:h * 192 + 192], wu_[l][:, h * 192 + 128:h * 192 + 160]], axis=1)
                                     for h in range(8)], axis=1) for l in range(2)])
    wi_ = inp['w_in']
    lay = lambda a, n: _f(a.reshape(a.shape[0], n, 128).transpose(0, 2, 1))
    shared = {
        'invf': _f(np.concatenate([invf, invf])[:, None]),
        'sgn': _f(np.concatenate([-np.ones(32, np.float32), np.ones(32, np.float32)])[:, None]),
        'wada': _f(inp['w_ada']),
        'bada_a': lay(inp['b_ada'][:, :4096], 32),
        'bada_c': lay(inp['b_ada'][:, 4096:], 64),
        'gmix': lay(inp['g_mix'], 16), 'gq': lay(inp['g_q'], 4), 'gkv': lay(inp['g_kv'], 4), 'gmoe': lay(inp['g_moe'], 16),
        'gfin': _f(inp['g_final'].reshape(16, 128).T),
        'win': _f(wi_), 'wsw': _f(np.concatenate([wi_[:, :, 1056:1088], wi_[:, :, 1024:1056]], axis=2)),
        'wuq': _f(wu_), 'wuqs': _f(wuqs), 'wukv': _f(inp['w_ukv']),
        'womla': _f(inp['w_o_mla']), 'wodil': _f(inp['w_o_dil']), 'wosb': _f(inp['w_o_sb']), 'wout': _f(inp['w_out']),
        'wr': _f(inp['w_router']), 'br': _f(np.broadcast_to(inp['b_router'][None, :], (128, 16))),
        'wg': _f(inp['w_gate']), 'wu': _f(inp['w_up']), 'wd': _f(inp['w_down']), 'ident': np.eye(128, dtype=np.float32),
    }
    shared.update(consts_B())
    maps = []
    for c in cores:
        b, j = c // 4, c % 4
        m = dict(shared)
        m['xT'] = _f(inp['x'][b, j * 2048:(j + 1) * 2048, :].T)
        m['jt'] = np.array([[j, (j // 2) * 5 * 4 * 128 * 4096 + (j % 2) * 2048]], np.int32)
        m['cT'] = _f(inp['c'][b].reshape(16, 128).T)
        m['pos'] = _f(pos[b, j * 2048:(j + 1) * 2048][None, :])
        pp = np.stack([pos[b][perm[g]] for g in range(3)]).astype(np.int32)
        m['dposq'] = _f(pp)
        m['dposk'] = _f(pp.reshape(3, S_ // 128, 128).transpose(0, 2, 1))
        m['nslope'] = _f(np.broadcast_to(-slopes[:, j][None, :], (128, 3)).astype(np.float32))
        maps.append(m)
    if "f" not in _PROG:
        _PROG["f"] = build_fused()
    res = run_bass_kernel_spmd(_PROG["f"], maps, core_ids=cores).results
    out = np.empty(inp['x'].shape, dtype=np.float32)
    for c in cores:
        b, j = c // 4, c % 4
        out[b, j * 2048:(j + 1) * 2048, :] = np.asarray(res[c]['xfT']).T
    return out
```

```python
import math
import numpy as np
import concourse.bass as bass
import concourse.mybir as mybir
from concourse.bass_utils import run_bass_kernel_spmd

F32 = mybir.dt.float32
BF16 = mybir.dt.bfloat16
I32 = mybir.dt.int32
AF = mybir.ActivationFunctionType
ALU = mybir.AluOpType
AX = mybir.AxisListType


class Buf:
    __slots__ = ("name", "w", "r")

    def __init__(self, name):
        self.name = name
        self.w = None
        self.r = {}


class _Rec:
    def __init__(self):
        self.call = None

    def __getattr__(self, name):
        def f(*a, **kw):
            self.call = (name, a, kw)
            return self
        return f


def _record(fn):
    r = _Rec()
    fn(r)
    assert r.call is not None
    return r.call


class Sched:
    ENGS = ("pe", "act", "dve", "pool", "sp")
    NDMA = 12

    def __init__(self, nc):
        self.nc = nc
        self.streams = {e: [] for e in self.ENGS}
        self.cnt = {e: 0 for e in self.ENGS}
        self.seen = {e: {} for e in self.ENGS}
        self.dma_i = {"sp": 0, "pool": 0, "act": 0}
        self.dma_val = {}
        self.sems = {}
        self._ctx = []
        self._perm = []
        self.cc_val = {}
        self._phase_mark = 0
        self.uses_dyn = False
        self._phase_no = 0
        self.jt_ap = None
        self.jsb = None
        self.jt_cnt = 0
        for e in ("pe", "act", "dve", "pool"):
            self._mk_sem("E_" + e)
        for q in ("sp", "pool", "act"):
            for k in range(self.NDMA):
                self._mk_sem("D_%s%d" % (q, k))
                self.dma_val["D_%s%d" % (q, k)] = 0

    def _mk_sem(self, key):
        cm = self.nc.semaphore(key)
        self.sems[key] = cm.__enter__()
        self._perm.append(cm)

    _mk_sem_perm = _mk_sem

    def enable_dyn(self, jt_ap):
        self.jt_ap = jt_ap
        self._mk_sem("JT")
        cm = self.nc.sbuf_tensor("jsb", [1, 2], I32)
        self.jsb = cm.__enter__()
        self._perm.append(cm)

    def sbuf(self, name, shape, dtype):
        cm = self.nc.sbuf_tensor("%s_p%d" % (name, self._phase_no), shape, dtype)
        t = cm.__enter__()
        self._ctx.append(cm)
        return t

    def psum(self, name, shape, dtype):
        cm = self.nc.psum_tensor("%s_p%d" % (name, self._phase_no), shape, dtype)
        t = cm.__enter__()
        self._ctx.append(cm)
        return t

    def _deps(self, eng, reads, writes):
        deps = {}

        def add(tok):
            if tok is None:
                return
            k, v = tok
            if deps.get(k, -1) < v:
                deps[k] = v
        for b in reads:
            add(b.w)
        for b in writes:
            add(b.w)
            for k, v in b.r.items():
                add((k, v))
        out = []
        seen = self.seen[eng]
        for k, v in deps.items():
            if eng == "pe" and k == "E_pe":
                continue
            if seen.get(k, -1) >= v:
                continue
            seen[k] = v
            out.append((k, v))
        return out

    def _mark(self, tok, reads, writes):
        k, v = tok
        for b in reads:
            if b.r.get(k, -1) < v:
                b.r[k] = v
        for b in writes:
            b.w = tok
            b.r = {}

    def op(self, eng, fn, reads=(), writes=()):
        waits = self._deps(eng, reads, writes)
        self.cnt[eng] += 1
        tok = ("E_" + eng, self.cnt[eng])
        self.streams[eng].append((waits, _record(fn), tok[0], 1))
        self._mark(tok, reads, writes)

    def dma(self, q, fn, reads=(), writes=()):
        i = self.dma_i[q]
        self.dma_i[q] += 1
        key = "D_%s%d" % (q, i % self.NDMA)
        waits = self._deps(q, reads, writes)
        prev = self.dma_val[key]
        if prev > 0 and self.seen[q].get(key, -1) < prev:
            self.seen[q][key] = prev
            waits.append((key, prev))
        self.dma_val[key] = prev + 16
        tok = (key, prev + 16)
        self.streams[q].append((waits, _record(fn), key, 16))
        self._mark(tok, reads, writes)

    def begin_phase(self):
        self._phase_mark = len(self._ctx)
        self._phase_no += 1

    def barrier(self, wait_cc=True):
        allv = {}
        for e in ("pe", "act", "dve", "pool"):
            if self.cnt[e] > 0:
                allv["E_" + e] = self.cnt[e]
        for k, v in self.dma_val.items():
            if v > 0:
                allv[k] = v
        for k, v in self.cc_val.items():
            if v > 0 and wait_cc:
                allv[k] = v
        for eng in self.ENGS:
            waits = []
            for k, v in allv.items():
                if self.seen[eng].get(k, -1) < v:
                    self.seen[eng][k] = v
                    waits.append((k, v))
            self.streams[eng].append((waits, None, None, 0))

    def end_phase(self, wait_cc=True):
        self.barrier(wait_cc)
        self.emit()
        self.streams = {e: [] for e in self.ENGS}
        while len(self._ctx) > self._phase_mark:
            self._ctx.pop().__exit__(None, None, None)

    def cc(self, fn, reads=(), writes=()):
        key = "CC"
        if key not in self.sems:
            self._mk_sem_perm(key)
            self.cc_val[key] = 0
        n = self.cc_val[key] + 1
        self.cc_val[key] = n
        waits = self._deps("pool", reads, writes)
        self.streams["pool"].append((waits, _record(fn), key, None))
        self._mark((key, n), reads, writes)

    def dma_dyn(self, out_ap, tensor, jmul, const, ap_list, reads=(), writes=(), which=0):
        q = "sp"
        i = self.dma_i[q]
        self.dma_i[q] += 1
        key = "D_%s%d" % (q, i % self.NDMA)
        waits = self._deps(q, reads, writes)
        prev = self.dma_val[key]
        if prev > 0 and self.seen[q].get(key, -1) < prev:
            self.seen[q][key] = prev
            waits.append((key, prev))
        self.dma_val[key] = prev + 16
        tok = (key, prev + 16)
        self.streams[q].append((waits, ("__dyn__", (out_ap, tensor, int(jmul), int(const), [list(x) for x in ap_list], which), {}), key, 16))
        self._mark(tok, reads, writes)
        self.uses_dyn = True

    def final_wait(self, eng, bufs):
        waits = self._deps(eng, bufs, ())
        self.streams[eng].append((waits, None, None, 0))

    def emit(self):
        nc = self.nc
        sems = self.sems
        streams = self.streams

        def run(engine, lst, regs=None):
            for waits, fn, key, inc in lst:
                for k, v in waits:
                    engine.wait_ge(sems[k], v)
                if fn is not None:
                    name, a, kw = fn
                    if name == "__dyn__":
                        out_ap, tensor, jmul, const, ap_list, which = a
                        rj, ro = regs[which], regs[2]
                        engine.reg_mul(ro, rj, jmul)
                        engine.reg_add(ro, ro, const)
                        ins = engine.dma_start(out=out_ap, in_=bass.AP(tensor, ro, ap_list))
                    else:
                        ins = getattr(engine, name)(*a, **kw)
                    if inc is None:
                        ins.then_inc(sems[key])
                    else:
                        ins.then_inc(sems[key], inc)

        with nc.Block() as block:
            @block.tensor
            def _(e):
                run(e, streams["pe"])

            @block.scalar
            def _(e):
                run(e, streams["act"])

            @block.vector
            def _(e):
                run(e, streams["dve"])

            @block.gpsimd
            def _(e):
                run(e, streams["pool"])

            @block.sync
            def _(e):
                if any(fn is not None and fn[0] == "__dyn__" for _, fn, _, _ in streams["sp"]):
                    self.jt_cnt += 1
                    with e.register("rj%d" % self.jt_cnt) as rj, e.register("ry%d" % self.jt_cnt) as ry, e.register("ro%d" % self.jt_cnt) as ro:
                        e.dma_start(out=self.jsb[:, :], in_=self.jt_ap).then_inc(sems["JT"], 16)
                        e.wait_ge(sems["JT"], 16 * self.jt_cnt)
                        e.reg_load(rj, self.jsb[0:1, 0:1])
                        e.reg_load(ry, self.jsb[0:1, 1:2])
                        run(e, streams["sp"], (rj, ry, ro))
                else:
                    run(e, streams["sp"])

    def close(self):
        for cm in reversed(self._ctx):
            cm.__exit__(None, None, None)
        self._ctx = []
        for cm in reversed(self._perm):
            cm.__exit__(None, None, None)
        self._perm = []


D = 2048
NK = 16
TS = 1024
TP = 128
EPS = 1e-6
TWO_PI = 2.0 * math.pi
C1 = 6.28125
C2 = TWO_PI - C1


def emit_A(S, nc, d, l, xT, QKs, Vs, gT, b_QKs_l, b_Vs_l, b_gT, ntok=2048, after_sup=None, tick=None):
    cT = d['cT']; wada = d['wada'][l]; bada = d['bada_a'][l]; gmix = d['gmix'][l]; win = d['win'][l]; wsw = d['wsw'][l]
    gq = d['gq'][l]; gkv = d['gkv'][l]; wuq = d['wuq'][l]; wuqs = d['wuqs'][l]; wukv = d['wukv'][l]
    pos = d['pos']; invf = d['invf']; sgn = d['sgn']
    b_QKs = b_QKs_l[0]; b_Vs = b_Vs_l[0]
    b_projT = b_QKs; b_mlaq = b_QKs; b_mlakv = b_QKs; b_mlakr = b_QKs
    aT = S.sbuf("aT", [128, NK, TS], BF16); b_aT = [Buf("aT%d" % i) for i in range(TS // TP)]
    wb = [S.sbuf("wb%d" % i, [128, NK, 512], BF16) for i in range(2)]; b_wb = [Buf("wb0"), Buf("wb1")]
    xt = S.sbuf("xt", [128, NK, TP], F32); b_xt = Buf("xt")
    sq = S.sbuf("sq", [128, NK, TP], BF16); b_sq = Buf("sq")
    rstd = S.sbuf("rstd", [128, 512], F32); b_rstd = Buf("rstd")
    lnt = S.sbuf("lnt", [128, 512], F32); b_lnt = Buf("lnt")
    tmp = [S.sbuf("tmp%d" % i, [128, 512], F32) for i in range(2)]; b_tmp = [Buf("tmp0"), Buf("tmp1")]
    ones = S.sbuf("ones", [128, 128], BF16); b_ones = Buf("ones")
    cin = S.sbuf("cin", [128, NK], F32); b_cin = Buf("cin")
    cact = S.sbuf("cact", [128, NK], BF16); b_cact = Buf("cact")
    badat = S.sbuf("badat", [128, 32], F32); b_badat = Buf("badat")
    gmixt = S.sbuf("gmixt", [128, NK], F32); b_gmixt = Buf("gmixt")
    modt = S.sbuf("modt", [128, 32], F32); b_modt = Buf("modt")
    s1 = S.sbuf("s1", [128, NK], F32); b_s1 = Buf("s1")
    cq = S.sbuf("cq", [128, 4, TS], F32); b_cq = Buf("cq")
    ckv = S.sbuf("ckv", [128, 4, TS], F32); b_ckv = Buf("ckv")
    kr = S.sbuf("kr", [64, TS], F32); b_kr = Buf("kr")
    krs = S.sbuf("krs", [64, TS], F32); b_krs = Buf("krs")
    wswb = S.sbuf("wswb", [128, NK, 64], BF16); b_wswb = Buf("wswb")
    wuqb = S.sbuf("wuqb", [128, 4, 1536], BF16); b_wuqb = Buf("wuqb")
    wuqsb = S.sbuf("wuqsb", [128, 4, 512], BF16); b_wuqsb = Buf("wuqsb")
    wukvb = S.sbuf("wukvb", [128, 4, 2048], BF16); b_wukvb = Buf("wukvb")
    gqt = S.sbuf("gqt", [128, 4], F32); b_gqt = Buf("gqt")
    gkvt = S.sbuf("gkvt", [128, 4], F32); b_gkvt = Buf("gkvt")
    ob = [S.sbuf("ob%d" % i, [128, TS], BF16) for i in range(2)]; b_ob = [Buf("ob0"), Buf("ob1")]
    of = [S.sbuf("of%d" % i, [128, TS], F32) for i in range(2)]; b_of = [Buf("of0"), Buf("of1")]
    lat = S.sbuf("lat", [128, 4, 512], BF16); b_lat = Buf("lat")
    posi = S.sbuf("posi", [64, 512], I32); b_posi = Buf("posi")
    ang = S.sbuf("ang", [64, 512], F32); b_ang = Buf("ang")
    kf = S.sbuf("kf", [64, 512], F32); b_kf = Buf("kf")
    ki = posi; b_ki = b_posi
    rr = S.sbuf("rr", [64, 512], F32); b_rr = Buf("rr")
    rc = S.sbuf("rc", [64, 512], F32); b_rc = Buf("rc")
    mm = S.sbuf("mm", [64, 512], F32); b_mm = Buf("mm")
    CS = S.sbuf("CS", [64, 512], F32); b_CS = Buf("CS")
    SN = S.sbuf("SN", [64, 512], F32); b_SN = Buf("SN")
    invft = S.sbuf("invft", [64, 1], F32); b_invft = Buf("invft")
    sgnt = S.sbuf("sgnt", [64, 1], F32); b_sgnt = Buf("sgnt")
    t1 = ang; b_t1 = b_ang
    t2 = kf; b_t2 = b_kf
    ps = S.psum("ps", [128, 8, 512], F32); b_ps = [Buf("ps%d" % i) for i in range(8)]
    st = {"bank": 0, "w": 0, "ob": 0, "of": 0, "tmp": 0, "ev": 0}

    def nbank():
        i = st["bank"]; st["bank"] = (i + 1) % 8
        return i

    def load_w(dst, bdst, src, nk, c0, ncols):
        srcv = src.rearrange("(k p) c -> p k c", p=128)
        h = max(1, nk // 2)
        for k0 in range(0, nk, h):
            S.dma("pool", lambda e, k0=k0: e.dma_start(out=dst[:, k0:k0 + h, 0:ncols],
                                                      in_=srcv[:, k0:k0 + h, c0:c0 + ncols]),
                  writes=[bdst])

    S.op("dve", lambda e: e.memset(ones[:], 1.0), writes=[b_ones])
    S.dma("sp", lambda e: e.dma_start(out=cin[:], in_=cT[:, :]), writes=[b_cin])
    S.dma("sp", lambda e: e.dma_start(out=badat[:], in_=bada[:, :]), writes=[b_badat])
    S.dma("sp", lambda e: e.dma_start(out=gmixt[:], in_=gmix[:, :]), writes=[b_gmixt])
    S.dma("sp", lambda e: e.dma_start(out=gqt[:], in_=gq[:, :]), writes=[b_gqt])
    S.dma("sp", lambda e: e.dma_start(out=gkvt[:], in_=gkv[:, :]), writes=[b_gkvt])
    S.dma("sp", lambda e: e.dma_start(out=invft[:], in_=invf[:, :]), writes=[b_invft])
    S.dma("sp", lambda e: e.dma_start(out=sgnt[:], in_=sgn[:, :]), writes=[b_sgnt])
    S.op("act", lambda e: e.activation(out=cact[:], in_=cin[:], func=AF.Silu), reads=[b_cin], writes=[b_cact])

    mb = nbank()
    for g in range(8):
        wi = st["w"]; st["w"] ^= 1
        load_w(wb[wi], b_wb[wi], wada, NK, g * 512, 512)
        for m in range(4):
            n = g * 4 + m
            for k in range(NK):
                S.op("pe", lambda e, wi=wi, m=m, k=k, n=n: e.matmul(
                    ps[:, mb, n:n + 1], lhsT=wb[wi][:, k, m * 128:(m + 1) * 128], rhs=cact[:, k:k + 1],
                    start=(k == 0), stop=(k == NK - 1)), reads=[b_wb[wi], b_cact], writes=[b_ps[mb]])
    S.op("dve", lambda e: e.tensor_tensor(out=modt[:], in0=ps[:, mb, 0:32], in1=badat[:], op=ALU.add),
         reads=[b_ps[mb], b_badat], writes=[b_modt])
    S.op("dve", lambda e: e.scalar_tensor_tensor(out=s1[:], in0=modt[:, 16:32], scalar=1.0, in1=gmixt[:],
                                                 op0=ALU.add, op1=ALU.mult),
         reads=[b_modt, b_gmixt], writes=[b_s1])

    load_w(wswb, b_wswb, wsw, NK, 0, 64)
    load_w(wuqb, b_wuqb, wuq, 4, 0, 1536)
    load_w(wuqsb, b_wuqsb, wuqs, 4, 0, 512)
    load_w(wukvb, b_wukvb, wukv, 4, 0, 2048)

    def rms_rstd(src_sq, bsrc, nk, width, dim):
        bk = nbank()
        for k in range(nk):
            S.op("pe", lambda e, k=k: e.matmul(ps[:, bk, 0:width], lhsT=ones[:], rhs=src_sq[:, k, 0:width],
                                               start=(k == 0), stop=(k == nk - 1)),
                 reads=[bsrc, b_ones], writes=[b_ps[bk]])
        S.op("act", lambda e: e.activation(out=lnt[:, 0:width], in_=ps[:, bk, 0:width], func=AF.Ln,
                                           scale=1.0 / dim, bias=EPS), reads=[b_ps[bk]], writes=[b_lnt])
        S.op("act", lambda e: e.activation(out=rstd[:, 0:width], in_=lnt[:, 0:width], func=AF.Exp, scale=-0.5),
             reads=[b_lnt], writes=[b_rstd])

    xTv = xT.rearrange("(k p) t -> p k t", p=128)
    for sup in range(ntok // TS):
        t0s = sup * TS
        b_QKs = b_QKs_l[sup]; b_Vs = b_Vs_l[sup]
        for pt in range(TS // TP):
            tok0 = t0s + pt * TP
            for k0 in (0, 8):
                S.dma("sp", lambda e, k0=k0, tok0=tok0: e.dma_start(out=xt[:, k0:k0 + 8, :],
                                                                    in_=xTv[:, k0:k0 + 8, tok0:tok0 + TP]),
                      writes=[b_xt])
            S.op("act", lambda e: e.activation(out=sq[:], in_=xt[:], func=AF.Square), reads=[b_xt], writes=[b_sq])
            rms_rstd(sq, b_sq, NK, TP, float(D))
            for k in range(NK):
                ti = st["tmp"]; st["tmp"] ^= 1
                S.op("dve", lambda e, k=k, ti=ti: e.tensor_tensor(out=tmp[ti][:, 0:TP], in0=xt[:, k, :],
                                                                  in1=rstd[:, 0:TP], op=ALU.mult),
                     reads=[b_xt, b_rstd], writes=[b_tmp[ti]])
                S.op("act", lambda e, k=k, ti=ti, pt=pt: e.activation(
                    out=aT[:, k, pt * TP:(pt + 1) * TP], in_=tmp[ti][:, 0:TP], func=AF.Identity,
                    scale=s1[:, k:k + 1], bias=modt[:, k:k + 1]),
                    reads=[b_tmp[ti], b_s1, b_modt], writes=[b_aT[pt]])

        def gemm_group(src, c0, ncols, epilogue):
            wi = st["w"]; st["w"] ^= 1
            load_w(wb[wi], b_wb[wi], src, NK, c0, ncols)
            if tick is not None:
                tick()
            for m in range((ncols + 127) // 128):
                mc = min(128, ncols - m * 128)
                for t in range(TS // 512):
                    bk = nbank()
                    for k in range(NK):
                        S.op("pe", lambda e, wi=wi, m=m, mc=mc, t=t, k=k, bk=bk: e.matmul(
                            ps[0:mc, bk, :], lhsT=wb[wi][:, k, m * 128:m * 128 + mc],
                            rhs=aT[:, k, t * 512:(t + 1) * 512], start=(k == 0), stop=(k == NK - 1)),
                            reads=[b_wb[wi]] + b_aT[4 * t:4 * t + 4], writes=[b_ps[bk]])
                    epilogue(m, mc, t, bk)

        def evac(out_ap, bout, bk, mc, scale=1.0, func=None):
            st["ev"] ^= 1
            if func is not None or st["ev"]:
                f = func if func is not None else AF.Copy
                S.op("act", lambda e: e.activation(out=out_ap, in_=ps[0:mc, bk, :], func=f, scale=scale),
                     reads=[b_ps[bk]], writes=[bout])
            else:
                S.op("dve", lambda e: e.tensor_scalar(out=out_ap, in0=ps[0:mc, bk, :], scalar1=scale, scalar2=None,
                                                      op0=ALU.mult), reads=[b_ps[bk]], writes=[bout])

        gemm_group(win, 0, 512, lambda m, mc, t, bk: evac(cq[:, m, t * 512:(t + 1) * 512], b_cq, bk, mc))
        gemm_group(win, 512, 512, lambda m, mc, t, bk: evac(ckv[:, m, t * 512:(t + 1) * 512], b_ckv, bk, mc))
        gemm_group(win, 1024, 64, lambda m, mc, t, bk: evac(kr[:, t * 512:(t + 1) * 512], b_kr, bk, mc))
        gemm_group(wsw, 0, 64, lambda m, mc, t, bk: evac(krs[:, t * 512:(t + 1) * 512], b_krs, bk, mc))

        def out_bf(dst, bdst, row0, scale):
            cur = {}

            def ep(m, mc, t, bk):
                if t == 0:
                    cur["i"] = st["ob"]; st["ob"] ^= 1
                i = cur["i"]
                evac(ob[i][:, t * 512:(t + 1) * 512], b_ob[i], bk, mc, scale=scale)
                if t == TS // 512 - 1:
                    r = row0 + m * 128
                    S.dma("sp", lambda e, i=i, r=r: e.dma_start(out=dst[r:r + 128, t0s:t0s + TS], in_=ob[i][:, :]),
                          reads=[b_ob[i]], writes=[bdst])
            return ep

        def out_f32(dst, bdst, row0, func):
            cur = {}

            def ep(m, mc, t, bk):
                if t == 0:
                    cur["i"] = st["of"]; st["of"] ^= 1
                i = cur["i"]
                evac(of[i][:, t * 512:(t + 1) * 512], b_of[i], bk, mc, func=func)
                if t == TS // 512 - 1:
                    r = row0 + m * 128
                    S.dma("sp", lambda e, i=i, r=r: e.dma_start(out=dst[r:r + 128, t0s:t0s + TS], in_=of[i][:, :]),
                          reads=[b_of[i]], writes=[bdst])
            return ep

        sc = 128.0 ** -0.5
        def out_qk(rowfn, scale):
            cur = {}

            def ep(m, mc, t, bk):
                if t == 0:
                    cur["i"] = st["ob"]; st["ob"] ^= 1
                i = cur["i"]
                evac(ob[i][:, t * 512:(t + 1) * 512], b_ob[i], bk, mc, scale=scale)
                if t == TS // 512 - 1:
                    sh, r0 = rowfn(m)
                    S.dma("sp", lambda e: e.dma_start(out=QKs[sh, sup, r0:r0 + 128, :], in_=ob[i][:, :]),
                          reads=[b_ob[i]], writes=[b_QKs])
            return ep

        def gemm_group_tm(c0, store):
            wi = st["w"]; st["w"] ^= 1
            load_w(wb[wi], b_wb[wi], win, NK, c0, 512)
            if tick is not None:
                tick()
            for s_ in range(TS // 128):
                bk = nbank()
                for k in range(NK):
                    S.op("pe", lambda e: e.matmul(ps[:, bk, :], lhsT=aT[:, k, s_ * 128:(s_ + 1) * 128], rhs=wb[wi][:, k, 0:512],
                                                  start=(k == 0), stop=(k == NK - 1)), reads=[b_wb[wi], b_aT[s_]], writes=[b_ps[bk]])
                oi = st["ob"]; st["ob"] ^= 1
                evac(ob[oi][:, 0:512], b_ob[oi], bk, 128)
                store(s_, oi)

        for g in range(3):
            gemm_group(win, 1088 + g * 512, 512, out_qk(lambda m, g=g: (m, g * 128), sc))
        for g in range(3):
            gemm_group(win, 1088 + 1536 + g * 512, 512, out_qk(lambda m, g=g: (m, 384 + g * 128), 1.0))
        for g in range(3):
            def st_dv(s_, oi, g=g):
                tk = t0s + s_ * 128
                S.dma("sp", lambda e: e.dma_start(out=Vs[:, tk:tk + 128, g * 128:(g + 1) * 128].rearrange("h p c -> p h c"),
                                                  in_=ob[oi][:, 0:512].rearrange("p (h c) -> p h c", c=128)),
                      reads=[b_ob[oi]], writes=[b_Vs])
            gemm_group_tm(1088 + 3072 + g * 512, st_dv)
        for gi in range(2):
            gemm_group(win, 5696 + gi * 512, 512, out_qk(lambda m, gi=gi: ((4 * gi + m) // 2, 768 + (m % 2) * 128), sc))
        for gi in range(2):
            gemm_group(win, 5696 + 1024 + gi * 512, 512, out_qk(lambda m, gi=gi: ((4 * gi + m) // 2, 1024 + (m % 2) * 128), 1.0))
        for gi in range(2):
            def st_sv(s_, oi, gi=gi):
                tk = t0s + s_ * 128
                S.dma("sp", lambda e: e.dma_start(out=Vs[2 * gi:2 * gi + 2, tk:tk + 128, 384:640].rearrange("j p c -> p j c"),
                                                  in_=ob[oi][:, 0:512].rearrange("p (j c) -> p j c", c=256)),
                      reads=[b_ob[oi]], writes=[b_Vs])
            gemm_group_tm(5696 + 2048 + gi * 512, st_sv)

        scm = 192.0 ** -0.5
        for tt in range(TS // 512):
            tok0 = t0s + tt * 512
            tsl = slice(tt * 512, (tt + 1) * 512)
            S.dma("sp", lambda e, tok0=tok0: e.dma_start(out=posi[:], in_=pos[0:1, tok0:tok0 + 512].partition_broadcast(64)),
                  writes=[b_posi])
            S.op("dve", lambda e: e.tensor_copy(out=ang[:], in_=posi[:]), reads=[b_posi], writes=[b_ang])
            S.op("dve", lambda e: e.tensor_scalar(out=ang[:], in0=ang[:], scalar1=invft[:, 0:1], scalar2=None, op0=ALU.mult),
                 reads=[b_ang, b_invft], writes=[b_ang])
            S.op("dve", lambda e: e.tensor_scalar(out=kf[:], in0=ang[:], scalar1=1.0 / TWO_PI, scalar2=None, op0=ALU.mult),
                 reads=[b_ang], writes=[b_kf])
            S.op("dve", lambda e: e.tensor_copy(out=ki[:], in_=kf[:]), reads=[b_kf], writes=[b_ki])
            S.op("dve", lambda e: e.tensor_copy(out=kf[:], in_=ki[:]), reads=[b_ki], writes=[b_kf])
            S.op("dve", lambda e: e.scalar_tensor_tensor(out=rr[:], in0=kf[:], scalar=-C1, in1=ang[:], op0=ALU.mult, op1=ALU.add),
                 reads=[b_kf, b_ang], writes=[b_rr])
            S.op("dve", lambda e: e.scalar_tensor_tensor(out=rr[:], in0=kf[:], scalar=-C2, in1=rr[:], op0=ALU.mult, op1=ALU.add),
                 reads=[b_kf, b_rr], writes=[b_rr])

            def wrap(r, br):
                S.op("dve", lambda e: e.tensor_scalar(out=mm[:], in0=r[:], scalar1=math.pi, scalar2=-TWO_PI, op0=ALU.is_gt, op1=ALU.mult),
                     reads=[br], writes=[b_mm])
                S.op("dve", lambda e: e.tensor_tensor(out=r[:], in0=r[:], in1=mm[:], op=ALU.add), reads=[br, b_mm], writes=[br])
                S.op("dve", lambda e: e.tensor_scalar(out=mm[:], in0=r[:], scalar1=-math.pi, scalar2=TWO_PI, op0=ALU.is_lt, op1=ALU.mult),
                     reads=[br], writes=[b_mm])
                S.op("dve", lambda e: e.tensor_tensor(out=r[:], in0=r[:], in1=mm[:], op=ALU.add), reads=[br, b_mm], writes=[br])
                S.op("dve", lambda e: e.tensor_scalar(out=r[:], in0=r[:], scalar1=3.1415925, scalar2=-3.1415925, op0=ALU.min, op1=ALU.max),
                     reads=[br], writes=[br])
            wrap(rr, b_rr)
            S.op("dve", lambda e: e.tensor_scalar(out=rc[:], in0=rr[:], scalar1=math.pi / 2, scalar2=None, op0=ALU.add),
                 reads=[b_rr], writes=[b_rc])
            wrap(rc, b_rc)
            S.op("act", lambda e: e.activation(out=CS[:], in_=rc[:], func=AF.Sin), reads=[b_rc], writes=[b_CS])
            S.op("act", lambda e: e.activation(out=SN[:], in_=rr[:], func=AF.Sin, scale=sgnt[:, 0:1]), reads=[b_rr, b_sgnt], writes=[b_SN])

            def rope_out(src_r, bsr, src_s, bss, scale, dst_ap, bdst):
                S.op("dve", lambda e: e.scalar_tensor_tensor(out=t1[:], in0=src_r, scalar=scale, in1=CS[:], op0=ALU.mult, op1=ALU.mult),
                     reads=[bsr, b_CS], writes=[b_t1])
                S.op("dve", lambda e: e.scalar_tensor_tensor(out=t2[:], in0=src_s, scalar=scale, in1=SN[:], op0=ALU.mult, op1=ALU.mult),
                     reads=[bss, b_SN], writes=[b_t2])
                S.op("dve", lambda e: e.tensor_tensor(out=dst_ap, in0=t1[:], in1=t2[:], op=ALU.add),
                     reads=[b_t1, b_t2], writes=[bdst])

            oi = st["ob"]; st["ob"] ^= 1
            rope_out(kr[:, tsl], b_kr, krs[:, tsl], b_krs, 1.0, ob[oi][0:64, 0:512], b_ob[oi])
            for sh in range(4):
                S.dma("sp", lambda e: e.dma_start(out=QKs[sh, sup, 1920:1984, tok0 - t0s:tok0 - t0s + 512], in_=ob[oi][0:64, 0:512]),
                      reads=[b_ob[oi]], writes=[b_QKs])

            def latent_norm(src, bsrc, gt, bgt):
                sqv = sq[:, :, :].rearrange("p (k a) t -> p k (a t)", a=4)
                S.op("act", lambda e: e.activation(out=sqv, in_=src[:, :, tsl], func=AF.Square), reads=[bsrc], writes=[b_sq])
                rms_rstd(sqv, b_sq, 4, 512, 512.0)
                for k in range(4):
                    S.op("dve", lambda e, k=k: e.scalar_tensor_tensor(out=lat[:, k, :], in0=src[:, k, tsl], scalar=gt[:, k:k + 1],
                                                                      in1=rstd[:, :], op0=ALU.mult, op1=ALU.mult),
                         reads=[bsrc, bgt, b_rstd], writes=[b_lat])

            latent_norm(cq, b_cq, gqt, b_gqt)
            for h in range(8):
                bk = nbank()
                for k in range(4):
                    S.op("pe", lambda e, h=h, k=k, bk=bk: e.matmul(ps[:, bk, :], lhsT=wuqb[:, k, h * 192:h * 192 + 128], rhs=lat[:, k, :],
                                                                   start=(k == 0), stop=(k == 3)), reads=[b_wuqb, b_lat], writes=[b_ps[bk]])
                oi = st["ob"]; st["ob"] ^= 1
                evac(ob[oi][:, 0:512], b_ob[oi], bk, 128, scale=scm)
                S.dma("sp", lambda e, oi=oi, h=h, tok0=tok0: e.dma_start(out=QKs[h // 2, sup, 1280 + (h % 2) * 128:1280 + (h % 2) * 128 + 128, tok0 - t0s:tok0 - t0s + 512], in_=ob[oi][:, 0:512]),
                      reads=[b_ob[oi]], writes=[b_mlaq])
                bk1 = nbank(); bk2 = nbank()
                for k in range(4):
                    S.op("pe", lambda e, h=h, k=k, bk1=bk1: e.matmul(ps[0:64, bk1, :], lhsT=wuqb[:, k, h * 192 + 128:h * 192 + 192], rhs=lat[:, k, :],
                                                                     start=(k == 0), stop=(k == 3)), reads=[b_wuqb, b_lat], writes=[b_ps[bk1]])
                for k in range(4):
                    S.op("pe", lambda e, h=h, k=k, bk2=bk2: e.matmul(ps[0:64, bk2, :], lhsT=wuqsb[:, k, h * 64:(h + 1) * 64], rhs=lat[:, k, :],
                                                                     start=(k == 0), stop=(k == 3)), reads=[b_wuqsb, b_lat], writes=[b_ps[bk2]])
                oi = st["ob"]; st["ob"] ^= 1
                rope_out(ps[0:64, bk1, :], b_ps[bk1], ps[0:64, bk2, :], b_ps[bk2], scm, ob[oi][0:64, 0:512], b_ob[oi])
                S.dma("sp", lambda e, oi=oi, h=h, tok0=tok0: e.dma_start(out=QKs[h // 2, sup, 1792 + (h % 2) * 64:1792 + (h % 2) * 64 + 64, tok0 - t0s:tok0 - t0s + 512], in_=ob[oi][0:64, 0:512]),
                      reads=[b_ob[oi]], writes=[b_mlaq])
            latent_norm(ckv, b_ckv, gkvt, b_gkvt)
            for h in range(8):
                bk = nbank()
                for k in range(4):
                    S.op("pe", lambda e: e.matmul(ps[:, bk, :], lhsT=wukvb[:, k, h * 256:h * 256 + 128], rhs=lat[:, k, :],
                                                  start=(k == 0), stop=(k == 3)), reads=[b_wukvb, b_lat], writes=[b_ps[bk]])
                oi = st["ob"]; st["ob"] ^= 1
                evac(ob[oi][:, 0:512], b_ob[oi], bk, 128)
                S.dma("sp", lambda e: e.dma_start(out=QKs[h // 2, sup, 1536 + (h % 2) * 128:1536 + (h % 2) * 128 + 128, tok0 - t0s:tok0 - t0s + 512], in_=ob[oi][:, 0:512]),
                      reads=[b_ob[oi]], writes=[b_QKs])
            wv = wukvb[:, :, :].rearrange("p k (h c) -> p k h c", c=256)
            for s4 in range(4):
                for hg in range(2):
                    bk = nbank()
                    for k in range(4):
                        S.op("pe", lambda e: e.matmul(ps[:, bk, :].rearrange("p (h c) -> p h c", c=128), lhsT=lat[:, k, s4 * 128:(s4 + 1) * 128],
                                                      rhs=wv[:, k, hg * 4:(hg + 1) * 4, 128:256], start=(k == 0), stop=(k == 3)),
                             reads=[b_wukvb, b_lat], writes=[b_ps[bk]])
                    oi = st["ob"]; st["ob"] ^= 1
                    evac(ob[oi][:, 0:512], b_ob[oi], bk, 128)
                    tk = tok0 + s4 * 128
                    S.dma("sp", lambda e: e.dma_start(out=Vs[2 * hg:2 * hg + 2, tk:tk + 128, 640:896].rearrange("j p c -> p j c"),
                                                      in_=ob[oi][:, 0:512].rearrange("p (j c) -> p j c", c=256)),
                          reads=[b_ob[oi]], writes=[b_Vs])
        if after_sup is not None:
            after_sup(sup)
        for gi in range(12):
            gemm_group(win, 8768 + gi * 512, 512, out_f32(gT, b_gT, gi * 512, AF.Sigmoid))


SEQ = 8192
NB = SEQ // 128
RATES = (1, 4, 16)
BIG = 1.0e6


def emit_B(S, nc, d, QKl, Vl, Ysrc, b_QKg, b_Vg, b_Ys, seq=SEQ, do_mla=True, do_sb=True, do_dil=True):
    NBk = seq // 128
    NG4 = seq // 512
    dposk = d['dposk']; dposq = d['dposq']; nslope = d['nslope']
    maskc_d = d['maskc']; masks_d = d['masks']; maskd_d = d['maskd']; m1_d = d['m1']; m2_d = d['m2']
    b_ymla = b_Ys; b_ysb = b_Ys; b_ydil = b_Ys
    SHR = 1984; SHV = 896
    Q1 = S.sbuf("Q1", [128, seq], BF16); bQ1 = Buf("Q1")
    K1 = S.sbuf("K1", [128, seq], BF16); bK1 = Buf("K1")
    if do_mla:
        Q2 = S.sbuf("Q2", [64, seq], BF16); bQ2 = Buf("Q2")
        K2 = S.sbuf("K2", [64, seq], BF16); bK2 = Buf("K2")
    V1 = S.sbuf("V1", [128, NBk, 128], BF16); bV1 = Buf("V1")
    two = do_mla or do_sb
    if two:
        Q1b = S.sbuf("Q1b", [128, seq], BF16); bQ1b = Buf("Q1b")
        K1b = S.sbuf("K1b", [128, seq], BF16); bK1b = Buf("K1b")
        V1b = S.sbuf("V1b", [128, NBk, 128], BF16); bV1b = Buf("V1b")
        if do_mla:
            Q2b = S.sbuf("Q2b", [64, seq], BF16); bQ2b = Buf("Q2b")
    NP = 8
    PT = [S.sbuf("PT%d" % i, [128, 512], BF16) for i in range(NP)]; bPT = [Buf("PT%d" % i) for i in range(NP)]
    SP = [S.sbuf("SP%d" % i, [128, 512], BF16) for i in range(NP)]; bSP = [Buf("SP%d" % i) for i in range(NP)]
    EN = [S.sbuf("EN%d" % i, [128, 512], F32) for i in range(4)]; bEN = [Buf("EN%d" % i) for i in range(4)]
    SNt = [S.sbuf("SN%d" % i, [128, 512], F32) for i in range(NP)]; bSN = [Buf("SN%d" % i) for i in range(NP)]
    UU = [S.sbuf("UU%d" % i, [128, 512], F32) for i in range(4)]; bUU = [Buf("UU%d" % i) for i in range(4)]
    YS = [S.sbuf("YS%d" % i, [128, 512], BF16) for i in range(2)]; bYS = [Buf("YS%d" % i) for i in range(2)]
    RD = S.sbuf("RD", [128, 512], F32); bRD = Buf("RD")
    maskc = S.sbuf("maskc_t", [128, 4, 512], BF16); bmaskc = Buf("maskc")
    masks = S.sbuf("masks_t", [128, 4, 512], BF16); bmasks = Buf("masks")
    maskd = S.sbuf("maskd_t", [128, 256], F32); bmaskd = Buf("maskd")
    M1 = S.sbuf("M1", [128, 128], BF16); bM1 = Buf("M1")
    M2 = S.sbuf("M2", [128, 128], BF16); bM2 = Buf("M2")
    ones = S.sbuf("ones", [128, 128], BF16); bones = Buf("ones")
    nsl = S.sbuf("nsl", [128, 3], F32); bnsl = Buf("nsl")
    ps = S.psum("ps", [128, 8, 512], F32); bps = [Buf("ps%d" % i) for i in range(8)]

    S.op("dve", lambda e: e.memset(ones[:], 1.0), writes=[bones])
    S.dma("sp", lambda e: e.dma_start(out=maskc[:], in_=maskc_d[:, :, :]), writes=[bmaskc])
    S.dma("sp", lambda e: e.dma_start(out=masks[:], in_=masks_d[:, :, :]), writes=[bmasks])
    S.dma("sp", lambda e: e.dma_start(out=maskd[:], in_=maskd_d[:, :]), writes=[bmaskd])
    S.dma("sp", lambda e: e.dma_start(out=M1[:], in_=m1_d[:, :]), writes=[bM1])
    S.dma("sp", lambda e: e.dma_start(out=M2[:], in_=m2_d[:, :]), writes=[bM2])
    S.dma("sp", lambda e: e.dma_start(out=nsl[:], in_=nslope[:, :]), writes=[bnsl])

    QK5 = QKl.rearrange("c r (p a) t -> c r p (a t)", a=1) if False else QKl
    Vl4 = Vl
    Vl6 = Vl.rearrange("c r (b p) d -> c r b p d", p=128)

    def load_fm(dst, bdst, R0, rows=128):
        dv4 = dst[0:rows, :].rearrange("p (r u t) -> p r u t", r=4, u=2)
        for u in range(2):
            S.dma("sp", lambda e: e.dma_start(out=dv4[:, :, u, :],
                                              in_=QK5[u * 4 + R0 // 512, :, R0 % 512:R0 % 512 + rows, :].rearrange("r p t -> p r t")),
                  reads=[b_QKg], writes=[bdst])

    def load_v(c0, Vt=None, bVt=None):
        Vt = V1 if Vt is None else Vt
        bVt = bV1 if bVt is None else bVt
        for r in range(4):
            for cp in range(4):
                S.dma("sp", lambda e: e.dma_start(out=Vt[:, r * 16 + cp * 4:r * 16 + cp * 4 + 4, :],
                                                  in_=Vl6[cp, r, :, :, c0:c0 + 128].rearrange("b p d -> p b d")),
                      reads=[b_Vg], writes=[bVt])

    def load_v_perm(c0, rt):
        if rt == 1:
            load_v(c0)
        elif rt == 4:
            for s_ in range(4):
                for r in range(4):
                    S.dma("sp", lambda e: e.dma_start(out=V1[:, s_ * 16 + 4 * r:s_ * 16 + 4 * r + 4, :],
                                                      in_=Vl4[:, r, s_:s_ + 4 * 127 + 1:4, c0:c0 + 128].rearrange("c p d -> p c d")),
                          reads=[b_Vg], writes=[bV1])
        else:
            for s_ in range(16):
                for cp in range(4):
                    S.dma("sp", lambda e: e.dma_start(out=V1[32 * cp:32 * cp + 32, s_ * 4:s_ * 4 + 4, :],
                                                      in_=Vl4[cp, :, s_:s_ + 16 * 31 + 1:16, c0:c0 + 128].rearrange("m p d -> p m d")),
                          reads=[b_Vg], writes=[bV1])

    Ys5 = Ysrc

    def yout(blk, g4):
        return Ys5[g4 // 8, blk, :, (g4 % 8) * 512:(g4 % 8) * 512 + 512]

    def pipeline(steps):
        prev = None
        for s1, s2 in steps:
            s1()
            if prev is not None:
                prev()
            prev = s2
        if prev is not None:
            prev()

    cnt = {"z": 0, "o": 0, "pt": 0, "ys": 0, "en": 0, "uu": 0}

    def interleave(a, b):
        out = []
        for x, y in zip(a, b):
            out.append(x); out.append(y)
        return out

    if do_mla:
        load_fm(K2, bK2, 1920, 64)
        hs = [dict(Q=Q1, bQ=bQ1, Qr=Q2, bQr=bQ2, K=K1, bK=bK1, V=V1, bV=bV1, zb=(0, 1), ob=2, db=3),
              dict(Q=Q1b, bQ=bQ1b, Qr=Q2b, bQr=bQ2b, K=K1b, bK=bK1b, V=V1b, bV=bV1b, zb=(6, 7), ob=4, db=5)]
        allsteps = []
        for h in range(2):
            H = hs[h]
            load_fm(H["Q"], H["bQ"], 1280 + h * 128)
            load_fm(H["Qr"], H["bQr"], 1792 + h * 64, 64)
            load_fm(H["K"], H["bK"], 1536 + h * 128)
            load_v(640 + h * 128, H["V"], H["bV"])
            steps = []
            zc = 0
            for g4 in range(NG4):
                qs = slice(g4 * 512, (g4 + 1) * 512)
                ob = H["ob"]; db = H["db"]
                nst = 4 * g4 + 4
                for j in range(nst):
                    zb = H["zb"][zc % 2]; zc += 1
                    ks = slice(j * 128, (j + 1) * 128)
                    box = {}

                    def s1(qs=qs, j=j, zb=zb, ks=ks, g4=g4, H=H, box=box):
                        pi = cnt["pt"] % NP; cnt["pt"] += 1
                        box["pi"] = pi
                        S.op("pe", lambda e: e.matmul(ps[:, zb, :], lhsT=H["K"][:, ks], rhs=H["Q"][:, qs], start=True, stop=False),
                             reads=[H["bK"], H["bQ"]], writes=[bps[zb]])
                        S.op("pe", lambda e: e.matmul(ps[:, zb, :], lhsT=K2[0:64, ks], rhs=H["Qr"][0:64, qs], start=False, stop=True),
                             reads=[bK2, H["bQr"]], writes=[bps[zb]])
                        S.op("act", lambda e: e.activation(out=PT[pi][:], in_=ps[:, zb, :], func=AF.Exp),
                             reads=[bps[zb]], writes=[bPT[pi]])
                        if j >= 4 * g4:
                            S.op("dve", lambda e: e.tensor_tensor(out=PT[pi][:], in0=PT[pi][:], in1=maskc[:, j - 4 * g4, :], op=ALU.mult),
                                 reads=[bPT[pi], bmaskc], writes=[bPT[pi]])

                    def s2(j=j, ob=ob, db=db, nst=nst, h=h, g4=g4, H=H, box=box):
                        pi = box["pi"]
                        S.op("pe", lambda e: e.matmul(ps[:, ob, :], lhsT=H["V"][:, j, :], rhs=PT[pi][:], start=(j == 0), stop=(j == nst - 1)),
                             reads=[H["bV"], bPT[pi]], writes=[bps[ob]])
                        S.op("pe", lambda e: e.matmul(ps[:, db, :], lhsT=ones[:], rhs=PT[pi][:], start=(j == 0), stop=(j == nst - 1)),
                             reads=[bones, bPT[pi]], writes=[bps[db]])
                        if j == nst - 1:
                            yi = cnt["ys"] % 2; cnt["ys"] += 1
                            ri = cnt["uu"] % 4; cnt["uu"] += 1
                            S.op("dve", lambda e: e.reciprocal(out=UU[ri][:], in_=ps[:, db, :]), reads=[bps[db]], writes=[bUU[ri]])
                            S.op("dve", lambda e: e.tensor_tensor(out=YS[yi][:], in0=ps[:, ob, :], in1=UU[ri][:], op=ALU.mult),
                                 reads=[bps[ob], bUU[ri]], writes=[bYS[yi]])
                            S.dma("sp", lambda e: e.dma_start(out=yout(h, g4), in_=YS[yi][:]), reads=[bYS[yi]], writes=[b_ymla])
                    steps.append((s1, s2))
            allsteps.append(steps)
        pipeline(interleave(allsteps[0], allsteps[1]))

    if do_sb:
        hs = [dict(Q=Q1, bQ=bQ1, K=K1, bK=bK1, V=V1, bV=bV1, zb=(0, 1), ob=2, rb=3),
              dict(Q=Q1b, bQ=bQ1b, K=K1b, bK=bK1b, V=V1b, bV=bV1b, zb=(6, 7), ob=4, rb=5)]
        allsteps = []
        for h in range(2):
            H = hs[h]
            load_fm(H["Q"], H["bQ"], 768 + h * 128)
            load_fm(H["K"], H["bK"], 1024 + h * 128)
            load_v(384 + h * 128, H["V"], H["bV"])
            steps = []
            zc = 0
            for g4 in range(NG4):
                qs = slice(g4 * 512, (g4 + 1) * 512)
                ob = H["ob"]; rb = H["rb"]
                nst = 4 * g4 + 4
                for idx, j in enumerate(reversed(range(nst))):
                    first = idx == 0; last = idx == nst - 1
                    zb = H["zb"][zc % 2]; zc += 1
                    ks = slice(j * 128, (j + 1) * 128)
                    box = {}

                    def t1(qs=qs, j=j, zb=zb, ks=ks, g4=g4, H=H, box=box):
                        pi = cnt["pt"] % NP; cnt["pt"] += 1
                        ei = cnt["en"] % 4; cnt["en"] += 1
                        box["pi"] = pi; box["ei"] = ei
                        S.op("pe", lambda e: e.matmul(ps[:, zb, :], lhsT=H["K"][:, ks], rhs=H["Q"][:, qs], start=True, stop=True),
                             reads=[H["bK"], H["bQ"]], writes=[bps[zb]])
                        S.op("act", lambda e: e.activation(out=EN[ei][:], in_=ps[:, zb, :], func=AF.Exp, scale=-1.0),
                             reads=[bps[zb]], writes=[bEN[ei]])

                    def t1b(j=j, zb=zb, g4=g4, box=box):
                        pi = box["pi"]; ei = box["ei"]
                        S.op("act", lambda e: e.activation(out=SNt[pi][:], in_=EN[ei][:], func=AF.Ln, bias=1.0),
                             reads=[bEN[ei]], writes=[bSN[pi]])
                        S.op("dve", lambda e: e.tensor_tensor(out=SP[pi][:], in0=ps[:, zb, :], in1=SNt[pi][:], op=ALU.add),
                             reads=[bps[zb], bSN[pi]], writes=[bSP[pi]])
                        if j >= 4 * g4:
                            S.op("dve", lambda e: e.tensor_tensor(out=SP[pi][:], in0=SP[pi][:], in1=masks[:, j - 4 * g4, :], op=ALU.mult),
                                 reads=[bSP[pi], bmasks], writes=[bSP[pi]])

                    def t2(j=j, rb=rb, first=first, g4=g4, box=box):
                        pi = box["pi"]
                        ui = cnt["uu"] % 4; cnt["uu"] += 1
                        S.op("pe", lambda e: e.matmul(ps[:, rb, :], lhsT=M1[:], rhs=SP[pi][:], start=first, stop=False, skip_group_check=True),
                             reads=[bM1, bSP[pi]], writes=[bps[rb]])
                        S.op("dve", lambda e: e.tensor_tensor(out=UU[ui][:], in0=SNt[pi][:], in1=ps[:, rb, :], op=ALU.add),
                             reads=[bSN[pi], bps[rb]], writes=[bUU[ui]])
                        S.op("act", lambda e: e.activation(out=PT[pi][:], in_=UU[ui][:], func=AF.Exp, scale=-1.0),
                             reads=[bUU[ui]], writes=[bPT[pi]])
                        if j >= 4 * g4:
                            S.op("dve", lambda e: e.tensor_tensor(out=PT[pi][:], in0=PT[pi][:], in1=masks[:, j - 4 * g4, :], op=ALU.mult),
                                 reads=[bPT[pi], bmasks], writes=[bPT[pi]])

                    def t3(rb=rb, last=last, box=box):
                        pi = box["pi"]
                        S.op("pe", lambda e: e.matmul(ps[:, rb, :], lhsT=M2[:], rhs=SP[pi][:], start=False, stop=last, skip_group_check=True),
                             reads=[bM2, bSP[pi]], writes=[bps[rb]])

                    def t4(j=j, ob=ob, first=first, last=last, h=h, g4=g4, H=H, box=box):
                        pi = box["pi"]
                        S.op("pe", lambda e: e.matmul(ps[:, ob, :], lhsT=H["V"][:, j, :], rhs=PT[pi][:], start=first, stop=last),
                             reads=[H["bV"], bPT[pi]], writes=[bps[ob]])
                        if last:
                            yi = cnt["ys"] % 2; cnt["ys"] += 1
                            S.op("act", lambda e: e.activation(out=YS[yi][:], in_=ps[:, ob, :], func=AF.Copy),
                                 reads=[bps[ob]], writes=[bYS[yi]])
                            S.dma("sp", lambda e: e.dma_start(out=yout(3 + h, g4), in_=YS[yi][:]), reads=[bYS[yi]], writes=[b_ysb])
                    steps.append((t1, t2, t3, t4, t1b))
            allsteps.append(steps)
        N_ = len(allsteps[0])
        for X in allsteps:
            X[0][0]()
        for X in allsteps:
            X[0][4]()
        for k in range(N_):
            for X in allsteps:
                X[k][1]()
            if k > 0:
                for X in allsteps:
                    X[k - 1][3]()
            if k + 1 < N_:
                for X in allsteps:
                    X[k + 1][0]()
                for X in allsteps:
                    X[k + 1][4]()
            for X in allsteps:
                X[k][2]()
        for X in allsteps:
            X[N_ - 1][3]()

    if do_dil:
        accn = S.sbuf("accn", [128, seq], F32); baccn = Buf("accn")
        accd = S.sbuf("accd", [128, seq], F32); baccd = Buf("accd")
        posk_i = S.sbuf("posk_i", [128, NBk], I32); bposk_i = Buf("posk_i")
        posk = S.sbuf("posk", [128, NBk], F32); bposk = Buf("posk")
        pq_i = [S.sbuf("pq_i%d" % i, [128, 128], I32) for i in range(3)]; bpq_i = [Buf("pq_i%d" % i) for i in range(3)]
        pq = [S.sbuf("pq%d" % i, [128, 128], F32) for i in range(3)]; bpq = [Buf("pq%d" % i) for i in range(3)]
        dd = [S.sbuf("dd%d" % i, [128, 256], F32) for i in range(3)]; bdd = [Buf("dd%d" % i) for i in range(3)]
        zt = [S.sbuf("zt%d" % i, [128, 256], F32) for i in range(3)]; bzt = [Buf("zt%d" % i) for i in range(3)]
        for g in range(3):
            r = RATES[g]
            L = seq // r
            nb = L // 128
            load_fm(Q1, bQ1, g * 128)
            load_fm(K1, bK1, 384 + g * 128)
            load_v_perm(g * 128, r)
            S.dma("sp", lambda e: e.dma_start(out=posk_i[:], in_=dposk[g, :, :]), writes=[bposk_i])
            S.op("dve", lambda e: e.tensor_copy(out=posk[:], in_=posk_i[:]), reads=[bposk_i], writes=[bposk])
            S.op("dve", lambda e: e.tensor_scalar(out=posk[:], in0=posk[:], scalar1=-1.0, scalar2=None, op0=ALU.mult), reads=[bposk], writes=[bposk])
            dsteps = []
            for i in range(NBk):
                s, m = divmod(i, nb)
                hp = m > 0
                c0 = 0 if hp else 128
                zb = (0, 1, 6, 7)[cnt["z"] % 4]; cnt["z"] += 1
                ob = 2 + (cnt["o"] % 2); db = 4 + (cnt["o"] % 2); cnt["o"] += 1
                pi = cnt["pt"] % NP; cnt["pt"] += 1
                bi = i % 3
                st0 = s + 128 * r * m
                qsl = slice(st0, st0 + 127 * r + 1, r) if r > 1 else slice(st0, st0 + 128)
                psl = (slice(st0 - 128 * r, st0 - 128 * r + 127 * r + 1, r) if r > 1 else slice(st0 - 128, st0)) if hp else None
                def s1(i=i, hp=hp, c0=c0, zb=zb, pi=pi, bi=bi, qsl=qsl, psl=psl, g=g):
                    if hp:
                        S.op("pe", lambda e: e.matmul(ps[:, zb, 0:128], lhsT=K1[:, psl], rhs=Q1[:, qsl], start=True, stop=True),
                             reads=[bK1, bQ1], writes=[bps[zb]])
                    S.op("pe", lambda e: e.matmul(ps[:, zb, 128:256], lhsT=K1[:, qsl], rhs=Q1[:, qsl], start=True, stop=True),
                         reads=[bK1, bQ1], writes=[bps[zb]])
                    S.dma("sp", lambda e: e.dma_start(out=pq_i[bi][:], in_=dposq[g:g + 1, i * 128:(i + 1) * 128].partition_broadcast(128)),
                          writes=[bpq_i[bi]])

                def s1x(i=i, hp=hp, bi=bi):
                    S.op("dve", lambda e: e.tensor_copy(out=pq[bi][:], in_=pq_i[bi][:]), reads=[bpq_i[bi]], writes=[bpq[bi]])
                    if hp:
                        S.op("act", lambda e: e.activation(out=dd[bi][:, 0:128], in_=pq[bi][:], func=AF.Abs, bias=posk[:, i - 1:i], scale=1.0),
                             reads=[bpq[bi], bposk], writes=[bdd[bi]])
                    S.op("act", lambda e: e.activation(out=dd[bi][:, 128:256], in_=pq[bi][:], func=AF.Abs, bias=posk[:, i:i + 1], scale=1.0),
                         reads=[bpq[bi], bposk], writes=[bdd[bi]])

                def s1b(i=i, hp=hp, c0=c0, zb=zb, pi=pi, bi=bi, g=g):
                    S.op("dve", lambda e: e.tensor_tensor(out=dd[bi][:, c0:256], in0=dd[bi][:, c0:256], in1=maskd[:, c0:256], op=ALU.add),
                         reads=[bdd[bi], bmaskd], writes=[bdd[bi]])
                    S.op("dve", lambda e: e.scalar_tensor_tensor(out=zt[bi][:, c0:256], in0=dd[bi][:, c0:256], scalar=nsl[:, g:g + 1],
                                                                 in1=ps[:, zb, c0:256], op0=ALU.mult, op1=ALU.add),
                         reads=[bdd[bi], bnsl, bps[zb]], writes=[bzt[bi]])
                    S.op("act", lambda e: e.activation(out=PT[pi][:, c0:256], in_=zt[bi][:, c0:256], func=AF.Exp),
                         reads=[bzt[bi]], writes=[bPT[pi]])

                def s2(i=i, hp=hp, ob=ob, db=db, pi=pi, s=s, m=m, r=r, g=g, st0=st0):
                    if hp:
                        S.op("pe", lambda e: e.matmul(ps[:, ob, 0:128], lhsT=V1[:, i - 1, :], rhs=PT[pi][:, 0:128], start=True, stop=False),
                             reads=[bV1, bPT[pi]], writes=[bps[ob]])
                    S.op("pe", lambda e: e.matmul(ps[:, ob, 0:128], lhsT=V1[:, i, :], rhs=PT[pi][:, 128:256], start=(not hp), stop=True),
                         reads=[bV1, bPT[pi]], writes=[bps[ob]])
                    if hp:
                        S.op("pe", lambda e: e.matmul(ps[:, db, 0:128], lhsT=ones[:], rhs=PT[pi][:, 0:128], start=True, stop=False),
                             reads=[bones, bPT[pi]], writes=[bps[db]])
                    S.op("pe", lambda e: e.matmul(ps[:, db, 0:128], lhsT=ones[:], rhs=PT[pi][:, 128:256], start=(not hp), stop=True),
                         reads=[bones, bPT[pi]], writes=[bps[db]])

                def s3(i=i, ob=ob, db=db, r=r, g=g, st0=st0):
                    an = accn[:, st0:st0 + 127 * r + 1:r] if r > 1 else accn[:, st0:st0 + 128]
                    ad = accd[:, st0:st0 + 127 * r + 1:r] if r > 1 else accd[:, st0:st0 + 128]
                    if g == 0:
                        S.op("act", lambda e: e.activation(out=an, in_=ps[:, ob, 0:128], func=AF.Copy), reads=[bps[ob]], writes=[baccn])
                        S.op("dve", lambda e: e.tensor_copy(out=ad, in_=ps[:, db, 0:128]), reads=[bps[db]], writes=[baccd])
                    else:
                        S.op("dve", lambda e: e.tensor_tensor(out=an, in0=an, in1=ps[:, ob, 0:128], op=ALU.add), reads=[baccn, bps[ob]], writes=[baccn])
                        S.op("dve", lambda e: e.tensor_tensor(out=ad, in0=ad, in1=ps[:, db, 0:128], op=ALU.add), reads=[baccd, bps[db]], writes=[baccd])
                dsteps.append((s1, s1x, s1b, s2, s3))
            nd = len(dsteps)
            NS = 5
            for k in range(-(NS - 1), nd):
                for st_ in range(NS):
                    idx = k + (NS - 1 - st_)
                    if 0 <= idx < nd:
                        dsteps[idx][st_]()
        for c in range(seq // 512):
            cs = slice(c * 512, (c + 1) * 512)
            yi = cnt["ys"] % 2; cnt["ys"] += 1
            S.op("dve", lambda e: e.reciprocal(out=RD[:], in_=accd[:, cs]), reads=[baccd], writes=[bRD])
            S.op("dve", lambda e: e.tensor_tensor(out=YS[yi][:], in0=accn[:, cs], in1=RD[:], op=ALU.mult), reads=[baccn, bRD], writes=[bYS[yi]])
            S.dma("sp", lambda e: e.dma_start(out=yout(2, c), in_=YS[yi][:]), reads=[bYS[yi]], writes=[b_ydil])


def consts_B():
    import ml_dtypes
    k = np.arange(128)[:, None]
    q = np.arange(512)[None, :]
    maskc = np.stack([(128 * i0 + k <= q) for i0 in range(4)], axis=1).astype(np.float32)
    masks = np.stack([(128 * i0 + k < q) for i0 in range(4)], axis=1).astype(np.float32)
    kl = np.arange(128)[:, None]; ql = np.arange(128)[None, :]
    maskd = np.concatenate([np.where(kl >= ql, 0.0, BIG), np.where(kl <= ql, 0.0, BIG)], axis=1).astype(np.float32)
    p = np.arange(128)[:, None]; m = np.arange(128)[None, :]
    m1 = (p > m).astype(np.float32); m2 = (p <= m).astype(np.float32)
    bf = ml_dtypes.bfloat16
    return dict(maskc=maskc.astype(bf), masks=masks.astype(bf), maskd=maskd, m1=m1.astype(bf), m2=m2.astype(bf))


D = 2048
NK = 16
EPS = 1e-6
NE = 16
BIGR = 1.0e4


def emit_C(S, nc, d, l, Yl, gT, xT, x2T, xfT, b_Yg, b_gT, b_xsrc, b_x2T, b_xfT, ntok=2048):
    NT = ntok // 512
    cT = d['cT']; wada = d['wada'][l]; bada = d['bada_c'][l]; gmoe = d['gmoe'][l]; gfin = d['gfin']
    womla = d['womla'][l]; wodil = d['wodil'][l]; wosb = d['wosb'][l]; wout = d['wout'][l]
    wr = d['wr']; br = d['br']; wg = d['wg'][l]; wu = d['wu'][l]; wd = d['wd'][l]; ident = d['ident']
    DBG = False
    xt = S.sbuf("xt", [128, NK, 512], F32); b_xt = [Buf("xt%d" % k) for k in range(NK)]
    scr = S.sbuf("scr", [128, 20, 512], BF16); b_scr = Buf("scr")
    mg = S.sbuf("mg", [128, NK, 512], BF16); b_mg = Buf("mg")
    a2b = S.sbuf("a2b", [128, NK, 512], BF16); b_a2b = Buf("a2b")
    NW = 5
    W = [S.sbuf("W%d" % i, [128, NK, 512], BF16) for i in range(NW)]; b_W = [Buf("W%d" % i) for i in range(NW)]
    gt = [S.sbuf("gt%d" % i, [128, 3, 512], F32) for i in range(2)]; b_gt = [Buf("gt0"), Buf("gt1")]
    tmp = [S.sbuf("tmp%d" % i, [128, 512], F32) for i in range(4)]; b_tmp = [Buf("tmp%d" % i) for i in range(4)]
    hp = [S.sbuf("hp%d" % i, [128, 4, 512], BF16) for i in range(2)]; b_hp = [Buf("hp0"), Buf("hp1")]
    rstd = S.sbuf("rstd", [128, 512], F32); b_rstd = Buf("rstd")
    lnt = S.sbuf("lnt", [128, 512], F32); b_lnt = Buf("lnt")
    ones = S.sbuf("ones", [128, 128], BF16); b_ones = Buf("ones")
    onesf = S.sbuf("onesf", [128, 128], F32); b_onesf = Buf("onesf")
    idt = S.sbuf("idt", [128, 128], F32); b_idt = Buf("idt")
    Gm = [S.sbuf("Gm%d" % i, [128, 128], F32) for i in range(2)]; b_Gm = [Buf("Gm0"), Buf("Gm1")]
    cin = S.sbuf("cin", [128, NK], F32); b_cin = Buf("cin")
    cact = S.sbuf("cact", [128, NK], BF16); b_cact = Buf("cact")
    badat = S.sbuf("badat", [128, 64], F32); b_badat = Buf("badat")
    gmoet = S.sbuf("gmoet", [128, NK], F32); b_gmoet = Buf("gmoet")
    gfint = S.sbuf("gfint", [128, NK], F32); b_gfint = Buf("gfint")
    modt = S.sbuf("modt", [128, 64], F32); b_modt = Buf("modt")
    s1m = S.sbuf("s1m", [128, NK], F32); b_s1m = Buf("s1m")
    wrt = S.sbuf("wrt", [128, NK, NE], F32); b_wrt = Buf("wrt")
    brt = S.sbuf("brt", [128, NE], F32); b_brt = Buf("brt")
    lt = S.sbuf("lt", [16, 512], F32); b_lt = Buf("lt")
    sc = S.sbuf("sc", [128, 64], F32); b_sc = Buf("sc")
    bi = S.sbuf("bi", [128, 64], F32); b_bi = Buf("bi")
    bi2 = S.sbuf("bi2", [128, 64], F32); b_bi2 = Buf("bi2")
    eq = S.sbuf("eq", [128, 64], F32); b_eq = Buf("eq")
    m1 = S.sbuf("m1", [128, 16], F32); b_m1 = Buf("m1")
    m2 = S.sbuf("m2", [128, 16], F32); b_m2 = Buf("m2")
    gs = S.sbuf("gs", [128, 16], F32); b_gs = Buf("gs")
    gmax = S.sbuf("gmax", [128, 4], F32); b_gmax = Buf("gmax")
    gsel = S.sbuf("gsel", [128, 16], F32); b_gsel = Buf("gsel")
    wsel = S.sbuf("wsel", [128, 64], F32); b_wsel = Buf("wsel")
    den = S.sbuf("den", [128, 4], F32); b_den = Buf("den")
    gates = S.sbuf("gates", [128, 64], F32); b_gates = Buf("gates")
    ps = S.psum("ps", [128, 8, 512], F32); b_ps = [Buf("ps%d" % i) for i in range(8)]
    st = {"bank": 0, "w": 0, "gt": 0, "tmp": 0, "hp": 0, "gm": 0}

    def nbank():
        i = st["bank"]; st["bank"] = (i + 1) % 6
        return i

    def nw():
        i = st["w"]; st["w"] = (i + 1) % NW
        return i

    def ntmp():
        i = st["tmp"]; st["tmp"] = (i + 1) % 4
        return i

    def load_w(wi, src2d, nk, c0, ncols, k_off=0):
        srcv = src2d.rearrange("(k p) c -> p k c", p=128)
        h = max(1, nk // 2)
        for k0 in range(0, nk, h):
            S.dma("pool", lambda e: e.dma_start(out=W[wi][:, k_off + k0:k_off + k0 + h, 0:ncols],
                                                in_=srcv[:, k0:k0 + h, c0:c0 + ncols]), writes=[b_W[wi]])

    S.op("dve", lambda e: e.memset(ones[:], 1.0), writes=[b_ones])
    S.op("dve", lambda e: e.memset(onesf[:], 1.0), writes=[b_onesf])
    for dst, bd, src in ((cin, b_cin, cT), (badat, b_badat, bada), (gmoet, b_gmoet, gmoe), (gfint, b_gfint, gfin),
                         (brt, b_brt, br), (idt, b_idt, ident)):
        S.dma("sp", lambda e: e.dma_start(out=dst[:], in_=src[:, :]), writes=[bd])
    S.dma("sp", lambda e: e.dma_start(out=wrt[:], in_=wr.rearrange("(k p) e -> p k e", p=128)), writes=[b_wrt])
    S.op("act", lambda e: e.activation(out=cact[:], in_=cin[:], func=AF.Silu), reads=[b_cin], writes=[b_cact])

    mb = nbank()
    for g in range(16):
        wi = nw()
        load_w(wi, wada, NK, 4096 + g * 512, 512)
        for m in range(4):
            n = g * 4 + m
            for k in range(NK):
                S.op("pe", lambda e: e.matmul(ps[:, mb, n:n + 1], lhsT=W[wi][:, k, m * 128:(m + 1) * 128], rhs=cact[:, k:k + 1],
                                              start=(k == 0), stop=(k == NK - 1)), reads=[b_W[wi], b_cact], writes=[b_ps[mb]])
    S.op("dve", lambda e: e.tensor_tensor(out=modt[:], in0=ps[:, mb, 0:64], in1=badat[:], op=ALU.add),
         reads=[b_ps[mb], b_badat], writes=[b_modt])
    S.op("dve", lambda e: e.scalar_tensor_tensor(out=s1m[:], in0=modt[:, 32:48], scalar=1.0, in1=gmoet[:], op0=ALU.add, op1=ALU.mult),
         reads=[b_modt, b_gmoet], writes=[b_s1m])

    def rms_rstd(src3, bsrcs):
        S.op("act", lambda e: e.activation(out=scr[:, 0:NK, :], in_=src3, func=AF.Square), reads=bsrcs, writes=[b_scr])
        bk = nbank()
        for k in range(NK):
            S.op("pe", lambda e: e.matmul(ps[:, bk, :], lhsT=ones[:], rhs=scr[:, k, :], start=(k == 0), stop=(k == NK - 1)),
                 reads=[b_scr, b_ones], writes=[b_ps[bk]])
        S.op("act", lambda e: e.activation(out=lnt[:], in_=ps[:, bk, :], func=AF.Ln, scale=1.0 / D, bias=EPS),
             reads=[b_ps[bk]], writes=[b_lnt])
        S.op("act", lambda e: e.activation(out=rstd[:], in_=lnt[:], func=AF.Exp, scale=-0.5), reads=[b_lnt], writes=[b_rstd])

    xTv = xT.rearrange("(k p) t -> p k t", p=128)
    gTv = gT.rearrange("(b n p) t -> p b n t", b=3, p=128)
    x2v = x2T.rearrange("(k p) t -> p k t", p=128) if x2T is not None else None
    xfv = xfT.rearrange("(k p) t -> p k t", p=128) if xfT is not None else None

    for t in range(NT):
        ts = slice(t * 512, (t + 1) * 512)
        for k0 in range(0, NK, 4):
            S.dma("sp", lambda e: e.dma_start(out=xt[:, k0:k0 + 4, :], in_=xTv[:, k0:k0 + 4, ts]), reads=[b_xsrc], writes=b_xt[k0:k0 + 4])
        for i_ in range(2):
            S.dma("sp", lambda e: e.dma_start(out=scr[:, i_:8:2, :], in_=Yl[i_, :, :, ts].rearrange("j p t -> p j t")), reads=[b_Yg], writes=[b_scr])
            S.dma("sp", lambda e: e.dma_start(out=scr[:, 12 + i_:20:2, :], in_=Yl[3 + i_, :, :, ts].rearrange("j p t -> p j t")), reads=[b_Yg], writes=[b_scr])
        S.dma("sp", lambda e: e.dma_start(out=scr[:, 8:12, :], in_=Yl[2, :, :, ts].rearrange("j p t -> p j t")), reads=[b_Yg], writes=[b_scr])
        for ng in range(4):
            wa = nw(); load_w(wa, womla, 8, ng * 512, 512); load_w(wa, wodil, 4, ng * 512, 512, k_off=8)
            wb_ = nw(); load_w(wb_, wosb, 8, ng * 512, 512)
            for m in range(4):
                n = ng * 4 + m
                gi = st["gt"]; st["gt"] ^= 1
                S.dma("sp", lambda e: e.dma_start(out=gt[gi][:], in_=gTv[:, :, n, ts]), reads=[b_gT], writes=[b_gt[gi]])
                banks = []
                for brn, (wi, koff, nk, yoff) in enumerate(((wa, 0, 8, 0), (wa, 8, 4, 8), (wb_, 0, 8, 12))):
                    bk = nbank(); banks.append(bk)
                    for k in range(nk):
                        S.op("pe", lambda e: e.matmul(ps[:, bk, :], lhsT=W[wi][:, koff + k, m * 128:(m + 1) * 128], rhs=scr[:, yoff + k, :],
                                                      start=(k == 0), stop=(k == nk - 1)), reads=[b_W[wi], b_scr], writes=[b_ps[bk]])
                ta = ntmp(); tb = ntmp()
                S.op("dve", lambda e: e.tensor_tensor(out=tmp[ta][:], in0=ps[:, banks[0], :], in1=gt[gi][:, 0, :], op=ALU.mult),
                     reads=[b_ps[banks[0]], b_gt[gi]], writes=[b_tmp[ta]])
                S.op("dve", lambda e: e.tensor_tensor(out=tmp[tb][:], in0=ps[:, banks[1], :], in1=gt[gi][:, 1, :], op=ALU.mult),
                     reads=[b_ps[banks[1]], b_gt[gi]], writes=[b_tmp[tb]])
                S.op("pool", lambda e: e.tensor_tensor(out=tmp[ta][:], in0=tmp[ta][:], in1=tmp[tb][:], op=ALU.add),
                     reads=[b_tmp[ta], b_tmp[tb]], writes=[b_tmp[ta]])
                S.op("dve", lambda e: e.tensor_tensor(out=tmp[tb][:], in0=ps[:, banks[2], :], in1=gt[gi][:, 2, :], op=ALU.mult),
                     reads=[b_ps[banks[2]], b_gt[gi]], writes=[b_tmp[tb]])
                S.op("pool", lambda e: e.tensor_tensor(out=mg[:, n, :], in0=tmp[ta][:], in1=tmp[tb][:], op=ALU.add),
                     reads=[b_tmp[ta], b_tmp[tb]], writes=[b_mg])
        for ng in range(4):
            wi = nw(); load_w(wi, wout, NK, ng * 512, 512)
            for m in range(4):
                n = ng * 4 + m
                bk = nbank()
                for k in range(NK):
                    S.op("pe", lambda e: e.matmul(ps[:, bk, :], lhsT=W[wi][:, k, m * 128:(m + 1) * 128], rhs=mg[:, k, :],
                                                  start=(k == 0), stop=(k == NK - 1)), reads=[b_W[wi], b_mg], writes=[b_ps[bk]])
                S.op("dve", lambda e: e.scalar_tensor_tensor(out=xt[:, n, :], in0=ps[:, bk, :], scalar=modt[:, n:n + 1], in1=xt[:, n, :],
                                                             op0=ALU.mult, op1=ALU.add), reads=[b_ps[bk], b_modt, b_xt[n]], writes=[b_xt[n]])
        if DBG:
            x1v = x1T.rearrange("(k p) t -> p k t", p=128)
            for k0 in range(0, NK, 4):
                S.dma("sp", lambda e: e.dma_start(out=x1v[:, k0:k0 + 4, ts], in_=xt[:, k0:k0 + 4, :]), reads=b_xt[k0:k0 + 4], writes=[b_x1T])
        rms_rstd(xt[:, :, :], b_xt)
        lb = nbank()
        for k in range(NK):
            ta = ntmp(); tb = ntmp()
            S.op("dve", lambda e: e.tensor_tensor(out=tmp[ta][:], in0=xt[:, k, :], in1=rstd[:], op=ALU.mult),
                 reads=[b_xt[k], b_rstd], writes=[b_tmp[ta]])
            S.op("act", lambda e: e.activation(out=tmp[tb][:], in_=tmp[ta][:], func=AF.Identity, scale=s1m[:, k:k + 1], bias=modt[:, 16 + k:17 + k]),
                 reads=[b_tmp[ta], b_s1m, b_modt], writes=[b_tmp[tb]])
            S.op("act", lambda e: e.activation(out=a2b[:, k, :], in_=tmp[ta][:], func=AF.Identity, scale=s1m[:, k:k + 1], bias=modt[:, 16 + k:17 + k]),
                 reads=[b_tmp[ta], b_s1m, b_modt], writes=[b_a2b])
            S.op("pe", lambda e: e.matmul(ps[0:16, lb, :], lhsT=wrt[:, k, :], rhs=tmp[tb][:], start=(k == 0), stop=(k == NK - 1)),
                 reads=[b_wrt, b_tmp[tb]], writes=[b_ps[lb]])
        S.op("act", lambda e: e.activation(out=lt[:], in_=ps[0:16, lb, :], func=AF.Copy), reads=[b_ps[lb]], writes=[b_lt])
        tb_ = nbank()
        for sub in range(4):
            S.op("pe", lambda e: e.matmul(ps[:, tb_, sub * 16:(sub + 1) * 16], lhsT=lt[0:16, sub * 128:(sub + 1) * 128], rhs=idt[0:16, 0:16],
                                          start=True, stop=True), reads=[b_lt, b_idt], writes=[b_ps[tb_]])
        S.op("act", lambda e: e.activation(out=sc[:], in_=ps[:, tb_, 0:64], func=AF.Sigmoid), reads=[b_ps[tb_]], writes=[b_sc])
        for sub in range(4):
            S.op("dve", lambda e: e.tensor_tensor(out=bi[:, sub * 16:(sub + 1) * 16], in0=sc[:, sub * 16:(sub + 1) * 16], in1=brt[:], op=ALU.add),
                 reads=[b_sc, b_brt], writes=[b_bi])
        v3 = lambda tl: tl[:, :].rearrange("p (g e) -> p g e", e=4)
        S.op("dve", lambda e: e.tensor_reduce(out=m1[:], in_=v3(bi), axis=AX.X, op=ALU.max), reads=[b_bi], writes=[b_m1])
        for ee in range(4):
            S.op("dve", lambda e: e.tensor_tensor(out=v3(eq)[:, :, ee], in0=v3(bi)[:, :, ee], in1=m1[:], op=ALU.is_equal),
                 reads=[b_bi, b_m1], writes=[b_eq])
        S.op("dve", lambda e: e.scalar_tensor_tensor(out=bi2[:], in0=eq[:], scalar=-BIGR, in1=bi[:], op0=ALU.mult, op1=ALU.add),
             reads=[b_eq, b_bi], writes=[b_bi2])
        S.op("dve", lambda e: e.tensor_reduce(out=m2[:], in_=v3(bi2), axis=AX.X, op=ALU.max), reads=[b_bi2], writes=[b_m2])
        S.op("dve", lambda e: e.tensor_tensor(out=gs[:], in0=m1[:], in1=m2[:], op=ALU.add), reads=[b_m1, b_m2], writes=[b_gs])
        S.op("dve", lambda e: e.tensor_reduce(out=gmax[:], in_=gs[:, :].rearrange("p (s g) -> p s g", g=4), axis=AX.X, op=ALU.max),
             reads=[b_gs], writes=[b_gmax])
        for g in range(4):
            S.op("dve", lambda e: e.tensor_tensor(out=gsel[:, :].rearrange("p (s g) -> p s g", g=4)[:, :, g],
                                                  in0=gs[:, :].rearrange("p (s g) -> p s g", g=4)[:, :, g], in1=gmax[:], op=ALU.is_equal),
                 reads=[b_gs, b_gmax], writes=[b_gsel])
        for ee in range(4):
            S.op("dve", lambda e: e.tensor_tensor(out=v3(eq)[:, :, ee], in0=v3(bi)[:, :, ee], in1=m2[:], op=ALU.is_ge),
                 reads=[b_bi, b_m2], writes=[b_eq])
            S.op("dve", lambda e: e.tensor_tensor(out=v3(eq)[:, :, ee], in0=v3(eq)[:, :, ee], in1=gsel[:], op=ALU.mult),
                 reads=[b_eq, b_gsel], writes=[b_eq])
        S.op("dve", lambda e: e.tensor_tensor(out=wsel[:], in0=sc[:], in1=eq[:], op=ALU.mult), reads=[b_sc, b_eq], writes=[b_wsel])
        S.op("dve", lambda e: e.tensor_reduce(out=den[:], in_=wsel[:, :].rearrange("p (s e) -> p s e", e=16), axis=AX.X, op=ALU.add),
             reads=[b_wsel], writes=[b_den])
        S.op("dve", lambda e: e.reciprocal(out=den[:], in_=den[:]), reads=[b_den], writes=[b_den])
        for sub in range(4):
            S.op("dve", lambda e: e.tensor_scalar(out=gates[:, sub * 16:(sub + 1) * 16], in0=wsel[:, sub * 16:(sub + 1) * 16],
                                                  scalar1=den[:, sub:sub + 1], scalar2=None, op0=ALU.mult), reads=[b_wsel, b_den], writes=[b_gates])
        if DBG and t == 0:
            for i_, (tl, bb, w_) in enumerate(((sc, b_sc, 64), (bi, b_bi, 64), (m1, b_m1, 16), (m2, b_m2, 16), (gsel, b_gsel, 16), (eq, b_eq, 64), (gates, b_gates, 64), (den, b_den, 4))):
                S.dma("sp", lambda e: e.dma_start(out=dbg[:, i_, 0:w_], in_=tl[:, 0:w_]), reads=[bb], writes=[b_dbg])
            a2v = a2T.rearrange("(k p) t -> p k t", p=128)
            S.dma("sp", lambda e: e.dma_start(out=a2v[:, :, ts], in_=a2b[:, :, :]), reads=[b_a2b], writes=[b_a2T])
        for ex in range(NE):
            w1 = nw(); load_w(w1, wg[ex], NK, 0, 512)
            w2 = nw(); load_w(w2, wu[ex], NK, 0, 512)
            w3 = nw()
            wdv = wd[ex].rearrange("(k p) c -> p k c", p=128)
            W3v = W[w3][:, :, :].rearrange("p (k a) c -> p k (a c)", a=4)
            for k0 in range(0, 4, 2):
                S.dma("pool", lambda e: e.dma_start(out=W3v[:, k0:k0 + 2, :], in_=wdv[:, k0:k0 + 2, :]), writes=[b_W[w3]])
            gb = 6 + (ex % 2)
            for sub in range(4):
                gi = st["gm"]; st["gm"] ^= 1
                S.op("dve", lambda e: e.tensor_scalar(out=Gm[gi][:], in0=onesf[:], scalar1=gates[:, sub * 16 + ex:sub * 16 + ex + 1], scalar2=None,
                                                      op0=ALU.mult), reads=[b_onesf, b_gates], writes=[b_Gm[gi]])
                S.op("pe", lambda e: e.matmul(ps[:, gb, sub * 128:(sub + 1) * 128], lhsT=Gm[gi][:], rhs=idt[:], start=True, stop=True),
                     reads=[b_Gm[gi], b_idt], writes=[b_ps[gb]])
            hi = st["hp"]; st["hp"] ^= 1
            for dc in range(4):
                b1 = nbank()
                for k in range(NK):
                    S.op("pe", lambda e: e.matmul(ps[:, b1, :], lhsT=W[w1][:, k, dc * 128:(dc + 1) * 128], rhs=a2b[:, k, :],
                                                  start=(k == 0), stop=(k == NK - 1)), reads=[b_W[w1], b_a2b], writes=[b_ps[b1]])
                b2 = nbank()
                for k in range(NK):
                    S.op("pe", lambda e: e.matmul(ps[:, b2, :], lhsT=W[w2][:, k, dc * 128:(dc + 1) * 128], rhs=a2b[:, k, :],
                                                  start=(k == 0), stop=(k == NK - 1)), reads=[b_W[w2], b_a2b], writes=[b_ps[b2]])
                ta = ntmp(); tb = ntmp()
                S.op("act", lambda e: e.activation(out=tmp[ta][:], in_=ps[:, b1, :], func=AF.Silu), reads=[b_ps[b1]], writes=[b_tmp[ta]])
                S.op("dve", lambda e: e.tensor_tensor(out=tmp[tb][:], in0=tmp[ta][:], in1=ps[:, b2, :], op=ALU.mult),
                     reads=[b_tmp[ta], b_ps[b2]], writes=[b_tmp[tb]])
                S.op("dve", lambda e: e.tensor_tensor(out=hp[hi][:, dc, :], in0=tmp[tb][:], in1=ps[:, gb, :], op=ALU.mult),
                     reads=[b_tmp[tb], b_ps[gb]], writes=[b_hp[hi]])
            for n in range(NK):
                bk = nbank()
                for dc in range(4):
                    S.op("pe", lambda e: e.matmul(ps[:, bk, :], lhsT=W3v[:, dc, n * 128:(n + 1) * 128], rhs=hp[hi][:, dc, :],
                                                  start=(dc == 0), stop=(dc == 3)), reads=[b_W[w3], b_hp[hi]], writes=[b_ps[bk]])
                S.op("dve", lambda e: e.scalar_tensor_tensor(out=xt[:, n, :], in0=ps[:, bk, :], scalar=modt[:, 48 + n:49 + n], in1=xt[:, n, :],
                                                             op0=ALU.mult, op1=ALU.add), reads=[b_ps[bk], b_modt, b_xt[n]], writes=[b_xt[n]])
        if x2v is not None:
            for k0 in range(0, NK, 4):
                S.dma("sp", lambda e: e.dma_start(out=x2v[:, k0:k0 + 4, ts], in_=xt[:, k0:k0 + 4, :]), reads=b_xt[k0:k0 + 4], writes=[b_x2T])
        if xfv is not None:
            rms_rstd(xt[:, :, :], b_xt)
            for k in range(NK):
                ta = ntmp()
                S.op("dve", lambda e: e.scalar_tensor_tensor(out=tmp[ta][:], in0=xt[:, k, :], scalar=gfint[:, k:k + 1], in1=rstd[:],
                                                             op0=ALU.mult, op1=ALU.mult), reads=[b_xt[k], b_gfint, b_rstd], writes=[b_tmp[ta]])
                S.dma("sp", lambda e: e.dma_start(out=xfv[:, k, ts], in_=tmp[ta][:]), reads=[b_tmp[ta]], writes=[b_xfT])


def build_fused():
    nc = bass.Bass("TRN2", target_bir_lowering=False)
    ext = lambda n, s, dt: nc.dram_tensor(n, s, dt, kind="ExternalInput").ap()
    d = {}
    d['xT'] = ext("xT", [2048, 2048], F32)
    jt = ext("jt", [1, 2], I32)
    d['cT'] = ext("cT", [128, 16], F32)
    d['pos'] = ext("pos", [1, 2048], I32)
    d['invf'] = ext("invf", [64, 1], F32)
    d['sgn'] = ext("sgn", [64, 1], F32)
    d['wada'] = ext("wada", [2, 2048, 12288], F32)
    d['bada_a'] = ext("bada_a", [2, 128, 32], F32)
    d['bada_c'] = ext("bada_c", [2, 128, 64], F32)
    d['gmix'] = ext("gmix", [2, 128, 16], F32)
    d['gq'] = ext("gq", [2, 128, 4], F32)
    d['gkv'] = ext("gkv", [2, 128, 4], F32)
    d['gmoe'] = ext("gmoe", [2, 128, 16], F32)
    d['gfin'] = ext("gfin", [128, 16], F32)
    d['win'] = ext("win", [2, 2048, 14912], F32)
    d['wsw'] = ext("wsw", [2, 2048, 64], F32)
    d['wuq'] = ext("wuq", [2, 512, 1536], F32)
    d['wuqs'] = ext("wuqs", [2, 512, 512], F32)
    d['wukv'] = ext("wukv", [2, 512, 2048], F32)
    d['womla'] = ext("womla", [2, 1024, 2048], F32)
    d['wodil'] = ext("wodil", [2, 512, 2048], F32)
    d['wosb'] = ext("wosb", [2, 1024, 2048], F32)
    d['wout'] = ext("wout", [2, 2048, 2048], F32)
    d['wr'] = ext("wr", [2048, 16], F32)
    d['br'] = ext("br", [128, 16], F32)
    d['wg'] = ext("wg", [2, 16, 2048, 512], F32)
    d['wu'] = ext("wu", [2, 16, 2048, 512], F32)
    d['wd'] = ext("wd", [2, 16, 512, 2048], F32)
    d['ident'] = ext("ident", [128, 128], F32)
    d['maskc'] = ext("maskc", [128, 4, 512], BF16)
    d['masks'] = ext("masks", [128, 4, 512], BF16)
    d['maskd'] = ext("maskd", [128, 256], F32)
    d['m1'] = ext("m1", [128, 128], BF16)
    d['m2'] = ext("m2", [128, 128], BF16)
    d['nslope'] = ext("nslope", [128, 3], F32)
    d['dposk'] = ext("dposk", [3, 128, 64], I32)
    d['dposq'] = ext("dposq", [3, 8192], I32)
    xfT = nc.dram_tensor("xfT", [2048, 2048], F32, kind="ExternalOutput").ap()
    b_xfT = Buf("xfT")
    x2s = nc.dram_tensor("x2scr", [2048, 2048], F32).ap()
    b_x2s = Buf("x2s")
    b_x0 = Buf("x0")
    GROUPS = [[0, 1, 2, 3], [4, 5, 6, 7]]

    S = Sched(nc)
    S.enable_dyn(jt[:, :])
    CH = 128 * 4096
    QKs_h = nc.dram_tensor("QKs", [32, 128, 4096], BF16)
    Vs_h = nc.dram_tensor("Vs", [16, 128, 4096], BF16)
    QKg_h = nc.dram_tensor("QKg", [32, 512, 4096], BF16)
    Vg_h = nc.dram_tensor("Vg", [16, 512, 4096], BF16)
    Ys_h = nc.dram_tensor("Ys", [10, 128, 4096], BF16)
    Yg_h = nc.dram_tensor("Yg", [10, 512, 4096], BF16)
    gsc = nc.dram_tensor("gscr", [6144, 2048], F32).ap()
    QKl_h = nc.dram_tensor("QKl", [4096, 4096], BF16)
    Vl_h = nc.dram_tensor("Vl", [2048, 4096], BF16)
    Yl_h = nc.dram_tensor("Yl", [2560, 2048], BF16)
    for l in range(2):
        b_QKs, b_Vs, b_QKg, b_Vg, b_Ys, b_Yg, b_g = (Buf(n) for n in ("QKs", "Vs", "QKg", "Vg", "Ys", "Yg", "g"))
        b_QKl, b_Vl, b_Yl = Buf("QKl"), Buf("Vl"), Buf("Yl")
        xsrc = d['xT'] if l == 0 else x2s
        b_xsrc = b_x0 if l == 0 else b_x2s
        QKs_v = QKs_h.ap().rearrange("c p (a t) -> (c p a) t", a=4).rearrange("(s u r) t -> s u r t", s=4, u=2)
        Vs_v = Vs_h.ap().rearrange("c p (a d) -> (c p a) d", a=4).rearrange("(s t) d -> s t d", s=4)
        S.begin_phase()
        b_QKs2 = [Buf("QKs0"), Buf("QKs1")]
        b_Vs2 = [Buf("Vs0"), Buf("Vs1")]

        pending = []
        rate = [1]

        def after_sup(sup):
            for sh in range(4):
                for q in range(4):
                    pending.append((QKs_h, QKg_h, sh * 8 + sup * 4 + q, b_QKs2[sup], b_QKg))
            for sh in range(4):
                for q in range(2):
                    pending.append((Vs_h, Vg_h, sh * 4 + sup * 2 + q, b_Vs2[sup], b_Vg))
            if sup == 1:
                rate[0] = 2

        def tick():
            for _ in range(rate[0]):
                if not pending:
                    break
                sh_, gh_, c, br_, bw_ = pending.pop(0)
                S.cc(lambda e: e.collective_compute("AllGather", ALU.bypass, replica_groups=GROUPS, ins=[sh_.ap()[c].opt()],
                                                    outs=[gh_.ap()[c].opt()]), reads=[br_], writes=[bw_])
        emit_A(S, nc, d, l, xsrc, QKs_v, Vs_v, gsc, b_QKs2, b_Vs2, b_g, after_sup=after_sup, tick=tick)
        while pending:
            tick()
        S.end_phase()
        S.begin_phase()
        for hh in range(2):
            S.dma_dyn(QKl_h.ap()[hh * 2048:(hh + 1) * 2048, :], QKg_h, 8 * 4 * CH, hh * 2048 * 4096, [[4096, 2048], [1, 4096]],
                      reads=[b_QKg], writes=[b_QKl])
        S.dma_dyn(Vl_h.ap()[:, :], Vg_h, 4 * 4 * CH, 0, [[4096, 2048], [1, 4096]], reads=[b_Vg], writes=[b_Vl])
        QKl = QKl_h.ap().rearrange("(c r p) (a t) -> c r (p a) t", c=8, r=4, a=4)
        Vl = Vl_h.ap().rearrange("(c r p) (a d) -> c r (p a) d", c=4, r=4, a=4)
        Ys_v = Ys_h.ap().rearrange("(th b) p t -> th b p t", th=2)
        S.barrier()
        emit_B(S, nc, d, QKl, Vl, Ys_v, b_QKl, b_Vl, b_Ys, do_mla=True, do_sb=False, do_dil=False)
        S.end_phase()
        S.begin_phase()
        emit_B(S, nc, d, QKl, Vl, Ys_v, b_QKl, b_Vl, b_Ys, do_mla=False, do_sb=True, do_dil=False)
        for c in (0, 1, 3, 4, 5, 6, 8, 9):
            S.cc(lambda e: e.collective_compute("AllGather", ALU.bypass, replica_groups=GROUPS, ins=[Ys_h.ap()[c].opt()], outs=[Yg_h.ap()[c].opt()]),
                 reads=[b_Ys], writes=[b_Yg])
        S.end_phase(wait_cc=False)
        S.begin_phase()
        emit_B(S, nc, d, QKl, Vl, Ys_v, b_QKl, b_Vl, b_Ys, do_mla=False, do_sb=False, do_dil=True)
        for c in (2, 7):
            S.cc(lambda e: e.collective_compute("AllGather", ALU.bypass, replica_groups=GROUPS, ins=[Ys_h.ap()[c].opt()], outs=[Yg_h.ap()[c].opt()]),
                 reads=[b_Ys], writes=[b_Yg])
        S.end_phase()
        S.begin_phase()
        for b0, nb_ in ((0, 3), (3, 2)):
            S.dma_dyn(Yl_h.ap()[b0 * 512:(b0 + nb_) * 512, :], Yg_h, 1, b0 * 4 * CH, [[4 * CH, nb_], [4096, 512], [1, 2048]],
                      reads=[b_Yg], writes=[b_Yl], which=1)
        Yl = Yl_h.ap().rearrange("(b j p) t -> b j p t", b=5, j=4)
        emit_C(S, nc, d, l, Yl, gsc, xsrc, x2s if l == 0 else None, xfT if l == 1 else None,
               b_Yl, b_g, b_xsrc, b_x2s, b_xfT)
        S.end_phase()
    S.close()
    return nc


_PROG = {}


def _f(a):
    return np.ascontiguousarray(a)


def kernel(**inputs):
    inp = {k: np.asarray(v) for k, v in inputs.items()}
    B_, S_ = inp['x'].shape[:2]
    cores = list(range(8))
    pos = inp['positions'].astype(np.int32)
    perm = [np.concatenate([np.arange(s, S_, r) for s in range(r)]) for r in RATES]
    slopes = (np.float32(2.0) ** (np.float32(-8.0) * np.arange(1, 13, dtype=np.float32) / np.float32(12))).reshape(3, 4)
    half = 32
    invf = (np.float32(10000.0) ** (-(np.arange(half, dtype=np.float32)) / np.float32(half))).astype(np.float32)
    wu_ = inp['w_uq']
    wuqs = np.stack([np.concatenate([np.concatenate([wu_[l][:, h * 192 + 160:h * 192 + 192], wu_[l][:, h * 192 + 128:h * 192 + 160]], axis=1)
                                     for h in range(8)], axis=1) for l in range(2)])
    wi_ = inp['w_in']
    lay = lambda a, n: _f(a.reshape(a.shape[0], n, 128).transpose(0, 2, 1))
    shared = {
        'invf': _f(np.concatenate([invf, invf])[:, None]),
        'sgn': _f(np.concatenate([-np.ones(32, np.float32), np.ones(32, np.float32)])[:, None]),
        'wada': _f(inp['w_ada']),
        'bada_a': lay(inp['b_ada'][:, :4096], 32),
        'bada_c': lay(inp['b_ada'][:, 4096:], 64),
        'gmix': lay(inp['g_mix'], 16), 'gq': lay(inp['g_q'], 4), 'gkv': lay(inp['g_kv'], 4), 'gmoe': lay(inp['g_moe'], 16),
        'gfin': _f(inp['g_final'].reshape(16, 128).T),
        'win': _f(wi_), 'wsw': _f(np.concatenate([wi_[:, :, 1056:1088], wi_[:, :, 1024:1056]], axis=2)),
        'wuq': _f(wu_), 'wuqs': _f(wuqs), 'wukv': _f(inp['w_ukv']),
        'womla': _f(inp['w_o_mla']), 'wodil': _f(inp['w_o_dil']), 'wosb': _f(inp['w_o_sb']), 'wout': _f(inp['w_out']),
        'wr': _f(inp['w_router']), 'br': _f(np.broadcast_to(inp['b_router'][None, :], (128, 16))),
        'wg': _f(inp['w_gate']), 'wu': _f(inp['w_up']), 'wd': _f(inp['w_down']), 'ident': np.eye(128, dtype=np.float32),
    }
    shared.update(consts_B())
    maps = []
    for c in cores:
        b, j = c // 4, c % 4
        m = dict(shared)
        m['xT'] = _f(inp['x'][b, j * 2048:(j + 1) * 2048, :].T)
        m['jt'] = np.array([[j, (j // 2) * 5 * 4 * 128 * 4096 + (j % 2) * 2048]], np.int32)
        m['cT'] = _f(inp['c'][b].reshape(16, 128).T)
        m['pos'] = _f(pos[b, j * 2048:(j + 1) * 2048][None, :])
        pp = np.stack([pos[b][perm[g]] for g in range(3)]).astype(np.int32)
        m['dposq'] = _f(pp)
        m['dposk'] = _f(pp.reshape(3, S_ // 128, 128).transpose(0, 2, 1))
        m['nslope'] = _f(np.broadcast_to(-slopes[:, j][None, :], (128, 3)).astype(np.float32))
        maps.append(m)
    if "f" not in _PROG:
        _PROG["f"] = build_fused()
    res = run_bass_kernel_spmd(_PROG["f"], maps, core_ids=cores).results
    out = np.empty(inp['x'].shape, dtype=np.float32)
    for c in cores:
        b, j = c // 4, c % 4
        out[b, j * 2048:(j + 1) * 2048, :] = np.asarray(res[c]['xfT']).T
    return out
```

```python
import math
import numpy as np
import concourse.bass as bass
import concourse.mybir as mybir
from concourse.bass_utils import run_bass_kernel_spmd

F32 = mybir.dt.float32
BF16 = mybir.dt.bfloat16
I32 = mybir.dt.int32
AF = mybir.ActivationFunctionType
ALU = mybir.AluOpType
AX = mybir.AxisListType


class Buf:
    __slots__ = ("name", "w", "r")

    def __init__(self, name):
        self.name = name
        self.w = None
        self.r = {}


class _Rec:
    def __init__(self):
        self.call = None

    def __getattr__(self, name):
        def f(*a, **kw):
            self.call = (name, a, kw)
            return self
        return f


def _record(fn):
    r = _Rec()
    fn(r)
    assert r.call is not None
    return r.call


class Sched:
    ENGS = ("pe", "act", "dve", "pool", "sp")
    NDMA = 12

    def __init__(self, nc):
        self.nc = nc
        self.streams = {e: [] for e in self.ENGS}
        self.cnt = {e: 0 for e in self.ENGS}
        self.seen = {e: {} for e in self.ENGS}
        self.dma_i = {"sp": 0, "pool": 0, "act": 0}
        self.dma_val = {}
        self.sems = {}
        self._ctx = []
        self._perm = []
        self.cc_val = {}
        self._phase_mark = 0
        self.uses_dyn = False
        self._phase_no = 0
        self.jt_ap = None
        self.jsb = None
        self.jt_cnt = 0
        for e in ("pe", "act", "dve", "pool"):
            self._mk_sem("E_" + e)
        for q in ("sp", "pool", "act"):
            for k in range(self.NDMA):
                self._mk_sem("D_%s%d" % (q, k))
                self.dma_val["D_%s%d" % (q, k)] = 0

    def _mk_sem(self, key):
        cm = self.nc.semaphore(key)
        self.sems[key] = cm.__enter__()
        self._perm.append(cm)

    _mk_sem_perm = _mk_sem

    def enable_dyn(self, jt_ap):
        self.jt_ap = jt_ap
        self._mk_sem("JT")
        cm = self.nc.sbuf_tensor("jsb", [1, 2], I32)
        self.jsb = cm.__enter__()
        self._perm.append(cm)

    def sbuf(self, name, shape, dtype):
        cm = self.nc.sbuf_tensor("%s_p%d" % (name, self._phase_no), shape, dtype)
        t = cm.__enter__()
        self._ctx.append(cm)
        return t

    def psum(self, name, shape, dtype):
        cm = self.nc.psum_tensor("%s_p%d" % (name, self._phase_no), shape, dtype)
        t = cm.__enter__()
        self._ctx.append(cm)
        return t

    def _deps(self, eng, reads, writes):
        deps = {}

        def add(tok):
            if tok is None:
                return
            k, v = tok
            if deps.get(k, -1) < v:
                deps[k] = v
        for b in reads:
            add(b.w)
        for b in writes:
            add(b.w)
            for k, v in b.r.items():
                add((k, v))
        out = []
        seen = self.seen[eng]
        for k, v in deps.items():
            if eng == "pe" and k == "E_pe":
                continue
            if seen.get(k, -1) >= v:
                continue
            seen[k] = v
            out.append((k, v))
        return out

    def _mark(self, tok, reads, writes):
        k, v = tok
        for b in reads:
            if b.r.get(k, -1) < v:
                b.r[k] = v
        for b in writes:
            b.w = tok
            b.r = {}

    def op(self, eng, fn, reads=(), writes=()):
        waits = self._deps(eng, reads, writes)
        self.cnt[eng] += 1
        tok = ("E_" + eng, self.cnt[eng])
        self.streams[eng].append((waits, _record(fn), tok[0], 1))
        self._mark(tok, reads, writes)

    def dma(self, q, fn, reads=(), writes=()):
        i = self.dma_i[q]
        self.dma_i[q] += 1
        key = "D_%s%d" % (q, i % self.NDMA)
        waits = self._deps(q, reads, writes)
        prev = self.dma_val[key]
        if prev > 0 and self.seen[q].get(key, -1) < prev:
            self.seen[q][key] = prev
            waits.append((key, prev))
        self.dma_val[key] = prev + 16
        tok = (key, prev + 16)
        self.streams[q].append((waits, _record(fn), key, 16))
        self._mark(tok, reads, writes)

    def begin_phase(self):
        self._phase_mark = len(self._ctx)
        self._phase_no += 1

    def barrier(self, wait_cc=True):
        allv = {}
        for e in ("pe", "act", "dve", "pool"):
            if self.cnt[e] > 0:
                allv["E_" + e] = self.cnt[e]
        for k, v in self.dma_val.items():
            if v > 0:
                allv[k] = v
        for k, v in self.cc_val.items():
            if v > 0 and wait_cc:
                allv[k] = v
        for eng in self.ENGS:
            waits = []
            for k, v in allv.items():
                if self.seen[eng].get(k, -1) < v:
                    self.seen[eng][k] = v
                    waits.append((k, v))
            self.streams[eng].append((waits, None, None, 0))

    def end_phase(self, wait_cc=True):
        self.barrier(wait_cc)
        self.emit()
        self.streams = {e: [] for e in self.ENGS}
        while len(self._ctx) > self._phase_mark:
            self._ctx.pop().__exit__(None, None, None)

    def cc(self, fn, reads=(), writes=()):
        key = "CC"
        if key not in self.sems:
            self._mk_sem_perm(key)
            self.cc_val[key] = 0
        n = self.cc_val[key] + 1
        self.cc_val[key] = n
        waits = self._deps("pool", reads, writes)
        self.streams["pool"].append((waits, _record(fn), key, None))
        self._mark((key, n), reads, writes)

    def dma_dyn(self, out_ap, tensor, jmul, const, ap_list, reads=(), writes=(), which=0):
        q = "sp"
        i = self.dma_i[q]
        self.dma_i[q] += 1
        key = "D_%s%d" % (q, i % self.NDMA)
        waits = self._deps(q, reads, writes)
        prev = self.dma_val[key]
        if prev > 0 and self.seen[q].get(key, -1) < prev:
            self.seen[q][key] = prev
            waits.append((key, prev))
        self.dma_val[key] = prev + 16
        tok = (key, prev + 16)
        self.streams[q].append((waits, ("__dyn__", (out_ap, tensor, int(jmul), int(const), [list(x) for x in ap_list], which), {}), key, 16))
        self._mark(tok, reads, writes)
        self.uses_dyn = True

    def final_wait(self, eng, bufs):
        waits = self._deps(eng, bufs, ())
        self.streams[eng].append((waits, None, None, 0))

    def emit(self):
        nc = self.nc
        sems = self.sems
        streams = self.streams

        def run(engine, lst, regs=None):
            for waits, fn, key, inc in lst:
                for k, v in waits:
                    engine.wait_ge(sems[k], v)
                if fn is not None:
                    name, a, kw = fn
                    if name == "__dyn__":
                        out_ap, tensor, jmul, const, ap_list, which = a
                        rj, ro = regs[which], regs[2]
                        engine.reg_mul(ro, rj, jmul)
                        engine.reg_add(ro, ro, const)
                        ins = engine.dma_start(out=out_ap, in_=bass.AP(tensor, ro, ap_list))
                    else:
                        ins = getattr(engine, name)(*a, **kw)
                    if inc is None:
                        ins.then_inc(sems[key])
                    else:
                        ins.then_inc(sems[key], inc)

        with nc.Block() as block:
            @block.tensor
            def _(e):
                run(e, streams["pe"])

            @block.scalar
            def _(e):
                run(e, streams["act"])

            @block.vector
            def _(e):
                run(e, streams["dve"])

            @block.gpsimd
            def _(e):
                run(e, streams["pool"])

            @block.sync
            def _(e):
                if any(fn is not None and fn[0] == "__dyn__" for _, fn, _, _ in streams["sp"]):
                    self.jt_cnt += 1
                    with e.register("rj%d" % self.jt_cnt) as rj, e.register("ry%d" % self.jt_cnt) as ry, e.register("ro%d" % self.jt_cnt) as ro:
                        e.dma_start(out=self.jsb[:, :], in_=self.jt_ap).then_inc(sems["JT"], 16)
                        e.wait_ge(sems["JT"], 16 * self.jt_cnt)
                        e.reg_load(rj, self.jsb[0:1, 0:1])
                        e.reg_load(ry, self.jsb[0:1, 1:2])
                        run(e, streams["sp"], (rj, ry, ro))
                else:
                    run(e, streams["sp"])

    def close(self):
        for cm in reversed(self._ctx):
            cm.__exit__(None, None, None)
        self._ctx = []
        for cm in reversed(self._perm):
            cm.__exit__(None, None, None)
        self._perm = []


D = 2048
NK = 16
TS = 1024
TP = 128
EPS = 1e-6
TWO_PI = 2.0 * math.pi
C1 = 6.28125
C2 = TWO_PI - C1


def emit_A(S, nc, d, l, xT, QKs, Vs, gT, b_QKs_l, b_Vs_l, b_gT, ntok=2048, after_sup=None, tick=None):
    cT = d['cT']; wada = d['wada'][l]; bada = d['bada_a'][l]; gmix = d['gmix'][l]; win = d['win'][l]; wsw = d['wsw'][l]
    gq = d['gq'][l]; gkv = d['gkv'][l]; wuq = d['wuq'][l]; wuqs = d['wuqs'][l]; wukv = d['wukv'][l]
    pos = d['pos']; invf = d['invf']; sgn = d['sgn']
    b_QKs = b_QKs_l[0]; b_Vs = b_Vs_l[0]
    b_projT = b_QKs; b_mlaq = b_QKs; b_mlakv = b_QKs; b_mlakr = b_QKs
    aT = S.sbuf("aT", [128, NK, TS], BF16); b_aT = [Buf("aT%d" % i) for i in range(TS // TP)]
    wb = [S.sbuf("wb%d" % i, [128, NK, 512], BF16) for i in range(2)]; b_wb = [Buf("wb0"), Buf("wb1")]
    xt = S.sbuf("xt", [128, NK, TP], F32); b_xt = Buf("xt")
    sq = S.sbuf("sq", [128, NK, TP], BF16); b_sq = Buf("sq")
    rstd = S.sbuf("rstd", [128, 512], F32); b_rstd = Buf("rstd")
    lnt = S.sbuf("lnt", [128, 512], F32); b_lnt = Buf("lnt")
    tmp = [S.sbuf("tmp%d" % i, [128, 512], F32) for i in range(2)]; b_tmp = [Buf("tmp0"), Buf("tmp1")]
    ones = S.sbuf("ones", [128, 128], BF16); b_ones = Buf("ones")
    cin = S.sbuf("cin", [128, NK], F32); b_cin = Buf("cin")
    cact = S.sbuf("cact", [128, NK], BF16); b_cact = Buf("cact")
    badat = S.sbuf("badat", [128, 32], F32); b_badat = Buf("badat")
    gmixt = S.sbuf("gmixt", [128, NK], F32); b_gmixt = Buf("gmixt")
    modt = S.sbuf("modt", [128, 32], F32); b_modt = Buf("modt")
    s1 = S.sbuf("s1", [128, NK], F32); b_s1 = Buf("s1")
    cq = S.sbuf("cq", [128, 4, TS], F32); b_cq = Buf("cq")
    ckv = S.sbuf("ckv", [128, 4, TS], F32); b_ckv = Buf("ckv")
    kr = S.sbuf("kr", [64, TS], F32); b_kr = Buf("kr")
    krs = S.sbuf("krs", [64, TS], F32); b_krs = Buf("krs")
    wswb = S.sbuf("wswb", [128, NK, 64], BF16); b_wswb = Buf("wswb")
    wuqb = S.sbuf("wuqb", [128, 4, 1536], BF16); b_wuqb = Buf("wuqb")
    wuqsb = S.sbuf("wuqsb", [128, 4, 512], BF16); b_wuqsb = Buf("wuqsb")
    wukvb = S.sbuf("wukvb", [128, 4, 2048], BF16); b_wukvb = Buf("wukvb")
    gqt = S.sbuf("gqt", [128, 4], F32); b_gqt = Buf("gqt")
    gkvt = S.sbuf("gkvt", [128, 4], F32); b_gkvt = Buf("gkvt")
    ob = [S.sbuf("ob%d" % i, [128, TS], BF16) for i in range(2)]; b_ob = [Buf("ob0"), Buf("ob1")]
    of = [S.sbuf("of%d" % i, [128, TS], F32) for i in range(2)]; b_of = [Buf("of0"), Buf("of1")]
    lat = S.sbuf("lat", [128, 4, 512], BF16); b_lat = Buf("lat")
    posi = S.sbuf("posi", [64, 512], I32); b_posi = Buf("posi")
    ang = S.sbuf("ang", [64, 512], F32); b_ang = Buf("ang")
    kf = S.sbuf("kf", [64, 512], F32); b_kf = Buf("kf")
    ki = posi; b_ki = b_posi
    rr = S.sbuf("rr", [64, 512], F32); b_rr = Buf("rr")
    rc = S.sbuf("rc", [64, 512], F32); b_rc = Buf("rc")
    mm = S.sbuf("mm", [64, 512], F32); b_mm = Buf("mm")
    CS = S.sbuf("CS", [64, 512], F32); b_CS = Buf("CS")
    SN = S.sbuf("SN", [64, 512], F32); b_SN = Buf("SN")
    invft = S.sbuf("invft", [64, 1], F32); b_invft = Buf("invft")
    sgnt = S.sbuf("sgnt", [64, 1], F32); b_sgnt = Buf("sgnt")
    t1 = ang; b_t1 = b_ang
    t2 = kf; b_t2 = b_kf
    ps = S.psum("ps", [128, 8, 512], F32); b_ps = [Buf("ps%d" % i) for i in range(8)]
    st = {"bank": 0, "w": 0, "ob": 0, "of": 0, "tmp": 0, "ev": 0}

    def nbank():
        i = st["bank"]; st["bank"] = (i + 1) % 8
        return i

    def load_w(dst, bdst, src, nk, c0, ncols):
        srcv = src.rearrange("(k p) c -> p k c", p=128)
        h = max(1, nk // 2)
        for k0 in range(0, nk, h):
            S.dma("pool", lambda e, k0=k0: e.dma_start(out=dst[:, k0:k0 + h, 0:ncols],
                                                      in_=srcv[:, k0:k0 + h, c0:c0 + ncols]),
                  writes=[bdst])

    S.op("dve", lambda e: e.memset(ones[:], 1.0), writes=[b_ones])
    S.dma("sp", lambda e: e.dma_start(out=cin[:], in_=cT[:, :]), writes=[b_cin])
    S.dma("sp", lambda e: e.dma_start(out=badat[:], in_=bada[:, :]), writes=[b_badat])
    S.dma("sp", lambda e: e.dma_start(out=gmixt[:], in_=gmix[:, :]), writes=[b_gmixt])
    S.dma("sp", lambda e: e.dma_start(out=gqt[:], in_=gq[:, :]), writes=[b_gqt])
    S.dma("sp", lambda e: e.dma_start(out=gkvt[:], in_=gkv[:, :]), writes=[b_gkvt])
    S.dma("sp", lambda e: e.dma_start(out=invft[:], in_=invf[:, :]), writes=[b_invft])
    S.dma("sp", lambda e: e.dma_start(out=sgnt[:], in_=sgn[:, :]), writes=[b_sgnt])
    S.op("act", lambda e: e.activation(out=cact[:], in_=cin[:], func=AF.Silu), reads=[b_cin], writes=[b_cact])

    mb = nbank()
    for g in range(8):
        wi = st["w"]; st["w"] ^= 1
        load_w(wb[wi], b_wb[wi], wada, NK, g * 512, 512)
        for m in range(4):
            n = g * 4 + m
            for k in range(NK):
                S.op("pe", lambda e, wi=wi, m=m, k=k, n=n: e.matmul(
                    ps[:, mb, n:n + 1], lhsT=wb[wi][:, k, m * 128:(m + 1) * 128], rhs=cact[:, k:k + 1],
                    start=(k == 0), stop=(k == NK - 1)), reads=[b_wb[wi], b_cact], writes=[b_ps[mb]])
    S.op("dve", lambda e: e.tensor_tensor(out=modt[:], in0=ps[:, mb, 0:32], in1=badat[:], op=ALU.add),
         reads=[b_ps[mb], b_badat], writes=[b_modt])
    S.op("dve", lambda e: e.scalar_tensor_tensor(out=s1[:], in0=modt[:, 16:32], scalar=1.0, in1=gmixt[:],
                                                 op0=ALU.add, op1=ALU.mult),
         reads=[b_modt, b_gmixt], writes=[b_s1])

    load_w(wswb, b_wswb, wsw, NK, 0, 64)
    load_w(wuqb, b_wuqb, wuq, 4, 0, 1536)
    load_w(wuqsb, b_wuqsb, wuqs, 4, 0, 512)
    load_w(wukvb, b_wukvb, wukv, 4, 0, 2048)

    def rms_rstd(src_sq, bsrc, nk, width, dim):
        bk = nbank()
        for k in range(nk):
            S.op("pe", lambda e, k=k: e.matmul(ps[:, bk, 0:width], lhsT=ones[:], rhs=src_sq[:, k, 0:width],
                                               start=(k == 0), stop=(k == nk - 1)),
                 reads=[bsrc, b_ones], writes=[b_ps[bk]])
        S.op("act", lambda e: e.activation(out=lnt[:, 0:width], in_=ps[:, bk, 0:width], func=AF.Ln,
                                           scale=1.0 / dim, bias=EPS), reads=[b_ps[bk]], writes=[b_lnt])
        S.op("act", lambda e: e.activation(out=rstd[:, 0:width], in_=lnt[:, 0:width], func=AF.Exp, scale=-0.5),
             reads=[b_lnt], writes=[b_rstd])

    xTv = xT.rearrange("(k p) t -> p k t", p=128)
    for sup in range(ntok // TS):
        t0s = sup * TS
        b_QKs = b_QKs_l[sup]; b_Vs = b_Vs_l[sup]
        for pt in range(TS // TP):
            tok0 = t0s + pt * TP
            for k0 in (0, 8):
                S.dma("sp", lambda e, k0=k0, tok0=tok0: e.dma_start(out=xt[:, k0:k0 + 8, :],
                                                                    in_=xTv[:, k0:k0 + 8, tok0:tok0 + TP]),
                      writes=[b_xt])
            S.op("act", lambda e: e.activation(out=sq[:], in_=xt[:], func=AF.Square), reads=[b_xt], writes=[b_sq])
            rms_rstd(sq, b_sq, NK, TP, float(D))
            for k in range(NK):
                ti = st["tmp"]; st["tmp"] ^= 1
                S.op("dve", lambda e, k=k, ti=ti: e.tensor_tensor(out=tmp[ti][:, 0:TP], in0=xt[:, k, :],
                                                                  in1=rstd[:, 0:TP], op=ALU.mult),
                     reads=[b_xt, b_rstd], writes=[b_tmp[ti]])
                S.op("act", lambda e, k=k, ti=ti, pt=pt: e.activation(
                    out=aT[:, k, pt * TP:(pt + 1) * TP], in_=tmp[ti][:, 0:TP], func=AF.Identity,
                    scale=s1[:, k:k + 1], bias=modt[:, k:k + 1]),
                    reads=[b_tmp[ti], b_s1, b_modt], writes=[b_aT[pt]])

        def gemm_group(src, c0, ncols, epilogue):
            wi = st["w"]; st["w"] ^= 1
            load_w(wb[wi], b_wb[wi], src, NK, c0, ncols)
            if tick is not None:
                tick()
            for m in range((ncols + 127) // 128):
                mc = min(128, ncols - m * 128)
                for t in range(TS // 512):
                    bk = nbank()
                    for k in range(NK):
                        S.op("pe", lambda e, wi=wi, m=m, mc=mc, t=t, k=k, bk=bk: e.matmul(
                            ps[0:mc, bk, :], lhsT=wb[wi][:, k, m * 128:m * 128 + mc],
                            rhs=aT[:, k, t * 512:(t + 1) * 512], start=(k == 0), stop=(k == NK - 1)),
                            reads=[b_wb[wi]] + b_aT[4 * t:4 * t + 4], writes=[b_ps[bk]])
                    epilogue(m, mc, t, bk)

        def evac(out_ap, bout, bk, mc, scale=1.0, func=None):
            st["ev"] ^= 1
            if func is not None or st["ev"]:
                f = func if func is not None else AF.Copy
                S.op("act", lambda e: e.activation(out=out_ap, in_=ps[0:mc, bk, :], func=f, scale=scale),
                     reads=[b_ps[bk]], writes=[bout])
            else:
                S.op("dve", lambda e: e.tensor_scalar(out=out_ap, in0=ps[0:mc, bk, :], scalar1=scale, scalar2=None,
                                                      op0=ALU.mult), reads=[b_ps[bk]], writes=[bout])

        gemm_group(win, 0, 512, lambda m, mc, t, bk: evac(cq[:, m, t * 512:(t + 1) * 512], b_cq, bk, mc))
        gemm_group(win, 512, 512, lambda m, mc, t, bk: evac(ckv[:, m, t * 512:(t + 1) * 512], b_ckv, bk, mc))
        gemm_group(win, 1024, 64, lambda m, mc, t, bk: evac(kr[:, t * 512:(t + 1) * 512], b_kr, bk, mc))
        gemm_group(wsw, 0, 64, lambda m, mc, t, bk: evac(krs[:, t * 512:(t + 1) * 512], b_krs, bk, mc))

        def out_bf(dst, bdst, row0, scale):
            cur = {}

            def ep(m, mc, t, bk):
                if t == 0:
                    cur["i"] = st["ob"]; st["ob"] ^= 1
                i = cur["i"]
                evac(ob[i][:, t * 512:(t + 1) * 512], b_ob[i], bk, mc, scale=scale)
                if t == TS // 512 - 1:
                    r = row0 + m * 128
                    S.dma("sp", lambda e, i=i, r=r: e.dma_start(out=dst[r:r + 128, t0s:t0s + TS], in_=ob[i][:, :]),
                          reads=[b_ob[i]], writes=[bdst])
            return ep

        def out_f32(dst, bdst, row0, func):
            cur = {}

            def ep(m, mc, t, bk):
                if t == 0:
                    cur["i"] = st["of"]; st["of"] ^= 1
                i = cur["i"]
                evac(of[i][:, t * 512:(t + 1) * 512], b_of[i], bk, mc, func=func)
                if t == TS // 512 - 1:
                    r = row0 + m * 128
                    S.dma("sp", lambda e, i=i, r=r: e.dma_start(out=dst[r:r + 128, t0s:t0s + TS], in_=of[i][:, :]),
                          reads=[b_of[i]], writes=[bdst])
            return ep

        sc = 128.0 ** -0.5
        def out_qk(rowfn, scale):
            cur = {}

            def ep(m, mc, t, bk):
                if t == 0:
                    cur["i"] = st["ob"]; st["ob"] ^= 1
                i = cur["i"]
                evac(ob[i][:, t * 512:(t + 1) * 512], b_ob[i], bk, mc, scale=scale)
                if t == TS // 512 - 1:
                    sh, r0 = rowfn(m)
                    S.dma("sp", lambda e: e.dma_start(out=QKs[sh, sup, r0:r0 + 128, :], in_=ob[i][:, :]),
                          reads=[b_ob[i]], writes=[b_QKs])
            return ep

        def gemm_group_tm(c0, store):
            wi = st["w"]; st["w"] ^= 1
            load_w(wb[wi], b_wb[wi], win, NK, c0, 512)
            if tick is not None:
                tick()
            for s_ in range(TS // 128):
                bk = nbank()
                for k in range(NK):
                    S.op("pe", lambda e: e.matmul(ps[:, bk, :], lhsT=aT[:, k, s_ * 128:(s_ + 1) * 128], rhs=wb[wi][:, k, 0:512],
                                                  start=(k == 0), stop=(k == NK - 1)), reads=[b_wb[wi], b_aT[s_]], writes=[b_ps[bk]])
                oi = st["ob"]; st["ob"] ^= 1
                evac(ob[oi][:, 0:512], b_ob[oi], bk, 128)
                store(s_, oi)

        for g in range(3):
            gemm_group(win, 1088 + g * 512, 512, out_qk(lambda m, g=g: (m, g * 128), sc))
        for g in range(3):
            gemm_group(win, 1088 + 1536 + g * 512, 512, out_qk(lambda m, g=g: (m, 384 + g * 128), 1.0))
        for g in range(3):
            def st_dv(s_, oi, g=g):
                tk = t0s + s_ * 128
                S.dma("sp", lambda e: e.dma_start(out=Vs[:, tk:tk + 128, g * 128:(g + 1) * 128].rearrange("h p c -> p h c"),
                                                  in_=ob[oi][:, 0:512].rearrange("p (h c) -> p h c", c=128)),
                      reads=[b_ob[oi]], writes=[b_Vs])
            gemm_group_tm(1088 + 3072 + g * 512, st_dv)
        for gi in range(2):
            gemm_group(win, 5696 + gi * 512, 512, out_qk(lambda m, gi=gi: ((4 * gi + m) // 2, 768 + (m % 2) * 128), sc))
        for gi in range(2):
            gemm_group(win, 5696 + 1024 + gi * 512, 512, out_qk(lambda m, gi=gi: ((4 * gi + m) // 2, 1024 + (m % 2) * 128), 1.0))
        for gi in range(2):
            def st_sv(s_, oi, gi=gi):
                tk = t0s + s_ * 128
                S.dma("sp", lambda e: e.dma_start(out=Vs[2 * gi:2 * gi + 2, tk:tk + 128, 384:640].rearrange("j p c -> p j c"),
                                                  in_=ob[oi][:, 0:512].rearrange("p (j c) -> p j c", c=256)),
                      reads=[b_ob[oi]], writes=[b_Vs])
            gemm_group_tm(5696 + 2048 + gi * 512, st_sv)

        scm = 192.0 ** -0.5
        for tt in range(TS // 512):
            tok0 = t0s + tt * 512
            tsl = slice(tt * 512, (tt + 1) * 512)
            S.dma("sp", lambda e, tok0=tok0: e.dma_start(out=posi[:], in_=pos[0:1, tok0:tok0 + 512].partition_broadcast(64)),
                  writes=[b_posi])
            S.op("dve", lambda e: e.tensor_copy(out=ang[:], in_=posi[:]), reads=[b_posi], writes=[b_ang])
            S.op("dve", lambda e: e.tensor_scalar(out=ang[:], in0=ang[:], scalar1=invft[:, 0:1], scalar2=None, op0=ALU.mult),
                 reads=[b_ang, b_invft], writes=[b_ang])
            S.op("dve", lambda e: e.tensor_scalar(out=kf[:], in0=ang[:], scalar1=1.0 / TWO_PI, scalar2=None, op0=ALU.mult),
                 reads=[b_ang], writes=[b_kf])
            S.op("dve", lambda e: e.tensor_copy(out=ki[:], in_=kf[:]), reads=[b_kf], writes=[b_ki])
            S.op("dve", lambda e: e.tensor_copy(out=kf[:], in_=ki[:]), reads=[b_ki], writes=[b_kf])
            S.op("dve", lambda e: e.scalar_tensor_tensor(out=rr[:], in0=kf[:], scalar=-C1, in1=ang[:], op0=ALU.mult, op1=ALU.add),
                 reads=[b_kf, b_ang], writes=[b_rr])
            S.op("dve", lambda e: e.scalar_tensor_tensor(out=rr[:], in0=kf[:], scalar=-C2, in1=rr[:], op0=ALU.mult, op1=ALU.add),
                 reads=[b_kf, b_rr], writes=[b_rr])

            def wrap(r, br):
                S.op("dve", lambda e: e.tensor_scalar(out=mm[:], in0=r[:], scalar1=math.pi, scalar2=-TWO_PI, op0=ALU.is_gt, op1=ALU.mult),
                     reads=[br], writes=[b_mm])
                S.op("dve", lambda e: e.tensor_tensor(out=r[:], in0=r[:], in1=mm[:], op=ALU.add), reads=[br, b_mm], writes=[br])
                S.op("dve", lambda e: e.tensor_scalar(out=mm[:], in0=r[:], scalar1=-math.pi, scalar2=TWO_PI, op0=ALU.is_lt, op1=ALU.mult),
                     reads=[br], writes=[b_mm])
                S.op("dve", lambda e: e.tensor_tensor(out=r[:], in0=r[:], in1=mm[:], op=ALU.add), reads=[br, b_mm], writes=[br])
                S.op("dve", lambda e: e.tensor_scalar(out=r[:], in0=r[:], scalar1=3.1415925, scalar2=-3.1415925, op0=ALU.min, op1=ALU.max),
                     reads=[br], writes=[br])
            wrap(rr, b_rr)
            S.op("dve", lambda e: e.tensor_scalar(out=rc[:], in0=rr[:], scalar1=math.pi / 2, scalar2=None, op0=ALU.add),
                 reads=[b_rr], writes=[b_rc])
            wrap(rc, b_rc)
            S.op("act", lambda e: e.activation(out=CS[:], in_=rc[:], func=AF.Sin), reads=[b_rc], writes=[b_CS])
            S.op("act", lambda e: e.activation(out=SN[:], in_=rr[:], func=AF.Sin, scale=sgnt[:, 0:1]), reads=[b_rr, b_sgnt], writes=[b_SN])

            def rope_out(src_r, bsr, src_s, bss, scale, dst_ap, bdst):
                S.op("dve", lambda e: e.scalar_tensor_tensor(out=t1[:], in0=src_r, scalar=scale, in1=CS[:], op0=ALU.mult, op1=ALU.mult),
                     reads=[bsr, b_CS], writes=[b_t1])
                S.op("dve", lambda e: e.scalar_tensor_tensor(out=t2[:], in0=src_s, scalar=scale, in1=SN[:], op0=ALU.mult, op1=ALU.mult),
                     reads=[bss, b_SN], writes=[b_t2])
                S.op("dve", lambda e: e.tensor_tensor(out=dst_ap, in0=t1[:], in1=t2[:], op=ALU.add),
                     reads=[b_t1, b_t2], writes=[bdst])

            oi = st["ob"]; st["ob"] ^= 1
            rope_out(kr[:, tsl], b_kr, krs[:, tsl], b_krs, 1.0, ob[oi][0:64, 0:512], b_ob[oi])
            for sh in range(4):
                S.dma("sp", lambda e: e.dma_start(out=QKs[sh, sup, 1920:1984, tok0 - t0s:tok0 - t0s + 512], in_=ob[oi][0:64, 0:512]),
                      reads=[b_ob[oi]], writes=[b_QKs])

            def latent_norm(src, bsrc, gt, bgt):
                sqv = sq[:, :, :].rearrange("p (k a) t -> p k (a t)", a=4)
                S.op("act", lambda e: e.activation(out=sqv, in_=src[:, :, tsl], func=AF.Square), reads=[bsrc], writes=[b_sq])
                rms_rstd(sqv, b_sq, 4, 512, 512.0)
                for k in range(4):
                    S.op("dve", lambda e, k=k: e.scalar_tensor_tensor(out=lat[:, k, :], in0=src[:, k, tsl], scalar=gt[:, k:k + 1],
                                                                      in1=rstd[:, :], op0=ALU.mult, op1=ALU.mult),
                         reads=[bsrc, bgt, b_rstd], writes=[b_lat])

            latent_norm(cq, b_cq, gqt, b_gqt)
            for h in range(8):
                bk = nbank()
                for k in range(4):
                    S.op("pe", lambda e, h=h, k=k, bk=bk: e.matmul(ps[:, bk, :], lhsT=wuqb[:, k, h * 192:h * 192 + 128], rhs=lat[:, k, :],
                                                                   start=(k == 0), stop=(k == 3)), reads=[b_wuqb, b_lat], writes=[b_ps[bk]])
                oi = st["ob"]; st["ob"] ^= 1
                evac(ob[oi][:, 0:512], b_ob[oi], bk, 128, scale=scm)
                S.dma("sp", lambda e, oi=oi, h=h, tok0=tok0: e.dma_start(out=QKs[h // 2, sup, 1280 + (h % 2) * 128:1280 + (h % 2) * 128 + 128, tok0 - t0s:tok0 - t0s + 512], in_=ob[oi][:, 0:512]),
                      reads=[b_ob[oi]], writes=[b_mlaq])
                bk1 = nbank(); bk2 = nbank()
                for k in range(4):
                    S.op("pe", lambda e, h=h, k=k, bk1=bk1: e.matmul(ps[0:64, bk1, :], lhsT=wuqb[:, k, h * 192 + 128:h * 192 + 192], rhs=lat[:, k, :],
                                                                     start=(k == 0), stop=(k == 3)), reads=[b_wuqb, b_lat], writes=[b_ps[bk1]])
                for k in range(4):
                    S.op("pe", lambda e, h=h, k=k, bk2=bk2: e.matmul(ps[0:64, bk2, :], lhsT=wuqsb[:, k, h * 64:(h + 1) * 64], rhs=lat[:, k, :],
                                                                     start=(k == 0), stop=(k == 3)), reads=[b_wuqsb, b_lat], writes=[b_ps[bk2]])
                oi = st["ob"]; st["ob"] ^= 1
                rope_out(ps[0:64, bk1, :], b_ps[bk1], ps[0:64, bk2, :], b_ps[bk2], scm, ob[oi][0:64, 0:512], b_ob[oi])
                S.dma("sp", lambda e, oi=oi, h=h, tok0=tok0: e.dma_start(out=QKs[h // 2, sup, 1792 + (h % 2) * 64:1792 + (h % 2) * 64 + 64, tok0 - t0s:tok0 - t0s + 512], in_=ob[oi][0:64, 0:512]),
                      reads=[b_ob[oi]], writes=[b_mlaq])
            latent_norm(ckv, b_ckv, gkvt, b_gkvt)
            for h in range(8):
                bk = nbank()
                for k in range(4):
                    S.op("pe", lambda e: e.matmul(ps[:, bk, :], lhsT=wukvb[:, k, h * 256:h * 256 + 128], rhs=lat[:, k, :],
                                                  start=(k == 0), stop=(k == 3)), reads=[b_wukvb, b_lat], writes=[b_ps[bk]])
                oi = st["ob"]; st["ob"] ^= 1
                evac(ob[oi][:, 0:512], b_ob[oi], bk, 128)
                S.dma("sp", lambda e: e.dma_start(out=QKs[h // 2, sup, 1536 + (h % 2) * 128:1536 + (h % 2) * 128 + 128, tok0 - t0s:tok0 - t0s + 512], in_=ob[oi][:, 0:512]),
                      reads=[b_ob[oi]], writes=[b_QKs])
            wv = wukvb[:, :, :].rearrange("p k (h c) -> p k h c", c=256)
            for s4 in range(4):
                for hg in range(2):
                    bk = nbank()
                    for k in range(4):
                        S.op("pe", lambda e: e.matmul(ps[:, bk, :].rearrange("p (h c) -> p h c", c=128), lhsT=lat[:, k, s4 * 128:(s4 + 1) * 128],
                                                      rhs=wv[:, k, hg * 4:(hg + 1) * 4, 128:256], start=(k == 0), stop=(k == 3)),
                             reads=[b_wukvb, b_lat], writes=[b_ps[bk]])
                    oi = st["ob"]; st["ob"] ^= 1
                    evac(ob[oi][:, 0:512], b_ob[oi], bk, 128)
                    tk = tok0 + s4 * 128
                    S.dma("sp", lambda e: e.dma_start(out=Vs[2 * hg:2 * hg + 2, tk:tk + 128, 640:896].rearrange("j p c -> p j c"),
                                                      in_=ob[oi][:, 0:512].rearrange("p (j c) -> p j c", c=256)),
                          reads=[b_ob[oi]], writes=[b_Vs])
        if after_sup is not None:
            after_sup(sup)
        for gi in range(12):
            gemm_group(win, 8768 + gi * 512, 512, out_f32(gT, b_gT, gi * 512, AF.Sigmoid))


SEQ = 8192
NB = SEQ // 128
RATES = (1, 4, 16)
BIG = 1.0e6


def emit_B(S, nc, d, QKl, Vl, Ysrc, b_QKg, b_Vg, b_Ys, seq=SEQ, do_mla=True, do_sb=True, do_dil=True):
    NBk = seq // 128
    NG4 = seq // 512
    dposk = d['dposk']; dposq = d['dposq']; nslope = d['nslope']
    maskc_d = d['maskc']; masks_d = d['masks']; maskd_d = d['maskd']; m1_d = d['m1']; m2_d = d['m2']
    b_ymla = b_Ys; b_ysb = b_Ys; b_ydil = b_Ys
    SHR = 1984; SHV = 896
    Q1 = S.sbuf("Q1", [128, seq], BF16); bQ1 = Buf("Q1")
    K1 = S.sbuf("K1", [128, seq], BF16); bK1 = Buf("K1")
    if do_mla:
        Q2 = S.sbuf("Q2", [64, seq], BF16); bQ2 = Buf("Q2")
        K2 = S.sbuf("K2", [64, seq], BF16); bK2 = Buf("K2")
    V1 = S.sbuf("V1", [128, NBk, 128], BF16); bV1 = Buf("V1")
    two = do_mla or do_sb
    if two:
        Q1b = S.sbuf("Q1b", [128, seq], BF16); bQ1b = Buf("Q1b")
        K1b = S.sbuf("K1b", [128, seq], BF16); bK1b = Buf("K1b")
        V1b = S.sbuf("V1b", [128, NBk, 128], BF16); bV1b = Buf("V1b")
        if do_mla:
            Q2b = S.sbuf("Q2b", [64, seq], BF16); bQ2b = Buf("Q2b")
    NP = 8
    PT = [S.sbuf("PT%d" % i, [128, 512], BF16) for i in range(NP)]; bPT = [Buf("PT%d" % i) for i in range(NP)]
    SP = [S.sbuf("SP%d" % i, [128, 512], BF16) for i in range(NP)]; bSP = [Buf("SP%d" % i) for i in range(NP)]
    EN = [S.sbuf("EN%d" % i, [128, 512], F32) for i in range(4)]; bEN = [Buf("EN%d" % i) for i in range(4)]
    SNt = [S.sbuf("SN%d" % i, [128, 512], F32) for i in range(NP)]; bSN = [Buf("SN%d" % i) for i in range(NP)]
    UU = [S.sbuf("UU%d" % i, [128, 512], F32) for i in range(4)]; bUU = [Buf("UU%d" % i) for i in range(4)]
    YS = [S.sbuf("YS%d" % i, [128, 512], BF16) for i in range(2)]; bYS = [Buf("YS%d" % i) for i in range(2)]
    RD = S.sbuf("RD", [128, 512], F32); bRD = Buf("RD")
    maskc = S.sbuf("maskc_t", [128, 4, 512], BF16); bmaskc = Buf("maskc")
    masks = S.sbuf("masks_t", [128, 4, 512], BF16); bmasks = Buf("masks")
    maskd = S.sbuf("maskd_t", [128, 256], F32); bmaskd = Buf("maskd")
    M1 = S.sbuf("M1", [128, 128], BF16); bM1 = Buf("M1")
    M2 = S.sbuf("M2", [128, 128], BF16); bM2 = Buf("M2")
    ones = S.sbuf("ones", [128, 128], BF16); bones = Buf("ones")
    nsl = S.sbuf("nsl", [128, 3], F32); bnsl = Buf("nsl")
    ps = S.psum("ps", [128, 8, 512], F32); bps = [Buf("ps%d" % i) for i in range(8)]

    S.op("dve", lambda e: e.memset(ones[:], 1.0), writes=[bones])
    S.dma("sp", lambda e: e.dma_start(out=maskc[:], in_=maskc_d[:, :, :]), writes=[bmaskc])
    S.dma("sp", lambda e: e.dma_start(out=masks[:], in_=masks_d[:, :, :]), writes=[bmasks])
    S.dma("sp", lambda e: e.dma_start(out=maskd[:], in_=maskd_d[:, :]), writes=[bmaskd])
    S.dma("sp", lambda e: e.dma_start(out=M1[:], in_=m1_d[:, :]), writes=[bM1])
    S.dma("sp", lambda e: e.dma_start(out=M2[:], in_=m2_d[:, :]), writes=[bM2])
    S.dma("sp", lambda e: e.dma_start(out=nsl[:], in_=nslope[:, :]), writes=[bnsl])

    QK5 = QKl.rearrange("c r (p a) t -> c r p (a t)", a=1) if False else QKl
    Vl4 = Vl
    Vl6 = Vl.rearrange("c r (b p) d -> c r b p d", p=128)

    def load_fm(dst, bdst, R0, rows=128):
        dv4 = dst[0:rows, :].rearrange("p (r u t) -> p r u t", r=4, u=2)
        for u in range(2):
            S.dma("sp", lambda e: e.dma_start(out=dv4[:, :, u, :],
                                              in_=QK5[u * 4 + R0 // 512, :, R0 % 512:R0 % 512 + rows, :].rearrange("r p t -> p r t")),
                  reads=[b_QKg], writes=[bdst])

    def load_v(c0, Vt=None, bVt=None):
        Vt = V1 if Vt is None else Vt
        bVt = bV1 if bVt is None else bVt
        for r in range(4):
            for cp in range(4):
                S.dma("sp", lambda e: e.dma_start(out=Vt[:, r * 16 + cp * 4:r * 16 + cp * 4 + 4, :],
                                                  in_=Vl6[cp, r, :, :, c0:c0 + 128].rearrange("b p d -> p b d")),
                      reads=[b_Vg], writes=[bVt])

    def load_v_perm(c0, rt):
        if rt == 1:
            load_v(c0)
        elif rt == 4:
            for s_ in range(4):
                for r in range(4):
                    S.dma("sp", lambda e: e.dma_start(out=V1[:, s_ * 16 + 4 * r:s_ * 16 + 4 * r + 4, :],
                                                      in_=Vl4[:, r, s_:s_ + 4 * 127 + 1:4, c0:c0 + 128].rearrange("c p d -> p c d")),
                          reads=[b_Vg], writes=[bV1])
        else:
            for s_ in range(16):
                for cp in range(4):
                    S.dma("sp", lambda e: e.dma_start(out=V1[32 * cp:32 * cp + 32, s_ * 4:s_ * 4 + 4, :],
                                                      in_=Vl4[cp, :, s_:s_ + 16 * 31 + 1:16, c0:c0 + 128].rearrange("m p d -> p m d")),
                          reads=[b_Vg], writes=[bV1])

    Ys5 = Ysrc

    def yout(blk, g4):
        return Ys5[g4 // 8, blk, :, (g4 % 8) * 512:(g4 % 8) * 512 + 512]

    def pipeline(steps):
        prev = None
        for s1, s2 in steps:
            s1()
            if prev is not None:
                prev()
            prev = s2
        if prev is not None:
            prev()

    cnt = {"z": 0, "o": 0, "pt": 0, "ys": 0, "en": 0, "uu": 0}

    def interleave(a, b):
        out = []
        for x, y in zip(a, b):
            out.append(x); out.append(y)
        return out

    if do_mla:
        load_fm(K2, bK2, 1920, 64)
        hs = [dict(Q=Q1, bQ=bQ1, Qr=Q2, bQr=bQ2, K=K1, bK=bK1, V=V1, bV=bV1, zb=(0, 1), ob=2, db=3),
              dict(Q=Q1b, bQ=bQ1b, Qr=Q2b, bQr=bQ2b, K=K1b, bK=bK1b, V=V1b, bV=bV1b, zb=(6, 7), ob=4, db=5)]
        allsteps = []
        for h in range(2):
            H = hs[h]
            load_fm(H["Q"], H["bQ"], 1280 + h * 128)
            load_fm(H["Qr"], H["bQr"], 1792 + h * 64, 64)
            load_fm(H["K"], H["bK"], 1536 + h * 128)
            load_v(640 + h * 128, H["V"], H["bV"])
            steps = []
            zc = 0
            for g4 in range(NG4):
                qs = slice(g4 * 512, (g4 + 1) * 512)
                ob = H["ob"]; db = H["db"]
                nst = 4 * g4 + 4
                for j in range(nst):
                    zb = H["zb"][zc % 2]; zc += 1
                    ks = slice(j * 128, (j + 1) * 128)
                    box = {}

                    def s1(qs=qs, j=j, zb=zb, ks=ks, g4=g4, H=H, box=box):
                        pi = cnt["pt"] % NP; cnt["pt"] += 1
                        box["pi"] = pi
                        S.op("pe", lambda e: e.matmul(ps[:, zb, :], lhsT=H["K"][:, ks], rhs=H["Q"][:, qs], start=True, stop=False),
                             reads=[H["bK"], H["bQ"]], writes=[bps[zb]])
                        S.op("pe", lambda e: e.matmul(ps[:, zb, :], lhsT=K2[0:64, ks], rhs=H["Qr"][0:64, qs], start=False, stop=True),
                             reads=[bK2, H["bQr"]], writes=[bps[zb]])
                        S.op("act", lambda e: e.activation(out=PT[pi][:], in_=ps[:, zb, :], func=AF.Exp),
                             reads=[bps[zb]], writes=[bPT[pi]])
                        if j >= 4 * g4:
                            S.op("dve", lambda e: e.tensor_tensor(out=PT[pi][:], in0=PT[pi][:], in1=maskc[:, j - 4 * g4, :], op=ALU.mult),
                                 reads=[bPT[pi], bmaskc], writes=[bPT[pi]])

                    def s2(j=j, ob=ob, db=db, nst=nst, h=h, g4=g4, H=H, box=box):
                        pi = box["pi"]
                        S.op("pe", lambda e: e.matmul(ps[:, ob, :], lhsT=H["V"][:, j, :], rhs=PT[pi][:], start=(j == 0), stop=(j == nst - 1)),
                             reads=[H["bV"], bPT[pi]], writes=[bps[ob]])
                        S.op("pe", lambda e: e.matmul(ps[:, db, :], lhsT=ones[:], rhs=PT[pi][:], start=(j == 0), stop=(j == nst - 1)),
                             reads=[bones, bPT[pi]], writes=[bps[db]])
                        if j == nst - 1:
                            yi = cnt["ys"] % 2; cnt["ys"] += 1
                            ri = cnt["uu"] % 4; cnt["uu"] += 1
                            S.op("dve", lambda e: e.reciprocal(out=UU[ri][:], in_=ps[:, db, :]), reads=[bps[db]], writes=[bUU[ri]])
                            S.op("dve", lambda e: e.tensor_tensor(out=YS[yi][:], in0=ps[:, ob, :], in1=UU[ri][:], op=ALU.mult),
                                 reads=[bps[ob], bUU[ri]], writes=[bYS[yi]])
                            S.dma("sp", lambda e: e.dma_start(out=yout(h, g4), in_=YS[yi][:]), reads=[bYS[yi]], writes=[b_ymla])
                    steps.append((s1, s2))
            allsteps.append(steps)
        ml = interleave(allsteps[0], allsteps[1])
        DEP = 3
        for k in range(-DEP, len(ml)):
            if k + DEP < len(ml):
                ml[k + DEP][0]()
            if k >= 0:
                ml[k][1]()

    if do_sb:
        hs = [dict(Q=Q1, bQ=bQ1, K=K1, bK=bK1, V=V1, bV=bV1, zb=(0, 1), ob=2, rb=3),
              dict(Q=Q1b, bQ=bQ1b, K=K1b, bK=bK1b, V=V1b, bV=bV1b, zb=(6, 7), ob=4, rb=5)]
        allsteps = []
        for h in range(2):
            H = hs[h]
            load_fm(H["Q"], H["bQ"], 768 + h * 128)
            load_fm(H["K"], H["bK"], 1024 + h * 128)
            load_v(384 + h * 128, H["V"], H["bV"])
            steps = []
            zc = 0
            for g4 in range(NG4):
                qs = slice(g4 * 512, (g4 + 1) * 512)
                ob = H["ob"]; rb = H["rb"]
                nst = 4 * g4 + 4
                for idx, j in enumerate(reversed(range(nst))):
                    first = idx == 0; last = idx == nst - 1
                    zb = H["zb"][zc % 2]; zc += 1
                    ks = slice(j * 128, (j + 1) * 128)
                    box = {}

                    def t1(qs=qs, j=j, zb=zb, ks=ks, g4=g4, H=H, box=box):
                        pi = cnt["pt"] % NP; cnt["pt"] += 1
                        ei = cnt["en"] % 4; cnt["en"] += 1
                        box["pi"] = pi; box["ei"] = ei
                        S.op("pe", lambda e: e.matmul(ps[:, zb, :], lhsT=H["K"][:, ks], rhs=H["Q"][:, qs], start=True, stop=True),
                             reads=[H["bK"], H["bQ"]], writes=[bps[zb]])
                        S.op("act", lambda e: e.activation(out=EN[ei][:], in_=ps[:, zb, :], func=AF.Exp, scale=-1.0),
                             reads=[bps[zb]], writes=[bEN[ei]])

                    def t1b(j=j, zb=zb, g4=g4, box=box):
                        pi = box["pi"]; ei = box["ei"]
                        S.op("act", lambda e: e.activation(out=SNt[pi][:], in_=EN[ei][:], func=AF.Ln, bias=1.0),
                             reads=[bEN[ei]], writes=[bSN[pi]])
                        S.op("dve", lambda e: e.tensor_tensor(out=SP[pi][:], in0=ps[:, zb, :], in1=SNt[pi][:], op=ALU.add),
                             reads=[bps[zb], bSN[pi]], writes=[bSP[pi]])
                        if j >= 4 * g4:
                            S.op("dve", lambda e: e.tensor_tensor(out=SP[pi][:], in0=SP[pi][:], in1=masks[:, j - 4 * g4, :], op=ALU.mult),
                                 reads=[bSP[pi], bmasks], writes=[bSP[pi]])

                    def t2(j=j, rb=rb, first=first, g4=g4, box=box):
                        pi = box["pi"]
                        ui = cnt["uu"] % 4; cnt["uu"] += 1
                        S.op("pe", lambda e: e.matmul(ps[:, rb, :], lhsT=M1[:], rhs=SP[pi][:], start=first, stop=False, skip_group_check=True),
                             reads=[bM1, bSP[pi]], writes=[bps[rb]])
                        S.op("dve", lambda e: e.tensor_tensor(out=UU[ui][:], in0=SNt[pi][:], in1=ps[:, rb, :], op=ALU.add),
                             reads=[bSN[pi], bps[rb]], writes=[bUU[ui]])
                        S.op("act", lambda e: e.activation(out=PT[pi][:], in_=UU[ui][:], func=AF.Exp, scale=-1.0),
                             reads=[bUU[ui]], writes=[bPT[pi]])
                        if j >= 4 * g4:
                            S.op("dve", lambda e: e.tensor_tensor(out=PT[pi][:], in0=PT[pi][:], in1=masks[:, j - 4 * g4, :], op=ALU.mult),
                                 reads=[bPT[pi], bmasks], writes=[bPT[pi]])

                    def t3(rb=rb, last=last, box=box):
                        pi = box["pi"]
                        S.op("pe", lambda e: e.matmul(ps[:, rb, :], lhsT=M2[:], rhs=SP[pi][:], start=False, stop=last, skip_group_check=True),
                             reads=[bM2, bSP[pi]], writes=[bps[rb]])

                    def t4(j=j, ob=ob, first=first, last=last, h=h, g4=g4, H=H, box=box):
                        pi = box["pi"]
                        S.op("pe", lambda e: e.matmul(ps[:, ob, :], lhsT=H["V"][:, j, :], rhs=PT[pi][:], start=first, stop=last),
                             reads=[H["bV"], bPT[pi]], writes=[bps[ob]])
                        if last:
                            yi = cnt["ys"] % 2; cnt["ys"] += 1
                            S.op("act", lambda e: e.activation(out=YS[yi][:], in_=ps[:, ob, :], func=AF.Copy),
                                 reads=[bps[ob]], writes=[bYS[yi]])
                            S.dma("sp", lambda e: e.dma_start(out=yout(3 + h, g4), in_=YS[yi][:]), reads=[bYS[yi]], writes=[b_ysb])
                    steps.append((t1, t2, t3, t4, t1b))
            allsteps.append(steps)
        N_ = len(allsteps[0])
        for X in allsteps:
            X[0][0]()
        for X in allsteps:
            X[0][4]()
        for k in range(N_):
            for X in allsteps:
                X[k][1]()
            if k > 0:
                for X in allsteps:
                    X[k - 1][3]()
            if k + 1 < N_:
                for X in allsteps:
                    X[k + 1][0]()
                for X in allsteps:
                    X[k + 1][4]()
            for X in allsteps:
                X[k][2]()
        for X in allsteps:
            X[N_ - 1][3]()

    if do_dil:
        accn = S.sbuf("accn", [128, seq], F32); baccn = Buf("accn")
        accd = S.sbuf("accd", [128, seq], F32); baccd = Buf("accd")
        posk_i = S.sbuf("posk_i", [128, NBk], I32); bposk_i = Buf("posk_i")
        posk = S.sbuf("posk", [128, NBk], F32); bposk = Buf("posk")
        pq_i = [S.sbuf("pq_i%d" % i, [128, 128], I32) for i in range(3)]; bpq_i = [Buf("pq_i%d" % i) for i in range(3)]
        pq = [S.sbuf("pq%d" % i, [128, 128], F32) for i in range(3)]; bpq = [Buf("pq%d" % i) for i in range(3)]
        dd = [S.sbuf("dd%d" % i, [128, 256], F32) for i in range(3)]; bdd = [Buf("dd%d" % i) for i in range(3)]
        zt = [S.sbuf("zt%d" % i, [128, 256], F32) for i in range(3)]; bzt = [Buf("zt%d" % i) for i in range(3)]
        for g in range(3):
            r = RATES[g]
            L = seq // r
            nb = L // 128
            load_fm(Q1, bQ1, g * 128)
            load_fm(K1, bK1, 384 + g * 128)
            load_v_perm(g * 128, r)
            S.dma("sp", lambda e: e.dma_start(out=posk_i[:], in_=dposk[g, :, :]), writes=[bposk_i])
            S.op("dve", lambda e: e.tensor_copy(out=posk[:], in_=posk_i[:]), reads=[bposk_i], writes=[bposk])
            S.op("dve", lambda e: e.tensor_scalar(out=posk[:], in0=posk[:], scalar1=-1.0, scalar2=None, op0=ALU.mult), reads=[bposk], writes=[bposk])
            dsteps = []
            for i in range(NBk):
                s, m = divmod(i, nb)
                hp = m > 0
                c0 = 0 if hp else 128
                zb = (0, 1, 6, 7)[cnt["z"] % 4]; cnt["z"] += 1
                ob = 2 + (cnt["o"] % 2); db = 4 + (cnt["o"] % 2); cnt["o"] += 1
                pi = cnt["pt"] % NP; cnt["pt"] += 1
                bi = i % 3
                st0 = s + 128 * r * m
                qsl = slice(st0, st0 + 127 * r + 1, r) if r > 1 else slice(st0, st0 + 128)
                psl = (slice(st0 - 128 * r, st0 - 128 * r + 127 * r + 1, r) if r > 1 else slice(st0 - 128, st0)) if hp else None
                def s1(i=i, hp=hp, c0=c0, zb=zb, pi=pi, bi=bi, qsl=qsl, psl=psl, g=g):
                    if hp:
                        S.op("pe", lambda e: e.matmul(ps[:, zb, 0:128], lhsT=K1[:, psl], rhs=Q1[:, qsl], start=True, stop=True),
                             reads=[bK1, bQ1], writes=[bps[zb]])
                    S.op("pe", lambda e: e.matmul(ps[:, zb, 128:256], lhsT=K1[:, qsl], rhs=Q1[:, qsl], start=True, stop=True),
                         reads=[bK1, bQ1], writes=[bps[zb]])
                    S.dma("sp", lambda e: e.dma_start(out=pq_i[bi][:], in_=dposq[g:g + 1, i * 128:(i + 1) * 128].partition_broadcast(128)),
                          writes=[bpq_i[bi]])

                def s1x(i=i, hp=hp, bi=bi):
                    S.op("dve", lambda e: e.tensor_copy(out=pq[bi][:], in_=pq_i[bi][:]), reads=[bpq_i[bi]], writes=[bpq[bi]])
                    if hp:
                        S.op("act", lambda e: e.activation(out=dd[bi][:, 0:128], in_=pq[bi][:], func=AF.Abs, bias=posk[:, i - 1:i], scale=1.0),
                             reads=[bpq[bi], bposk], writes=[bdd[bi]])
                    S.op("act", lambda e: e.activation(out=dd[bi][:, 128:256], in_=pq[bi][:], func=AF.Abs, bias=posk[:, i:i + 1], scale=1.0),
                         reads=[bpq[bi], bposk], writes=[bdd[bi]])

                def s1b(i=i, hp=hp, c0=c0, zb=zb, pi=pi, bi=bi, g=g):
                    S.op("dve", lambda e: e.tensor_tensor(out=dd[bi][:, c0:256], in0=dd[bi][:, c0:256], in1=maskd[:, c0:256], op=ALU.add),
                         reads=[bdd[bi], bmaskd], writes=[bdd[bi]])
                    S.op("dve", lambda e: e.scalar_tensor_tensor(out=zt[bi][:, c0:256], in0=dd[bi][:, c0:256], scalar=nsl[:, g:g + 1],
                                                                 in1=ps[:, zb, c0:256], op0=ALU.mult, op1=ALU.add),
                         reads=[bdd[bi], bnsl, bps[zb]], writes=[bzt[bi]])
                    S.op("act", lambda e: e.activation(out=PT[pi][:, c0:256], in_=zt[bi][:, c0:256], func=AF.Exp),
                         reads=[bzt[bi]], writes=[bPT[pi]])

                def s2(i=i, hp=hp, ob=ob, db=db, pi=pi, s=s, m=m, r=r, g=g, st0=st0):
                    if hp:
                        S.op("pe", lambda e: e.matmul(ps[:, ob, 0:128], lhsT=V1[:, i - 1, :], rhs=PT[pi][:, 0:128], start=True, stop=False),
                             reads=[bV1, bPT[pi]], writes=[bps[ob]])
                    S.op("pe", lambda e: e.matmul(ps[:, ob, 0:128], lhsT=V1[:, i, :], rhs=PT[pi][:, 128:256], start=(not hp), stop=True),
                         reads=[bV1, bPT[pi]], writes=[bps[ob]])
                    if hp:
                        S.op("pe", lambda e: e.matmul(ps[:, db, 0:128], lhsT=ones[:], rhs=PT[pi][:, 0:128], start=True, stop=False),
                             reads=[bones, bPT[pi]], writes=[bps[db]])
                    S.op("pe", lambda e: e.matmul(ps[:, db, 0:128], lhsT=ones[:], rhs=PT[pi][:, 128:256], start=(not hp), stop=True),
                         reads=[bones, bPT[pi]], writes=[bps[db]])

                def s3(i=i, ob=ob, db=db, r=r, g=g, st0=st0):
                    an = accn[:, st0:st0 + 127 * r + 1:r] if r > 1 else accn[:, st0:st0 + 128]
                    ad = accd[:, st0:st0 + 127 * r + 1:r] if r > 1 else accd[:, st0:st0 + 128]
                    if g == 0:
                        S.op("act", lambda e: e.activation(out=an, in_=ps[:, ob, 0:128], func=AF.Copy), reads=[bps[ob]], writes=[baccn])
                        S.op("dve", lambda e: e.tensor_copy(out=ad, in_=ps[:, db, 0:128]), reads=[bps[db]], writes=[baccd])
                    else:
                        S.op("dve", lambda e: e.tensor_tensor(out=an, in0=an, in1=ps[:, ob, 0:128], op=ALU.add), reads=[baccn, bps[ob]], writes=[baccn])
                        S.op("dve", lambda e: e.tensor_tensor(out=ad, in0=ad, in1=ps[:, db, 0:128], op=ALU.add), reads=[baccd, bps[db]], writes=[baccd])
                dsteps.append((s1, s1x, s1b, s2, s3))
            nd = len(dsteps)
            NS = 5
            for k in range(-(NS - 1), nd):
                for st_ in range(NS):
                    idx = k + (NS - 1 - st_)
                    if 0 <= idx < nd:
                        dsteps[idx][st_]()
        for c in range(seq // 512):
            cs = slice(c * 512, (c + 1) * 512)
            yi = cnt["ys"] % 2; cnt["ys"] += 1
            S.op("dve", lambda e: e.reciprocal(out=RD[:], in_=accd[:, cs]), reads=[baccd], writes=[bRD])
            S.op("dve", lambda e: e.tensor_tensor(out=YS[yi][:], in0=accn[:, cs], in1=RD[:], op=ALU.mult), reads=[baccn, bRD], writes=[bYS[yi]])
            S.dma("sp", lambda e: e.dma_start(out=yout(2, c), in_=YS[yi][:]), reads=[bYS[yi]], writes=[b_ydil])


def consts_B():
    import ml_dtypes
    k = np.arange(128)[:, None]
    q = np.arange(512)[None, :]
    maskc = np.stack([(128 * i0 + k <= q) for i0 in range(4)], axis=1).astype(np.float32)
    masks = np.stack([(128 * i0 + k < q) for i0 in range(4)], axis=1).astype(np.float32)
    kl = np.arange(128)[:, None]; ql = np.arange(128)[None, :]
    maskd = np.concatenate([np.where(kl >= ql, 0.0, BIG), np.where(kl <= ql, 0.0, BIG)], axis=1).astype(np.float32)
    p = np.arange(128)[:, None]; m = np.arange(128)[None, :]
    m1 = (p > m).astype(np.float32); m2 = (p <= m).astype(np.float32)
    bf = ml_dtypes.bfloat16
    return dict(maskc=maskc.astype(bf), masks=masks.astype(bf), maskd=maskd, m1=m1.astype(bf), m2=m2.astype(bf))


D = 2048
NK = 16
EPS = 1e-6
NE = 16
BIGR = 1.0e4


def emit_C(S, nc, d, l, Yl, gT, xT, x2T, xfT, b_Yg, b_gT, b_xsrc, b_x2T, b_xfT, ntok=2048):
    NT = ntok // 512
    cT = d['cT']; wada = d['wada'][l]; bada = d['bada_c'][l]; gmoe = d['gmoe'][l]; gfin = d['gfin']
    womla = d['womla'][l]; wodil = d['wodil'][l]; wosb = d['wosb'][l]; wout = d['wout'][l]
    wr = d['wr']; br = d['br']; wg = d['wg'][l]; wu = d['wu'][l]; wd = d['wd'][l]; ident = d['ident']
    DBG = False
    xt = S.sbuf("xt", [128, NK, 512], F32); b_xt = [Buf("xt%d" % k) for k in range(NK)]
    scr = S.sbuf("scr", [128, 20, 512], BF16); b_scr = Buf("scr")
    mg = S.sbuf("mg", [128, NK, 512], BF16); b_mg = Buf("mg")
    a2b = S.sbuf("a2b", [128, NK, 512], BF16); b_a2b = Buf("a2b")
    NW = 5
    W = [S.sbuf("W%d" % i, [128, NK, 512], BF16) for i in range(NW)]; b_W = [Buf("W%d" % i) for i in range(NW)]
    gt = [S.sbuf("gt%d" % i, [128, 3, 512], F32) for i in range(2)]; b_gt = [Buf("gt0"), Buf("gt1")]
    tmp = [S.sbuf("tmp%d" % i, [128, 512], F32) for i in range(4)]; b_tmp = [Buf("tmp%d" % i) for i in range(4)]
    hp = [S.sbuf("hp%d" % i, [128, 4, 512], BF16) for i in range(2)]; b_hp = [Buf("hp0"), Buf("hp1")]
    rstd = S.sbuf("rstd", [128, 512], F32); b_rstd = Buf("rstd")
    lnt = S.sbuf("lnt", [128, 512], F32); b_lnt = Buf("lnt")
    ones = S.sbuf("ones", [128, 128], BF16); b_ones = Buf("ones")
    onesf = S.sbuf("onesf", [128, 128], F32); b_onesf = Buf("onesf")
    idt = S.sbuf("idt", [128, 128], F32); b_idt = Buf("idt")
    Gm = [S.sbuf("Gm%d" % i, [128, 128], F32) for i in range(2)]; b_Gm = [Buf("Gm0"), Buf("Gm1")]
    cin = S.sbuf("cin", [128, NK], F32); b_cin = Buf("cin")
    cact = S.sbuf("cact", [128, NK], BF16); b_cact = Buf("cact")
    badat = S.sbuf("badat", [128, 64], F32); b_badat = Buf("badat")
    gmoet = S.sbuf("gmoet", [128, NK], F32); b_gmoet = Buf("gmoet")
    gfint = S.sbuf("gfint", [128, NK], F32); b_gfint = Buf("gfint")
    modt = S.sbuf("modt", [128, 64], F32); b_modt = Buf("modt")
    s1m = S.sbuf("s1m", [128, NK], F32); b_s1m = Buf("s1m")
    wrt = S.sbuf("wrt", [128, NK, NE], F32); b_wrt = Buf("wrt")
    brt = S.sbuf("brt", [128, NE], F32); b_brt = Buf("brt")
    lt = S.sbuf("lt", [16, 512], F32); b_lt = Buf("lt")
    sc = S.sbuf("sc", [128, 64], F32); b_sc = Buf("sc")
    bi = S.sbuf("bi", [128, 64], F32); b_bi = Buf("bi")
    bi2 = S.sbuf("bi2", [128, 64], F32); b_bi2 = Buf("bi2")
    eq = S.sbuf("eq", [128, 64], F32); b_eq = Buf("eq")
    m1 = S.sbuf("m1", [128, 16], F32); b_m1 = Buf("m1")
    m2 = S.sbuf("m2", [128, 16], F32); b_m2 = Buf("m2")
    gs = S.sbuf("gs", [128, 16], F32); b_gs = Buf("gs")
    gmax = S.sbuf("gmax", [128, 4], F32); b_gmax = Buf("gmax")
    gsel = S.sbuf("gsel", [128, 16], F32); b_gsel = Buf("gsel")
    wsel = S.sbuf("wsel", [128, 64], F32); b_wsel = Buf("wsel")
    den = S.sbuf("den", [128, 4], F32); b_den = Buf("den")
    gates = S.sbuf("gates", [128, 64], F32); b_gates = Buf("gates")
    ps = S.psum("ps", [128, 8, 512], F32); b_ps = [Buf("ps%d" % i) for i in range(8)]
    st = {"bank": 0, "w": 0, "gt": 0, "tmp": 0, "hp": 0, "gm": 0}

    def nbank():
        i = st["bank"]; st["bank"] = (i + 1) % 6
        return i

    def nw():
        i = st["w"]; st["w"] = (i + 1) % NW
        return i

    def ntmp():
        i = st["tmp"]; st["tmp"] = (i + 1) % 4
        return i

    def load_w(wi, src2d, nk, c0, ncols, k_off=0):
        srcv = src2d.rearrange("(k p) c -> p k c", p=128)
        h = max(1, nk // 2)
        for k0 in range(0, nk, h):
            S.dma("pool", lambda e: e.dma_start(out=W[wi][:, k_off + k0:k_off + k0 + h, 0:ncols],
                                                in_=srcv[:, k0:k0 + h, c0:c0 + ncols]), writes=[b_W[wi]])

    S.op("dve", lambda e: e.memset(ones[:], 1.0), writes=[b_ones])
    S.op("dve", lambda e: e.memset(onesf[:], 1.0), writes=[b_onesf])
    for dst, bd, src in ((cin, b_cin, cT), (badat, b_badat, bada), (gmoet, b_gmoet, gmoe), (gfint, b_gfint, gfin),
                         (brt, b_brt, br), (idt, b_idt, ident)):
        S.dma("sp", lambda e: e.dma_start(out=dst[:], in_=src[:, :]), writes=[bd])
    S.dma("sp", lambda e: e.dma_start(out=wrt[:], in_=wr.rearrange("(k p) e -> p k e", p=128)), writes=[b_wrt])
    S.op("act", lambda e: e.activation(out=cact[:], in_=cin[:], func=AF.Silu), reads=[b_cin], writes=[b_cact])

    mb = nbank()
    for g in range(16):
        wi = nw()
        load_w(wi, wada, NK, 4096 + g * 512, 512)
        for m in range(4):
            n = g * 4 + m
            for k in range(NK):
                S.op("pe", lambda e: e.matmul(ps[:, mb, n:n + 1], lhsT=W[wi][:, k, m * 128:(m + 1) * 128], rhs=cact[:, k:k + 1],
                                              start=(k == 0), stop=(k == NK - 1)), reads=[b_W[wi], b_cact], writes=[b_ps[mb]])
    S.op("dve", lambda e: e.tensor_tensor(out=modt[:], in0=ps[:, mb, 0:64], in1=badat[:], op=ALU.add),
         reads=[b_ps[mb], b_badat], writes=[b_modt])
    S.op("dve", lambda e: e.scalar_tensor_tensor(out=s1m[:], in0=modt[:, 32:48], scalar=1.0, in1=gmoet[:], op0=ALU.add, op1=ALU.mult),
         reads=[b_modt, b_gmoet], writes=[b_s1m])

    def rms_rstd(src3, bsrcs):
        S.op("act", lambda e: e.activation(out=scr[:, 0:NK, :], in_=src3, func=AF.Square), reads=bsrcs, writes=[b_scr])
        bk = nbank()
        for k in range(NK):
            S.op("pe", lambda e: e.matmul(ps[:, bk, :], lhsT=ones[:], rhs=scr[:, k, :], start=(k == 0), stop=(k == NK - 1)),
                 reads=[b_scr, b_ones], writes=[b_ps[bk]])
        S.op("act", lambda e: e.activation(out=lnt[:], in_=ps[:, bk, :], func=AF.Ln, scale=1.0 / D, bias=EPS),
             reads=[b_ps[bk]], writes=[b_lnt])
        S.op("act", lambda e: e.activation(out=rstd[:], in_=lnt[:], func=AF.Exp, scale=-0.5), reads=[b_lnt], writes=[b_rstd])

    xTv = xT.rearrange("(k p) t -> p k t", p=128)
    gTv = gT.rearrange("(b n p) t -> p b n t", b=3, p=128)
    x2v = x2T.rearrange("(k p) t -> p k t", p=128) if x2T is not None else None
    xfv = xfT.rearrange("(k p) t -> p k t", p=128) if xfT is not None else None

    for t in range(NT):
        ts = slice(t * 512, (t + 1) * 512)
        for k0 in range(0, NK, 4):
            S.dma("sp", lambda e: e.dma_start(out=xt[:, k0:k0 + 4, :], in_=xTv[:, k0:k0 + 4, ts]), reads=[b_xsrc], writes=b_xt[k0:k0 + 4])
        for i_ in range(2):
            S.dma("sp", lambda e: e.dma_start(out=scr[:, i_:8:2, :], in_=Yl[i_, :, :, ts].rearrange("j p t -> p j t")), reads=[b_Yg], writes=[b_scr])
            S.dma("sp", lambda e: e.dma_start(out=scr[:, 12 + i_:20:2, :], in_=Yl[3 + i_, :, :, ts].rearrange("j p t -> p j t")), reads=[b_Yg], writes=[b_scr])
        S.dma("sp", lambda e: e.dma_start(out=scr[:, 8:12, :], in_=Yl[2, :, :, ts].rearrange("j p t -> p j t")), reads=[b_Yg], writes=[b_scr])
        for ng in range(4):
            wa = nw(); load_w(wa, womla, 8, ng * 512, 512); load_w(wa, wodil, 4, ng * 512, 512, k_off=8)
            wb_ = nw(); load_w(wb_, wosb, 8, ng * 512, 512)
            for m in range(4):
                n = ng * 4 + m
                gi = st["gt"]; st["gt"] ^= 1
                S.dma("sp", lambda e: e.dma_start(out=gt[gi][:], in_=gTv[:, :, n, ts]), reads=[b_gT], writes=[b_gt[gi]])
                banks = []
                for brn, (wi, koff, nk, yoff) in enumerate(((wa, 0, 8, 0), (wa, 8, 4, 8), (wb_, 0, 8, 12))):
                    bk = nbank(); banks.append(bk)
                    for k in range(nk):
                        S.op("pe", lambda e: e.matmul(ps[:, bk, :], lhsT=W[wi][:, koff + k, m * 128:(m + 1) * 128], rhs=scr[:, yoff + k, :],
                                                      start=(k == 0), stop=(k == nk - 1)), reads=[b_W[wi], b_scr], writes=[b_ps[bk]])
                ta = ntmp(); tb = ntmp()
                S.op("dve", lambda e: e.tensor_tensor(out=tmp[ta][:], in0=ps[:, banks[0], :], in1=gt[gi][:, 0, :], op=ALU.mult),
                     reads=[b_ps[banks[0]], b_gt[gi]], writes=[b_tmp[ta]])
                S.op("dve", lambda e: e.tensor_tensor(out=tmp[tb][:], in0=ps[:, banks[1], :], in1=gt[gi][:, 1, :], op=ALU.mult),
                     reads=[b_ps[banks[1]], b_gt[gi]], writes=[b_tmp[tb]])
                S.op("pool", lambda e: e.tensor_tensor(out=tmp[ta][:], in0=tmp[ta][:], in1=tmp[tb][:], op=ALU.add),
                     reads=[b_tmp[ta], b_tmp[tb]], writes=[b_tmp[ta]])
                S.op("dve", lambda e: e.tensor_tensor(out=tmp[tb][:], in0=ps[:, banks[2], :], in1=gt[gi][:, 2, :], op=ALU.mult),
                     reads=[b_ps[banks[2]], b_gt[gi]], writes=[b_tmp[tb]])
                S.op("pool", lambda e: e.tensor_tensor(out=mg[:, n, :], in0=tmp[ta][:], in1=tmp[tb][:], op=ALU.add),
                     reads=[b_tmp[ta], b_tmp[tb]], writes=[b_mg])
        for ng in range(4):
            wi = nw(); load_w(wi, wout, NK, ng * 512, 512)
            for m in range(4):
                n = ng * 4 + m
                bk = nbank()
                for k in range(NK):
                    S.op("pe", lambda e: e.matmul(ps[:, bk, :], lhsT=W[wi][:, k, m * 128:(m + 1) * 128], rhs=mg[:, k, :],
                                                  start=(k == 0), stop=(k == NK - 1)), reads=[b_W[wi], b_mg], writes=[b_ps[bk]])
                S.op("dve", lambda e: e.scalar_tensor_tensor(out=xt[:, n, :], in0=ps[:, bk, :], scalar=modt[:, n:n + 1], in1=xt[:, n, :],
                                                             op0=ALU.mult, op1=ALU.add), reads=[b_ps[bk], b_modt, b_xt[n]], writes=[b_xt[n]])
        if DBG:
            x1v = x1T.rearrange("(k p) t -> p k t", p=128)
            for k0 in range(0, NK, 4):
                S.dma("sp", lambda e: e.dma_start(out=x1v[:, k0:k0 + 4, ts], in_=xt[:, k0:k0 + 4, :]), reads=b_xt[k0:k0 + 4], writes=[b_x1T])
        rms_rstd(xt[:, :, :], b_xt)
        lb = nbank()
        for k in range(NK):
            ta = ntmp(); tb = ntmp()
            S.op("dve", lambda e: e.tensor_tensor(out=tmp[ta][:], in0=xt[:, k, :], in1=rstd[:], op=ALU.mult),
                 reads=[b_xt[k], b_rstd], writes=[b_tmp[ta]])
            S.op("act", lambda e: e.activation(out=tmp[tb][:], in_=tmp[ta][:], func=AF.Identity, scale=s1m[:, k:k + 1], bias=modt[:, 16 + k:17 + k]),
                 reads=[b_tmp[ta], b_s1m, b_modt], writes=[b_tmp[tb]])
            S.op("act", lambda e: e.activation(out=a2b[:, k, :], in_=tmp[ta][:], func=AF.Identity, scale=s1m[:, k:k + 1], bias=modt[:, 16 + k:17 + k]),
                 reads=[b_tmp[ta], b_s1m, b_modt], writes=[b_a2b])
            S.op("pe", lambda e: e.matmul(ps[0:16, lb, :], lhsT=wrt[:, k, :], rhs=tmp[tb][:], start=(k == 0), stop=(k == NK - 1)),
                 reads=[b_wrt, b_tmp[tb]], writes=[b_ps[lb]])
        S.op("act", lambda e: e.activation(out=lt[:], in_=ps[0:16, lb, :], func=AF.Copy), reads=[b_ps[lb]], writes=[b_lt])
        tb_ = nbank()
        for sub in range(4):
            S.op("pe", lambda e: e.matmul(ps[:, tb_, sub * 16:(sub + 1) * 16], lhsT=lt[0:16, sub * 128:(sub + 1) * 128], rhs=idt[0:16, 0:16],
                                          start=True, stop=True), reads=[b_lt, b_idt], writes=[b_ps[tb_]])
        S.op("act", lambda e: e.activation(out=sc[:], in_=ps[:, tb_, 0:64], func=AF.Sigmoid), reads=[b_ps[tb_]], writes=[b_sc])
        for sub in range(4):
            S.op("dve", lambda e: e.tensor_tensor(out=bi[:, sub * 16:(sub + 1) * 16], in0=sc[:, sub * 16:(sub + 1) * 16], in1=brt[:], op=ALU.add),
                 reads=[b_sc, b_brt], writes=[b_bi])
        v3 = lambda tl: tl[:, :].rearrange("p (g e) -> p g e", e=4)
        S.op("dve", lambda e: e.tensor_reduce(out=m1[:], in_=v3(bi), axis=AX.X, op=ALU.max), reads=[b_bi], writes=[b_m1])
        for ee in range(4):
            S.op("dve", lambda e: e.tensor_tensor(out=v3(eq)[:, :, ee], in0=v3(bi)[:, :, ee], in1=m1[:], op=ALU.is_equal),
                 reads=[b_bi, b_m1], writes=[b_eq])
        S.op("dve", lambda e: e.scalar_tensor_tensor(out=bi2[:], in0=eq[:], scalar=-BIGR, in1=bi[:], op0=ALU.mult, op1=ALU.add),
             reads=[b_eq, b_bi], writes=[b_bi2])
        S.op("dve", lambda e: e.tensor_reduce(out=m2[:], in_=v3(bi2), axis=AX.X, op=ALU.max), reads=[b_bi2], writes=[b_m2])
        S.op("dve", lambda e: e.tensor_tensor(out=gs[:], in0=m1[:], in1=m2[:], op=ALU.add), reads=[b_m1, b_m2], writes=[b_gs])
        S.op("dve", lambda e: e.tensor_reduce(out=gmax[:], in_=gs[:, :].rearrange("p (s g) -> p s g", g=4), axis=AX.X, op=ALU.max),
             reads=[b_gs], writes=[b_gmax])
        for g in range(4):
            S.op("dve", lambda e: e.tensor_tensor(out=gsel[:, :].rearrange("p (s g) -> p s g", g=4)[:, :, g],
                                                  in0=gs[:, :].rearrange("p (s g) -> p s g", g=4)[:, :, g], in1=gmax[:], op=ALU.is_equal),
                 reads=[b_gs, b_gmax], writes=[b_gsel])
        for ee in range(4):
            S.op("dve", lambda e: e.tensor_tensor(out=v3(eq)[:, :, ee], in0=v3(bi)[:, :, ee], in1=m2[:], op=ALU.is_ge),
                 reads=[b_bi, b_m2], writes=[b_eq])
            S.op("dve", lambda e: e.tensor_tensor(out=v3(eq)[:, :, ee], in0=v3(eq)[:, :, ee], in1=gsel[:], op=ALU.mult),
                 reads=[b_eq, b_gsel], writes=[b_eq])
        S.op("dve", lambda e: e.tensor_tensor(out=wsel[:], in0=sc[:], in1=eq[:], op=ALU.mult), reads=[b_sc, b_eq], writes=[b_wsel])
        S.op("dve", lambda e: e.tensor_reduce(out=den[:], in_=wsel[:, :].rearrange("p (s e) -> p s e", e=16), axis=AX.X, op=ALU.add),
             reads=[b_wsel], writes=[b_den])
        S.op("dve", lambda e: e.reciprocal(out=den[:], in_=den[:]), reads=[b_den], writes=[b_den])
        for sub in range(4):
            S.op("dve", lambda e: e.tensor_scalar(out=gates[:, sub * 16:(sub + 1) * 16], in0=wsel[:, sub * 16:(sub + 1) * 16],
                                                  scalar1=den[:, sub:sub + 1], scalar2=None, op0=ALU.mult), reads=[b_wsel, b_den], writes=[b_gates])
        if DBG and t == 0:
            for i_, (tl, bb, w_) in enumerate(((sc, b_sc, 64), (bi, b_bi, 64), (m1, b_m1, 16), (m2, b_m2, 16), (gsel, b_gsel, 16), (eq, b_eq, 64), (gates, b_gates, 64), (den, b_den, 4))):
                S.dma("sp", lambda e: e.dma_start(out=dbg[:, i_, 0:w_], in_=tl[:, 0:w_]), reads=[bb], writes=[b_dbg])
            a2v = a2T.rearrange("(k p) t -> p k t", p=128)
            S.dma("sp", lambda e: e.dma_start(out=a2v[:, :, ts], in_=a2b[:, :, :]), reads=[b_a2b], writes=[b_a2T])
        for ex in range(NE):
            w1 = nw(); load_w(w1, wg[ex], NK, 0, 512)
            w2 = nw(); load_w(w2, wu[ex], NK, 0, 512)
            w3 = nw()
            wdv = wd[ex].rearrange("(k p) c -> p k c", p=128)
            W3v = W[w3][:, :, :].rearrange("p (k a) c -> p k (a c)", a=4)
            for k0 in range(0, 4, 2):
                S.dma("pool", lambda e: e.dma_start(out=W3v[:, k0:k0 + 2, :], in_=wdv[:, k0:k0 + 2, :]), writes=[b_W[w3]])
            gb = 6 + (ex % 2)
            for sub in range(4):
                gi = st["gm"]; st["gm"] ^= 1
                S.op("dve", lambda e: e.tensor_scalar(out=Gm[gi][:], in0=onesf[:], scalar1=gates[:, sub * 16 + ex:sub * 16 + ex + 1], scalar2=None,
                                                      op0=ALU.mult), reads=[b_onesf, b_gates], writes=[b_Gm[gi]])
                S.op("pe", lambda e: e.matmul(ps[:, gb, sub * 128:(sub + 1) * 128], lhsT=Gm[gi][:], rhs=idt[:], start=True, stop=True),
                     reads=[b_Gm[gi], b_idt], writes=[b_ps[gb]])
            hi = st["hp"]; st["hp"] ^= 1
            for dc in range(4):
                b1 = nbank()
                for k in range(NK):
                    S.op("pe", lambda e: e.matmul(ps[:, b1, :], lhsT=W[w1][:, k, dc * 128:(dc + 1) * 128], rhs=a2b[:, k, :],
                                                  start=(k == 0), stop=(k == NK - 1)), reads=[b_W[w1], b_a2b], writes=[b_ps[b1]])
                b2 = nbank()
                for k in range(NK):
                    S.op("pe", lambda e: e.matmul(ps[:, b2, :], lhsT=W[w2][:, k, dc * 128:(dc + 1) * 128], rhs=a2b[:, k, :],
                                                  start=(k == 0), stop=(k == NK - 1)), reads=[b_W[w2], b_a2b], writes=[b_ps[b2]])
                ta = ntmp(); tb = ntmp()
                S.op("act", lambda e: e.activation(out=tmp[ta][:], in_=ps[:, b1, :], func=AF.Silu), reads=[b_ps[b1]], writes=[b_tmp[ta]])
                S.op("dve", lambda e: e.tensor_tensor(out=tmp[tb][:], in0=tmp[ta][:], in1=ps[:, b2, :], op=ALU.mult),
                     reads=[b_tmp[ta], b_ps[b2]], writes=[b_tmp[tb]])
                S.op("dve", lambda e: e.tensor_tensor(out=hp[hi][:, dc, :], in0=tmp[tb][:], in1=ps[:, gb, :], op=ALU.mult),
                     reads=[b_tmp[tb], b_ps[gb]], writes=[b_hp[hi]])
            for n in range(NK):
                bk = nbank()
                for dc in range(4):
                    S.op("pe", lambda e: e.matmul(ps[:, bk, :], lhsT=W3v[:, dc, n * 128:(n + 1) * 128], rhs=hp[hi][:, dc, :],
                                                  start=(dc == 0), stop=(dc == 3)), reads=[b_W[w3], b_hp[hi]], writes=[b_ps[bk]])
                S.op("dve", lambda e: e.scalar_tensor_tensor(out=xt[:, n, :], in0=ps[:, bk, :], scalar=modt[:, 48 + n:49 + n], in1=xt[:, n, :],
                                                             op0=ALU.mult, op1=ALU.add), reads=[b_ps[bk], b_modt, b_xt[n]], writes=[b_xt[n]])
        if x2v is not None:
            for k0 in range(0, NK, 4):
                S.dma("sp", lambda e: e.dma_start(out=x2v[:, k0:k0 + 4, ts], in_=xt[:, k0:k0 + 4, :]), reads=b_xt[k0:k0 + 4], writes=[b_x2T])
        if xfv is not None:
            rms_rstd(xt[:, :, :], b_xt)
            for k in range(NK):
                ta = ntmp()
                S.op("dve", lambda e: e.scalar_tensor_tensor(out=tmp[ta][:], in0=xt[:, k, :], scalar=gfint[:, k:k + 1], in1=rstd[:],
                                                             op0=ALU.mult, op1=ALU.mult), reads=[b_xt[k], b_gfint, b_rstd], writes=[b_tmp[ta]])
                S.dma("sp", lambda e: e.dma_start(out=xfv[:, k, ts], in_=tmp[ta][:]), reads=[b_tmp[ta]], writes=[b_xfT])


def build_fused():
    nc = bass.Bass("TRN2", target_bir_lowering=False)
    ext = lambda n, s, dt: nc.dram_tensor(n, s, dt, kind="ExternalInput").ap()
    d = {}
    d['xT'] = ext("xT", [2048, 2048], F32)
    jt = ext("jt", [1, 2], I32)
    d['cT'] = ext("cT", [128, 16], F32)
    d['pos'] = ext("pos", [1, 2048], I32)
    d['invf'] = ext("invf", [64, 1], F32)
    d['sgn'] = ext("sgn", [64, 1], F32)
    d['wada'] = ext("wada", [2, 2048, 12288], F32)
    d['bada_a'] = ext("bada_a", [2, 128, 32], F32)
    d['bada_c'] = ext("bada_c", [2, 128, 64], F32)
    d['gmix'] = ext("gmix", [2, 128, 16], F32)
    d['gq'] = ext("gq", [2, 128, 4], F32)
    d['gkv'] = ext("gkv", [2, 128, 4], F32)
    d['gmoe'] = ext("gmoe", [2, 128, 16], F32)
    d['gfin'] = ext("gfin", [128, 16], F32)
    d['win'] = ext("win", [2, 2048, 14912], F32)
    d['wsw'] = ext("wsw", [2, 2048, 64], F32)
    d['wuq'] = ext("wuq", [2, 512, 1536], F32)
    d['wuqs'] = ext("wuqs", [2, 512, 512], F32)
    d['wukv'] = ext("wukv", [2, 512, 2048], F32)
    d['womla'] = ext("womla", [2, 1024, 2048], F32)
    d['wodil'] = ext("wodil", [2, 512, 2048], F32)
    d['wosb'] = ext("wosb", [2, 1024, 2048], F32)
    d['wout'] = ext("wout", [2, 2048, 2048], F32)
    d['wr'] = ext("wr", [2048, 16], F32)
    d['br'] = ext("br", [128, 16], F32)
    d['wg'] = ext("wg", [2, 16, 2048, 512], F32)
    d['wu'] = ext("wu", [2, 16, 2048, 512], F32)
    d['wd'] = ext("wd", [2, 16, 512, 2048], F32)
    d['ident'] = ext("ident", [128, 128], F32)
    d['maskc'] = ext("maskc", [128, 4, 512], BF16)
    d['masks'] = ext("masks", [128, 4, 512], BF16)
    d['maskd'] = ext("maskd", [128, 256], F32)
    d['m1'] = ext("m1", [128, 128], BF16)
    d['m2'] = ext("m2", [128, 128], BF16)
    d['nslope'] = ext("nslope", [128, 3], F32)
    d['dposk'] = ext("dposk", [3, 128, 64], I32)
    d['dposq'] = ext("dposq", [3, 8192], I32)
    xfT = nc.dram_tensor("xfT", [2048, 2048], F32, kind="ExternalOutput").ap()
    b_xfT = Buf("xfT")
    x2s = nc.dram_tensor("x2scr", [2048, 2048], F32).ap()
    b_x2s = Buf("x2s")
    b_x0 = Buf("x0")
    GROUPS = [[0, 1, 2, 3], [4, 5, 6, 7]]

    S = Sched(nc)
    S.enable_dyn(jt[:, :])
    CH = 128 * 4096
    QKs_h = nc.dram_tensor("QKs", [32, 128, 4096], BF16)
    Vs_h = nc.dram_tensor("Vs", [16, 128, 4096], BF16)
    QKg_h = nc.dram_tensor("QKg", [32, 512, 4096], BF16)
    Vg_h = nc.dram_tensor("Vg", [16, 512, 4096], BF16)
    Ys_h = nc.dram_tensor("Ys", [10, 128, 4096], BF16)
    Yg_h = nc.dram_tensor("Yg", [10, 512, 4096], BF16)
    gsc = nc.dram_tensor("gscr", [6144, 2048], F32).ap()
    QKl_h = nc.dram_tensor("QKl", [4096, 4096], BF16)
    Vl_h = nc.dram_tensor("Vl", [2048, 4096], BF16)
    Yl_h = nc.dram_tensor("Yl", [2560, 2048], BF16)
    for l in range(2):
        b_QKs, b_Vs, b_QKg, b_Vg, b_Ys, b_Yg, b_g = (Buf(n) for n in ("QKs", "Vs", "QKg", "Vg", "Ys", "Yg", "g"))
        b_QKl, b_Vl, b_Yl = Buf("QKl"), Buf("Vl"), Buf("Yl")
        xsrc = d['xT'] if l == 0 else x2s
        b_xsrc = b_x0 if l == 0 else b_x2s
        QKs_v = QKs_h.ap().rearrange("c p (a t) -> (c p a) t", a=4).rearrange("(s u r) t -> s u r t", s=4, u=2)
        Vs_v = Vs_h.ap().rearrange("c p (a d) -> (c p a) d", a=4).rearrange("(s t) d -> s t d", s=4)
        S.begin_phase()
        b_QKs2 = [Buf("QKs0"), Buf("QKs1")]
        b_Vs2 = [Buf("Vs0"), Buf("Vs1")]

        pending = []
        rate = [1]

        def after_sup(sup):
            for sh in range(4):
                for q in range(4):
                    pending.append((QKs_h, QKg_h, sh * 8 + sup * 4 + q, b_QKs2[sup], b_QKg))
            for sh in range(4):
                for q in range(2):
                    pending.append((Vs_h, Vg_h, sh * 4 + sup * 2 + q, b_Vs2[sup], b_Vg))
            if sup == 1:
                rate[0] = 2

        def tick():
            for _ in range(rate[0]):
                if not pending:
                    break
                sh_, gh_, c, br_, bw_ = pending.pop(0)
                S.cc(lambda e: e.collective_compute("AllGather", ALU.bypass, replica_groups=GROUPS, ins=[sh_.ap()[c].opt()],
                                                    outs=[gh_.ap()[c].opt()]), reads=[br_], writes=[bw_])
        emit_A(S, nc, d, l, xsrc, QKs_v, Vs_v, gsc, b_QKs2, b_Vs2, b_g, after_sup=after_sup, tick=tick)
        while pending:
            tick()
        S.end_phase()
        S.begin_phase()
        for hh in range(2):
            S.dma_dyn(QKl_h.ap()[hh * 2048:(hh + 1) * 2048, :], QKg_h, 8 * 4 * CH, hh * 2048 * 4096, [[4096, 2048], [1, 4096]],
                      reads=[b_QKg], writes=[b_QKl])
        S.dma_dyn(Vl_h.ap()[:, :], Vg_h, 4 * 4 * CH, 0, [[4096, 2048], [1, 4096]], reads=[b_Vg], writes=[b_Vl])
        QKl = QKl_h.ap().rearrange("(c r p) (a t) -> c r (p a) t", c=8, r=4, a=4)
        Vl = Vl_h.ap().rearrange("(c r p) (a d) -> c r (p a) d", c=4, r=4, a=4)
        Ys_v = Ys_h.ap().rearrange("(th b) p t -> th b p t", th=2)
        S.barrier()
        emit_B(S, nc, d, QKl, Vl, Ys_v, b_QKl, b_Vl, b_Ys, do_mla=True, do_sb=False, do_dil=False)
        S.end_phase()
        S.begin_phase()
        emit_B(S, nc, d, QKl, Vl, Ys_v, b_QKl, b_Vl, b_Ys, do_mla=False, do_sb=True, do_dil=False)
        for c in (0, 1, 3, 4, 5, 6, 8, 9):
            S.cc(lambda e: e.collective_compute("AllGather", ALU.bypass, replica_groups=GROUPS, ins=[Ys_h.ap()[c].opt()], outs=[Yg_h.ap()[c].opt()]),
                 reads=[b_Ys], writes=[b_Yg])
        S.end_phase(wait_cc=False)
        S.begin_phase()
        emit_B(S, nc, d, QKl, Vl, Ys_v, b_QKl, b_Vl, b_Ys, do_mla=False, do_sb=False, do_dil=True)
        for c in (2, 7):
            S.cc(lambda e: e.collective_compute("AllGather", ALU.bypass, replica_groups=GROUPS, ins=[Ys_h.ap()[c].opt()], outs=[Yg_h.ap()[c].opt()]),
                 reads=[b_Ys], writes=[b_Yg])
        S.end_phase()
        S.begin_phase()
        for b0, nb_ in ((0, 3), (3, 2)):
            S.dma_dyn(Yl_h.ap()[b0 * 512:(b0 + nb_) * 512, :], Yg_h, 1, b0 * 4 * CH, [[4 * CH, nb_], [4096, 512], [1, 2048]],
                      reads=[b_Yg], writes=[b_Yl], which=1)
        Yl = Yl_h.ap().rearrange("(b j p) t -> b j p t", b=5, j=4)
        emit_C(S, nc, d, l, Yl, gsc, xsrc, x2s if l == 0 else None, xfT if l == 1 else None,
               b_Yl, b_g, b_xsrc, b_x2s, b_xfT)
        S.end_phase()
    S.close()
    return nc


_PROG = {}


def _f(a):
    return np.ascontiguousarray(a)


def kernel(**inputs):
    inp = {k: np.asarray(v) for k, v in inputs.items()}
    B_, S_ = inp['x'].shape[:2]
    cores = list(range(8))
    pos = inp['positions'].astype(np.int32)
    perm = [np.concatenate([np.arange(s, S_, r) for s in range(r)]) for r in RATES]
    slopes = (np.float32(2.0) ** (np.float32(-8.0) * np.arange(1, 13, dtype=np.float32) / np.float32(12))).reshape(3, 4)
    half = 32
    invf = (np.float32(10000.0) ** (-(np.arange(half, dtype=np.float32)) / np.float32(half))).astype(np.float32)
    wu_ = inp['w_uq']
    wuqs = np.stack([np.concatenate([np.concatenate([wu_[l][:, h * 192 + 160:h * 192 + 192], wu_[l][:, h * 192 + 128:h * 192 + 160]], axis=1)
                                     for h in range(8)], axis=1) for l in range(2)])
    wi_ = inp['w_in']
    lay = lambda a, n: _f(a.reshape(a.shape[0], n, 128).transpose(0, 2, 1))
    shared = {
        'invf': _f(np.concatenate([invf, invf])[:, None]),
        'sgn': _f(np.concatenate([-np.ones(32, np.float32), np.ones(32, np.float32)])[:, None]),
        'wada': _f(inp['w_ada']),
        'bada_a': lay(inp['b_ada'][:, :4096], 32),
        'bada_c': lay(inp['b_ada'][:, 4096:], 64),
        'gmix': lay(inp['g_mix'], 16), 'gq': lay(inp['g_q'], 4), 'gkv': lay(inp['g_kv'], 4), 'gmoe': lay(inp['g_moe'], 16),
        'gfin': _f(inp['g_final'].reshape(16, 128).T),
        'win': _f(wi_), 'wsw': _f(np.concatenate([wi_[:, :, 1056:1088], wi_[:, :, 1024:1056]], axis=2)),
        'wuq': _f(wu_), 'wuqs': _f(wuqs), 'wukv': _f(inp['w_ukv']),
        'womla': _f(inp['w_o_mla']), 'wodil': _f(inp['w_o_dil']), 'wosb': _f(inp['w_o_sb']), 'wout': _f(inp['w_out']),
        'wr': _f(inp['w_router']), 'br': _f(np.broadcast_to(inp['b_router'][None, :], (128, 16))),
        'wg': _f(inp['w_gate']), 'wu': _f(inp['w_up']), 'wd': _f(inp['w_down']), 'ident': np.eye(128, dtype=np.float32),
    }
    shared.update(consts_B())
    maps = []
    for c in cores:
        b, j = c // 4, c % 4
        m = dict(shared)
        m['xT'] = _f(inp['x'][b, j * 2048:(j + 1) * 2048, :].T)
        m['jt'] = np.array([[j, (j // 2) * 5 * 4 * 128 * 4096 + (j % 2) * 2048]], np.int32)
        m['cT'] = _f(inp['c'][b].reshape(16, 128).T)
        m['pos'] = _f(pos[b, j * 2048:(j + 1) * 2048][None, :])
        pp = np.stack([pos[b][perm[g]] for g in range(3)]).astype(np.int32)
        m['dposq'] = _f(pp)
        m['dposk'] = _f(pp.reshape(3, S_ // 128, 128).transpose(0, 2, 1))
        m['nslope'] = _f(np.broadcast_to(-slopes[:, j][None, :], (128, 3)).astype(np.float32))
        maps.append(m)
    if "f" not in _PROG:
        _PROG["f"] = build_fused()
    res = run_bass_kernel_spmd(_PROG["f"], maps, core_ids=cores).results
    out = np.empty(inp['x'].shape, dtype=np.float32)
    for c in cores:
        b, j = c // 4, c % 4
        out[b, j * 2048:(j + 1) * 2048, :] = np.asarray(res[c]['xfT']).T
    return out
```
